# Optimizing a Trainium2 kernel written in Bass

```python
import jax, jax.numpy as jnp
from jax import lax
import numpy as np

D_MODEL = 2048
BATCH = 1
SEQ = 16384
DEPTH = 1

N_HEADS_SWA = 8
HEAD_DIM_SWA = 128
SWA_PATTERNS = ((128, 1), (512, 4), (2048, 16))
SWA_BLOCK = 128
N_HEADS_MLA = 8
Q_LORA_RANK = 512
KV_LORA_RANK = 256
QK_NOPE_DIM = 128
QK_ROPE_DIM = 64
V_HEAD_DIM = 128
ROPE_THETA = 10000.0
Q_BLOCK = 128
D_SWA = N_HEADS_SWA * HEAD_DIM_SWA
D_MLA = N_HEADS_MLA * V_HEAD_DIM
D_MIX = D_SWA + D_MLA
D_IN = 3 * D_SWA + Q_LORA_RANK + KV_LORA_RANK + QK_ROPE_DIM
N_EXPERTS = 64
N_GROUPS = 8
TOPK_GROUPS = 4
TOP_K = 8
D_EXPERT = 512
ROUTED_SCALE = 2.5
MOE_BLOCK = 128
N_ADA = 6
EPS = 1e-6
NEG_INF = -1e30

kernel_name = 'hybrid_dilated_mla_moe_adaln_layer'


def rms_norm(x, g):
    xf = x.astype(jnp.float32)
    y = xf * lax.rsqrt(jnp.mean(xf * xf, axis=-1, keepdims=True) + EPS)
    return (y * g.astype(jnp.float32)).astype(x.dtype)


def swiglu(x, w_gate, w_up, w_down):
    return (jax.nn.silu(x @ w_gate) * (x @ w_up)) @ w_down


def rope_tables(positions):
    half = QK_ROPE_DIM // 2
    inv_freq = ROPE_THETA ** (-jnp.arange(half, dtype=jnp.float32) / half)
    ang = positions.astype(jnp.float32)[..., None] * inv_freq
    return jnp.cos(ang), jnp.sin(ang)


def apply_rope(x, cos, sin):
    x1, x2 = jnp.split(x.astype(jnp.float32), 2, axis=-1)
    return jnp.concatenate([x1 * cos - x2 * sin, x2 * cos + x1 * sin], axis=-1).astype(x.dtype)


def dilated_window_pattern(q, k, v, pos, window, dilation, slopes):
    B, S, H, Dh = q.shape
    seg = dilation * SWA_BLOCK
    L = -(-S // seg) * seg
    pad = L - S
    nb = L // seg
    n_window = window // dilation

    def pad_seq(a):
        return jnp.pad(a, [(0, 0), (0, pad)] + [(0, 0)] * (a.ndim - 2))

    def strided(a):
        rest = a.shape[2:]
        a = a.reshape((B, L // dilation, dilation) + rest)
        a = jnp.moveaxis(a, 2, 1)
        return a.reshape((B, dilation, nb, SWA_BLOCK) + rest)

    def band(a):
        prev = jnp.pad(a[:, :, :-1], [(0, 0), (0, 0), (1, 0)] + [(0, 0)] * (a.ndim - 3))
        return jnp.concatenate([prev, a], axis=3)

    def unstrided(a):
        rest = a.shape[4:]
        a = a.reshape((B, dilation, L // dilation) + rest)
        a = jnp.moveaxis(a, 1, 2).reshape((B, L) + rest)
        return a[:, :S]

    posf = jnp.pad(pos.astype(jnp.float32), ((0, 0), (0, pad)), mode='edge')
    qb = strided(pad_seq(q))
    kb = band(strided(pad_seq(k)))
    vb = band(strided(pad_seq(v)))
    pq = strided(posf)
    pk = band(strided(posf))

    s = jnp.einsum('brnqhd,brnkhd->brnhqk', qb, kb,
                   preferred_element_type=jnp.float32) * (Dh ** -0.5)
    dist = jnp.abs(pq[..., :, None] - pk[..., None, :])
    s = s - slopes[:, None, None] * dist[:, :, :, None]
    i = jnp.arange(SWA_BLOCK)[:, None] + SWA_BLOCK
    j = jnp.arange(2 * SWA_BLOCK)[None, :]
    rel = i - j
    band_ok = (rel >= 0) & (rel <= n_window)
    first_ok = (jnp.arange(nb)[:, None, None] > 0) | (j[None] >= SWA_BLOCK)
    valid = band_ok[None] & first_ok
    s = jnp.where(valid[:, None], s, NEG_INF)
    m = jnp.max(s, axis=-1, keepdims=True)
    p = jnp.exp(s - m)
    den = jnp.sum(p, axis=-1)
    o = jnp.einsum('brnhqk,brnkhd->brnqhd', p, vb.astype(jnp.float32))
    o = o / jnp.swapaxes(den, 3, 4)[..., None]
    lse = jnp.swapaxes(m[..., 0] + jnp.log(den), 3, 4)
    return unstrided(o), unstrided(lse)


def dilated_attention(q, k, v, pos):
    H = q.shape[2]
    slopes = jnp.exp2(-8.0 * jnp.arange(1, H + 1, dtype=jnp.float32) / H)
    outs, lses = [], []
    for window, dilation in SWA_PATTERNS:
        o, lse = dilated_window_pattern(q, k, v, pos, window, dilation, slopes)
        outs.append(o)
        lses.append(lse)
    alpha = jax.nn.softmax(jnp.stack(lses, axis=0), axis=0)
    o = jnp.einsum('pbsh,pbshd->bshd', alpha, jnp.stack(outs, axis=0))
    return o.astype(q.dtype)


def causal_block_attention(q, k, v):
    B, S, H, Dk = q.shape
    nq = S // Q_BLOCK
    qb = jnp.moveaxis(q.reshape(B, nq, Q_BLOCK, H, Dk), 1, 0)
    kpos = jnp.arange(S)

    def one_block(args):
        qblk, bi = args
        s = jnp.einsum('bqhd,bkhd->bhqk', qblk, k,
                       preferred_element_type=jnp.float32) * (Dk ** -0.5)
        qpos = bi * Q_BLOCK + jnp.arange(Q_BLOCK)
        s = jnp.where(kpos[None, :] <= qpos[:, None], s, NEG_INF)
        p = jax.nn.softmax(s, axis=-1)
        return jnp.einsum('bhqk,bkhd->bqhd', p.astype(v.dtype), v)

    ob = lax.map(one_block, (qb, jnp.arange(nq)))
    return jnp.moveaxis(ob, 0, 1).reshape(B, S, H, v.shape[-1])


def latent_attention(c_q, c_kv, k_rope, positions, g_q, w_uq, g_kv, w_ukv):
    B, S, _ = c_q.shape
    q = (rms_norm(c_q, g_q) @ w_uq).reshape(B, S, N_HEADS_MLA, QK_NOPE_DIM + QK_ROPE_DIM)
    kv = (rms_norm(c_kv, g_kv) @ w_ukv).reshape(B, S, N_HEADS_MLA, QK_NOPE_DIM + V_HEAD_DIM)
    q_nope, q_rope = jnp.split(q, [QK_NOPE_DIM], axis=-1)
    k_nope, v = jnp.split(kv, [QK_NOPE_DIM], axis=-1)
    cos, sin = rope_tables(positions)
    q_rope = apply_rope(q_rope, cos[:, :, None], sin[:, :, None])
    k_rope = apply_rope(k_rope, cos, sin)[:, :, None, :]
    q = jnp.concatenate([q_nope, q_rope], axis=-1)
    k = jnp.concatenate([k_nope, jnp.broadcast_to(k_rope, k_nope.shape[:-1] + (QK_ROPE_DIM,))], axis=-1)
    return causal_block_attention(q, k, v)


def routed_experts(xt, eidx, wts, w_gate, w_up, w_down):
    N, D = xt.shape
    A = N * TOP_K
    flat_e = eidx.reshape(A)
    order = jnp.argsort(flat_e)
    sorted_e = flat_e[order]
    sorted_tok = (order // TOP_K).astype(jnp.int32)
    sorted_w = wts.reshape(A)[order]
    counts = jnp.bincount(flat_e, length=N_EXPERTS)
    padded = (counts + MOE_BLOCK - 1) // MOE_BLOCK * MOE_BLOCK
    seg_start = jnp.cumsum(counts) - counts
    pad_end = jnp.cumsum(padded)
    pad_start = pad_end - padded
    dest = pad_start[sorted_e] + jnp.arange(A) - seg_start[sorted_e]
    n_slots = -(-A // MOE_BLOCK) * MOE_BLOCK + N_EXPERTS * MOE_BLOCK
    n_blocks = n_slots // MOE_BLOCK
    slot_tok = jnp.full((n_slots,), N, jnp.int32).at[dest].set(sorted_tok)
    slot_w = jnp.zeros((n_slots,), jnp.float32).at[dest].set(sorted_w)
    blk_e = jnp.minimum(jnp.searchsorted(pad_end, jnp.arange(n_blocks) * MOE_BLOCK, side='right'),
                        N_EXPERTS - 1)
    x_pad = jnp.concatenate([xt, jnp.zeros((1, D), xt.dtype)], axis=0)

    def body(y, args):
        tok, wt, e = args
        yb = swiglu(x_pad[tok], w_gate[e], w_up[e], w_down[e])
        return y.at[tok].add(yb * wt[:, None].astype(yb.dtype)), None

    y, _ = lax.scan(body, jnp.zeros_like(x_pad),
                    (slot_tok.reshape(n_blocks, MOE_BLOCK), slot_w.reshape(n_blocks, MOE_BLOCK), blk_e))
    return y[:N]


def moe_ffn(h, w_router, router_bias, w_exp_gate, w_exp_up, w_exp_down, w_sh_gate, w_sh_up, w_sh_down):
    B, S, D = h.shape
    N = B * S
    xt = h.reshape(N, D)
    logits = jnp.einsum('nd,de->ne', xt, w_router, preferred_element_type=jnp.float32)
    scores = jax.nn.sigmoid(logits)
    choice = scores + router_bias.astype(jnp.float32)
    grp = choice.reshape(N, N_GROUPS, N_EXPERTS // N_GROUPS)
    grp_score = jnp.sum(lax.top_k(grp, 2)[0], axis=-1)
    _, gidx = lax.top_k(grp_score, TOPK_GROUPS)
    gmask = jnp.sum(jax.nn.one_hot(gidx, N_GROUPS, dtype=jnp.float32), axis=1)
    emask = jnp.repeat(gmask, N_EXPERTS // N_GROUPS, axis=1) > 0
    _, eidx = lax.top_k(jnp.where(emask, choice, NEG_INF), TOP_K)
    w = jnp.take_along_axis(scores, eidx, axis=-1)
    w = w / jnp.sum(w, axis=-1, keepdims=True) * ROUTED_SCALE
    routed = routed_experts(xt, eidx, w, w_exp_gate, w_exp_up, w_exp_down)
    shared = swiglu(xt, w_sh_gate, w_sh_up, w_sh_down)
    return (routed + shared).reshape(B, S, D)


def hybrid_layer(x, c, positions, norm_attn_g, w_ada, b_ada, w_in, g_q, w_uq, g_kv, w_ukv,
                 g_out_swa, g_out_mla, w_o, norm_ffn_g, w_router, router_bias,
                 w_exp_gate, w_exp_up, w_exp_down, w_sh_gate, w_sh_up, w_sh_down):
    B, S, _ = x.shape
    mod = jnp.einsum('bd,de->be', jax.nn.silu(c), w_ada) + b_ada
    shift_a, scale_a, gate_a, shift_f, scale_f, gate_f = [m[:, None, :] for m in jnp.split(mod, N_ADA, axis=-1)]

    h = rms_norm(x, norm_attn_g) * (1 + scale_a) + shift_a
    proj = h @ w_in
    q_a, k_a, v_a, c_q, c_kv, k_rope = jnp.split(
        proj, [D_SWA, 2 * D_SWA, 3 * D_SWA, 3 * D_SWA + Q_LORA_RANK,
               3 * D_SWA + Q_LORA_RANK + KV_LORA_RANK], axis=-1)
    heads = lambda a: a.reshape(B, S, N_HEADS_SWA, HEAD_DIM_SWA)
    o_a = dilated_attention(heads(q_a), heads(k_a), heads(v_a), positions).reshape(B, S, D_SWA)
    o_b = latent_attention(c_q, c_kv, k_rope, positions, g_q, w_uq, g_kv, w_ukv).reshape(B, S, D_MLA)
    mix = jnp.concatenate([rms_norm(o_a, g_out_swa), rms_norm(o_b, g_out_mla)], axis=-1)
    x = x + gate_a * (mix @ w_o)

    h = rms_norm(x, norm_ffn_g) * (1 + scale_f) + shift_f
    x = x + gate_f * moe_ffn(h, w_router, router_bias, w_exp_gate, w_exp_up, w_exp_down,
                             w_sh_gate, w_sh_up, w_sh_down)
    return x


def setup_inputs(seed: int = 0) -> dict:
    key = jax.random.key(seed)
    ks = jax.random.split(key, 24)
    f32 = jnp.float32
    L, D, E = DEPTH, D_MODEL, N_EXPERTS

    def nrm(k, shape, scale):
        return jax.random.normal(k, shape, f32) * scale

    def gain(k, shape):
        return 1.0 + 0.02 * jax.random.normal(k, shape, f32)

    start = jax.random.randint(ks[2], (BATCH, 1), 0, 4096, dtype=jnp.int32)
    positions = start + jnp.arange(SEQ, dtype=jnp.int32)[None, :]
    return {
        'x': nrm(ks[0], (BATCH, SEQ, D), 1.0),
        'c': nrm(ks[1], (BATCH, D), 1.0),
        'positions': positions,
        'norm_attn_g': gain(ks[3], (L, D)),
        'w_ada': nrm(ks[4], (L, D, N_ADA * D), 0.5 * D ** -0.5),
        'b_ada': nrm(ks[5], (L, N_ADA * D), 0.02),
        'w_in': nrm(ks[6], (L, D, D_IN), D ** -0.5),
        'g_q': gain(ks[7], (L, Q_LORA_RANK)),
        'w_uq': nrm(ks[8], (L, Q_LORA_RANK, N_HEADS_MLA * (QK_NOPE_DIM + QK_ROPE_DIM)), Q_LORA_RANK ** -0.5),
        'g_kv': gain(ks[9], (L, KV_LORA_RANK)),
        'w_ukv': nrm(ks[10], (L, KV_LORA_RANK, N_HEADS_MLA * (QK_NOPE_DIM + V_HEAD_DIM)), KV_LORA_RANK ** -0.5),
        'g_out_swa': gain(ks[11], (L, D_SWA)),
        'g_out_mla': gain(ks[12], (L, D_MLA)),
        'w_o': nrm(ks[13], (L, D_MIX, D), D_MIX ** -0.5),
        'norm_ffn_g': gain(ks[14], (L, D)),
        'w_router': nrm(ks[15], (L, D, E), D ** -0.5),
        'router_bias': nrm(ks[16], (L, E), 0.01),
        'w_exp_gate': nrm(ks[17], (L, E, D, D_EXPERT), D ** -0.5),
        'w_exp_up': nrm(ks[18], (L, E, D, D_EXPERT), D ** -0.5),
        'w_exp_down': nrm(ks[19], (L, E, D_EXPERT, D), D_EXPERT ** -0.5),
        'w_sh_gate': nrm(ks[20], (L, D, D_EXPERT), D ** -0.5),
        'w_sh_up': nrm(ks[21], (L, D, D_EXPERT), D ** -0.5),
        'w_sh_down': nrm(ks[22], (L, D_EXPERT, D), D_EXPERT ** -0.5),
        'final_norm_g': gain(ks[23], (D,)),
    }


def reference(x, c, positions, norm_attn_g, w_ada, b_ada, w_in, g_q, w_uq, g_kv, w_ukv,
              g_out_swa, g_out_mla, w_o, norm_ffn_g, w_router, router_bias,
              w_exp_gate, w_exp_up, w_exp_down, w_sh_gate, w_sh_up, w_sh_down, final_norm_g):
    for l in range(DEPTH):
        x = hybrid_layer(x, c, positions, norm_attn_g[l], w_ada[l], b_ada[l], w_in[l], g_q[l], w_uq[l],
                         g_kv[l], w_ukv[l], g_out_swa[l], g_out_mla[l], w_o[l], norm_ffn_g[l],
                         w_router[l], router_bias[l], w_exp_gate[l], w_exp_up[l], w_exp_down[l],
                         w_sh_gate[l], w_sh_up[l], w_sh_down[l])
    return rms_norm(x, final_norm_g)
```

```python
import contextlib
import numpy as np
import concourse.bass as bass
import concourse.mybir as mybir
from concourse.bass_utils import run_bass_kernel_spmd

F32 = mybir.dt.float32
BF16 = mybir.dt.bfloat16
I32 = mybir.dt.int32
AF = mybir.ActivationFunctionType
ALU = mybir.AluOpType
AX = mybir.AxisListType

SAME_ENGINE_SYNC = True


class Cfg:
    D = 2048
    CH = 2048
    NSLOT = 8
    NCORES = 8
    NE = 64
    NG = 8
    TOPG = 4
    TOPK = 8
    DE = 512
    NH = 8
    HD = 128
    QL = 512
    KVL = 256
    ROPE = 64
    NADA = 6
    EPS = 1e-6
    ROUTED_SCALE = 2.5
    WINDOWS = ((128, 1), (512, 4), (2048, 16))
    CAP = 1024
    debug = False
    stop_after = 99


class Sem:
    def __init__(self, h, name):
        self.h = h
        self.name = name
        self.count = 0


class KB:
    def __init__(self, nc):
        self.nc = nc
        self.E = {"pe": nc.tensor, "act": nc.scalar, "dve": nc.vector, "pool": nc.gpsimd, "sp": nc.sync}
        self.esem = {}
        self.allsems = []
        for e in ["pe", "act", "dve", "pool"]:
            self.esem[e] = self.newsem("es_" + e)
        self.waited = {e: {} for e in self.E}
        self.lastw = {}
        self.readers = {}
        self.free_dsems = []
        self.nwaits = 0
        self.nops = 0

    def newsem(self, name):
        s = Sem(self.nc.alloc_semaphore(name=name), name)
        self.allsems.append(s)
        return s

    def dsem(self, name="d"):
        if self.free_dsems:
            return self.free_dsems.pop()
        return self.newsem("ds%d_%s" % (len(self.allsems), name))

    def release_dsems(self, sems):
        self.free_dsems.extend(sems)

    def _wait(self, eng, sem, val):
        if self.waited[eng].get(sem, 0) >= val:
            return
        self.E[eng].wait_ge(sem.h, val)
        self.waited[eng][sem] = val
        self.nwaits += 1

    def op(self, eng, issue, reads=(), writes=(), dsem=None):
        need = {}
        def add(ev):
            sem, val, peng = ev
            if eng == "pe" and peng == "pe":
                return
            if (not SAME_ENGINE_SYNC) and peng == eng and peng != "dma":
                return
            if need.get(sem, 0) < val:
                need[sem] = val
        for r in reads:
            if r in self.lastw:
                add(self.lastw[r])
        for w in writes:
            if w in self.lastw:
                add(self.lastw[w])
            for ev in self.readers.get(w, ()):
                add(ev)
        for sem, val in need.items():
            self._wait(eng, sem, val)
        inst = issue(self.E[eng])
        if dsem is not None:
            if dsem.count > 0 and not any(w.get(dsem, 0) >= dsem.count for w in self.waited.values()):
                raise RuntimeError("DMA semaphore %s reused while previous DMA may be in flight" % dsem.name)
            dsem.count += 16
            inst.then_inc(dsem.h, 16)
            ev = (dsem, dsem.count, "dma")
        else:
            s = self.esem[eng]
            s.count += 1
            inst.then_inc(s.h, 1)
            ev = (s, s.count, eng)
        for w in writes:
            self.lastw[w] = ev
            self.readers[w] = []
        for r in reads:
            if r not in writes:
                self.readers.setdefault(r, []).append(ev)
        self.nops += 1
        return ev

    def barrier(self, engines=("pe", "act", "dve", "pool", "sp")):
        for e in engines:
            for s in self.allsems:
                if s.count > 0:
                    self._wait(e, s, s.count)
        self.lastw = {}
        self.readers = {}


def _slopes(nh):
    return [float(np.float32(2.0) ** np.float32(-8.0 * (h + 1) / nh)) for h in range(nh)]


class Builder:
    def __init__(self, cfg):
        self.cfg = cfg
        self.nc = bass.Bass("TRN2", target_bir_lowering=False)
        self.kb = KB(self.nc)
        self.dram_in = {}
        self.dram_out = {}
        self.scratch = {}

    def din(self, name, shape, dtype=F32):
        t = self.nc.dram_tensor(name, list(shape), dtype, kind="ExternalInput")
        self.dram_in[name] = (tuple(shape), dtype)
        return t.ap()

    def dscr(self, name, shape, dtype, internal=False):
        kind = "ExternalOutput" if (self.cfg.debug and not internal) else "Internal"
        t = self.nc.dram_tensor(name, list(shape), dtype, kind=kind)
        self.scratch[name] = (tuple(shape), dtype)
        return t.ap()

    def dout(self, name, shape, dtype=F32):
        t = self.nc.dram_tensor(name, list(shape), dtype, kind="ExternalOutput")
        self.dram_out[name] = (tuple(shape), dtype)
        return t.ap()


MAGIC = 12582912.0
TWO_PI = 2.0 * np.pi
CW1 = 6.28125
CW2 = float(np.float32(TWO_PI - CW1))
CW3 = float(TWO_PI - CW1 - CW2)


def _b(cls):
    return cls


class Phases(Builder):
    _uid = 0

    def _nm(self, name):
        Phases._uid += 1
        return "%s_u%d" % (name, Phases._uid)

    def sb(self, st, name, shape, dtype):
        return st.enter_context(self.nc.sbuf_tensor(self._nm(name), list(shape), dtype))

    def ps(self, st, name, shape, dtype=F32):
        return st.enter_context(self.nc.psum_tensor(self._nm(name), list(shape), dtype))

    def dma(self, eng, out, in_, reads, writes, sem, **kw):
        return self.kb.op(eng, lambda e: e.dma_start(out=out, in_=in_, **kw), reads=reads, writes=writes, dsem=sem)

    def declare_io(self):
        c = self.cfg
        NT = c.NSLOT * c.CH
        self.xk = self.din("xk", [NT, c.D])
        self.posk = self.din("posk", [1, NT], I32)
        self.flags = self.din("flags", [1, c.NSLOT * 8])
        self.cT = self.din("cT", [128, 16])
        self.w_ada = self.din("w_ada", [c.D, c.NADA * c.D])
        self.b_adaT = self.din("b_adaT", [128, 96])
        self.gvecT = self.din("gvecT", [128, 48])
        self.w_in = self.din("w_in", [c.D, 3968])
        self.gsmallT = self.din("gsmallT", [128, 22])
        self.w_uq = self.din("w_uq", [c.QL, 2048])
        self.w_ukv = self.din("w_ukv", [c.KVL, 2048])
        self.w_o = self.din("w_o", [c.D, c.D])
        self.w_router = self.din("w_router", [c.D, c.NE])
        self.rbias = self.din("rbias", [1, c.NE])
        self.w_eg = self.din("w_eg", [c.NE, c.D, c.DE])
        self.w_eu = self.din("w_eu", [c.NE, c.D, c.DE])
        self.w_ed = self.din("w_ed", [c.NE, c.DE, c.D])
        self.w_sg = self.din("w_sg", [c.D, c.DE])
        self.w_su = self.din("w_su", [c.D, c.DE])
        self.w_sd = self.din("w_sd", [c.DE, c.D])
        self.ident_d = self.din("ident", [128, 128])
        self.invfreq2 = self.din("invfreq2", [64, 1])
        self.lnmult = self.din("lnmult", [128, 17, 128])
        self.tri_d = self.din("tri", [128, 128])
        self.lstrict_d = self.din("lstrict", [128, 128])
        self.iota_d = self.din("iota64", [128, c.NE])
        self.out = self.dout("out", [c.CH, c.D])
        self.HT = self.dscr("HT", [2, 128, 16, c.CH], BF16)
        self.KT_A = self.dscr("KT_A", [c.NH, 128, NT], BF16)
        self.KT_B = self.dscr("KT_B", [65, NT], BF16)
        self.V_AUG = self.dscr("V_AUG", [NT, c.NH * 129], BF16)
        self.KAT = self.dscr("KAT", [c.NH, 128, 2 * c.CH], BF16)
        self.VA_AUG = self.dscr("VA_AUG", [2 * c.CH, c.NH * 129], BF16)
        self.QAT = self.dscr("QAT", [c.NH, 128, c.CH], BF16)
        self.NEGCA = self.dscr("NEGCA", [c.NH, c.CH], BF16)
        self.QT_A = self.dscr("QT_A", [c.NH, 128, c.CH], BF16)
        self.QT_B = self.dscr("QT_B", [c.NH, 65, c.CH], BF16)
        self.OB = self.dscr("OB", [c.CH, 1024], F32)
        self.MIXT = self.dscr("MIXT", [128, 16, c.CH], BF16)
        self.XMID = self.dscr("XMID", [c.CH, c.D], F32)
        self.H2T = self.dscr("H2T", [128, 16, c.CH], BF16)
        self.MODT = self.dscr("MODT", [128, 96], F32)
        self.XG = self.dscr("XG", [c.NE * max(c.CAP, 128), c.D], BF16, internal=True)
        self.YS = self.dscr("YS", [c.NE * max(c.CAP, 128), c.D], BF16, internal=True)
        self.YSH = self.dscr("YSH", [c.CH, c.D], F32)

    def phase0(self, st):
        c, kb, nc = self.cfg, self.kb, self.nc
        P = self.P = {}
        P["ident_f"] = self.sb(st, "ident_f", [128, 128], F32)
        P["ident_b"] = self.sb(st, "ident_b", [128, 128], BF16)
        P["ones_f"] = self.sb(st, "ones_f", [128, 128], F32)
        P["ones_b"] = self.sb(st, "ones_b", [128, 128], BF16)
        P["modT"] = self.sb(st, "modT", [128, 96], F32)
        P["gvec"] = self.sb(st, "gvec", [128, 48], F32)
        P["gsm"] = self.sb(st, "gsm", [128, 22], F32)
        P["gmodA"] = self.sb(st, "gmodA", [128, 16], F32)
        P["gmodF"] = self.sb(st, "gmodF", [128, 16], F32)
        P["flag8"] = self.sb(st, "flag8", [128, c.NSLOT * 8], F32)
        P["invf"] = self.sb(st, "invf", [64, 1], F32)
        P["sgn"] = self.sb(st, "sgn", [64, 1], F32)
        P["kmax2"] = self.sb(st, "kmax2", [1, 2], F32)
        cs = [kb.dsem("c%d" % i) for i in range(8)]
        s0 = cs[0]
        self.dma("sp", P["ident_f"][:], self.ident_d, [], ["ident_f"], cs[0])
        self.dma("pool", P["ident_b"][:], self.ident_d, [], ["ident_b"], cs[1])
        self.dma("sp", P["gvec"][:], self.gvecT, [], ["gvec"], cs[2])
        self.dma("sp", P["gsm"][:], self.gsmallT, [], ["gsm"], cs[3])
        self.dma("sp", P["flag8"][:], self.flags.partition_broadcast(128), [], ["flag8"], cs[4])
        self.dma("sp", P["invf"][:], self.invfreq2, [], ["invf"], cs[5])
        kb.op("dve", lambda e: e.memset(P["ones_f"][:], 1.0), writes=["ones_f"])
        kb.op("dve", lambda e: e.memset(P["ones_b"][:], 1.0), writes=["ones_b"])
        kb.op("dve", lambda e: e.memset(P["sgn"][0:32, :], -1.0), writes=["sgn0"])
        kb.op("dve", lambda e: e.memset(P["sgn"][32:64, :], 1.0), writes=["sgn1"])
        kb.op("dve", lambda e: e.memset(P["kmax2"][:], 0.0), writes=["kmax2"])

        with contextlib.ExitStack() as ph:
            cT = self.sb(ph, "cT_s", [128, 16], F32)
            sc = self.sb(ph, "sc_s", [128, 16], F32)
            bT = self.sb(ph, "bT_s", [128, 96], F32)
            wbuf = [self.sb(ph, "wada%d" % i, [128, 16, 512], F32) for i in range(2)]
            wsem = [kb.dsem("wada%d" % i) for i in range(2)]
            mps = self.ps(ph, "mod_ps", [128, 96], F32)
            self.dma("sp", cT[:], self.cT, [], ["cT"], cs[6])
            self.dma("sp", bT[:], self.b_adaT, [], ["bT"], cs[7])
            kb.op("act", lambda e: e.activation(out=sc[:], in_=cT[:], func=AF.Silu), reads=["cT"], writes=["sc"])
            wv = self.w_ada.rearrange("(kc p) n -> p kc n", p=128)
            npieces = (c.NADA * c.D) // 512
            for pi in range(npieces):
                b = pi % 2
                eng = "sp" if b == 0 else "pool"
                self.dma(eng, wbuf[b][:], wv[:, :, pi * 512:(pi + 1) * 512], [], ["wada%d" % b], wsem[b])
                for fb in range(4):
                    col = pi * 4 + fb
                    for kc in range(16):
                        kb.op("pe", lambda e, b=b, fb=fb, kc=kc, col=col: e.matmul(
                            mps[:, col:col + 1], lhsT=wbuf[b][:, kc, fb * 128:(fb + 1) * 128], rhs=sc[:, kc:kc + 1],
                            start=(kc == 0), stop=(kc == 15)),
                            reads=["wada%d" % b, "sc"], writes=["mod_ps"])
            kb.op("dve", lambda e: e.tensor_tensor(out=P["modT"][:], in0=mps[:], in1=bT[:], op=ALU.add),
                  reads=["mod_ps", "bT"], writes=["modT"])
            kb.op("dve", lambda e: e.scalar_tensor_tensor(out=P["gmodA"][:], in0=P["modT"][:, 16:32], scalar=1.0,
                                                          in1=P["gvec"][:, 0:16], op0=ALU.add, op1=ALU.mult),
                  reads=["modT", "gvec"], writes=["gmodA"])
            kb.op("dve", lambda e: e.scalar_tensor_tensor(out=P["gmodF"][:], in0=P["modT"][:, 64:80], scalar=1.0,
                                                          in1=P["gvec"][:, 16:32], op0=ALU.add, op1=ALU.mult),
                  reads=["modT", "gvec"], writes=["gmodF"])
            if c.debug:
                self.dma("sp", self.MODT, P["modT"][:], ["modT"], [], kb.dsem("dbg"))
            kb.barrier()
            kb.release_dsems(wsem + cs)

    def bcast_tile(self, st, ph, name, srcT, key):
        kb, P = self.kb, self.P
        dst = self.sb(st, name, [128, 2048], F32)
        dg = self.sb(ph, name + "_dg", [128, 128], F32)
        for ci in range(16):
            pst_full, pkk = self.pbank()
            pst = pst_full[:, 0:128]
            kb.op("dve", lambda e, ci=ci: e.tensor_scalar(out=dg[:], in0=P["ident_f"][:], scalar1=srcT[:, ci:ci + 1],
                                                         scalar2=None, op0=ALU.mult),
                  reads=["ident_f", key], writes=[name + "_dg"])
            kb.op("pe", lambda e, pst=pst: e.matmul(pst, lhsT=P["ones_f"][:], rhs=dg[:], start=True, stop=True),
                  reads=["ones_f", name + "_dg"], writes=[pkk])
            kb.op("act", lambda e, ci=ci, pst=pst: e.activation(out=dst[:, ci * 128:(ci + 1) * 128], in_=pst, func=AF.Copy),
                  reads=[pkk], writes=[name])
        return dst


def _T128(v):
    v = np.asarray(v).reshape(-1, 128)
    return np.ascontiguousarray(v.T)


def lnmult_table():
    tab = np.full((17, 128, 128), -1e30, np.float32)
    k = np.arange(128)[:, None]
    q = np.arange(128)[None, :]
    for dl in range(17):
        diff = 128 * dl + q - k
        cnt = np.zeros((128, 128), np.int32)
        for w, d in Cfg.WINDOWS:
            cnt += ((diff >= 0) & (diff <= w) & (diff % d == 0)).astype(np.int32)
        tab[dl] = np.where(cnt > 0, np.log(np.maximum(cnt, 1)).astype(np.float32), np.float32(-1e30))
    return np.ascontiguousarray(tab.transpose(1, 0, 2))


def prepare_shared(inp, cfg):
    c = cfg
    sh = {}
    sh["cT"] = _T128(inp["c"][0])
    sh["w_ada"] = np.ascontiguousarray(inp["w_ada"][0])
    sh["b_adaT"] = _T128(inp["b_ada"][0])
    sh["gvecT"] = np.concatenate([_T128(inp["norm_attn_g"][0]), _T128(inp["norm_ffn_g"][0]),
                                  _T128(inp["final_norm_g"])], axis=1)
    w_in = inp["w_in"][0]
    rope = w_in[:, 3840:3904]
    sh["w_in"] = np.concatenate([w_in, rope[:, 32:64], rope[:, 0:32]], axis=1)
    sh["gsmallT"] = np.concatenate([_T128(inp["g_q"][0]), _T128(inp["g_kv"][0]),
                                    _T128(inp["g_out_swa"][0]), _T128(inp["g_out_mla"][0])], axis=1)
    wq = inp["w_uq"][0].reshape(c.QL, c.NH, 192)
    sh["w_uq"] = np.concatenate([wq[:, :, 0:128].reshape(c.QL, -1), wq[:, :, 128:192].reshape(c.QL, -1),
                                 np.concatenate([wq[:, :, 160:192], wq[:, :, 128:160]], axis=2).reshape(c.QL, -1)],
                                axis=1)
    wkv = inp["w_ukv"][0].reshape(c.KVL, c.NH, 256)
    sh["w_ukv"] = np.concatenate([wkv[:, :, 0:128].reshape(c.KVL, -1), wkv[:, :, 128:256].reshape(c.KVL, -1)], axis=1)
    sh["w_o"] = np.ascontiguousarray(inp["w_o"][0])
    sh["w_router"] = np.ascontiguousarray(inp["w_router"][0])
    sh["rbias"] = np.ascontiguousarray(inp["router_bias"][0][None, :])
    sh["w_eg"] = inp["w_exp_gate"][0]
    sh["w_eu"] = inp["w_exp_up"][0]
    sh["w_ed"] = inp["w_exp_down"][0]
    sh["w_sg"] = inp["w_sh_gate"][0]
    sh["w_su"] = inp["w_sh_up"][0]
    sh["w_sd"] = inp["w_sh_down"][0]
    sh["ident"] = np.eye(128, dtype=np.float32)
    half = c.ROPE // 2
    invf = (np.float32(10000.0) ** (-np.arange(half, dtype=np.float32) / np.float32(half))).astype(np.float32)
    sh["invfreq2"] = np.concatenate([invf, invf])[:, None].astype(np.float32)
    sh["lnmult"] = lnmult_table()
    sh["tri"] = (np.arange(128)[:, None] <= np.arange(128)[None, :]).astype(np.float32)
    sh["lstrict"] = (np.arange(128)[:, None] < np.arange(128)[None, :]).astype(np.float32)
    sh["iota64"] = np.ascontiguousarray(np.broadcast_to(np.arange(c.NE, dtype=np.float32)[None, :], (128, c.NE)))
    return sh


def slot_order(core, nchunks, nslot):
    order = [core - s for s in range(core + 1)]
    rest = [j for j in range(nchunks) if j > core]
    order = order + rest
    order = order[:nslot] + [-1] * max(0, nslot - len(order))
    valid = [1.0 if s <= core and order[s] >= 0 else 0.0 for s in range(nslot)]
    return order, valid


def prepare_core(inp, cfg, core, nchunks):
    c = cfg
    x = inp["x"][0]
    pos = inp["positions"][0]
    order, valid = slot_order(core, nchunks, c.NSLOT)
    xs, ps = [], []
    for s, j in enumerate(order):
        if j >= 0:
            xs.append(x[j * c.CH:(j + 1) * c.CH])
            ps.append(pos[j * c.CH:(j + 1) * c.CH])
        else:
            xs.append(x[0:c.CH])
            ps.append(pos[0:c.CH])
    d = {}
    d["xk"] = np.concatenate(xs, axis=0)
    d["posk"] = np.concatenate(ps)[None, :].astype(np.int32)
    d["flags"] = np.repeat(np.asarray(valid, np.float32), 8)[None, :]
    return d


class Phases2(Phases):
    def alloc_psum(self, ph, nf32=6, nbf=2):
        self.pbanks = [self.ps(ph, "pb%d" % i, [128, 512], F32) for i in range(nf32)]
        self.pbi = 0
        self.tbanks = [self.ps(ph, "tb%d" % i, [128, 8, 128], BF16) for i in range(nbf)]
        self.tbi = 0

    def pbank(self):
        i = self.pbi % len(self.pbanks)
        self.pbi += 1
        return self.pbanks[i], "pb%d" % i

    def tbank(self):
        i = self.tbi % len(self.tbanks)
        self.tbi += 1
        return self.tbanks[i], "tb%d" % i

    def alloc_normT(self, ph, pref, Fmax, nslots=2):
        W = {"pref": pref, "n": nslots, "i": 0}
        W["junk"] = self.sb(ph, pref + "_junk", [128, Fmax], BF16)
        W["xn"] = [self.sb(ph, pref + "_xn%d" % i, [128, Fmax], BF16) for i in range(nslots)]
        W["st"] = [self.sb(ph, pref + "_st%d" % i, [128, 4], F32) for i in range(nslots)]
        return W

    def norm_T(self, W, src, src_keys, F, gT, shiftT, gkeys, dst_of, dst_keys, eps_scale=None):
        kb, P, c = self.kb, self.P, self.cfg
        i = W["i"] % W["n"]
        W["i"] += 1
        pref = W["pref"]
        junk, xn, stt = W["junk"], W["xn"][i], W["st"][i]
        kj, kx, ks = pref + "_junk", pref + "_xn%d" % i, pref + "_st%d" % i
        kb.op("act", lambda e: e.activation(out=junk[:, 0:F], in_=src, func=AF.Square, accum_out=stt[:, 0:1]),
              reads=src_keys, writes=[kj, ks])
        kb.op("dve", lambda e: e.tensor_scalar(out=stt[:, 1:2], in0=stt[:, 0:1], scalar1=1.0 / F, scalar2=c.EPS,
                                               op0=ALU.mult, op1=ALU.add), reads=[ks], writes=[ks])
        kb.op("act", lambda e: e.activation(out=stt[:, 2:3], in_=stt[:, 1:2], func=AF.Sqrt), reads=[ks], writes=[ks])
        kb.op("dve", lambda e: e.reciprocal(out=stt[:, 3:4], in_=stt[:, 2:3]), reads=[ks], writes=[ks])
        kb.op("act", lambda e: e.activation(out=xn[:, 0:F], in_=src, func=AF.Copy, scale=stt[:, 3:4]),
              reads=list(src_keys) + [ks], writes=[kx])
        nchunk = F // 128
        for c0 in range(0, nchunk, 8):
            tb, tk = self.tbank()
            n = min(8, nchunk - c0)
            for j in range(n):
                ci = c0 + j
                kb.op("pe", lambda e, j=j, ci=ci: e.transpose(out=tb[:, j, :], in_=xn[:, ci * 128:(ci + 1) * 128],
                                                              identity=P["ident_b"][:]),
                      reads=[kx, "ident_b"], writes=[tk])
            for j in range(n):
                ci = c0 + j
                if shiftT is not None:
                    kb.op("dve", lambda e, j=j, ci=ci: e.tensor_scalar(
                        out=dst_of(ci), in0=tb[:, j, :], scalar1=gT[:, ci:ci + 1], scalar2=shiftT[:, ci:ci + 1],
                        op0=ALU.mult, op1=ALU.add), reads=[tk] + gkeys, writes=dst_keys)
                else:
                    kb.op("dve", lambda e, j=j, ci=ci: e.tensor_scalar(
                        out=dst_of(ci), in0=tb[:, j, :], scalar1=gT[:, ci:ci + 1], scalar2=None,
                        op0=ALU.mult), reads=[tk] + gkeys, writes=dst_keys)

    def alloc_rope(self, ph, pref):
        R = {"pref": pref}
        for n in ["posi"]:
            R[n] = self.sb(ph, pref + n, [64, 512], I32)
        for n in ["ang", "t", "kk", "r", "r2", "sin", "cos"]:
            R[n] = self.sb(ph, pref + n, [64, 512], F32)
        R["sem"] = self.kb.dsem(pref)
        return R

    def rope_tables(self, R, pos_ap):
        kb, P = self.kb, self.P
        p = R["pref"]
        K = lambda n: p + n
        self.dma("sp", R["posi"][:], pos_ap.partition_broadcast(64), [], [K("posi")], R["sem"])
        kb.op("dve", lambda e: e.tensor_copy(out=R["ang"][:], in_=R["posi"][:]), reads=[K("posi")], writes=[K("ang")])
        kb.op("dve", lambda e: e.tensor_scalar(out=R["ang"][:], in0=R["ang"][:], scalar1=P["invf"][:, 0:1], scalar2=None,
                                               op0=ALU.mult), reads=["invf"], writes=[K("ang")])
        kb.op("dve", lambda e: e.tensor_scalar(out=R["t"][:], in0=R["ang"][:], scalar1=float(1.0 / TWO_PI), scalar2=MAGIC,
                                               op0=ALU.mult, op1=ALU.add), reads=[K("ang")], writes=[K("t")])
        kb.op("dve", lambda e: e.tensor_scalar(out=R["kk"][:], in0=R["t"][:], scalar1=-MAGIC, scalar2=None,
                                               op0=ALU.add), reads=[K("t")], writes=[K("kk")])
        kb.op("dve", lambda e: e.scalar_tensor_tensor(out=R["r"][:], in0=R["kk"][:], scalar=-CW1, in1=R["ang"][:],
                                                      op0=ALU.mult, op1=ALU.add), reads=[K("ang"), K("kk")], writes=[K("r")])
        for cw in (CW2, CW3):
            kb.op("dve", lambda e, cw=cw: e.scalar_tensor_tensor(out=R["r"][:], in0=R["kk"][:], scalar=-cw, in1=R["r"][:],
                                                                 op0=ALU.mult, op1=ALU.add), reads=[K("kk")], writes=[K("r")])
        kb.op("dve", lambda e: e.tensor_scalar(out=R["t"][:], in0=R["r"][:], scalar1=float(np.pi / 2), scalar2=None,
                                               op0=ALU.is_gt), reads=[K("r")], writes=[K("t")])
        kb.op("dve", lambda e: e.scalar_tensor_tensor(out=R["r2"][:], in0=R["t"][:], scalar=-float(TWO_PI), in1=R["r"][:],
                                                      op0=ALU.mult, op1=ALU.add), reads=[K("t"), K("r")], writes=[K("r2")])
        kb.op("dve", lambda e: e.tensor_scalar(out=R["r2"][:], in0=R["r2"][:], scalar1=float(np.pi / 2), scalar2=None,
                                               op0=ALU.add), reads=[], writes=[K("r2")])
        lim = 3.1415925
        for n in ["r", "r2"]:
            kb.op("dve", lambda e, n=n: e.tensor_scalar(out=R[n][:], in0=R[n][:], scalar1=lim, scalar2=-lim,
                                                        op0=ALU.min, op1=ALU.max), reads=[], writes=[K(n)])
        kb.op("act", lambda e: e.activation(out=R["sin"][:], in_=R["r"][:], func=AF.Sin), reads=[K("r")], writes=[K("sin")])
        kb.op("act", lambda e: e.activation(out=R["cos"][:], in_=R["r2"][:], func=AF.Sin), reads=[K("r2")], writes=[K("cos")])
        kb.op("dve", lambda e: e.tensor_scalar(out=R["sin"][:], in0=R["sin"][:], scalar1=P["sgn"][:, 0:1], scalar2=None,
                                               op0=ALU.mult), reads=["sgn0", "sgn1"], writes=[K("sin")])

    def apply_rope(self, R, a_ps, b_ps, ps_keys, out_ap, out_keys, tmp, tmpk):
        kb = self.kb
        p = R["pref"]
        kb.op("dve", lambda e: e.tensor_tensor(out=tmp[0][:], in0=a_ps, in1=R["cos"][:], op=ALU.mult),
              reads=ps_keys + [p + "cos"], writes=[tmpk + "0"])
        kb.op("dve", lambda e: e.tensor_tensor(out=tmp[1][:], in0=b_ps, in1=R["sin"][:], op=ALU.mult),
              reads=ps_keys + [p + "sin"], writes=[tmpk + "1"])
        kb.op("pool", lambda e: e.tensor_tensor(out=out_ap, in0=tmp[0][:], in1=tmp[1][:], op=ALU.add),
              reads=[tmpk + "0", tmpk + "1"], writes=out_keys)


class Phases3(Phases2):
    def phase1a(self):
        c, kb, P = self.cfg, self.kb, self.P
        NG = c.NSLOT * c.CH // 512
        gps = c.CH // 512
        with contextlib.ExitStack() as ph:
            self.alloc_psum(ph, 6, 2)
            wkv = self.sb(ph, "wkv", [128, 16, 384], BF16)
            wukv = self.sb(ph, "wukv", [128, 2, 2048], BF16)
            ws = kb.dsem("w1a")
            self.dma("pool", wkv[:], self.w_in[:, 3584:3968].rearrange("(kc p) n -> p kc n", p=128), [], ["wkv"], ws)
            ws2 = kb.dsem("w1a2")
            self.dma("pool", wukv[:], self.w_ukv.rearrange("(kc p) n -> p kc n", p=128), [], ["wukv"], ws2)
            NX = 3
            xt = [self.sb(ph, "xt%d" % i, [128, 2048], F32) for i in range(NX)]
            xs = [kb.dsem("xt%d" % i) for i in range(NX)]
            WN = self.alloc_normT(ph, "n1", 2048, 2)
            hT = [self.sb(ph, "hT%d" % i, [128, 16, 512], BF16) for i in range(2)]
            hs = [kb.dsem("hT%d" % i) for i in range(2)]
            ckvnT = [self.sb(ph, "ckvnT%d" % i, [128, 2, 512], BF16) for i in range(2)]
            kT_st = [self.sb(ph, "kTst%d" % i, [128, 8, 512], BF16) for i in range(2)]
            kTs = [kb.dsem("kTst%d" % i) for i in range(2)]
            kb_st = [self.sb(ph, "kbst%d" % i, [65, 512], BF16) for i in range(2)]
            kbs = [kb.dsem("kbst%d" % i) for i in range(2)]
            va_st = [self.sb(ph, "vast%d" % i, [128, 4, 8 * 129], BF16) for i in range(2)]
            vas = [kb.dsem("vast%d" % i) for i in range(2)]
            sq = self.sb(ph, "sq1a", [128, 8, 512], BF16)
            sqr = self.sb(ph, "sqr1a", [64, 512], BF16)
            sqmax = self.sb(ph, "sqmax1a", [128, 512], BF16)
            kmrun = self.sb(ph, "kmrun", [1, 512], F32)
            tmp = [self.sb(ph, "rtmp%d" % i, [64, 512], F32) for i in range(2)]
            R = self.alloc_rope(ph, "r1a")
            kb.op("dve", lambda e: e.memset(kmrun[:], 0.0), writes=["kmrun"])
            for i in range(2):
                kb.op("dve", lambda e, i=i: e.memset(kb_st[i][64:65, :], 1.0), writes=["kbst%d_one" % i])
            xv = self.xk.rearrange("(n p) d -> n p d", p=128)
            tile_i = 0
            for g in range(NG):
                slot = g // gps
                tok0 = g * 512
                b = g % 2
                self.rope_tables(R, self.posk[:, tok0:tok0 + 512])
                for t in range(4):
                    xi = tile_i % NX
                    tile_i += 1
                    self.dma("sp", xt[xi][:], xv[g * 4 + t], [], ["xt%d" % xi], xs[xi])
                    self.norm_T(WN, xt[xi][:], ["xt%d" % xi], 2048, P["gmodA"], P["modT"], ["gmodA", "modT"],
                                lambda ci, t=t, b=b: hT[b][:, ci, t * 128:(t + 1) * 128], ["hT%d" % b])
                if slot < 2:
                    self.dma("sp", self.HT[slot, :, :, (g % gps) * 512:(g % gps + 1) * 512], hT[b][:], ["hT%d" % b], [], hs[b])
                for t in range(4):
                    pb, pk = self.pbank()
                    for ci in range(16):
                        kb.op("pe", lambda e, ci=ci, t=t, pb=pb: e.matmul(
                            pb[:, 0:256], lhsT=hT[b][:, ci, t * 128:(t + 1) * 128], rhs=wkv[:, ci, 0:256],
                            start=(ci == 0), stop=(ci == 15)), reads=["hT%d" % b, "wkv"], writes=[pk])
                    self.norm_T(WN, pb[:, 0:256], [pk], 256, P["gsm"][:, 4:6], None, ["gsm"],
                                lambda ci, t=t, b=b: ckvnT[b][:, ci, t * 128:(t + 1) * 128], ["ckvnT%d" % b])
                pa, pak = self.pbank()
                pbb, pbk = self.pbank()
                for (pp, ppk, c0) in ((pa, pak, 256), (pbb, pbk, 320)):
                    for ci in range(16):
                        kb.op("pe", lambda e, ci=ci, pp=pp, c0=c0: e.matmul(
                            pp[0:64, :], lhsT=wkv[:, ci, c0:c0 + 64], rhs=hT[b][:, ci, :],
                            start=(ci == 0), stop=(ci == 15)), reads=["hT%d" % b, "wkv"], writes=[ppk])
                self.apply_rope(R, pa[0:64, :], pbb[0:64, :], [pak, pbk], kb_st[b][0:64, :], ["kbst%d" % b], tmp, "rtmp")
                kb.op("act", lambda e: e.activation(out=sqr[:], in_=kb_st[b][0:64, :], func=AF.Square),
                      reads=["kbst%d" % b], writes=["sqr1a"])
                for h in range(c.NH):
                    pb, pk = self.pbank()
                    for kc in range(2):
                        kb.op("pe", lambda e, kc=kc, h=h, pb=pb: e.matmul(
                            pb[:], lhsT=wukv[:, kc, h * 128:(h + 1) * 128], rhs=ckvnT[b][:, kc, :],
                            start=(kc == 0), stop=(kc == 1)), reads=["ckvnT%d" % b, "wukv"], writes=[pk])
                    kb.op("act", lambda e, h=h, pb=pb: e.activation(out=kT_st[b][:, h, :], in_=pb[:], func=AF.Copy),
                          reads=[pk], writes=["kTst%d" % b])
                    kb.op("act", lambda e, h=h, pb=pb: e.activation(out=sq[:, h, :], in_=pb[:], func=AF.Square),
                          reads=[pk], writes=["sq1a"])
                kb.op("dve", lambda e: e.tensor_reduce(out=sqmax[:], in_=sq[:].rearrange("p h t -> p t h"),
                                                       axis=AX.X, op=ALU.max), reads=["sq1a"], writes=["sqmax1a"])
                pu, puk = self.pbank()
                kb.op("pe", lambda e: e.matmul(pu[0:1, :], lhsT=P["ones_b"][:, 0:1], rhs=sqmax[:], start=True, stop=False),
                      reads=["ones_b", "sqmax1a"], writes=[puk])
                kb.op("pe", lambda e: e.matmul(pu[0:1, :], lhsT=P["ones_b"][0:64, 0:1], rhs=sqr[:], start=False, stop=True),
                      reads=["ones_b", "sqr1a"], writes=[puk])
                kb.op("dve", lambda e: e.tensor_tensor(out=kmrun[:], in0=pu[0:1, :], in1=kmrun[:], op=ALU.max),
                      reads=[puk], writes=["kmrun"])
                for t in range(4):
                    for half in range(2):
                        pb, pk = self.pbank()
                        for kc in range(2):
                            kb.op("pe", lambda e, kc=kc, t=t, half=half, pb=pb: e.matmul(
                                pb[:], lhsT=ckvnT[b][:, kc, t * 128:(t + 1) * 128],
                                rhs=wukv[:, kc, 1024 + half * 512:1024 + (half + 1) * 512],
                                start=(kc == 0), stop=(kc == 1)), reads=["ckvnT%d" % b, "wukv"], writes=[pk])
                        dstv = va_st[b][:, t, :].rearrange("p (h f) -> p h f", f=129)[:, half * 4:(half + 1) * 4, 0:128]
                        kb.op("dve", lambda e, pb=pb, dstv=dstv: e.tensor_scalar(
                            out=dstv, in0=pb[:].rearrange("p (h f) -> p h f", f=128),
                            scalar1=P["flag8"][:, slot * 8:slot * 8 + 1], scalar2=None, op0=ALU.mult),
                            reads=[pk, "flag8"], writes=["vast%d" % b])
                    onec = va_st[b][:, t, :].rearrange("p (h f) -> p h f", f=129)[:, :, 128:129]
                    kb.op("pool", lambda e, onec=onec: e.tensor_copy(
                        out=onec, in_=P["flag8"][:, slot * 8:(slot + 1) * 8].unsqueeze(2)),
                        reads=["flag8"], writes=["vast%d" % b])
                self.dma("sp", self.KT_A[:, :, tok0:tok0 + 512].rearrange("h p t -> p h t"), kT_st[b][:],
                         ["kTst%d" % b], [], kTs[b])
                self.dma("sp", self.KT_B[:, tok0:tok0 + 512], kb_st[b][:], ["kbst%d" % b, "kbst%d_one" % b], [], kbs[b])
                self.dma("sp", self.V_AUG[tok0:tok0 + 512, :].rearrange("(t p) f -> p t f", p=128), va_st[b][:],
                         ["vast%d" % b], [], vas[b])
            kb.op("dve", lambda e: e.tensor_reduce(out=P["kmax2"][0:1, 0:1], in_=kmrun[:], axis=AX.X, op=ALU.max),
                  reads=["kmrun"], writes=["kmax2"])
            kb.barrier()
            kb.release_dsems([ws, ws2] + xs + hs + kTs + kbs + vas + [R["sem"]])


class Phases4(Phases3):
    def phase1b(self):
        c, kb, P = self.cfg, self.kb, self.P
        gps = c.CH // 512
        with contextlib.ExitStack() as ph:
            self.alloc_psum(ph, 6, 2)
            wkva = self.sb(ph, "wkva", [128, 16, 2048], BF16)
            ws = [kb.dsem("w1b%d" % i) for i in range(2)]
            for i in range(2):
                self.dma("pool", wkva[:, :, i * 1024:(i + 1) * 1024],
                         self.w_in[:, 1024 + i * 1024:2048 + i * 1024].rearrange("(kc p) n -> p kc n", p=128),
                         [], ["wkva%d" % i], ws[i])
            hT = [self.sb(ph, "hTb%d" % i, [128, 16, 512], BF16) for i in range(2)]
            hs = [kb.dsem("hTb%d" % i) for i in range(2)]
            kT_st = [self.sb(ph, "kaTst%d" % i, [128, 8, 512], BF16) for i in range(2)]
            kTs = [kb.dsem("kaTst%d" % i) for i in range(2)]
            va_st = [self.sb(ph, "vastb%d" % i, [128, 4, 8 * 129], BF16) for i in range(2)]
            vas = [kb.dsem("vastb%d" % i) for i in range(2)]
            sq = self.sb(ph, "sq1b", [128, 8, 512], BF16)
            sqmax = self.sb(ph, "sqmax1b", [128, 512], BF16)
            kmrun = self.sb(ph, "kmrunb", [1, 512], F32)
            kb.op("dve", lambda e: e.memset(kmrun[:], 0.0), writes=["kmrunb"])
            for g in range(2 * gps):
                slot = g // gps
                b = g % 2
                tokd = (1 - slot) * c.CH + (g % gps) * 512
                self.dma("sp", hT[b][:], self.HT[slot, :, :, (g % gps) * 512:(g % gps + 1) * 512], [], ["hTb%d" % b], hs[b])
                for h in range(c.NH):
                    pb, pk = self.pbank()
                    for ci in range(16):
                        kb.op("pe", lambda e, ci=ci, h=h, pb=pb: e.matmul(
                            pb[:], lhsT=wkva[:, ci, h * 128:(h + 1) * 128], rhs=hT[b][:, ci, :],
                            start=(ci == 0), stop=(ci == 15)), reads=["hTb%d" % b, "wkva0"], writes=[pk])
                    kb.op("act", lambda e, h=h, pb=pb: e.activation(out=kT_st[b][:, h, :], in_=pb[:], func=AF.Copy),
                          reads=[pk], writes=["kaTst%d" % b])
                    kb.op("act", lambda e, h=h, pb=pb: e.activation(out=sq[:, h, :], in_=pb[:], func=AF.Square),
                          reads=[pk], writes=["sq1b"])
                kb.op("dve", lambda e: e.tensor_reduce(out=sqmax[:], in_=sq[:].rearrange("p h t -> p t h"),
                                                       axis=AX.X, op=ALU.max), reads=["sq1b"], writes=["sqmax1b"])
                pu, puk = self.pbank()
                kb.op("pe", lambda e: e.matmul(pu[0:1, :], lhsT=P["ones_b"][:, 0:1], rhs=sqmax[:], start=True, stop=True),
                      reads=["ones_b", "sqmax1b"], writes=[puk])
                kb.op("dve", lambda e: e.tensor_tensor(out=kmrun[:], in0=pu[0:1, :], in1=kmrun[:], op=ALU.max),
                      reads=[puk], writes=["kmrunb"])
                for t in range(4):
                    for half in range(2):
                        pb, pk = self.pbank()
                        for ci in range(16):
                            kb.op("pe", lambda e, ci=ci, t=t, half=half, pb=pb: e.matmul(
                                pb[:], lhsT=hT[b][:, ci, t * 128:(t + 1) * 128],
                                rhs=wkva[:, ci, 1024 + half * 512:1024 + (half + 1) * 512],
                                start=(ci == 0), stop=(ci == 15)), reads=["hTb%d" % b, "wkva1"], writes=[pk])
                        dstv = va_st[b][:, t, :].rearrange("p (h f) -> p h f", f=129)[:, half * 4:(half + 1) * 4, 0:128]
                        kb.op("dve", lambda e, pb=pb, dstv=dstv: e.tensor_scalar(
                            out=dstv, in0=pb[:].rearrange("p (h f) -> p h f", f=128),
                            scalar1=P["flag8"][:, slot * 8:slot * 8 + 1], scalar2=None, op0=ALU.mult),
                            reads=[pk, "flag8"], writes=["vastb%d" % b])
                    onec = va_st[b][:, t, :].rearrange("p (h f) -> p h f", f=129)[:, :, 128:129]
                    kb.op("pool", lambda e, onec=onec: e.tensor_copy(
                        out=onec, in_=P["flag8"][:, slot * 8:(slot + 1) * 8].unsqueeze(2)),
                        reads=["flag8"], writes=["vastb%d" % b])
                self.dma("sp", self.KAT[:, :, tokd:tokd + 512].rearrange("h p t -> p h t"), kT_st[b][:],
                         ["kaTst%d" % b], [], kTs[b])
                self.dma("sp", self.VA_AUG[tokd:tokd + 512, :].rearrange("(t p) f -> p t f", p=128), va_st[b][:],
                         ["vastb%d" % b], [], vas[b])
            kb.op("dve", lambda e: e.tensor_reduce(out=P["kmax2"][0:1, 1:2], in_=kmrun[:], axis=AX.X, op=ALU.max),
                  reads=["kmrunb"], writes=["kmax2"])
            kb.barrier()
            kb.release_dsems(ws + hs + kTs + vas)

    def negc_row(self, u_ps, upk, kcol, dst, dstk, tmpf):
        kb, P = self.kb, self.P
        kb.op("act", lambda e: e.activation(out=tmpf[:], in_=u_ps, func=AF.Sqrt, scale=P["kmax2"][0:1, kcol:kcol + 1]),
              reads=[upk, "kmax2"], writes=["negc_tmp"])
        kb.op("dve", lambda e: e.tensor_scalar(out=dst, in0=tmpf[:], scalar1=-1.0, scalar2=None, op0=ALU.mult),
              reads=["negc_tmp"], writes=[dstk])

    def phase1c(self):
        c, kb, P = self.cfg, self.kb, self.P
        gps = c.CH // 512
        with contextlib.ExitStack() as ph:
            self.alloc_psum(ph, 6, 2)
            wq = self.sb(ph, "wq", [128, 16, 1536], BF16)
            wuq = self.sb(ph, "wuq", [128, 4, 2048], BF16)
            ws = [kb.dsem("w1c%d" % i) for i in range(3)]
            self.dma("pool", wq[:, :, 0:1024], self.w_in[:, 0:1024].rearrange("(kc p) n -> p kc n", p=128), [], ["wq0"], ws[0])
            self.dma("pool", wq[:, :, 1024:1536], self.w_in[:, 3072:3584].rearrange("(kc p) n -> p kc n", p=128), [], ["wq1"], ws[1])
            self.dma("pool", wuq[:], self.w_uq.rearrange("(kc p) n -> p kc n", p=128), [], ["wuq"], ws[2])
            WN = self.alloc_normT(ph, "n1c", 512, 2)
            hT = [self.sb(ph, "hTc%d" % i, [128, 16, 512], BF16) for i in range(2)]
            hs = [kb.dsem("hTc%d" % i) for i in range(2)]
            cqnT = [self.sb(ph, "cqnT%d" % i, [128, 4, 512], BF16) for i in range(2)]
            qa_st = [self.sb(ph, "qast%d" % i, [128, 8, 512], BF16) for i in range(2)]
            qas = [kb.dsem("qast%d" % i) for i in range(2)]
            qn_st = [self.sb(ph, "qnst%d" % i, [128, 8, 512], BF16) for i in range(2)]
            qns = [kb.dsem("qnst%d" % i) for i in range(2)]
            qb_st = [self.sb(ph, "qbst%d" % i, [64, 8, 512], BF16) for i in range(2)]
            qbs = [kb.dsem("qbst%d" % i) for i in range(2)]
            nca_st = [self.sb(ph, "ncast0", [1, 8, 512], BF16)] * 2
            ncas = [kb.dsem("ncast0")] * 2
            ncb_st = [self.sb(ph, "ncbst0", [1, 8, 512], BF16)] * 2
            ncbs = [kb.dsem("ncbst0")] * 2
            sq = self.sb(ph, "sq1c", [128, 512], BF16)
            sqr = self.sb(ph, "sqr1c", [64, 512], BF16)
            tmpf = self.sb(ph, "negc_tmp", [1, 512], F32)
            tmp = [self.sb(ph, "rtmpc%d" % i, [64, 512], F32) for i in range(2)]
            R = self.alloc_rope(ph, "r1c")
            for g in range(gps):
                b = g % 2
                tok0 = g * 512
                self.rope_tables(R, self.posk[:, tok0:tok0 + 512])
                self.dma("sp", hT[b][:], self.HT[0, :, :, tok0:tok0 + 512], [], ["hTc%d" % b], hs[b])
                for h in range(c.NH):
                    pb, pk = self.pbank()
                    for ci in range(16):
                        kb.op("pe", lambda e, ci=ci, h=h, pb=pb: e.matmul(
                            pb[:], lhsT=wq[:, ci, h * 128:(h + 1) * 128], rhs=hT[b][:, ci, :],
                            start=(ci == 0), stop=(ci == 15)), reads=["hTc%d" % b, "wq0"], writes=[pk])
                    kb.op("act", lambda e, h=h, pb=pb: e.activation(out=qa_st[b][:, h, :], in_=pb[:], func=AF.Copy),
                          reads=[pk], writes=["qast%d" % b])
                    kb.op("act", lambda e, pb=pb: e.activation(out=sq[:], in_=pb[:], func=AF.Square),
                          reads=[pk], writes=["sq1c"])
                    pu, puk = self.pbank()
                    kb.op("pe", lambda e, pu=pu: e.matmul(pu[0:1, :], lhsT=P["ones_b"][:, 0:1], rhs=sq[:], start=True, stop=True),
                          reads=["ones_b", "sq1c"], writes=[puk])
                    self.negc_row(pu[0:1, :], puk, 1, nca_st[b][0:1, h, :], "ncast0", tmpf)
                for t in range(4):
                    pb, pk = self.pbank()
                    for ci in range(16):
                        kb.op("pe", lambda e, ci=ci, t=t, pb=pb: e.matmul(
                            pb[:], lhsT=hT[b][:, ci, t * 128:(t + 1) * 128], rhs=wq[:, ci, 1024:1536],
                            start=(ci == 0), stop=(ci == 15)), reads=["hTc%d" % b, "wq1"], writes=[pk])
                    self.norm_T(WN, pb[:], [pk], 512, P["gsm"][:, 0:4], None, ["gsm"],
                                lambda ci, t=t, b=b: cqnT[b][:, ci, t * 128:(t + 1) * 128], ["cqnT%d" % b])
                for h in range(c.NH):
                    pb, pk = self.pbank()
                    for kc in range(4):
                        kb.op("pe", lambda e, kc=kc, h=h, pb=pb: e.matmul(
                            pb[:], lhsT=wuq[:, kc, h * 128:(h + 1) * 128], rhs=cqnT[b][:, kc, :],
                            start=(kc == 0), stop=(kc == 3)), reads=["cqnT%d" % b, "wuq"], writes=[pk])
                    kb.op("act", lambda e, h=h, pb=pb: e.activation(out=qn_st[b][:, h, :], in_=pb[:], func=AF.Copy),
                          reads=[pk], writes=["qnst%d" % b])
                    kb.op("act", lambda e, pb=pb: e.activation(out=sq[:], in_=pb[:], func=AF.Square),
                          reads=[pk], writes=["sq1c"])
                    pa, pak = self.pbank()
                    pbb, pbk = self.pbank()
                    for (pp, ppk, c0) in ((pa, pak, 1024), (pbb, pbk, 1536)):
                        for kc in range(4):
                            kb.op("pe", lambda e, kc=kc, pp=pp, c0=c0, h=h: e.matmul(
                                pp[0:64, :], lhsT=wuq[:, kc, c0 + h * 64:c0 + (h + 1) * 64], rhs=cqnT[b][:, kc, :],
                                start=(kc == 0), stop=(kc == 3)), reads=["cqnT%d" % b, "wuq"], writes=[ppk])
                    self.apply_rope(R, pa[0:64, :], pbb[0:64, :], [pak, pbk], qb_st[b][:, h, :], ["qbst%d" % b], tmp, "rtmpc")
                    kb.op("act", lambda e, h=h: e.activation(out=sqr[:], in_=qb_st[b][:, h, :], func=AF.Square),
                          reads=["qbst%d" % b], writes=["sqr1c"])
                    pu, puk = self.pbank()
                    kb.op("pe", lambda e, pu=pu: e.matmul(pu[0:1, :], lhsT=P["ones_b"][:, 0:1], rhs=sq[:], start=True, stop=False),
                          reads=["ones_b", "sq1c"], writes=[puk])
                    kb.op("pe", lambda e, pu=pu: e.matmul(pu[0:1, :], lhsT=P["ones_b"][0:64, 0:1], rhs=sqr[:], start=False, stop=True),
                          reads=["ones_b", "sqr1c"], writes=[puk])
                    self.negc_row(pu[0:1, :], puk, 0, ncb_st[b][0:1, h, :], "ncbst0", tmpf)
                self.dma("sp", self.QAT[:, :, tok0:tok0 + 512].rearrange("h p t -> p h t"), qa_st[b][:], ["qast%d" % b], [], qas[b])
                self.dma("sp", self.NEGCA[:, tok0:tok0 + 512].rearrange("(o h) t -> o h t", o=1), nca_st[b][:], ["ncast0"], [], ncas[b])
                self.dma("sp", self.QT_A[:, :, tok0:tok0 + 512].rearrange("h p t -> p h t"), qn_st[b][:], ["qnst%d" % b], [], qns[b])
                self.dma("sp", self.QT_B[:, 0:64, tok0:tok0 + 512].rearrange("h p t -> p h t"), qb_st[b][:], ["qbst%d" % b], [], qbs[b])
                self.dma("sp", self.QT_B[:, 64:65, tok0:tok0 + 512].rearrange("h o t -> o h t"), ncb_st[b][:], ["ncbst0"], [], ncbs[b])
            kb.barrier()
            kb.release_dsems(ws + hs + qas + qns + qbs + [ncas[0], ncbs[0], R["sem"]])


class Phases5(Phases4):
    def phase2(self):
        c, kb, P = self.cfg, self.kb, self.P
        NQ = c.CH // 128
        slopes = _slopes(c.NH)
        scale = float(c.HD ** -0.5)
        with contextlib.ExitStack() as ph:
            stp = [self.ps(ph, "stp%d" % i, [128, 8, 128], F32) for i in range(2)]
            accp = [self.ps(ph, "accp%d" % i, [128, 3, 129], F32) for i in range(3)]
            self.tbanks = [self.ps(ph, "tbs", [128, 8, 128], BF16)]
            self.tbi = 0
            def acc_of(h):
                return accp[h // 3][:, h % 3, :], "accp%d" % (h // 3)
            kaT = self.sb(ph, "kaT", [128, 8, 2 * c.CH], BF16)
            VA = self.sb(ph, "VAr", [128, 2 * NQ, 8 * 129], BF16)
            sems = [kb.dsem("p2_%d" % i) for i in range(12)]
            self.dma("sp", kaT[:], self.KAT.rearrange("h p t -> p h t"), [], ["kaT"], sems[0])
            nva = 4
            per = 2 * NQ // nva
            for i in range(nva):
                self.dma("sp", VA[:, i * per:(i + 1) * per, :],
                         self.VA_AUG[i * per * 128:(i + 1) * per * 128, :].rearrange("(t p) f -> p t f", p=128),
                         [], ["VA%d" % i], sems[8 + i])
            posq_i = self.sb(ph, "posq_i", [128, c.CH], I32)
            posq = self.sb(ph, "posq", [128, c.CH], F32)
            posk_i = self.sb(ph, "posk_i", [128, 2 * NQ], I32)
            poskc = self.sb(ph, "poskc", [128, 2 * NQ], F32)
            lnm = self.sb(ph, "lnm", [128, 17, 128], F32)
            NS = self.sb(ph, "NS", [128, 8, 128], F32)
            self.dma("sp", posq_i[:], self.posk[:, 0:c.CH].partition_broadcast(128), [], ["posq_i"], sems[3])
            self.dma("sp", posk_i[:, 0:NQ], self.posk[:, c.CH:2 * c.CH].rearrange("o (t p) -> p (o t)", p=128),
                     [], ["posk_i0"], sems[4], allow_slow_non_contiguous=True)
            self.dma("sp", posk_i[:, NQ:2 * NQ], self.posk[:, 0:c.CH].rearrange("o (t p) -> p (o t)", p=128),
                     [], ["posk_i1"], sems[5], allow_slow_non_contiguous=True)
            self.dma("sp", lnm[:], self.lnmult, [], ["lnm"], sems[6])
            kb.op("dve", lambda e: e.tensor_copy(out=posq[:], in_=posq_i[:]), reads=["posq_i"], writes=["posq"])
            kb.op("dve", lambda e: e.tensor_copy(out=poskc[:], in_=posk_i[:]), reads=["posk_i0", "posk_i1"], writes=["poskc"])
            kb.op("dve", lambda e: e.tensor_scalar(out=poskc[:], in0=poskc[:], scalar1=-1.0, scalar2=None, op0=ALU.mult),
                  reads=[], writes=["poskc"])
            for h in range(c.NH):
                kb.op("dve", lambda e, h=h: e.memset(NS[:, h, :], -slopes[h]), writes=["NS"])
            qa = [self.sb(ph, "qa%d" % i, [128, 8, 128], BF16) for i in range(2)]
            qsem = [kb.dsem("qa%d" % i) for i in range(2)]
            ngc = [self.sb(ph, "ngc%d" % i, [1, 8, 128], BF16) for i in range(2)]
            nsem = [kb.dsem("ngc%d" % i) for i in range(2)]
            dist = [self.sb(ph, "dist%d" % i, [128, 128], F32) for i in range(2)]
            u = [self.sb(ph, "u%d" % i, [128, 8, 128], F32) for i in range(2)]
            sp_ = [self.sb(ph, "sp%d" % i, [128, 8, 128], F32) for i in range(2)]
            pT = [self.sb(ph, "pT%d" % i, [128, 8, 128], BF16) for i in range(2)]
            oa = self.sb(ph, "oa", [128, 1024], F32)
            rl = self.sb(ph, "rl", [128, 8], F32)
            WN = self.alloc_normT(ph, "n2", 1024, 1)
            mx = [self.sb(ph, "mxa%d" % i, [128, 8, 128], BF16) for i in range(2)]
            msem = [kb.dsem("mxa%d" % i) for i in range(2)]
            it = 0
            for b in range(NQ):
                qb = b % 2
                self.dma("sp", qa[qb][:], self.QAT[:, :, b * 128:(b + 1) * 128].rearrange("h p t -> p h t"), [], ["qa%d" % qb], qsem[qb])
                self.dma("sp", ngc[qb][:], self.NEGCA[:, b * 128:(b + 1) * 128].rearrange("(o h) t -> o h t", o=1), [], ["ngc%d" % qb], nsem[qb])
                for i3 in range(3):
                    kb.op("dve", lambda e, i3=i3: e.memset(accp[i3][:], 0.0), writes=["accp%d" % i3])
                for dl in range(16, -1, -1):
                    j = NQ + b - dl
                    k = it % 2
                    it += 1
                    vak = "VA%d" % (j // per)
                    kb.op("act", lambda e, k=k, j=j: e.activation(
                        out=dist[k][:], in_=posq[:, b * 128:(b + 1) * 128], func=AF.Abs, bias=poskc[:, j:j + 1], scale=1.0),
                        reads=["posq", "poskc"], writes=["dist%d" % k])
                    kb.op("pool", lambda e, k=k: e.tensor_tensor(
                        out=u[k][:], in0=dist[k][:].unsqueeze(1).broadcast_to([128, 8, 128]), in1=NS[:], op=ALU.mult),
                        reads=["dist%d" % k, "NS"], writes=["u%d" % k])
                    kb.op("pool", lambda e, k=k, dl=dl: e.tensor_tensor(
                        out=u[k][:], in0=u[k][:], in1=lnm[:, dl, :].unsqueeze(1).broadcast_to([128, 8, 128]), op=ALU.add),
                        reads=["lnm"], writes=["u%d" % k])
                    for h in range(c.NH):
                        kb.op("pe", lambda e, h=h, k=k, j=j: e.matmul(
                            stp[k][:, h, :], lhsT=kaT[:, h, j * 128:(j + 1) * 128], rhs=qa[qb][:, h, :], start=True, stop=False),
                            reads=["kaT", "qa%d" % qb], writes=["stp%d" % k])
                        kb.op("pe", lambda e, h=h, k=k: e.matmul(
                            stp[k][:, h, :], lhsT=P["ones_b"][0:1, :], rhs=ngc[qb][0:1, h, :], start=False, stop=True),
                            reads=["ones_b", "ngc%d" % qb], writes=["stp%d" % k])
                    kb.op("dve", lambda e, k=k: e.scalar_tensor_tensor(
                        out=sp_[k][:], in0=stp[k][:], scalar=scale, in1=u[k][:], op0=ALU.mult, op1=ALU.add),
                        reads=["stp%d" % k, "u%d" % k], writes=["sp%d" % k])
                    kb.op("act", lambda e, k=k: e.activation(out=pT[k][:], in_=sp_[k][:], func=AF.Exp),
                          reads=["sp%d" % k], writes=["pT%d" % k])
                    for h in range(c.NH):
                        ah, ak = acc_of(h)
                        kb.op("pe", lambda e, h=h, k=k, j=j, ah=ah: e.matmul(
                            ah, lhsT=pT[k][:, h, :], rhs=VA[:, j, h * 129:(h + 1) * 129], start=False, stop=(dl == 0)),
                            reads=["pT%d" % k, vak], writes=[ak])
                for i3 in range(3):
                    nh = 3 if i3 < 2 else 2
                    kb.op("dve", lambda e, i3=i3, nh=nh: e.reciprocal(
                        out=rl[:, i3 * 3:i3 * 3 + nh].unsqueeze(2), in_=accp[i3][:, 0:nh, 128:129]),
                        reads=["accp%d" % i3], writes=["rl"])
                for h in range(c.NH):
                    ah, ak = acc_of(h)
                    kb.op("dve", lambda e, h=h, ah=ah: e.tensor_scalar(
                        out=oa[:, h * 128:(h + 1) * 128], in0=ah[:, 0:128], scalar1=rl[:, h:h + 1], scalar2=None, op0=ALU.mult),
                        reads=[ak, "rl"], writes=["oa"])
                mb = b % 2
                self.norm_T(WN, oa[:], ["oa"], 1024, P["gsm"][:, 6:14], None, ["gsm"],
                            lambda ci, mb=mb: mx[mb][:, ci, :], ["mxa%d" % mb])
                self.dma("sp", self.MIXT[:, 0:8, b * 128:(b + 1) * 128], mx[mb][:], ["mxa%d" % mb], [], msem[mb])
            kb.barrier()
            kb.release_dsems(sems + qsem + nsem + msem)


class Phases6(Phases5):
    def phase3(self):
        c, kb, P = self.cfg, self.kb, self.P
        NT = c.NSLOT * c.CH
        TPS = c.CH // 128
        NGQ = c.CH // 512
        scale = float((128 + c.ROPE) ** -0.5)
        with contextlib.ExitStack() as ph:
            self.alloc_psum(ph, 3, 0)
            accs = []
            for i in range(2):
                accs.append((self.ps(ph, "macc%da" % i, [128, 3, 129], F32), self.ps(ph, "macc%db" % i, [128, 1, 129], F32)))
            tri = self.sb(ph, "tri_b", [128, 128], BF16)
            ts = kb.dsem("tri")
            self.dma("pool", tri[:], self.tri_d, [], ["tri"], ts)
            ktB = self.sb(ph, "ktB", [65, NT], BF16)
            kbs = kb.dsem("ktB")
            self.dma("sp", ktB[:], self.KT_B, [], ["ktB"], kbs)
            ktA = [self.sb(ph, "ktA%d" % i, [128, NT], BF16) for i in range(2)]
            Vh = [self.sb(ph, "Vh%d" % i, [128, NT // 128, 129], BF16) for i in range(2)]
            kas = [[kb.dsem("ktA%d_%d" % (i, p)) for p in range(c.NSLOT)] for i in range(2)]
            vs = [[kb.dsem("Vh%d_%d" % (i, p)) for p in range(c.NSLOT)] for i in range(2)]
            qTa = [self.sb(ph, "qTa%d" % i, [128, c.CH], BF16) for i in range(2)]
            qTb = [self.sb(ph, "qTb%d" % i, [65, c.CH], BF16) for i in range(2)]
            qsa = [kb.dsem("qTa%d" % i) for i in range(2)]
            qsb = [kb.dsem("qTb%d" % i) for i in range(2)]
            pT = [self.sb(ph, "mpT%d" % i, [128, 512], BF16) for i in range(3)]
            obst = [self.sb(ph, "obst%d" % i, [128, 4, 128], F32) for i in range(2)]
            obs = [kb.dsem("obst%d" % i) for i in range(2)]
            rl = self.sb(ph, "mrl", [128, 4], F32)
            pti = 0
            it = 0
            for h in range(c.NH):
                hb = h % 2
                self.dma("sp", qTa[hb][:], self.QT_A[h], [], ["qTa%d" % hb], qsa[hb])
                self.dma("sp", qTb[hb][:], self.QT_B[h], [], ["qTb%d" % hb], qsb[hb])
                for p in range(c.NSLOT):
                    self.dma("sp", ktA[hb][:, p * c.CH:(p + 1) * c.CH], self.KT_A[h, :, p * c.CH:(p + 1) * c.CH],
                             [], ["ktA%d_%d" % (hb, p)], kas[hb][p])
                    self.dma("sp", Vh[hb][:, p * TPS:(p + 1) * TPS, :],
                             self.V_AUG[p * c.CH:(p + 1) * c.CH, h * 129:(h + 1) * 129].rearrange("(t p) f -> p t f", p=128),
                             [], ["Vh%d_%d" % (hb, p)], vs[hb][p])
                for g in range(NGQ):
                    ab = it % 2
                    it += 1
                    accA, accB = accs[ab]
                    def acc_of(i):
                        return (accA[:, i, :], "macc%da" % ab) if i < 3 else (accB[:, 0, :], "macc%db" % ab)
                    tiles = [(jt, max(0, jt - 4 * g), (jt - 4 * g) if jt >= 4 * g else None) for jt in range(4 * g + 4)]
                    tiles += [(kt, 0, None) for kt in range(TPS, c.NSLOT * TPS)]
                    kb.op("dve", lambda e: e.memset(accA[:], 0.0), writes=["macc%da" % ab])
                    kb.op("dve", lambda e: e.memset(accB[:], 0.0), writes=["macc%db" % ab])
                    first = {}
                    last = {}
                    for n, (kt, imin, idg) in enumerate(tiles):
                        for i in range(imin, 4):
                            first.setdefault(i, n)
                            last[i] = n
                    for n, (kt, imin, idg) in enumerate(tiles):
                        p = kt // TPS
                        pb, pk = self.pbank()
                        q0 = g * 512 + imin * 128
                        q1 = (g + 1) * 512
                        kb.op("pe", lambda e, pb=pb, kt=kt, imin=imin, q0=q0, q1=q1: e.matmul(
                            pb[:, imin * 128:512], lhsT=ktA[hb][:, kt * 128:(kt + 1) * 128], rhs=qTa[hb][:, q0:q1],
                            start=True, stop=False), reads=["ktA%d_%d" % (hb, p), "qTa%d" % hb], writes=[pk])
                        kb.op("pe", lambda e, pb=pb, kt=kt, imin=imin, q0=q0, q1=q1: e.matmul(
                            pb[:, imin * 128:512], lhsT=ktB[:, kt * 128:(kt + 1) * 128], rhs=qTb[hb][:, q0:q1],
                            start=False, stop=True), reads=["ktB", "qTb%d" % hb], writes=[pk])
                        pi = pti % 3
                        pti += 1
                        kb.op("act", lambda e, pb=pb, pi=pi, imin=imin: e.activation(
                            out=pT[pi][:, imin * 128:512], in_=pb[:, imin * 128:512], func=AF.Exp, scale=scale),
                            reads=[pk], writes=["mpT%d" % pi])
                        if idg is not None:
                            kb.op("pool", lambda e, pi=pi, idg=idg: e.tensor_tensor(
                                out=pT[pi][:, idg * 128:(idg + 1) * 128], in0=pT[pi][:, idg * 128:(idg + 1) * 128],
                                in1=tri[:], op=ALU.mult), reads=["tri"], writes=["mpT%d" % pi])
                        for i in range(imin, 4):
                            ai, ak = acc_of(i)
                            kb.op("pe", lambda e, ai=ai, pi=pi, i=i, kt=kt, n=n: e.matmul(
                                ai, lhsT=pT[pi][:, i * 128:(i + 1) * 128], rhs=Vh[hb][:, kt, :],
                                start=False, stop=(last[i] == n)),
                                reads=["mpT%d" % pi, "Vh%d_%d" % (hb, p)], writes=[ak])
                    ob = obst[ab]
                    kb.op("dve", lambda e: e.reciprocal(out=rl[:, 0:3].unsqueeze(2), in_=accA[:, :, 128:129]),
                          reads=["macc%da" % ab], writes=["mrl"])
                    kb.op("dve", lambda e: e.reciprocal(out=rl[:, 3:4].unsqueeze(2), in_=accB[:, :, 128:129]),
                          reads=["macc%db" % ab], writes=["mrl"])
                    for i in range(4):
                        ai, ak = acc_of(i)
                        kb.op("dve", lambda e, ai=ai, i=i, ob=ob: e.tensor_scalar(
                            out=ob[:, i, :], in0=ai[:, 0:128], scalar1=rl[:, i:i + 1], scalar2=None, op0=ALU.mult),
                            reads=[ak, "mrl"], writes=["obst%d" % ab])
                    self.dma("sp", self.OB[g * 512:(g + 1) * 512, h * 128:(h + 1) * 128].rearrange("(i p) f -> p i f", p=128),
                             ob[:], ["obst%d" % ab], [], obs[ab])
            kb.barrier()
            kb.release_dsems([ts, kbs] + kas[0] + kas[1] + vs[0] + vs[1] + qsa + qsb + obs)


class Phases7(Phases6):
    def phase4(self, st):
        c, kb, P = self.cfg, self.kb, self.P
        NQ = c.CH // 128
        NE = c.NE
        GS = NE // c.NG
        P["Wall"] = self.sb(st, "Wall", [128, NQ, NE], F32)
        P["slotI"] = self.sb(st, "slotI", [128, NQ, 8], I32)
        self.bnd_reg = self.nc.gpsimd.alloc_register("moe_bnd")
        self.nc.gpsimd.reg_mov(self.bnd_reg, NE * c.CAP - 1)
        P["w8"] = self.sb(st, "w8", [128, NQ, 8], F32)
        CAP = c.CAP
        BIGK = 131072.0
        with contextlib.ExitStack() as ph:
            self.alloc_psum(ph, 5, 2)
            Ls = self.sb(ph, "Ls", [128, 128], BF16)
            iot = self.sb(ph, "iot", [128, NE], F32)
            ebase = self.sb(ph, "ebase", [128, NE], F32)
            selb = self.sb(ph, "selb", [128, NQ, NE], BF16)
            lss = kb.dsem("Ls")
            ios = kb.dsem("iot")
            self.dma("pool", Ls[:], self.lstrict_d, [], ["Ls"], lss)
            self.dma("sp", iot[:], self.iota_d, [], ["iot"], ios)
            kb.op("dve", lambda e: e.tensor_scalar(out=ebase[:], in0=iot[:], scalar1=float(CAP), scalar2=None, op0=ALU.mult),
                  reads=["iot"], writes=["ebase"])
            d_selv = self.sb(ph, "d_selv", [128, NE], F32)
            d_slot = self.sb(ph, "d_slot", [128, NE], F32)
            d_k8 = self.sb(ph, "d_k8", [128, 8], F32)
            d_s8 = self.sb(ph, "d_s8", [128, 8], F32)
            d_e8i = self.sb(ph, "d_e8i", [128, 8], I32)
            d_e8f = self.sb(ph, "d_e8f", [128, 8], F32)
            d_junk = self.sb(ph, "d_junk", [128, NE], F32)
            scs = [kb.dsem("scat%d" % i) for i in range(16)]
            sci = 0
            gate_a = self.bcast_tile(ph, ph, "gate_a_bc", P["modT"][:, 32:48], "modT")
            wo = self.sb(ph, "wo", [128, 16, 2048], BF16)
            wos = [kb.dsem("wo%d" % i) for i in range(4)]
            for i in range(4):
                self.dma("pool", wo[:, :, i * 512:(i + 1) * 512],
                         self.w_o[:, i * 512:(i + 1) * 512].rearrange("(kc p) n -> p kc n", p=128), [], ["wo%d" % i], wos[i])
            wr = self.sb(ph, "wr", [128, 16, NE], BF16)
            wrs = kb.dsem("wr")
            self.dma("pool", wr[:], self.w_router.rearrange("(kc p) n -> p kc n", p=128), [], ["wr"], wrs)
            rb = self.sb(ph, "rb_bc", [128, NE], F32)
            rbs = kb.dsem("rb")
            self.dma("sp", rb[:], self.rbias.partition_broadcast(128), [], ["rb"], rbs)
            obt = [self.sb(ph, "obt%d" % i, [128, 1024], F32) for i in range(2)]
            obts = [kb.dsem("obt%d" % i) for i in range(2)]
            mixa = [self.sb(ph, "mixa%d" % i, [128, 8, 128], BF16) for i in range(2)]
            mas = [kb.dsem("mixa%d" % i) for i in range(2)]
            mixb = [self.sb(ph, "mixb%d" % i, [128, 8, 128], BF16) for i in range(2)]
            xt = [self.sb(ph, "xt4_%d" % i, [128, 2048], F32) for i in range(2)]
            xts = [kb.dsem("xt4_%d" % i) for i in range(2)]
            xm = [self.sb(ph, "xm%d" % i, [128, 2048], F32) for i in range(2)]
            xms = [kb.dsem("xm%d" % i) for i in range(2)]
            tmpm = [self.sb(ph, "tmpm%d" % i, [128, 512], F32) for i in range(2)]
            h2 = [self.sb(ph, "h2st%d" % i, [128, 16, 128], BF16) for i in range(2)]
            h2s = [kb.dsem("h2st%d" % i) for i in range(2)]
            WN1 = self.alloc_normT(ph, "n4a", 1024, 1)
            WN2 = self.alloc_normT(ph, "n4b", 2048, 2)
            sc = self.sb(ph, "r_sc", [128, NE], F32)
            ch = self.sb(ph, "r_ch", [128, NE], F32)
            cm = self.sb(ph, "r_cm", [128, NE], F32)
            m8 = self.sb(ph, "r_m8", [128, c.NG, 8], F32)
            grp = self.sb(ph, "r_grp", [128, 8], F32)
            g8 = self.sb(ph, "r_g8", [128, 8], F32)
            gm = self.sb(ph, "r_gm", [128, 8], F32)
            e8 = self.sb(ph, "r_e8", [128, 8], F32)
            sel = self.sb(ph, "r_sel", [128, NE], F32)
            ws_ = self.sb(ph, "r_ws", [128, 2], F32)
            xv = self.xk.rearrange("(n p) d -> n p d", p=128)
            for b in range(NQ):
                k = b % 2
                self.dma("sp", obt[k][:], self.OB[b * 128:(b + 1) * 128, :], [], ["obt%d" % k], obts[k])
                self.dma("sp", mixa[k][:], self.MIXT[:, 0:8, b * 128:(b + 1) * 128], [], ["mixa%d" % k], mas[k])
                self.dma("sp", xt[k][:], xv[b], [], ["xt4_%d" % k], xts[k])
                self.norm_T(WN1, obt[k][:], ["obt%d" % k], 1024, P["gsm"][:, 14:22], None, ["gsm"],
                            lambda ci, k=k: mixb[k][:, ci, :], ["mixb%d" % k])
                for nb in range(4):
                    pb, pk = self.pbank()
                    for ci in range(16):
                        src = mixa[k] if ci < 8 else mixb[k]
                        sk = ("mixa%d" % k) if ci < 8 else ("mixb%d" % k)
                        kb.op("pe", lambda e, ci=ci, nb=nb, pb=pb, src=src: e.matmul(
                            pb[:], lhsT=src[:, ci % 8, :], rhs=wo[:, ci, nb * 512:(nb + 1) * 512],
                            start=(ci == 0), stop=(ci == 15)), reads=[sk, "wo%d" % nb], writes=[pk])
                    tk = nb % 2
                    kb.op("dve", lambda e, nb=nb, pb=pb, tk=tk: e.tensor_tensor(
                        out=tmpm[tk][:], in0=pb[:], in1=gate_a[:, nb * 512:(nb + 1) * 512], op=ALU.mult),
                        reads=[pk, "gate_a_bc"], writes=["tmpm%d" % tk])
                    kb.op("pool", lambda e, nb=nb, tk=tk: e.tensor_tensor(
                        out=xm[k][:, nb * 512:(nb + 1) * 512], in0=tmpm[tk][:], in1=xt[k][:, nb * 512:(nb + 1) * 512], op=ALU.add),
                        reads=["tmpm%d" % tk, "xt4_%d" % k], writes=["xm%d" % k])
                self.dma("sp", self.XMID[b * 128:(b + 1) * 128, :], xm[k][:], ["xm%d" % k], [], xms[k])
                self.norm_T(WN2, xm[k][:], ["xm%d" % k], 2048, P["gmodF"], P["modT"][:, 48:64], ["gmodF", "modT"],
                            lambda ci, k=k: h2[k][:, ci, :], ["h2st%d" % k])
                self.dma("sp", self.H2T[:, :, b * 128:(b + 1) * 128], h2[k][:], ["h2st%d" % k], [], h2s[k])
                pb, pk = self.pbank()
                for ci in range(16):
                    kb.op("pe", lambda e, ci=ci, pb=pb: e.matmul(pb[:, 0:NE], lhsT=h2[k][:, ci, :], rhs=wr[:, ci, :],
                                                                 start=(ci == 0), stop=(ci == 15)),
                          reads=["h2st%d" % k, "wr"], writes=[pk])
                D = lambda fn, r, w: kb.op("dve", fn, reads=r, writes=w)
                kb.op("act", lambda e, pb=pb: e.activation(out=sc[:], in_=pb[:, 0:NE], func=AF.Sigmoid), reads=[pk], writes=["r_sc"])
                D(lambda e: e.tensor_tensor(out=ch[:], in0=sc[:], in1=rb[:], op=ALU.add), ["r_sc", "rb"], ["r_ch"])
                for g in range(c.NG):
                    D(lambda e, g=g: e.max(out=m8[:, g, :], in_=ch[:, g * GS:(g + 1) * GS]), ["r_ch"], ["r_m8"])
                D(lambda e: e.tensor_tensor(out=grp[:].unsqueeze(2), in0=m8[:, :, 0:1], in1=m8[:, :, 1:2], op=ALU.add), ["r_m8"], ["r_grp"])
                D(lambda e: e.max(out=g8[:], in_=grp[:]), ["r_grp"], ["r_g8"])
                D(lambda e: e.tensor_scalar(out=gm[:], in0=grp[:], scalar1=g8[:, c.TOPG - 1:c.TOPG], scalar2=None, op0=ALU.is_ge),
                  ["r_grp", "r_g8"], ["r_gm"])
                D(lambda e: e.tensor_scalar(out=gm[:], in0=gm[:], scalar1=-1.0, scalar2=1e30, op0=ALU.add, op1=ALU.mult), [], ["r_gm"])
                D(lambda e: e.tensor_tensor(out=cm[:].rearrange("p (g s) -> p g s", s=GS), in0=ch[:].rearrange("p (g s) -> p g s", s=GS),
                                            in1=gm[:].unsqueeze(2).broadcast_to([128, c.NG, GS]), op=ALU.add), ["r_ch", "r_gm"], ["r_cm"])
                D(lambda e: e.max(out=e8[:], in_=cm[:]), ["r_cm"], ["r_e8"])
                D(lambda e: e.tensor_scalar(out=sel[:], in0=cm[:], scalar1=e8[:, c.TOPK - 1:c.TOPK], scalar2=None, op0=ALU.is_ge),
                  ["r_cm", "r_e8"], ["r_sel"])
                D(lambda e, b=b: e.tensor_copy(out=selb[:, b, :], in_=sel[:]), ["r_sel"], ["selb%d" % b])
                pp, ppk = self.pbank()
                for b2 in range(b):
                    kb.op("pe", lambda e, b2=b2, pp=pp: e.matmul(pp[:, 0:NE], lhsT=P["ones_b"][:], rhs=selb[:, b2, :],
                                                                 start=(b2 == 0), stop=False),
                          reads=["ones_b", "selb%d" % b2], writes=[ppk])
                kb.op("pe", lambda e, b=b, pp=pp: e.matmul(pp[:, 0:NE], lhsT=Ls[:], rhs=selb[:, b, :], start=(b == 0), stop=True),
                      reads=["Ls", "selb%d" % b], writes=[ppk])
                D(lambda e, pp=pp: e.tensor_scalar(out=d_selv[:], in0=pp[:, 0:NE], scalar1=float(CAP), scalar2=None, op0=ALU.is_lt),
                  [ppk], ["d_selv"])
                D(lambda e: e.tensor_tensor(out=d_selv[:], in0=d_selv[:], in1=sel[:], op=ALU.mult), ["r_sel"], ["d_selv"])
                D(lambda e, pp=pp: e.tensor_tensor(out=d_slot[:], in0=pp[:, 0:NE], in1=ebase[:], op=ALU.add), [ppk, "ebase"], ["d_slot"])
                D(lambda e: e.tensor_scalar(out=d_slot[:], in0=d_slot[:], scalar1=-1.0, scalar2=BIGK, op0=ALU.mult, op1=ALU.add),
                  [], ["d_slot"])
                D(lambda e: e.tensor_tensor(out=d_slot[:], in0=d_slot[:], in1=d_selv[:], op=ALU.mult), ["d_selv"], ["d_slot"])
                D(lambda e: e.max(out=d_k8[:], in_=d_slot[:]), ["d_slot"], ["d_k8"])
                D(lambda e: e.tensor_scalar(out=d_s8[:], in0=d_k8[:], scalar1=-1.0, scalar2=BIGK, op0=ALU.mult, op1=ALU.add),
                  ["d_k8"], ["d_s8"])
                D(lambda e, b=b: e.tensor_copy(out=P["slotI"][:, b, :], in_=d_s8[:]), ["d_s8"], ["slotI%d" % b])
                D(lambda e, b=b: e.tensor_scalar(out=d_e8i[:], in0=P["slotI"][:, b, :], scalar1=int(np.log2(CAP)), scalar2=None,
                                                 op0=ALU.arith_shift_right), ["slotI%d" % b], ["d_e8i"])
                D(lambda e: e.tensor_copy(out=d_e8f[:], in_=d_e8i[:]), ["d_e8i"], ["d_e8f"])
                xn_cur = WN2["xn"][(WN2["i"] - 1) % WN2["n"]]
                xn_key = "n4b_xn%d" % ((WN2["i"] - 1) % WN2["n"])
                for k8 in range(8):
                    sm = scs[sci % 16]
                    sci += 1
                    kb.op("pool", lambda e, b=b, k8=k8, xn_cur=xn_cur: e.indirect_dma_start(
                        out=self.XG[:, :], out_offset=bass.IndirectOffsetOnAxis(ap=P["slotI"][:, b, k8:k8 + 1], axis=0),
                        in_=xn_cur[:, :], in_offset=None, bounds_check=self.bnd_reg, oob_is_err=False),
                        reads=[xn_key, "slotI%d" % b], writes=[], dsem=sm)
                D(lambda e: e.tensor_tensor(out=sel[:], in0=sel[:], in1=sc[:], op=ALU.mult), ["r_sc"], ["r_sel"])
                D(lambda e: e.tensor_reduce(out=ws_[:, 0:1], in_=sel[:], axis=AX.X, op=ALU.add), ["r_sel"], ["r_ws"])
                D(lambda e: e.reciprocal(out=ws_[:, 1:2], in_=ws_[:, 0:1]), [], ["r_ws"])
                D(lambda e, b=b: e.tensor_scalar(out=P["Wall"][:, b, :], in0=sel[:], scalar1=ws_[:, 1:2], scalar2=float(c.ROUTED_SCALE),
                                                 op0=ALU.mult, op1=ALU.mult), ["r_sel", "r_ws"], ["Wall"])
                for k8 in range(8):
                    D(lambda e, b=b, k8=k8: e.scalar_tensor_tensor(
                        out=d_junk[:], in0=iot[:], scalar=d_e8f[:, k8:k8 + 1], in1=P["Wall"][:, b, :],
                        op0=ALU.is_equal, op1=ALU.mult, accum_out=P["w8"][:, b, k8:k8 + 1]),
                      ["iot", "d_e8f", "Wall"], ["d_junk", "w8_%d" % b])
            if c.debug:
                self.WALL = self.dscr("WALL", [128, NQ * NE], F32)
                self.dma("sp", self.WALL, P["Wall"][:].rearrange("p a b -> p (a b)"), ["Wall"], [], rbs)
            kb.barrier()
            kb.release_dsems(wos + [wrs, rbs, lss, ios] + obts + mas + xts + xms + h2s + scs)

    def phase6(self, shared_only=False):
        c, kb, P = self.cfg, self.kb, self.P
        NE = c.NE
        TH = c.CH // 2
        NTT = TH // 128
        with contextlib.ExitStack() as ph:
            self.alloc_psum(ph, 8, 0)
            gate_f = self.bcast_tile(ph, ph, "gate_f_bc", P["modT"][:, 80:96], "modT")
            fg = self.bcast_tile(ph, ph, "fg_bc", P["gvec"][:, 32:48], "gvec")
            h2T = self.sb(ph, "h2T_h", [128, 16, TH], BF16)
            h2s = kb.dsem("h2T_h")
            acc = self.sb(ph, "moe_acc", [128, NTT, 2048], F32)
            NW = 2
            wg = [self.sb(ph, "wg%d" % i, [128, 16, 128], BF16) for i in range(NW)]
            wu = [self.sb(ph, "wu%d" % i, [128, 16, 128], BF16) for i in range(NW)]
            wgs = [kb.dsem("wg%d" % i) for i in range(NW)]
            wus = [kb.dsem("wu%d" % i) for i in range(NW)]
            wd = [self.sb(ph, "wd%d" % i, [128, 4, 2048], BF16) for i in range(2)]
            wds = [kb.dsem("wd%d" % i) for i in range(2)]
            hT = [self.sb(ph, "ehT%d" % i, [128, 4, TH], BF16) for i in range(2)]
            sg = [self.sb(ph, "sg%d" % i, [128, 512], F32) for i in range(2)]
            xmt = self.sb(ph, "xmt", [128, 2048], F32)
            xmts = kb.dsem("xmt")
            st3 = self.sb(ph, "st3", [128, 4], F32)
            junk = self.sb(ph, "junk6", [128, 2048], BF16)
            wi = 0
            sgi = 0
            yshs = [kb.dsem("ysh%d" % i) for i in range(NTT)] if shared_only else []
            for half in range(2):
                self.dma("sp", h2T[:], self.H2T[:, :, half * TH:(half + 1) * TH], [], ["h2T_h"], h2s)
                for tt in range(NTT):
                    kb.op("pool", lambda e, tt=tt: e.memset(acc[:, tt, :], 0.0), writes=["acc%d" % tt])
                for ex in ([NE] if shared_only else range(NE + 1)):
                    eb = ex % 2
                    if ex < NE:
                        g_src, u_src, d_src = self.w_eg[ex], self.w_eu[ex], self.w_ed[ex]
                    else:
                        g_src, u_src, d_src = self.w_sg, self.w_su, self.w_sd
                    self.dma("pool", wd[eb][:], d_src.rearrange("(kc p) n -> p kc n", p=128), [], ["wd%d" % eb], wds[eb])
                    for hb in range(4):
                        w = wi % NW
                        wi += 1
                        self.dma("pool", wg[w][:], g_src[:, hb * 128:(hb + 1) * 128].rearrange("(kc p) n -> p kc n", p=128),
                                 [], ["wg%d" % w], wgs[w])
                        self.dma("pool", wu[w][:], u_src[:, hb * 128:(hb + 1) * 128].rearrange("(kc p) n -> p kc n", p=128),
                                 [], ["wu%d" % w], wus[w])
                        for tg in range(TH // 512):
                            pg, pgk = self.pbank()
                            pu, puk = self.pbank()
                            for (pp, ppk, wsrc, wk) in ((pg, pgk, wg[w], "wg%d" % w), (pu, puk, wu[w], "wu%d" % w)):
                                for kc in range(16):
                                    kb.op("pe", lambda e, pp=pp, wsrc=wsrc, kc=kc, tg=tg: e.matmul(
                                        pp[:], lhsT=wsrc[:, kc, :], rhs=h2T[:, kc, tg * 512:(tg + 1) * 512],
                                        start=(kc == 0), stop=(kc == 15)), reads=[wk, "h2T_h"], writes=[ppk])
                            si = sgi % 2
                            sgi += 1
                            kb.op("act", lambda e, pg=pg, si=si: e.activation(out=sg[si][:], in_=pg[:], func=AF.Silu),
                                  reads=[pgk], writes=["sg%d" % si])
                            kb.op("dve", lambda e, pu=pu, si=si, hb=hb, tg=tg: e.tensor_tensor(
                                out=hT[eb][:, hb, tg * 512:(tg + 1) * 512], in0=pu[:], in1=sg[si][:], op=ALU.mult),
                                reads=[puk, "sg%d" % si], writes=["ehT%d" % eb])
                    for tt in range(NTT):
                        tile = half * NTT + tt
                        for nb in range(4):
                            py, pyk = self.pbank()
                            for hb in range(4):
                                kb.op("pe", lambda e, py=py, hb=hb, tt=tt, nb=nb: e.matmul(
                                    py[:], lhsT=hT[eb][:, hb, tt * 128:(tt + 1) * 128], rhs=wd[eb][:, hb, nb * 512:(nb + 1) * 512],
                                    start=(hb == 0), stop=(hb == 3)), reads=["ehT%d" % eb, "wd%d" % eb], writes=[pyk])
                            scal = P["Wall"][:, tile, ex:ex + 1] if ex < NE else 1.0
                            kb.op("dve", lambda e, py=py, tt=tt, nb=nb, scal=scal: e.scalar_tensor_tensor(
                                out=acc[:, tt, nb * 512:(nb + 1) * 512], in0=py[:], scalar=scal,
                                in1=acc[:, tt, nb * 512:(nb + 1) * 512], op0=ALU.mult, op1=ALU.add),
                                reads=[pyk, "Wall"], writes=["acc%d" % tt])
                if shared_only:
                    for tt in range(NTT):
                        tile = half * NTT + tt
                        self.dma("sp", self.YSH[tile * 128:(tile + 1) * 128, :], acc[:, tt, :], ["acc%d" % tt], [], yshs[tt])
                    continue
                for tt in range(NTT):
                    tile = half * NTT + tt
                    self.dma("sp", xmt[:], self.XMID[tile * 128:(tile + 1) * 128, :], [], ["xmt"], xmts)
                    kb.op("dve", lambda e, tt=tt: e.tensor_tensor(out=acc[:, tt, :], in0=acc[:, tt, :], in1=gate_f[:], op=ALU.mult),
                          reads=["gate_f_bc"], writes=["acc%d" % tt])
                    kb.op("pool", lambda e, tt=tt: e.tensor_tensor(out=xmt[:], in0=xmt[:], in1=acc[:, tt, :], op=ALU.add),
                          reads=["acc%d" % tt], writes=["xmt"])
                    kb.op("act", lambda e: e.activation(out=junk[:], in_=xmt[:], func=AF.Square, accum_out=st3[:, 0:1]),
                          reads=["xmt"], writes=["junk6", "st3"])
                    kb.op("dve", lambda e: e.tensor_scalar(out=st3[:, 1:2], in0=st3[:, 0:1], scalar1=1.0 / c.D, scalar2=c.EPS,
                                                           op0=ALU.mult, op1=ALU.add), reads=[], writes=["st3"])
                    kb.op("act", lambda e: e.activation(out=st3[:, 2:3], in_=st3[:, 1:2], func=AF.Sqrt), reads=[], writes=["st3"])
                    kb.op("dve", lambda e: e.reciprocal(out=st3[:, 3:4], in_=st3[:, 2:3]), reads=[], writes=["st3"])
                    kb.op("dve", lambda e: e.scalar_tensor_tensor(out=xmt[:], in0=xmt[:], scalar=st3[:, 3:4], in1=fg[:],
                                                                  op0=ALU.mult, op1=ALU.mult), reads=["st3", "fg_bc"], writes=["xmt"])
                    self.dma("sp", self.out[tile * 128:(tile + 1) * 128, :], xmt[:], ["xmt"], [], xmts)
            kb.barrier()
            kb.release_dsems([h2s, xmts] + wgs + wus + wds + yshs)


class Phases8(Phases7):
    def phase6_routed(self):
        c, kb, P = self.cfg, self.kb, self.P
        NE, CAP = c.NE, c.CAP
        NB = CAP // 128
        with contextlib.ExitStack() as ph:
            self.alloc_psum(ph, 6, 2)
            xg = [self.sb(ph, "xg%d" % i, [128, 2048], BF16) for i in range(3)]
            xgs = [kb.dsem("xg%d" % i) for i in range(3)]
            xT = self.sb(ph, "xTe", [128, 16, CAP], BF16)
            NW = 2
            wg = [self.sb(ph, "rwg%d" % i, [128, 16, 128], BF16) for i in range(NW)]
            wu = [self.sb(ph, "rwu%d" % i, [128, 16, 128], BF16) for i in range(NW)]
            wgs = [kb.dsem("rwg%d" % i) for i in range(NW)]
            wus = [kb.dsem("rwu%d" % i) for i in range(NW)]
            wd = [self.sb(ph, "rwd%d" % i, [128, 4, 2048], BF16) for i in range(2)]
            wds = [kb.dsem("rwd%d" % i) for i in range(2)]
            hT = [self.sb(ph, "rhT%d" % i, [128, 4, CAP], BF16) for i in range(2)]
            sg = [self.sb(ph, "rsg%d" % i, [128, 512], F32) for i in range(2)]
            yst = [self.sb(ph, "yst%d" % i, [128, 2048], BF16) for i in range(2)]
            ysts = [kb.dsem("yst%d" % i) for i in range(2)]
            xgi = 0
            wi = 0
            sgi = 0
            yi = 0
            for ex in range(NE):
                eb = ex % 2
                self.dma("pool", wd[eb][:], self.w_ed[ex].rearrange("(kc p) n -> p kc n", p=128), [], ["rwd%d" % eb], wds[eb])
                for blk in range(NB):
                    xi = xgi % 3
                    xgi += 1
                    r0 = ex * CAP + blk * 128
                    self.dma("sp", xg[xi][:], self.XG[r0:r0 + 128, :], [], ["xg%d" % xi], xgs[xi])
                    for c0 in range(0, 16, 8):
                        tb, tk = self.tbank()
                        for j in range(8):
                            ci = c0 + j
                            kb.op("pe", lambda e, j=j, ci=ci, tb=tb, xi=xi: e.transpose(
                                out=tb[:, j, :], in_=xg[xi][:, ci * 128:(ci + 1) * 128], identity=P["ident_b"][:]),
                                reads=["xg%d" % xi, "ident_b"], writes=[tk])
                        for j in range(8):
                            ci = c0 + j
                            kb.op("dve", lambda e, j=j, ci=ci, tb=tb, blk=blk: e.tensor_scalar(
                                out=xT[:, ci, blk * 128:(blk + 1) * 128], in0=tb[:, j, :], scalar1=P["gmodF"][:, ci:ci + 1],
                                scalar2=P["modT"][:, 48 + ci:49 + ci], op0=ALU.mult, op1=ALU.add),
                                reads=[tk, "gmodF", "modT"], writes=["xTe%d" % (blk // 4)])
                for hb in range(4):
                    w = wi % NW
                    wi += 1
                    self.dma("pool", wg[w][:], self.w_eg[ex][:, hb * 128:(hb + 1) * 128].rearrange("(kc p) n -> p kc n", p=128),
                             [], ["rwg%d" % w], wgs[w])
                    self.dma("pool", wu[w][:], self.w_eu[ex][:, hb * 128:(hb + 1) * 128].rearrange("(kc p) n -> p kc n", p=128),
                             [], ["rwu%d" % w], wus[w])
                    for tg in range(CAP // 512):
                        pg, pgk = self.pbank()
                        pu, puk = self.pbank()
                        for (pp, ppk, wsrc, wk) in ((pg, pgk, wg[w], "rwg%d" % w), (pu, puk, wu[w], "rwu%d" % w)):
                            for kc in range(16):
                                kb.op("pe", lambda e, pp=pp, wsrc=wsrc, kc=kc, tg=tg: e.matmul(
                                    pp[:], lhsT=wsrc[:, kc, :], rhs=xT[:, kc, tg * 512:(tg + 1) * 512],
                                    start=(kc == 0), stop=(kc == 15)), reads=[wk, "xTe%d" % tg], writes=[ppk])
                        si = sgi % 2
                        sgi += 1
                        kb.op("act", lambda e, pg=pg, si=si: e.activation(out=sg[si][:], in_=pg[:], func=AF.Silu),
                              reads=[pgk], writes=["rsg%d" % si])
                        kb.op("dve", lambda e, pu=pu, si=si, hb=hb, tg=tg: e.tensor_tensor(
                            out=hT[eb][:, hb, tg * 512:(tg + 1) * 512], in0=pu[:], in1=sg[si][:], op=ALU.mult),
                            reads=[puk, "rsg%d" % si], writes=["rhT%d" % eb])
                for blk in range(NB):
                    y = yi % 2
                    yi += 1
                    for nb in range(4):
                        py, pyk = self.pbank()
                        for hb in range(4):
                            kb.op("pe", lambda e, py=py, hb=hb, blk=blk, nb=nb: e.matmul(
                                py[:], lhsT=hT[eb][:, hb, blk * 128:(blk + 1) * 128], rhs=wd[eb][:, hb, nb * 512:(nb + 1) * 512],
                                start=(hb == 0), stop=(hb == 3)), reads=["rhT%d" % eb, "rwd%d" % eb], writes=[pyk])
                        kb.op("act", lambda e, py=py, y=y, nb=nb: e.activation(out=yst[y][:, nb * 512:(nb + 1) * 512], in_=py[:], func=AF.Copy),
                              reads=[pyk], writes=["yst%d" % y])
                    r0 = ex * CAP + blk * 128
                    self.dma("sp", self.YS[r0:r0 + 128, :], yst[y][:], ["yst%d" % y], [], ysts[y])
            kb.barrier()
            kb.release_dsems(xgs + wgs + wus + wds + ysts)

    def phase7(self):
        c, kb, P = self.cfg, self.kb, self.P
        NQ = c.CH // 128
        NE, CAP = c.NE, c.CAP
        with contextlib.ExitStack() as ph:
            self.alloc_psum(ph, 2, 0)
            gate_f = self.bcast_tile(ph, ph, "gate_f_bc2", P["modT"][:, 80:96], "modT")
            fg = self.bcast_tile(ph, ph, "fg_bc2", P["gvec"][:, 32:48], "gvec")
            acc = [self.sb(ph, "cacc%d" % i, [128, 2048], F32) for i in range(2)]
            accs = [kb.dsem("cacc%d" % i) for i in range(2)]
            xmt = [self.sb(ph, "cxm%d" % i, [128, 2048], F32) for i in range(2)]
            xmts = [kb.dsem("cxm%d" % i) for i in range(2)]
            NGB = 4
            gb = [self.sb(ph, "gb%d" % i, [128, 2048], BF16) for i in range(NGB)]
            gbs = [kb.dsem("gb%d" % i) for i in range(NGB)]
            st3 = self.sb(ph, "cst3", [128, 4], F32)
            junk = self.sb(ph, "cjunk", [128, 2048], BF16)
            for i in range(NGB):
                kb.op("pool", lambda e, i=i: e.memset(gb[i][:], 0.0), writes=["gb%d" % i])
            gi = 0
            for b in range(NQ):
                k = b % 2
                self.dma("sp", acc[k][:], self.YSH[b * 128:(b + 1) * 128, :], [], ["cacc%d" % k], accs[k])
                self.dma("sp", xmt[k][:], self.XMID[b * 128:(b + 1) * 128, :], [], ["cxm%d" % k], xmts[k])
                for k8 in range(8):
                    g = gi % NGB
                    gi += 1
                    kb.op("pool", lambda e, g=g, b=b, k8=k8: e.indirect_dma_start(
                        out=gb[g][:, :], out_offset=None, in_=self.YS[:, :],
                        in_offset=bass.IndirectOffsetOnAxis(ap=P["slotI"][:, b, k8:k8 + 1], axis=0),
                        bounds_check=self.bnd_reg, oob_is_err=False),
                        reads=["slotI%d" % b], writes=["gb%d" % g], dsem=gbs[g])
                    kb.op("dve", lambda e, g=g, b=b, k8=k8, k=k: e.scalar_tensor_tensor(
                        out=acc[k][:], in0=gb[g][:], scalar=P["w8"][:, b, k8:k8 + 1], in1=acc[k][:], op0=ALU.mult, op1=ALU.add),
                        reads=["gb%d" % g, "w8_%d" % b], writes=["cacc%d" % k])
                kb.op("dve", lambda e, k=k: e.tensor_tensor(out=acc[k][:], in0=acc[k][:], in1=gate_f[:], op=ALU.mult),
                      reads=["gate_f_bc2"], writes=["cacc%d" % k])
                kb.op("pool", lambda e, k=k: e.tensor_tensor(out=xmt[k][:], in0=xmt[k][:], in1=acc[k][:], op=ALU.add),
                      reads=["cacc%d" % k], writes=["cxm%d" % k])
                kb.op("act", lambda e, k=k: e.activation(out=junk[:], in_=xmt[k][:], func=AF.Square, accum_out=st3[:, 0:1]),
                      reads=["cxm%d" % k], writes=["cjunk", "cst3"])
                kb.op("dve", lambda e: e.tensor_scalar(out=st3[:, 1:2], in0=st3[:, 0:1], scalar1=1.0 / c.D, scalar2=c.EPS,
                                                       op0=ALU.mult, op1=ALU.add), reads=[], writes=["cst3"])
                kb.op("act", lambda e: e.activation(out=st3[:, 2:3], in_=st3[:, 1:2], func=AF.Sqrt), reads=[], writes=["cst3"])
                kb.op("dve", lambda e: e.reciprocal(out=st3[:, 3:4], in_=st3[:, 2:3]), reads=[], writes=["cst3"])
                kb.op("dve", lambda e, k=k: e.scalar_tensor_tensor(out=xmt[k][:], in0=xmt[k][:], scalar=st3[:, 3:4], in1=fg[:],
                                                                   op0=ALU.mult, op1=ALU.mult), reads=["cst3", "fg_bc2"], writes=["cxm%d" % k])
                self.dma("sp", self.out[b * 128:(b + 1) * 128, :], xmt[k][:], ["cxm%d" % k], [], xmts[k])
            kb.barrier()
            kb.release_dsems(accs + xmts + gbs)


def build(cfg):
    b = Phases8(cfg)
    b.declare_io()
    kb = b.kb
    with contextlib.ExitStack() as st:
        b.phase0(st)
        if cfg.stop_after >= 1:
            b.phase1a()
        if cfg.stop_after >= 2:
            b.phase1b()
            b.phase1c()
        if cfg.stop_after >= 3:
            b.phase2()
        if cfg.stop_after >= 4:
            b.phase3()
        if cfg.stop_after >= 5:
            b.phase4(st)
        if cfg.stop_after >= 6:
            if cfg.CAP:
                b.phase6(shared_only=True)
                b.phase6_routed()
                b.phase7()
            else:
                b.phase6()
        kb.barrier()
    return b


_BUILD_CACHE = {}


def kernel(**inputs):
    cfg = Cfg
    inp = {k: np.asarray(v) for k, v in inputs.items()}
    S = inp["x"].shape[1]
    nchunks = S // cfg.CH
    assert nchunks == cfg.NCORES and nchunks == cfg.NSLOT
    if "b" not in _BUILD_CACHE:
        _BUILD_CACHE["b"] = build(cfg)
    b = _BUILD_CACHE["b"]
    sh = prepare_shared(inp, cfg)
    maps = []
    for core in range(cfg.NCORES):
        m = dict(sh)
        m.update(prepare_core(inp, cfg, core, nchunks))
        maps.append(m)
    res = run_bass_kernel_spmd(b.nc, maps, core_ids=list(range(cfg.NCORES)))
    outs = [np.asarray(r["out"]) for r in res.results]
    return np.concatenate(outs, axis=0)[None].astype(np.float32)
```

```python
import contextlib
import numpy as np
import concourse.bass as bass
import concourse.mybir as mybir
from concourse.bass_utils import run_bass_kernel_spmd

F32 = mybir.dt.float32
BF16 = mybir.dt.bfloat16
I32 = mybir.dt.int32
AF = mybir.ActivationFunctionType
ALU = mybir.AluOpType
AX = mybir.AxisListType

SAME_ENGINE_SYNC = True


class Cfg:
    D = 2048
    CH = 2048
    NSLOT = 8
    NCORES = 8
    NE = 64
    NG = 8
    TOPG = 4
    TOPK = 8
    DE = 512
    NH = 8
    HD = 128
    QL = 512
    KVL = 256
    ROPE = 64
    NADA = 6
    EPS = 1e-6
    ROUTED_SCALE = 2.5
    WINDOWS = ((128, 1), (512, 4), (2048, 16))
    CAP = 1024
    debug = False
    stop_after = 99


class Sem:
    def __init__(self, h, name):
        self.h = h
        self.name = name
        self.count = 0


class KB:
    def __init__(self, nc):
        self.nc = nc
        self.E = {"pe": nc.tensor, "act": nc.scalar, "dve": nc.vector, "pool": nc.gpsimd, "sp": nc.sync}
        self.esem = {}
        self.allsems = []
        for e in ["pe", "act", "dve", "pool"]:
            self.esem[e] = self.newsem("es_" + e)
        self.waited = {e: {} for e in self.E}
        self.lastw = {}
        self.readers = {}
        self.free_dsems = []
        self.nwaits = 0
        self.nops = 0

    def newsem(self, name):
        s = Sem(self.nc.alloc_semaphore(name=name), name)
        self.allsems.append(s)
        return s

    def dsem(self, name="d"):
        if self.free_dsems:
            return self.free_dsems.pop()
        return self.newsem("ds%d_%s" % (len(self.allsems), name))

    def release_dsems(self, sems):
        self.free_dsems.extend(sems)

    def _wait(self, eng, sem, val):
        if self.waited[eng].get(sem, 0) >= val:
            return
        self.E[eng].wait_ge(sem.h, val)
        self.waited[eng][sem] = val
        self.nwaits += 1

    def op(self, eng, issue, reads=(), writes=(), dsem=None):
        need = {}
        def add(ev):
            sem, val, peng = ev
            if eng == "pe" and peng == "pe":
                return
            if (not SAME_ENGINE_SYNC) and peng == eng and peng != "dma":
                return
            if need.get(sem, 0) < val:
                need[sem] = val
        for r in reads:
            if r in self.lastw:
                add(self.lastw[r])
        for w in writes:
            if w in self.lastw:
                add(self.lastw[w])
            for ev in self.readers.get(w, ()):
                add(ev)
        for sem, val in need.items():
            self._wait(eng, sem, val)
        inst = issue(self.E[eng])
        if dsem is not None:
            if dsem.count > 0 and not any(w.get(dsem, 0) >= dsem.count for w in self.waited.values()):
                raise RuntimeError("DMA semaphore %s reused while previous DMA may be in flight" % dsem.name)
            dsem.count += 16
            inst.then_inc(dsem.h, 16)
            ev = (dsem, dsem.count, "dma")
        else:
            s = self.esem[eng]
            s.count += 1
            inst.then_inc(s.h, 1)
            ev = (s, s.count, eng)
        for w in writes:
            self.lastw[w] = ev
            self.readers[w] = []
        for r in reads:
            if r not in writes:
                self.readers.setdefault(r, []).append(ev)
        self.nops += 1
        return ev

    def barrier(self, engines=("pe", "act", "dve", "pool", "sp")):
        for e in engines:
            for s in self.allsems:
                if s.count > 0:
                    self._wait(e, s, s.count)
        self.lastw = {}
        self.readers = {}


def _slopes(nh):
    return [float(np.float32(2.0) ** np.float32(-8.0 * (h + 1) / nh)) for h in range(nh)]


class Builder:
    def __init__(self, cfg):
        self.cfg = cfg
        self.nc = bass.Bass("TRN2", target_bir_lowering=False)
        self.kb = KB(self.nc)
        self.dram_in = {}
        self.dram_out = {}
        self.scratch = {}

    def din(self, name, shape, dtype=F32):
        t = self.nc.dram_tensor(name, list(shape), dtype, kind="ExternalInput")
        self.dram_in[name] = (tuple(shape), dtype)
        return t.ap()

    def dscr(self, name, shape, dtype, internal=False):
        kind = "ExternalOutput" if (self.cfg.debug and not internal) else "Internal"
        t = self.nc.dram_tensor(name, list(shape), dtype, kind=kind)
        self.scratch[name] = (tuple(shape), dtype)
        return t.ap()

    def dout(self, name, shape, dtype=F32):
        t = self.nc.dram_tensor(name, list(shape), dtype, kind="ExternalOutput")
        self.dram_out[name] = (tuple(shape), dtype)
        return t.ap()


MAGIC = 12582912.0
TWO_PI = 2.0 * np.pi
CW1 = 6.28125
CW2 = float(np.float32(TWO_PI - CW1))
CW3 = float(TWO_PI - CW1 - CW2)


def _b(cls):
    return cls


class Phases(Builder):
    _uid = 0

    def _nm(self, name):
        Phases._uid += 1
        return "%s_u%d" % (name, Phases._uid)

    def sb(self, st, name, shape, dtype):
        return st.enter_context(self.nc.sbuf_tensor(self._nm(name), list(shape), dtype))

    def ps(self, st, name, shape, dtype=F32):
        return st.enter_context(self.nc.psum_tensor(self._nm(name), list(shape), dtype))

    def dma(self, eng, out, in_, reads, writes, sem, **kw):
        return self.kb.op(eng, lambda e: e.dma_start(out=out, in_=in_, **kw), reads=reads, writes=writes, dsem=sem)

    def declare_io(self):
        c = self.cfg
        NT = c.NSLOT * c.CH
        self.xk = self.din("xk", [NT, c.D])
        self.posk = self.din("posk", [1, NT], I32)
        self.flags = self.din("flags", [1, c.NSLOT * 8])
        self.cT = self.din("cT", [128, 16])
        self.w_ada = self.din("w_ada", [c.D, c.NADA * c.D])
        self.b_adaT = self.din("b_adaT", [128, 96])
        self.gvecT = self.din("gvecT", [128, 48])
        self.w_in = self.din("w_in", [c.D, 3968])
        self.gsmallT = self.din("gsmallT", [128, 22])
        self.w_uq = self.din("w_uq", [c.QL, 2048])
        self.w_ukv = self.din("w_ukv", [c.KVL, 2048])
        self.w_o = self.din("w_o", [c.D, c.D])
        self.w_router = self.din("w_router", [c.D, c.NE])
        self.rbias = self.din("rbias", [1, c.NE])
        nexp = c.NE if c.stop_after >= 6 else 1
        self.w_eg = self.din("w_eg", [nexp, c.D, c.DE])
        self.w_eu = self.din("w_eu", [nexp, c.D, c.DE])
        self.w_ed = self.din("w_ed", [nexp, c.DE, c.D])
        self.w_sg = self.din("w_sg", [c.D, c.DE])
        self.w_su = self.din("w_su", [c.D, c.DE])
        self.w_sd = self.din("w_sd", [c.DE, c.D])
        self.ident_d = self.din("ident", [128, 128])
        self.invfreq2 = self.din("invfreq2", [64, 1])
        self.lnmult = self.din("lnmult", [128, 17, 128])
        self.tri_d = self.din("tri", [128, 128])
        self.lstrict_d = self.din("lstrict", [128, 128])
        self.iota_d = self.din("iota64", [128, c.NE])
        self.out = self.dout("out", [c.CH, c.D])
        self.HT = self.dscr("HT", [2, 128, 16, c.CH], BF16)
        self.KT_A = self.dscr("KT_A", [c.NH, 128, NT], BF16)
        self.KT_B = self.dscr("KT_B", [65, NT], BF16)
        self.V_AUG = self.dscr("V_AUG", [NT, c.NH * 129], BF16)
        self.KAT = self.dscr("KAT", [c.NH, 128, 2 * c.CH], BF16)
        self.VA_AUG = self.dscr("VA_AUG", [2 * c.CH, c.NH * 129], BF16)
        self.QAT = self.dscr("QAT", [c.NH, 128, c.CH], BF16)
        self.NEGCA = self.dscr("NEGCA", [c.NH, c.CH], BF16)
        self.QT_A = self.dscr("QT_A", [c.NH, 128, c.CH], BF16)
        self.QT_B = self.dscr("QT_B", [c.NH, 65, c.CH], BF16)
        self.OB = self.dscr("OB", [c.CH, 1024], F32)
        self.MIXT = self.dscr("MIXT", [128, 16, c.CH], BF16)
        self.XMID = self.dscr("XMID", [c.CH, c.D], F32)
        self.H2T = self.dscr("H2T", [128, 16, c.CH], BF16)
        self.MODT = self.dscr("MODT", [128, 96], F32)
        self.XG = self.dscr("XG", [c.NE * max(c.CAP, 128), c.D], BF16, internal=True)
        self.YS = self.dscr("YS", [c.NE * max(c.CAP, 128), c.D], BF16, internal=True)
        self.YSH = self.dscr("YSH", [c.CH, c.D], F32)

    def phase0(self, st):
        c, kb, nc = self.cfg, self.kb, self.nc
        P = self.P = {}
        P["ident_f"] = self.sb(st, "ident_f", [128, 128], F32)
        P["ident_b"] = self.sb(st, "ident_b", [128, 128], BF16)
        P["ones_f"] = self.sb(st, "ones_f", [128, 128], F32)
        P["ones_b"] = self.sb(st, "ones_b", [128, 128], BF16)
        P["modT"] = self.sb(st, "modT", [128, 96], F32)
        P["gvec"] = self.sb(st, "gvec", [128, 48], F32)
        P["gsm"] = self.sb(st, "gsm", [128, 22], F32)
        P["gmodA"] = self.sb(st, "gmodA", [128, 16], F32)
        P["gmodF"] = self.sb(st, "gmodF", [128, 16], F32)
        P["flag8"] = self.sb(st, "flag8", [128, c.NSLOT * 8], F32)
        P["invf"] = self.sb(st, "invf", [64, 1], F32)
        P["sgn"] = self.sb(st, "sgn", [64, 1], F32)
        P["kmax2"] = self.sb(st, "kmax2", [1, 2], F32)
        cs = [kb.dsem("c%d" % i) for i in range(8)]
        s0 = cs[0]
        self.dma("sp", P["ident_f"][:], self.ident_d, [], ["ident_f"], cs[0])
        self.dma("pool", P["ident_b"][:], self.ident_d, [], ["ident_b"], cs[1])
        self.dma("sp", P["gvec"][:], self.gvecT, [], ["gvec"], cs[2])
        self.dma("sp", P["gsm"][:], self.gsmallT, [], ["gsm"], cs[3])
        self.dma("sp", P["flag8"][:], self.flags.partition_broadcast(128), [], ["flag8"], cs[4])
        self.dma("sp", P["invf"][:], self.invfreq2, [], ["invf"], cs[5])
        kb.op("dve", lambda e: e.memset(P["ones_f"][:], 1.0), writes=["ones_f"])
        kb.op("dve", lambda e: e.memset(P["ones_b"][:], 1.0), writes=["ones_b"])
        kb.op("dve", lambda e: e.memset(P["sgn"][0:32, :], -1.0), writes=["sgn0"])
        kb.op("dve", lambda e: e.memset(P["sgn"][32:64, :], 1.0), writes=["sgn1"])
        kb.op("dve", lambda e: e.memset(P["kmax2"][:], 0.0), writes=["kmax2"])

        with contextlib.ExitStack() as ph:
            cT = self.sb(ph, "cT_s", [128, 16], F32)
            sc = self.sb(ph, "sc_s", [128, 16], F32)
            bT = self.sb(ph, "bT_s", [128, 96], F32)
            wbuf = [self.sb(ph, "wada%d" % i, [128, 16, 512], F32) for i in range(2)]
            wsem = [kb.dsem("wada%d" % i) for i in range(2)]
            mps = self.ps(ph, "mod_ps", [128, 96], F32)
            self.dma("sp", cT[:], self.cT, [], ["cT"], cs[6])
            self.dma("sp", bT[:], self.b_adaT, [], ["bT"], cs[7])
            kb.op("act", lambda e: e.activation(out=sc[:], in_=cT[:], func=AF.Silu), reads=["cT"], writes=["sc"])
            wv = self.w_ada.rearrange("(kc p) n -> p kc n", p=128)
            npieces = (c.NADA * c.D) // 512
            for pi in range(npieces):
                b = pi % 2
                eng = "sp" if b == 0 else "pool"
                self.dma(eng, wbuf[b][:], wv[:, :, pi * 512:(pi + 1) * 512], [], ["wada%d" % b], wsem[b])
                for fb in range(4):
                    col = pi * 4 + fb
                    for kc in range(16):
                        kb.op("pe", lambda e, b=b, fb=fb, kc=kc, col=col: e.matmul(
                            mps[:, col:col + 1], lhsT=wbuf[b][:, kc, fb * 128:(fb + 1) * 128], rhs=sc[:, kc:kc + 1],
                            start=(kc == 0), stop=(kc == 15)),
                            reads=["wada%d" % b, "sc"], writes=["mod_ps"])
            kb.op("dve", lambda e: e.tensor_tensor(out=P["modT"][:], in0=mps[:], in1=bT[:], op=ALU.add),
                  reads=["mod_ps", "bT"], writes=["modT"])
            kb.op("dve", lambda e: e.scalar_tensor_tensor(out=P["gmodA"][:], in0=P["modT"][:, 16:32], scalar=1.0,
                                                          in1=P["gvec"][:, 0:16], op0=ALU.add, op1=ALU.mult),
                  reads=["modT", "gvec"], writes=["gmodA"])
            kb.op("dve", lambda e: e.scalar_tensor_tensor(out=P["gmodF"][:], in0=P["modT"][:, 64:80], scalar=1.0,
                                                          in1=P["gvec"][:, 16:32], op0=ALU.add, op1=ALU.mult),
                  reads=["modT", "gvec"], writes=["gmodF"])
            if c.debug:
                self.dma("sp", self.MODT, P["modT"][:], ["modT"], [], kb.dsem("dbg"))
            kb.barrier()
            kb.release_dsems(wsem + cs)

    def bcast_tile(self, st, ph, name, srcT, key):
        kb, P = self.kb, self.P
        dst = self.sb(st, name, [128, 2048], F32)
        dg = self.sb(ph, name + "_dg", [128, 128], F32)
        for ci in range(16):
            pst_full, pkk = self.pbank()
            pst = pst_full[:, 0:128]
            kb.op("dve", lambda e, ci=ci: e.tensor_scalar(out=dg[:], in0=P["ident_f"][:], scalar1=srcT[:, ci:ci + 1],
                                                         scalar2=None, op0=ALU.mult),
                  reads=["ident_f", key], writes=[name + "_dg"])
            kb.op("pe", lambda e, pst=pst: e.matmul(pst, lhsT=P["ones_f"][:], rhs=dg[:], start=True, stop=True),
                  reads=["ones_f", name + "_dg"], writes=[pkk])
            kb.op("act", lambda e, ci=ci, pst=pst: e.activation(out=dst[:, ci * 128:(ci + 1) * 128], in_=pst, func=AF.Copy),
                  reads=[pkk], writes=[name])
        return dst


def _T128(v):
    v = np.asarray(v).reshape(-1, 128)
    return np.ascontiguousarray(v.T)


def lnmult_table():
    tab = np.full((17, 128, 128), -1e30, np.float32)
    k = np.arange(128)[:, None]
    q = np.arange(128)[None, :]
    for dl in range(17):
        diff = 128 * dl + q - k
        cnt = np.zeros((128, 128), np.int32)
        for w, d in Cfg.WINDOWS:
            cnt += ((diff >= 0) & (diff <= w) & (diff % d == 0)).astype(np.int32)
        tab[dl] = np.where(cnt > 0, np.log(np.maximum(cnt, 1)).astype(np.float32), np.float32(-1e30))
    return np.ascontiguousarray(tab.transpose(1, 0, 2))


def prepare_shared(inp, cfg):
    c = cfg
    sh = {}
    sh["cT"] = _T128(inp["c"][0])
    sh["w_ada"] = np.ascontiguousarray(inp["w_ada"][0])
    sh["b_adaT"] = _T128(inp["b_ada"][0])
    sh["gvecT"] = np.concatenate([_T128(inp["norm_attn_g"][0]), _T128(inp["norm_ffn_g"][0]),
                                  _T128(inp["final_norm_g"])], axis=1)
    w_in = inp["w_in"][0]
    rope = w_in[:, 3840:3904]
    sh["w_in"] = np.concatenate([w_in, rope[:, 32:64], rope[:, 0:32]], axis=1)
    sh["gsmallT"] = np.concatenate([_T128(inp["g_q"][0]), _T128(inp["g_kv"][0]),
                                    _T128(inp["g_out_swa"][0]), _T128(inp["g_out_mla"][0])], axis=1)
    wq = inp["w_uq"][0].reshape(c.QL, c.NH, 192)
    sh["w_uq"] = np.concatenate([wq[:, :, 0:128].reshape(c.QL, -1), wq[:, :, 128:192].reshape(c.QL, -1),
                                 np.concatenate([wq[:, :, 160:192], wq[:, :, 128:160]], axis=2).reshape(c.QL, -1)],
                                axis=1)
    wkv = inp["w_ukv"][0].reshape(c.KVL, c.NH, 256)
    sh["w_ukv"] = np.concatenate([wkv[:, :, 0:128].reshape(c.KVL, -1), wkv[:, :, 128:256].reshape(c.KVL, -1)], axis=1)
    sh["w_o"] = np.ascontiguousarray(inp["w_o"][0])
    sh["w_router"] = np.ascontiguousarray(inp["w_router"][0])
    sh["rbias"] = np.ascontiguousarray(inp["router_bias"][0][None, :])
    sh["w_eg"] = inp["w_exp_gate"][0]
    sh["w_eu"] = inp["w_exp_up"][0]
    sh["w_ed"] = inp["w_exp_down"][0]
    sh["w_sg"] = inp["w_sh_gate"][0]
    sh["w_su"] = inp["w_sh_up"][0]
    sh["w_sd"] = inp["w_sh_down"][0]
    sh["ident"] = np.eye(128, dtype=np.float32)
    half = c.ROPE // 2
    invf = (np.float32(10000.0) ** (-np.arange(half, dtype=np.float32) / np.float32(half))).astype(np.float32)
    sh["invfreq2"] = np.concatenate([invf, invf])[:, None].astype(np.float32)
    sh["lnmult"] = lnmult_table()
    sh["tri"] = (np.arange(128)[:, None] <= np.arange(128)[None, :]).astype(np.float32)
    sh["lstrict"] = (np.arange(128)[:, None] < np.arange(128)[None, :]).astype(np.float32)
    sh["iota64"] = np.ascontiguousarray(np.broadcast_to(np.arange(c.NE, dtype=np.float32)[None, :], (128, c.NE)))
    return sh


def slot_order(core, nchunks, nslot):
    order = [core - s for s in range(core + 1)]
    rest = [j for j in range(nchunks) if j > core]
    order = order + rest
    order = order[:nslot] + [-1] * max(0, nslot - len(order))
    valid = [1.0 if s <= core and order[s] >= 0 else 0.0 for s in range(nslot)]
    return order, valid


def prepare_core(inp, cfg, core, nchunks):
    c = cfg
    x = inp["x"][0]
    pos = inp["positions"][0]
    order, valid = slot_order(core, nchunks, c.NSLOT)
    xs, ps = [], []
    for s, j in enumerate(order):
        if j >= 0:
            xs.append(x[j * c.CH:(j + 1) * c.CH])
            ps.append(pos[j * c.CH:(j + 1) * c.CH])
        else:
            xs.append(x[0:c.CH])
            ps.append(pos[0:c.CH])
    d = {}
    d["xk"] = np.concatenate(xs, axis=0)
    d["posk"] = np.concatenate(ps)[None, :].astype(np.int32)
    d["flags"] = np.repeat(np.asarray(valid, np.float32), 8)[None, :]
    return d


class Phases2(Phases):
    def alloc_psum(self, ph, nf32=6, nbf=2):
        self.pbanks = [self.ps(ph, "pb%d" % i, [128, 512], F32) for i in range(nf32)]
        self.pbi = 0
        self.tbanks = [self.ps(ph, "tb%d" % i, [128, 8, 128], BF16) for i in range(nbf)]
        self.tbi = 0

    def pbank(self):
        i = self.pbi % len(self.pbanks)
        self.pbi += 1
        return self.pbanks[i], "pb%d" % i

    def tbank(self):
        i = self.tbi % len(self.tbanks)
        self.tbi += 1
        return self.tbanks[i], "tb%d" % i

    def alloc_normT(self, ph, pref, Fmax, nslots=2):
        W = {"pref": pref, "n": nslots, "i": 0}
        W["junk"] = self.sb(ph, pref + "_junk", [128, Fmax], BF16)
        W["xn"] = [self.sb(ph, pref + "_xn%d" % i, [128, Fmax], BF16) for i in range(nslots)]
        W["st"] = [self.sb(ph, pref + "_st%d" % i, [128, 4], F32) for i in range(nslots)]
        return W

    def norm_T(self, W, src, src_keys, F, gT, shiftT, gkeys, dst_of, dst_keys, eps_scale=None):
        kb, P, c = self.kb, self.P, self.cfg
        i = W["i"] % W["n"]
        W["i"] += 1
        pref = W["pref"]
        junk, xn, stt = W["junk"], W["xn"][i], W["st"][i]
        kj, kx, ks = pref + "_junk", pref + "_xn%d" % i, pref + "_st%d" % i
        kb.op("act", lambda e: e.activation(out=junk[:, 0:F], in_=src, func=AF.Square, accum_out=stt[:, 0:1]),
              reads=src_keys, writes=[kj, ks])
        kb.op("dve", lambda e: e.tensor_scalar(out=stt[:, 1:2], in0=stt[:, 0:1], scalar1=1.0 / F, scalar2=c.EPS,
                                               op0=ALU.mult, op1=ALU.add), reads=[ks], writes=[ks])
        kb.op("act", lambda e: e.activation(out=stt[:, 2:3], in_=stt[:, 1:2], func=AF.Sqrt), reads=[ks], writes=[ks])
        kb.op("dve", lambda e: e.reciprocal(out=stt[:, 3:4], in_=stt[:, 2:3]), reads=[ks], writes=[ks])
        kb.op("act", lambda e: e.activation(out=xn[:, 0:F], in_=src, func=AF.Copy, scale=stt[:, 3:4]),
              reads=list(src_keys) + [ks], writes=[kx])
        nchunk = F // 128
        for c0 in range(0, nchunk, 8):
            tb, tk = self.tbank()
            n = min(8, nchunk - c0)
            for j in range(n):
                ci = c0 + j
                kb.op("pe", lambda e, j=j, ci=ci: e.transpose(out=tb[:, j, :], in_=xn[:, ci * 128:(ci + 1) * 128],
                                                              identity=P["ident_b"][:]),
                      reads=[kx, "ident_b"], writes=[tk])
            for j in range(n):
                ci = c0 + j
                if shiftT is not None:
                    kb.op("dve", lambda e, j=j, ci=ci: e.tensor_scalar(
                        out=dst_of(ci), in0=tb[:, j, :], scalar1=gT[:, ci:ci + 1], scalar2=shiftT[:, ci:ci + 1],
                        op0=ALU.mult, op1=ALU.add), reads=[tk] + gkeys, writes=dst_keys)
                else:
                    kb.op("dve", lambda e, j=j, ci=ci: e.tensor_scalar(
                        out=dst_of(ci), in0=tb[:, j, :], scalar1=gT[:, ci:ci + 1], scalar2=None,
                        op0=ALU.mult), reads=[tk] + gkeys, writes=dst_keys)

    def alloc_rope(self, ph, pref):
        R = {"pref": pref}
        for n in ["posi"]:
            R[n] = self.sb(ph, pref + n, [64, 512], I32)
        for n in ["ang", "t", "kk", "r", "r2", "sin", "cos"]:
            R[n] = self.sb(ph, pref + n, [64, 512], F32)
        R["sem"] = self.kb.dsem(pref)
        return R

    def rope_tables(self, R, pos_ap):
        kb, P = self.kb, self.P
        p = R["pref"]
        K = lambda n: p + n
        self.dma("sp", R["posi"][:], pos_ap.partition_broadcast(64), [], [K("posi")], R["sem"])
        kb.op("dve", lambda e: e.tensor_copy(out=R["ang"][:], in_=R["posi"][:]), reads=[K("posi")], writes=[K("ang")])
        kb.op("dve", lambda e: e.tensor_scalar(out=R["ang"][:], in0=R["ang"][:], scalar1=P["invf"][:, 0:1], scalar2=None,
                                               op0=ALU.mult), reads=["invf"], writes=[K("ang")])
        kb.op("dve", lambda e: e.tensor_scalar(out=R["t"][:], in0=R["ang"][:], scalar1=float(1.0 / TWO_PI), scalar2=MAGIC,
                                               op0=ALU.mult, op1=ALU.add), reads=[K("ang")], writes=[K("t")])
        kb.op("dve", lambda e: e.tensor_scalar(out=R["kk"][:], in0=R["t"][:], scalar1=-MAGIC, scalar2=None,
                                               op0=ALU.add), reads=[K("t")], writes=[K("kk")])
        kb.op("dve", lambda e: e.scalar_tensor_tensor(out=R["r"][:], in0=R["kk"][:], scalar=-CW1, in1=R["ang"][:],
                                                      op0=ALU.mult, op1=ALU.add), reads=[K("ang"), K("kk")], writes=[K("r")])
        for cw in (CW2, CW3):
            kb.op("dve", lambda e, cw=cw: e.scalar_tensor_tensor(out=R["r"][:], in0=R["kk"][:], scalar=-cw, in1=R["r"][:],
                                                                 op0=ALU.mult, op1=ALU.add), reads=[K("kk")], writes=[K("r")])
        kb.op("dve", lambda e: e.tensor_scalar(out=R["t"][:], in0=R["r"][:], scalar1=float(np.pi / 2), scalar2=None,
                                               op0=ALU.is_gt), reads=[K("r")], writes=[K("t")])
        kb.op("dve", lambda e: e.scalar_tensor_tensor(out=R["r2"][:], in0=R["t"][:], scalar=-float(TWO_PI), in1=R["r"][:],
                                                      op0=ALU.mult, op1=ALU.add), reads=[K("t"), K("r")], writes=[K("r2")])
        kb.op("dve", lambda e: e.tensor_scalar(out=R["r2"][:], in0=R["r2"][:], scalar1=float(np.pi / 2), scalar2=None,
                                               op0=ALU.add), reads=[], writes=[K("r2")])
        lim = 3.1415925
        for n in ["r", "r2"]:
            kb.op("dve", lambda e, n=n: e.tensor_scalar(out=R[n][:], in0=R[n][:], scalar1=lim, scalar2=-lim,
                                                        op0=ALU.min, op1=ALU.max), reads=[], writes=[K(n)])
        kb.op("act", lambda e: e.activation(out=R["sin"][:], in_=R["r"][:], func=AF.Sin), reads=[K("r")], writes=[K("sin")])
        kb.op("act", lambda e: e.activation(out=R["cos"][:], in_=R["r2"][:], func=AF.Sin), reads=[K("r2")], writes=[K("cos")])
        kb.op("dve", lambda e: e.tensor_scalar(out=R["sin"][:], in0=R["sin"][:], scalar1=P["sgn"][:, 0:1], scalar2=None,
                                               op0=ALU.mult), reads=["sgn0", "sgn1"], writes=[K("sin")])

    def apply_rope(self, R, a_ps, b_ps, ps_keys, out_ap, out_keys, tmp, tmpk):
        kb = self.kb
        p = R["pref"]
        kb.op("dve", lambda e: e.tensor_tensor(out=tmp[0][:], in0=a_ps, in1=R["cos"][:], op=ALU.mult),
              reads=ps_keys + [p + "cos"], writes=[tmpk + "0"])
        kb.op("dve", lambda e: e.tensor_tensor(out=tmp[1][:], in0=b_ps, in1=R["sin"][:], op=ALU.mult),
              reads=ps_keys + [p + "sin"], writes=[tmpk + "1"])
        kb.op("pool", lambda e: e.tensor_tensor(out=out_ap, in0=tmp[0][:], in1=tmp[1][:], op=ALU.add),
              reads=[tmpk + "0", tmpk + "1"], writes=out_keys)


class Phases3(Phases2):
    def phase1a(self):
        c, kb, P = self.cfg, self.kb, self.P
        NG = c.NSLOT * c.CH // 512
        gps = c.CH // 512
        with contextlib.ExitStack() as ph:
            self.alloc_psum(ph, 6, 2)
            wkv = self.sb(ph, "wkv", [128, 16, 384], BF16)
            wukv = self.sb(ph, "wukv", [128, 2, 2048], BF16)
            ws = kb.dsem("w1a")
            self.dma("pool", wkv[:], self.w_in[:, 3584:3968].rearrange("(kc p) n -> p kc n", p=128), [], ["wkv"], ws)
            ws2 = kb.dsem("w1a2")
            self.dma("pool", wukv[:], self.w_ukv.rearrange("(kc p) n -> p kc n", p=128), [], ["wukv"], ws2)
            NX = 3
            xt = [self.sb(ph, "xt%d" % i, [128, 2048], F32) for i in range(NX)]
            xs = [kb.dsem("xt%d" % i) for i in range(NX)]
            WN = self.alloc_normT(ph, "n1", 2048, 2)
            WNb = self.alloc_normT(ph, "n1b", 256, 2)
            hT = [self.sb(ph, "hT%d" % i, [128, 16, 512], BF16) for i in range(2)]
            hs = [kb.dsem("hT%d" % i) for i in range(2)]
            ckvnT = [self.sb(ph, "ckvnT%d" % i, [128, 2, 512], BF16) for i in range(2)]
            kT_st = [self.sb(ph, "kTst%d" % i, [128, 8, 512], BF16) for i in range(2)]
            kTs = [kb.dsem("kTst%d" % i) for i in range(2)]
            kb_st = [self.sb(ph, "kbst%d" % i, [65, 512], BF16) for i in range(2)]
            kbs = [kb.dsem("kbst%d" % i) for i in range(2)]
            va_st = [self.sb(ph, "vast%d" % i, [128, 4, 8 * 129], BF16) for i in range(2)]
            vas = [kb.dsem("vast%d" % i) for i in range(2)]
            sq = self.sb(ph, "sq1a", [128, 8, 512], BF16)
            sqr = self.sb(ph, "sqr1a", [64, 512], BF16)
            sqmax = self.sb(ph, "sqmax1a", [128, 512], BF16)
            kmrun = self.sb(ph, "kmrun", [1, 512], F32)
            tmp = [self.sb(ph, "rtmp%d" % i, [64, 512], F32) for i in range(2)]
            R = self.alloc_rope(ph, "r1a")
            kb.op("dve", lambda e: e.memset(kmrun[:], 0.0), writes=["kmrun"])
            for i in range(2):
                kb.op("dve", lambda e, i=i: e.memset(kb_st[i][64:65, :], 1.0), writes=["kbst%d_one" % i])
            xv = self.xk.rearrange("(n p) d -> n p d", p=128)
            cnt1a = [0]

            def stageA(g):
                slot = g // gps
                tok0 = g * 512
                b = g % 2
                for t in range(4):
                    xi = cnt1a[0] % NX
                    cnt1a[0] += 1
                    self.dma("sp", xt[xi][:], xv[g * 4 + t], [], ["xt%d" % xi], xs[xi])
                    self.norm_T(WN, xt[xi][:], ["xt%d" % xi], 2048, P["gmodA"], P["modT"], ["gmodA", "modT"],
                                lambda ci, t=t, b=b: hT[b][:, ci, t * 128:(t + 1) * 128], ["hT%d" % b])
                if slot < 2:
                    self.dma("sp", self.HT[slot, :, :, (g % gps) * 512:(g % gps + 1) * 512], hT[b][:], ["hT%d" % b], [], hs[b])

            def stageB(g):
                slot = g // gps
                tok0 = g * 512
                b = g % 2
                self.rope_tables(R, self.posk[:, tok0:tok0 + 512])
                for t in range(4):
                    pb, pk = self.pbank()
                    for ci in range(16):
                        kb.op("pe", lambda e, ci=ci, t=t, pb=pb: e.matmul(
                            pb[:, 0:256], lhsT=hT[b][:, ci, t * 128:(t + 1) * 128], rhs=wkv[:, ci, 0:256],
                            start=(ci == 0), stop=(ci == 15)), reads=["hT%d" % b, "wkv"], writes=[pk])
                    self.norm_T(WNb, pb[:, 0:256], [pk], 256, P["gsm"][:, 4:6], None, ["gsm"],
                                lambda ci, t=t, b=b: ckvnT[b][:, ci, t * 128:(t + 1) * 128], ["ckvnT%d" % b])
                pa, pak = self.pbank()
                pbb, pbk = self.pbank()
                for (pp, ppk, c0) in ((pa, pak, 256), (pbb, pbk, 320)):
                    for ci in range(16):
                        kb.op("pe", lambda e, ci=ci, pp=pp, c0=c0: e.matmul(
                            pp[0:64, :], lhsT=wkv[:, ci, c0:c0 + 64], rhs=hT[b][:, ci, :],
                            start=(ci == 0), stop=(ci == 15)), reads=["hT%d" % b, "wkv"], writes=[ppk])
                self.apply_rope(R, pa[0:64, :], pbb[0:64, :], [pak, pbk], kb_st[b][0:64, :], ["kbst%d" % b], tmp, "rtmp")
                kb.op("act", lambda e: e.activation(out=sqr[:], in_=kb_st[b][0:64, :], func=AF.Square),
                      reads=["kbst%d" % b], writes=["sqr1a"])
                for h in range(c.NH):
                    pb, pk = self.pbank()
                    for kc in range(2):
                        kb.op("pe", lambda e, kc=kc, h=h, pb=pb: e.matmul(
                            pb[:], lhsT=wukv[:, kc, h * 128:(h + 1) * 128], rhs=ckvnT[b][:, kc, :],
                            start=(kc == 0), stop=(kc == 1)), reads=["ckvnT%d" % b, "wukv"], writes=[pk])
                    kb.op("act", lambda e, h=h, pb=pb: e.activation(out=kT_st[b][:, h, :], in_=pb[:], func=AF.Copy),
                          reads=[pk], writes=["kTst%d" % b])
                    kb.op("act", lambda e, h=h, pb=pb: e.activation(out=sq[:, h, :], in_=pb[:], func=AF.Square),
                          reads=[pk], writes=["sq1a"])
                kb.op("dve", lambda e: e.tensor_reduce(out=sqmax[:], in_=sq[:].rearrange("p h t -> p t h"),
                                                       axis=AX.X, op=ALU.max), reads=["sq1a"], writes=["sqmax1a"])
                pu, puk = self.pbank()
                kb.op("pe", lambda e: e.matmul(pu[0:1, :], lhsT=P["ones_b"][:, 0:1], rhs=sqmax[:], start=True, stop=False),
                      reads=["ones_b", "sqmax1a"], writes=[puk])
                kb.op("pe", lambda e: e.matmul(pu[0:1, :], lhsT=P["ones_b"][0:64, 0:1], rhs=sqr[:], start=False, stop=True),
                      reads=["ones_b", "sqr1a"], writes=[puk])
                kb.op("dve", lambda e: e.tensor_tensor(out=kmrun[:], in0=pu[0:1, :], in1=kmrun[:], op=ALU.max),
                      reads=[puk], writes=["kmrun"])
                for t in range(4):
                    for half in range(2):
                        pb, pk = self.pbank()
                        for kc in range(2):
                            kb.op("pe", lambda e, kc=kc, t=t, half=half, pb=pb: e.matmul(
                                pb[:], lhsT=ckvnT[b][:, kc, t * 128:(t + 1) * 128],
                                rhs=wukv[:, kc, 1024 + half * 512:1024 + (half + 1) * 512],
                                start=(kc == 0), stop=(kc == 1)), reads=["ckvnT%d" % b, "wukv"], writes=[pk])
                        dstv = va_st[b][:, t, :].rearrange("p (h f) -> p h f", f=129)[:, half * 4:(half + 1) * 4, 0:128]
                        kb.op("dve", lambda e, pb=pb, dstv=dstv: e.tensor_scalar(
                            out=dstv, in0=pb[:].rearrange("p (h f) -> p h f", f=128),
                            scalar1=P["flag8"][:, slot * 8:slot * 8 + 1], scalar2=None, op0=ALU.mult),
                            reads=[pk, "flag8"], writes=["vast%d" % b])
                    onec = va_st[b][:, t, :].rearrange("p (h f) -> p h f", f=129)[:, :, 128:129]
                    kb.op("pool", lambda e, onec=onec: e.tensor_copy(
                        out=onec, in_=P["flag8"][:, slot * 8:(slot + 1) * 8].unsqueeze(2)),
                        reads=["flag8"], writes=["vast%d" % b])
                self.dma("sp", self.KT_A[:, :, tok0:tok0 + 512].rearrange("h p t -> p h t"), kT_st[b][:],
                         ["kTst%d" % b], [], kTs[b])
                self.dma("sp", self.KT_B[:, tok0:tok0 + 512], kb_st[b][:], ["kbst%d" % b, "kbst%d_one" % b], [], kbs[b])
                self.dma("sp", self.V_AUG[tok0:tok0 + 512, :].rearrange("(t p) f -> p t f", p=128), va_st[b][:],
                         ["vast%d" % b], [], vas[b])

            stageA(0)
            for g in range(NG):
                if g + 1 < NG:
                    stageA(g + 1)
                stageB(g)
            kb.op("dve", lambda e: e.tensor_reduce(out=P["kmax2"][0:1, 0:1], in_=kmrun[:], axis=AX.X, op=ALU.max),
                  reads=["kmrun"], writes=["kmax2"])
            kb.barrier()
            kb.release_dsems([ws, ws2] + xs + hs + kTs + kbs + vas + [R["sem"]])


class Phases4(Phases3):
    def phase1b(self):
        c, kb, P = self.cfg, self.kb, self.P
        gps = c.CH // 512
        with contextlib.ExitStack() as ph:
            self.alloc_psum(ph, 6, 2)
            wkva = self.sb(ph, "wkva", [128, 16, 2048], BF16)
            ws = [kb.dsem("w1b%d" % i) for i in range(2)]
            for i in range(2):
                self.dma("pool", wkva[:, :, i * 1024:(i + 1) * 1024],
                         self.w_in[:, 1024 + i * 1024:2048 + i * 1024].rearrange("(kc p) n -> p kc n", p=128),
                         [], ["wkva%d" % i], ws[i])
            hT = [self.sb(ph, "hTb%d" % i, [128, 16, 512], BF16) for i in range(2)]
            hs = [kb.dsem("hTb%d" % i) for i in range(2)]
            kT_st = [self.sb(ph, "kaTst%d" % i, [128, 8, 512], BF16) for i in range(2)]
            kTs = [kb.dsem("kaTst%d" % i) for i in range(2)]
            va_st = [self.sb(ph, "vastb%d" % i, [128, 4, 8 * 129], BF16) for i in range(2)]
            vas = [kb.dsem("vastb%d" % i) for i in range(2)]
            sq = self.sb(ph, "sq1b", [128, 8, 512], BF16)
            sqmax = self.sb(ph, "sqmax1b", [128, 512], BF16)
            kmrun = self.sb(ph, "kmrunb", [1, 512], F32)
            kb.op("dve", lambda e: e.memset(kmrun[:], 0.0), writes=["kmrunb"])
            for g in range(2 * gps):
                slot = g // gps
                b = g % 2
                tokd = (1 - slot) * c.CH + (g % gps) * 512
                self.dma("sp", hT[b][:], self.HT[slot, :, :, (g % gps) * 512:(g % gps + 1) * 512], [], ["hTb%d" % b], hs[b])
                for h in range(c.NH):
                    pb, pk = self.pbank()
                    for ci in range(16):
                        kb.op("pe", lambda e, ci=ci, h=h, pb=pb: e.matmul(
                            pb[:], lhsT=wkva[:, ci, h * 128:(h + 1) * 128], rhs=hT[b][:, ci, :],
                            start=(ci == 0), stop=(ci == 15)), reads=["hTb%d" % b, "wkva0"], writes=[pk])
                    kb.op("act", lambda e, h=h, pb=pb: e.activation(out=kT_st[b][:, h, :], in_=pb[:], func=AF.Copy),
                          reads=[pk], writes=["kaTst%d" % b])
                    kb.op("act", lambda e, h=h, pb=pb: e.activation(out=sq[:, h, :], in_=pb[:], func=AF.Square),
                          reads=[pk], writes=["sq1b"])
                kb.op("dve", lambda e: e.tensor_reduce(out=sqmax[:], in_=sq[:].rearrange("p h t -> p t h"),
                                                       axis=AX.X, op=ALU.max), reads=["sq1b"], writes=["sqmax1b"])
                pu, puk = self.pbank()
                kb.op("pe", lambda e: e.matmul(pu[0:1, :], lhsT=P["ones_b"][:, 0:1], rhs=sqmax[:], start=True, stop=True),
                      reads=["ones_b", "sqmax1b"], writes=[puk])
                kb.op("dve", lambda e: e.tensor_tensor(out=kmrun[:], in0=pu[0:1, :], in1=kmrun[:], op=ALU.max),
                      reads=[puk], writes=["kmrunb"])
                for t in range(4):
                    for half in range(2):
                        pb, pk = self.pbank()
                        for ci in range(16):
                            kb.op("pe", lambda e, ci=ci, t=t, half=half, pb=pb: e.matmul(
                                pb[:], lhsT=hT[b][:, ci, t * 128:(t + 1) * 128],
                                rhs=wkva[:, ci, 1024 + half * 512:1024 + (half + 1) * 512],
                                start=(ci == 0), stop=(ci == 15)), reads=["hTb%d" % b, "wkva1"], writes=[pk])
                        dstv = va_st[b][:, t, :].rearrange("p (h f) -> p h f", f=129)[:, half * 4:(half + 1) * 4, 0:128]
                        kb.op("dve", lambda e, pb=pb, dstv=dstv: e.tensor_scalar(
                            out=dstv, in0=pb[:].rearrange("p (h f) -> p h f", f=128),
                            scalar1=P["flag8"][:, slot * 8:slot * 8 + 1], scalar2=None, op0=ALU.mult),
                            reads=[pk, "flag8"], writes=["vastb%d" % b])
                    onec = va_st[b][:, t, :].rearrange("p (h f) -> p h f", f=129)[:, :, 128:129]
                    kb.op("pool", lambda e, onec=onec: e.tensor_copy(
                        out=onec, in_=P["flag8"][:, slot * 8:(slot + 1) * 8].unsqueeze(2)),
                        reads=["flag8"], writes=["vastb%d" % b])
                self.dma("sp", self.KAT[:, :, tokd:tokd + 512].rearrange("h p t -> p h t"), kT_st[b][:],
                         ["kaTst%d" % b], [], kTs[b])
                self.dma("sp", self.VA_AUG[tokd:tokd + 512, :].rearrange("(t p) f -> p t f", p=128), va_st[b][:],
                         ["vastb%d" % b], [], vas[b])
            kb.op("dve", lambda e: e.tensor_reduce(out=P["kmax2"][0:1, 1:2], in_=kmrun[:], axis=AX.X, op=ALU.max),
                  reads=["kmrunb"], writes=["kmax2"])
            kb.barrier()
            kb.release_dsems(ws + hs + kTs + vas)

    def negc_row(self, u_ps, upk, kcol, dst, dstk, tmpf):
        kb, P = self.kb, self.P
        kb.op("act", lambda e: e.activation(out=tmpf[:], in_=u_ps, func=AF.Sqrt, scale=P["kmax2"][0:1, kcol:kcol + 1]),
              reads=[upk, "kmax2"], writes=["negc_tmp"])
        kb.op("dve", lambda e: e.tensor_scalar(out=dst, in0=tmpf[:], scalar1=-1.0, scalar2=None, op0=ALU.mult),
              reads=["negc_tmp"], writes=[dstk])

    def phase1c(self):
        c, kb, P = self.cfg, self.kb, self.P
        gps = c.CH // 512
        with contextlib.ExitStack() as ph:
            self.alloc_psum(ph, 6, 2)
            wq = self.sb(ph, "wq", [128, 16, 1536], BF16)
            wuq = self.sb(ph, "wuq", [128, 4, 2048], BF16)
            ws = [kb.dsem("w1c%d" % i) for i in range(3)]
            self.dma("pool", wq[:, :, 0:1024], self.w_in[:, 0:1024].rearrange("(kc p) n -> p kc n", p=128), [], ["wq0"], ws[0])
            self.dma("pool", wq[:, :, 1024:1536], self.w_in[:, 3072:3584].rearrange("(kc p) n -> p kc n", p=128), [], ["wq1"], ws[1])
            self.dma("pool", wuq[:], self.w_uq.rearrange("(kc p) n -> p kc n", p=128), [], ["wuq"], ws[2])
            WN = self.alloc_normT(ph, "n1c", 512, 2)
            hT = [self.sb(ph, "hTc%d" % i, [128, 16, 512], BF16) for i in range(2)]
            hs = [kb.dsem("hTc%d" % i) for i in range(2)]
            cqnT = [self.sb(ph, "cqnT%d" % i, [128, 4, 512], BF16) for i in range(2)]
            qa_st = [self.sb(ph, "qast%d" % i, [128, 8, 512], BF16) for i in range(2)]
            qas = [kb.dsem("qast%d" % i) for i in range(2)]
            qn_st = [self.sb(ph, "qnst%d" % i, [128, 8, 512], BF16) for i in range(2)]
            qns = [kb.dsem("qnst%d" % i) for i in range(2)]
            qb_st = [self.sb(ph, "qbst%d" % i, [64, 8, 512], BF16) for i in range(2)]
            qbs = [kb.dsem("qbst%d" % i) for i in range(2)]
            nca_st = [self.sb(ph, "ncast0", [1, 8, 512], BF16)] * 2
            ncas = [kb.dsem("ncast0")] * 2
            ncb_st = [self.sb(ph, "ncbst0", [1, 8, 512], BF16)] * 2
            ncbs = [kb.dsem("ncbst0")] * 2
            sq = self.sb(ph, "sq1c", [128, 512], BF16)
            sqr = self.sb(ph, "sqr1c", [64, 512], BF16)
            tmpf = self.sb(ph, "negc_tmp", [1, 512], F32)
            tmp = [self.sb(ph, "rtmpc%d" % i, [64, 512], F32) for i in range(2)]
            R = self.alloc_rope(ph, "r1c")
            for g in range(gps):
                b = g % 2
                tok0 = g * 512
                self.rope_tables(R, self.posk[:, tok0:tok0 + 512])
                self.dma("sp", hT[b][:], self.HT[0, :, :, tok0:tok0 + 512], [], ["hTc%d" % b], hs[b])
                for h in range(c.NH):
                    pb, pk = self.pbank()
                    for ci in range(16):
                        kb.op("pe", lambda e, ci=ci, h=h, pb=pb: e.matmul(
                            pb[:], lhsT=wq[:, ci, h * 128:(h + 1) * 128], rhs=hT[b][:, ci, :],
                            start=(ci == 0), stop=(ci == 15)), reads=["hTc%d" % b, "wq0"], writes=[pk])
                    kb.op("act", lambda e, h=h, pb=pb: e.activation(out=qa_st[b][:, h, :], in_=pb[:], func=AF.Copy),
                          reads=[pk], writes=["qast%d" % b])
                    kb.op("act", lambda e, pb=pb: e.activation(out=sq[:], in_=pb[:], func=AF.Square),
                          reads=[pk], writes=["sq1c"])
                    pu, puk = self.pbank()
                    kb.op("pe", lambda e, pu=pu: e.matmul(pu[0:1, :], lhsT=P["ones_b"][:, 0:1], rhs=sq[:], start=True, stop=True),
                          reads=["ones_b", "sq1c"], writes=[puk])
                    self.negc_row(pu[0:1, :], puk, 1, nca_st[b][0:1, h, :], "ncast0", tmpf)
                for t in range(4):
                    pb, pk = self.pbank()
                    for ci in range(16):
                        kb.op("pe", lambda e, ci=ci, t=t, pb=pb: e.matmul(
                            pb[:], lhsT=hT[b][:, ci, t * 128:(t + 1) * 128], rhs=wq[:, ci, 1024:1536],
                            start=(ci == 0), stop=(ci == 15)), reads=["hTc%d" % b, "wq1"], writes=[pk])
                    self.norm_T(WN, pb[:], [pk], 512, P["gsm"][:, 0:4], None, ["gsm"],
                                lambda ci, t=t, b=b: cqnT[b][:, ci, t * 128:(t + 1) * 128], ["cqnT%d" % b])
                for h in range(c.NH):
                    pb, pk = self.pbank()
                    for kc in range(4):
                        kb.op("pe", lambda e, kc=kc, h=h, pb=pb: e.matmul(
                            pb[:], lhsT=wuq[:, kc, h * 128:(h + 1) * 128], rhs=cqnT[b][:, kc, :],
                            start=(kc == 0), stop=(kc == 3)), reads=["cqnT%d" % b, "wuq"], writes=[pk])
                    kb.op("act", lambda e, h=h, pb=pb: e.activation(out=qn_st[b][:, h, :], in_=pb[:], func=AF.Copy),
                          reads=[pk], writes=["qnst%d" % b])
                    kb.op("act", lambda e, pb=pb: e.activation(out=sq[:], in_=pb[:], func=AF.Square),
                          reads=[pk], writes=["sq1c"])
                    pa, pak = self.pbank()
                    pbb, pbk = self.pbank()
                    for (pp, ppk, c0) in ((pa, pak, 1024), (pbb, pbk, 1536)):
                        for kc in range(4):
                            kb.op("pe", lambda e, kc=kc, pp=pp, c0=c0, h=h: e.matmul(
                                pp[0:64, :], lhsT=wuq[:, kc, c0 + h * 64:c0 + (h + 1) * 64], rhs=cqnT[b][:, kc, :],
                                start=(kc == 0), stop=(kc == 3)), reads=["cqnT%d" % b, "wuq"], writes=[ppk])
                    self.apply_rope(R, pa[0:64, :], pbb[0:64, :], [pak, pbk], qb_st[b][:, h, :], ["qbst%d" % b], tmp, "rtmpc")
                    kb.op("act", lambda e, h=h: e.activation(out=sqr[:], in_=qb_st[b][:, h, :], func=AF.Square),
                          reads=["qbst%d" % b], writes=["sqr1c"])
                    pu, puk = self.pbank()
                    kb.op("pe", lambda e, pu=pu: e.matmul(pu[0:1, :], lhsT=P["ones_b"][:, 0:1], rhs=sq[:], start=True, stop=False),
                          reads=["ones_b", "sq1c"], writes=[puk])
                    kb.op("pe", lambda e, pu=pu: e.matmul(pu[0:1, :], lhsT=P["ones_b"][0:64, 0:1], rhs=sqr[:], start=False, stop=True),
                          reads=["ones_b", "sqr1c"], writes=[puk])
                    self.negc_row(pu[0:1, :], puk, 0, ncb_st[b][0:1, h, :], "ncbst0", tmpf)
                self.dma("sp", self.QAT[:, :, tok0:tok0 + 512].rearrange("h p t -> p h t"), qa_st[b][:], ["qast%d" % b], [], qas[b])
                self.dma("sp", self.NEGCA[:, tok0:tok0 + 512].rearrange("(o h) t -> o h t", o=1), nca_st[b][:], ["ncast0"], [], ncas[b])
                self.dma("sp", self.QT_A[:, :, tok0:tok0 + 512].rearrange("h p t -> p h t"), qn_st[b][:], ["qnst%d" % b], [], qns[b])
                self.dma("sp", self.QT_B[:, 0:64, tok0:tok0 + 512].rearrange("h p t -> p h t"), qb_st[b][:], ["qbst%d" % b], [], qbs[b])
                self.dma("sp", self.QT_B[:, 64:65, tok0:tok0 + 512].rearrange("h o t -> o h t"), ncb_st[b][:], ["ncbst0"], [], ncbs[b])
            kb.barrier()
            kb.release_dsems(ws + hs + qas + qns + qbs + [ncas[0], ncbs[0], R["sem"]])


class Phases5(Phases4):
    def phase2(self):
        c, kb, P = self.cfg, self.kb, self.P
        NQ = c.CH // 128
        slopes = _slopes(c.NH)
        scale = float(c.HD ** -0.5)
        with contextlib.ExitStack() as ph:
            stp = [self.ps(ph, "stp%d" % i, [128, 8, 128], F32) for i in range(2)]
            accp = [self.ps(ph, "accp%d" % i, [128, 3, 129], F32) for i in range(3)]
            self.tbanks = [self.ps(ph, "tbs", [128, 8, 128], BF16)]
            self.tbi = 0
            def acc_of(h):
                return accp[h // 3][:, h % 3, :], "accp%d" % (h // 3)
            kaT = self.sb(ph, "kaT", [128, 8, 2 * c.CH], BF16)
            VA = self.sb(ph, "VAr", [128, 2 * NQ, 8 * 129], BF16)
            sems = [kb.dsem("p2_%d" % i) for i in range(12)]
            self.dma("sp", kaT[:], self.KAT.rearrange("h p t -> p h t"), [], ["kaT"], sems[0])
            nva = 4
            per = 2 * NQ // nva
            for i in range(nva):
                self.dma("sp", VA[:, i * per:(i + 1) * per, :],
                         self.VA_AUG[i * per * 128:(i + 1) * per * 128, :].rearrange("(t p) f -> p t f", p=128),
                         [], ["VA%d" % i], sems[8 + i])
            posq_i = self.sb(ph, "posq_i", [128, c.CH], I32)
            posq = self.sb(ph, "posq", [128, c.CH], F32)
            posk_i = self.sb(ph, "posk_i", [128, 2 * NQ], I32)
            poskc = self.sb(ph, "poskc", [128, 2 * NQ], F32)
            lnm = self.sb(ph, "lnm", [128, 17, 128], F32)
            NS = self.sb(ph, "NS", [128, 8, 128], F32)
            self.dma("sp", posq_i[:], self.posk[:, 0:c.CH].partition_broadcast(128), [], ["posq_i"], sems[3])
            self.dma("sp", posk_i[:, 0:NQ], self.posk[:, c.CH:2 * c.CH].rearrange("o (t p) -> p (o t)", p=128),
                     [], ["posk_i0"], sems[4], allow_slow_non_contiguous=True)
            self.dma("sp", posk_i[:, NQ:2 * NQ], self.posk[:, 0:c.CH].rearrange("o (t p) -> p (o t)", p=128),
                     [], ["posk_i1"], sems[5], allow_slow_non_contiguous=True)
            self.dma("sp", lnm[:], self.lnmult, [], ["lnm"], sems[6])
            kb.op("dve", lambda e: e.tensor_copy(out=posq[:], in_=posq_i[:]), reads=["posq_i"], writes=["posq"])
            kb.op("dve", lambda e: e.tensor_copy(out=poskc[:], in_=posk_i[:]), reads=["posk_i0", "posk_i1"], writes=["poskc"])
            kb.op("dve", lambda e: e.tensor_scalar(out=poskc[:], in0=poskc[:], scalar1=-1.0, scalar2=None, op0=ALU.mult),
                  reads=[], writes=["poskc"])
            for h in range(c.NH):
                kb.op("dve", lambda e, h=h: e.memset(NS[:, h, :], -slopes[h]), writes=["NS"])
            qa = [self.sb(ph, "qa%d" % i, [128, 8, 128], BF16) for i in range(2)]
            qsem = [kb.dsem("qa%d" % i) for i in range(2)]
            ngc = [self.sb(ph, "ngc%d" % i, [1, 8, 128], BF16) for i in range(2)]
            nsem = [kb.dsem("ngc%d" % i) for i in range(2)]
            dist = [self.sb(ph, "dist%d" % i, [128, 128], F32) for i in range(2)]
            u = [self.sb(ph, "u%d" % i, [128, 8, 128], F32) for i in range(2)]
            sp_ = [self.sb(ph, "sp%d" % i, [128, 8, 128], F32) for i in range(2)]
            pT = [self.sb(ph, "pT%d" % i, [128, 8, 128], BF16) for i in range(2)]
            oa = self.sb(ph, "oa", [128, 1024], F32)
            rl = self.sb(ph, "rl", [128, 8], F32)
            WN = self.alloc_normT(ph, "n2", 1024, 1)
            mx = [self.sb(ph, "mxa%d" % i, [128, 8, 128], BF16) for i in range(2)]
            msem = [kb.dsem("mxa%d" % i) for i in range(2)]
            seq = [(b, dl) for b in range(NQ) for dl in range(16, -1, -1)]

            def front(n):
                b, dl = seq[n]
                qb = b % 2
                k = n % 2
                if dl == 16:
                    self.dma("sp", qa[qb][:], self.QAT[:, :, b * 128:(b + 1) * 128].rearrange("h p t -> p h t"), [], ["qa%d" % qb], qsem[qb])
                    self.dma("sp", ngc[qb][:], self.NEGCA[:, b * 128:(b + 1) * 128].rearrange("(o h) t -> o h t", o=1), [], ["ngc%d" % qb], nsem[qb])
                j = NQ + b - dl
                kb.op("act", lambda e: e.activation(
                    out=dist[k][:], in_=posq[:, b * 128:(b + 1) * 128], func=AF.Abs, bias=poskc[:, j:j + 1], scale=1.0),
                    reads=["posq", "poskc"], writes=["dist%d" % k])
                kb.op("pool", lambda e: e.tensor_tensor(
                    out=u[k][:], in0=dist[k][:].unsqueeze(1).broadcast_to([128, 8, 128]), in1=NS[:], op=ALU.mult),
                    reads=["dist%d" % k, "NS"], writes=["u%d" % k])
                kb.op("pool", lambda e: e.tensor_tensor(
                    out=u[k][:], in0=u[k][:], in1=lnm[:, dl, :].unsqueeze(1).broadcast_to([128, 8, 128]), op=ALU.add),
                    reads=["lnm"], writes=["u%d" % k])
                for h in range(c.NH):
                    kb.op("pe", lambda e, h=h: e.matmul(
                        stp[k][:, h, :], lhsT=kaT[:, h, j * 128:(j + 1) * 128], rhs=qa[qb][:, h, :], start=True, stop=False),
                        reads=["kaT", "qa%d" % qb], writes=["stp%d" % k])
                    kb.op("pe", lambda e, h=h: e.matmul(
                        stp[k][:, h, :], lhsT=P["ones_b"][0:1, :], rhs=ngc[qb][0:1, h, :], start=False, stop=True),
                        reads=["ones_b", "ngc%d" % qb], writes=["stp%d" % k])
                kb.op("dve", lambda e: e.scalar_tensor_tensor(
                    out=sp_[k][:], in0=stp[k][:], scalar=scale, in1=u[k][:], op0=ALU.mult, op1=ALU.add),
                    reads=["stp%d" % k, "u%d" % k], writes=["sp%d" % k])
                kb.op("act", lambda e: e.activation(out=pT[k][:], in_=sp_[k][:], func=AF.Exp),
                      reads=["sp%d" % k], writes=["pT%d" % k])

            def back(n):
                b, dl = seq[n]
                k = n % 2
                j = NQ + b - dl
                vak = "VA%d" % (j // per)
                if dl == 16:
                    for i3 in range(3):
                        kb.op("dve", lambda e, i3=i3: e.memset(accp[i3][:], 0.0), writes=["accp%d" % i3])
                for h in range(c.NH):
                    ah, ak = acc_of(h)
                    kb.op("pe", lambda e, h=h, ah=ah: e.matmul(
                        ah, lhsT=pT[k][:, h, :], rhs=VA[:, j, h * 129:(h + 1) * 129], start=False, stop=(dl == 0)),
                        reads=["pT%d" % k, vak], writes=[ak])
                if dl != 0:
                    return
                for i3 in range(3):
                    nh = 3 if i3 < 2 else 2
                    kb.op("dve", lambda e, i3=i3, nh=nh: e.reciprocal(
                        out=rl[:, i3 * 3:i3 * 3 + nh].unsqueeze(2), in_=accp[i3][:, 0:nh, 128:129]),
                        reads=["accp%d" % i3], writes=["rl"])
                for h in range(c.NH):
                    ah, ak = acc_of(h)
                    kb.op("dve", lambda e, h=h, ah=ah: e.tensor_scalar(
                        out=oa[:, h * 128:(h + 1) * 128], in0=ah[:, 0:128], scalar1=rl[:, h:h + 1], scalar2=None, op0=ALU.mult),
                        reads=[ak, "rl"], writes=["oa"])
                mb = b % 2
                self.norm_T(WN, oa[:], ["oa"], 1024, P["gsm"][:, 6:14], None, ["gsm"],
                            lambda ci, mb=mb: mx[mb][:, ci, :], ["mxa%d" % mb])
                self.dma("sp", self.MIXT[:, 0:8, b * 128:(b + 1) * 128], mx[mb][:], ["mxa%d" % mb], [], msem[mb])

            front(0)
            for n in range(len(seq)):
                if n + 1 < len(seq):
                    front(n + 1)
                back(n)
            kb.barrier()
            kb.release_dsems(sems + qsem + nsem + msem)


class Phases6(Phases5):
    def phase3(self):
        c, kb, P = self.cfg, self.kb, self.P
        NT = c.NSLOT * c.CH
        TPS = c.CH // 128
        NGQ = c.CH // 512
        scale = float((128 + c.ROPE) ** -0.5)
        with contextlib.ExitStack() as ph:
            self.alloc_psum(ph, 4, 0)
            NPT = 4
            accs = []
            for i in range(2):
                accs.append((self.ps(ph, "macc%da" % i, [128, 3, 129], F32), self.ps(ph, "macc%db" % i, [128, 1, 129], F32)))
            tri = self.sb(ph, "tri_b", [128, 128], BF16)
            ts = kb.dsem("tri")
            self.dma("pool", tri[:], self.tri_d, [], ["tri"], ts)
            ktB = self.sb(ph, "ktB", [65, NT], BF16)
            kbs = kb.dsem("ktB")
            self.dma("sp", ktB[:], self.KT_B, [], ["ktB"], kbs)
            ktA = [self.sb(ph, "ktA%d" % i, [128, NT], BF16) for i in range(2)]
            Vh = [self.sb(ph, "Vh%d" % i, [128, NT // 128, 129], BF16) for i in range(2)]
            kas = [[kb.dsem("ktA%d_%d" % (i, p)) for p in range(c.NSLOT)] for i in range(2)]
            vs = [[kb.dsem("Vh%d_%d" % (i, p)) for p in range(c.NSLOT)] for i in range(2)]
            qTa = [self.sb(ph, "qTa%d" % i, [128, c.CH], BF16) for i in range(2)]
            qTb = [self.sb(ph, "qTb%d" % i, [65, c.CH], BF16) for i in range(2)]
            qsa = [kb.dsem("qTa%d" % i) for i in range(2)]
            qsb = [kb.dsem("qTb%d" % i) for i in range(2)]
            pT = [self.sb(ph, "mpT%d" % i, [128, 512], BF16) for i in range(NPT)]
            obst = [self.sb(ph, "obst%d" % i, [128, 4, 128], F32) for i in range(2)]
            obs = [kb.dsem("obst%d" % i) for i in range(2)]
            rl = self.sb(ph, "mrl", [128, 4], F32)
            pti = 0
            it = 0
            for h in range(c.NH):
                hb = h % 2
                self.dma("sp", qTa[hb][:], self.QT_A[h], [], ["qTa%d" % hb], qsa[hb])
                self.dma("sp", qTb[hb][:], self.QT_B[h], [], ["qTb%d" % hb], qsb[hb])
                for p in range(c.NSLOT):
                    self.dma("sp", ktA[hb][:, p * c.CH:(p + 1) * c.CH], self.KT_A[h, :, p * c.CH:(p + 1) * c.CH],
                             [], ["ktA%d_%d" % (hb, p)], kas[hb][p])
                    self.dma("sp", Vh[hb][:, p * TPS:(p + 1) * TPS, :],
                             self.V_AUG[p * c.CH:(p + 1) * c.CH, h * 129:(h + 1) * 129].rearrange("(t p) f -> p t f", p=128),
                             [], ["Vh%d_%d" % (hb, p)], vs[hb][p])
                for g in range(NGQ):
                    ab = it % 2
                    it += 1
                    accA, accB = accs[ab]
                    def acc_of(i):
                        return (accA[:, i, :], "macc%da" % ab) if i < 3 else (accB[:, 0, :], "macc%db" % ab)
                    tiles = [(jt, max(0, jt - 4 * g), (jt - 4 * g) if jt >= 4 * g else None) for jt in range(4 * g + 4)]
                    tiles += [(kt, 0, None) for kt in range(TPS, c.NSLOT * TPS)]
                    kb.op("dve", lambda e: e.memset(accA[:], 0.0), writes=["macc%da" % ab])
                    kb.op("dve", lambda e: e.memset(accB[:], 0.0), writes=["macc%db" % ab])
                    first = {}
                    last = {}
                    for n, (kt, imin, idg) in enumerate(tiles):
                        for i in range(imin, 4):
                            first.setdefault(i, n)
                            last[i] = n
                    LAG = 2
                    pend = {}
                    for n2 in range(len(tiles) + LAG):
                        if n2 < len(tiles):
                            n = n2
                            kt, imin, idg = tiles[n]
                            p = kt // TPS
                            pb, pk = self.pbank()
                            q0 = g * 512 + imin * 128
                            q1 = (g + 1) * 512
                            kb.op("pe", lambda e, pb=pb, kt=kt, imin=imin, q0=q0, q1=q1: e.matmul(
                                pb[:, imin * 128:512], lhsT=ktA[hb][:, kt * 128:(kt + 1) * 128], rhs=qTa[hb][:, q0:q1],
                                start=True, stop=False), reads=["ktA%d_%d" % (hb, p), "qTa%d" % hb], writes=[pk])
                            kb.op("pe", lambda e, pb=pb, kt=kt, imin=imin, q0=q0, q1=q1: e.matmul(
                                pb[:, imin * 128:512], lhsT=ktB[:, kt * 128:(kt + 1) * 128], rhs=qTb[hb][:, q0:q1],
                                start=False, stop=True), reads=["ktB", "qTb%d" % hb], writes=[pk])
                            pi = pti % NPT
                            pti += 1
                            kb.op("act", lambda e, pb=pb, pi=pi, imin=imin: e.activation(
                                out=pT[pi][:, imin * 128:512], in_=pb[:, imin * 128:512], func=AF.Exp, scale=scale),
                                reads=[pk], writes=["mpT%d" % pi])
                            if idg is not None:
                                kb.op("pool", lambda e, pi=pi, idg=idg: e.tensor_tensor(
                                    out=pT[pi][:, idg * 128:(idg + 1) * 128], in0=pT[pi][:, idg * 128:(idg + 1) * 128],
                                    in1=tri[:], op=ALU.mult), reads=["tri"], writes=["mpT%d" % pi])
                            pend[n] = pi
                        if n2 >= LAG:
                            n = n2 - LAG
                            kt, imin, idg = tiles[n]
                            p = kt // TPS
                            pi = pend.pop(n)
                            for i in range(imin, 4):
                                ai, ak = acc_of(i)
                                kb.op("pe", lambda e, ai=ai, pi=pi, i=i, kt=kt, n=n: e.matmul(
                                    ai, lhsT=pT[pi][:, i * 128:(i + 1) * 128], rhs=Vh[hb][:, kt, :],
                                    start=False, stop=(last[i] == n)),
                                    reads=["mpT%d" % pi, "Vh%d_%d" % (hb, p)], writes=[ak])
                    ob = obst[ab]
                    kb.op("dve", lambda e: e.reciprocal(out=rl[:, 0:3].unsqueeze(2), in_=accA[:, :, 128:129]),
                          reads=["macc%da" % ab], writes=["mrl"])
                    kb.op("dve", lambda e: e.reciprocal(out=rl[:, 3:4].unsqueeze(2), in_=accB[:, :, 128:129]),
                          reads=["macc%db" % ab], writes=["mrl"])
                    for i in range(4):
                        ai, ak = acc_of(i)
                        kb.op("dve", lambda e, ai=ai, i=i, ob=ob: e.tensor_scalar(
                            out=ob[:, i, :], in0=ai[:, 0:128], scalar1=rl[:, i:i + 1], scalar2=None, op0=ALU.mult),
                            reads=[ak, "mrl"], writes=["obst%d" % ab])
                    self.dma("sp", self.OB[g * 512:(g + 1) * 512, h * 128:(h + 1) * 128].rearrange("(i p) f -> p i f", p=128),
                             ob[:], ["obst%d" % ab], [], obs[ab])
            kb.barrier()
            kb.release_dsems([ts, kbs] + kas[0] + kas[1] + vs[0] + vs[1] + qsa + qsb + obs)


class Phases7(Phases6):
    def phase4(self, st):
        c, kb, P = self.cfg, self.kb, self.P
        NQ = c.CH // 128
        NE = c.NE
        GS = NE // c.NG
        P["Wall"] = self.sb(st, "Wall", [128, NQ, NE], F32)
        P["slotI"] = self.sb(st, "slotI", [128, NQ, 8], I32)
        self.bnd_reg = self.nc.gpsimd.alloc_register("moe_bnd")
        self.nc.gpsimd.reg_mov(self.bnd_reg, NE * c.CAP - 1)
        P["w8"] = self.sb(st, "w8", [128, NQ, 8], F32)
        CAP = c.CAP
        BIGK = 131072.0
        with contextlib.ExitStack() as ph:
            self.alloc_psum(ph, 5, 2)
            Ls = self.sb(ph, "Ls", [128, 128], BF16)
            iot = self.sb(ph, "iot", [128, NE], F32)
            ebase = self.sb(ph, "ebase", [128, NE], F32)
            selb = self.sb(ph, "selb", [128, NQ, NE], BF16)
            lss = kb.dsem("Ls")
            ios = kb.dsem("iot")
            self.dma("pool", Ls[:], self.lstrict_d, [], ["Ls"], lss)
            self.dma("sp", iot[:], self.iota_d, [], ["iot"], ios)
            kb.op("dve", lambda e: e.tensor_scalar(out=ebase[:], in0=iot[:], scalar1=float(CAP), scalar2=None, op0=ALU.mult),
                  reads=["iot"], writes=["ebase"])
            d_selv = self.sb(ph, "d_selv", [128, NE], F32)
            d_slot = self.sb(ph, "d_slot", [128, NE], F32)
            d_k8 = self.sb(ph, "d_k8", [128, 8], F32)
            d_s8 = self.sb(ph, "d_s8", [128, 8], F32)
            d_e8i = self.sb(ph, "d_e8i", [128, 8], I32)
            d_e8f = self.sb(ph, "d_e8f", [128, 8], F32)
            d_junk = self.sb(ph, "d_junk", [128, NE], F32)
            scs = [kb.dsem("scat%d" % i) for i in range(16)]
            sci = 0
            gate_a = self.bcast_tile(ph, ph, "gate_a_bc", P["modT"][:, 32:48], "modT")
            wo = self.sb(ph, "wo", [128, 16, 2048], BF16)
            wos = [kb.dsem("wo%d" % i) for i in range(4)]
            for i in range(4):
                self.dma("pool", wo[:, :, i * 512:(i + 1) * 512],
                         self.w_o[:, i * 512:(i + 1) * 512].rearrange("(kc p) n -> p kc n", p=128), [], ["wo%d" % i], wos[i])
            wr = self.sb(ph, "wr", [128, 16, NE], BF16)
            wrs = kb.dsem("wr")
            self.dma("pool", wr[:], self.w_router.rearrange("(kc p) n -> p kc n", p=128), [], ["wr"], wrs)
            rb = self.sb(ph, "rb_bc", [128, NE], F32)
            rbs = kb.dsem("rb")
            self.dma("sp", rb[:], self.rbias.partition_broadcast(128), [], ["rb"], rbs)
            obt = [self.sb(ph, "obt%d" % i, [128, 1024], F32) for i in range(2)]
            obts = [kb.dsem("obt%d" % i) for i in range(2)]
            mixa = [self.sb(ph, "mixa%d" % i, [128, 8, 128], BF16) for i in range(2)]
            mas = [kb.dsem("mixa%d" % i) for i in range(2)]
            mixb = [self.sb(ph, "mixb%d" % i, [128, 8, 128], BF16) for i in range(2)]
            xt = [self.sb(ph, "xt4_%d" % i, [128, 2048], F32) for i in range(2)]
            xts = [kb.dsem("xt4_%d" % i) for i in range(2)]
            xm = [self.sb(ph, "xm%d" % i, [128, 2048], F32) for i in range(2)]
            xms = [kb.dsem("xm%d" % i) for i in range(2)]
            tmpm = [self.sb(ph, "tmpm%d" % i, [128, 512], F32) for i in range(2)]
            h2 = [self.sb(ph, "h2st%d" % i, [128, 16, 128], BF16) for i in range(2)]
            h2s = [kb.dsem("h2st%d" % i) for i in range(2)]
            WN1 = self.alloc_normT(ph, "n4a", 1024, 1)
            WN2 = self.alloc_normT(ph, "n4b", 2048, 2)
            sc = self.sb(ph, "r_sc", [128, NE], F32)
            ch = self.sb(ph, "r_ch", [128, NE], F32)
            cm = self.sb(ph, "r_cm", [128, NE], F32)
            m8 = self.sb(ph, "r_m8", [128, c.NG, 8], F32)
            grp = self.sb(ph, "r_grp", [128, 8], F32)
            g8 = self.sb(ph, "r_g8", [128, 8], F32)
            gm = self.sb(ph, "r_gm", [128, 8], F32)
            e8 = self.sb(ph, "r_e8", [128, 8], F32)
            sel = self.sb(ph, "r_sel", [128, NE], F32)
            ws_ = self.sb(ph, "r_ws", [128, 2], F32)
            xv = self.xk.rearrange("(n p) d -> n p d", p=128)
            for b in range(NQ):
                k = b % 2
                self.dma("sp", obt[k][:], self.OB[b * 128:(b + 1) * 128, :], [], ["obt%d" % k], obts[k])
                self.dma("sp", mixa[k][:], self.MIXT[:, 0:8, b * 128:(b + 1) * 128], [], ["mixa%d" % k], mas[k])
                self.dma("sp", xt[k][:], xv[b], [], ["xt4_%d" % k], xts[k])
                self.norm_T(WN1, obt[k][:], ["obt%d" % k], 1024, P["gsm"][:, 14:22], None, ["gsm"],
                            lambda ci, k=k: mixb[k][:, ci, :], ["mixb%d" % k])
                for nb in range(4):
                    pb, pk = self.pbank()
                    for ci in range(16):
                        src = mixa[k] if ci < 8 else mixb[k]
                        sk = ("mixa%d" % k) if ci < 8 else ("mixb%d" % k)
                        kb.op("pe", lambda e, ci=ci, nb=nb, pb=pb, src=src: e.matmul(
                            pb[:], lhsT=src[:, ci % 8, :], rhs=wo[:, ci, nb * 512:(nb + 1) * 512],
                            start=(ci == 0), stop=(ci == 15)), reads=[sk, "wo%d" % nb], writes=[pk])
                    tk = nb % 2
                    kb.op("dve", lambda e, nb=nb, pb=pb, tk=tk: e.tensor_tensor(
                        out=tmpm[tk][:], in0=pb[:], in1=gate_a[:, nb * 512:(nb + 1) * 512], op=ALU.mult),
                        reads=[pk, "gate_a_bc"], writes=["tmpm%d" % tk])
                    kb.op("pool", lambda e, nb=nb, tk=tk: e.tensor_tensor(
                        out=xm[k][:, nb * 512:(nb + 1) * 512], in0=tmpm[tk][:], in1=xt[k][:, nb * 512:(nb + 1) * 512], op=ALU.add),
                        reads=["tmpm%d" % tk, "xt4_%d" % k], writes=["xm%d" % k])
                self.dma("sp", self.XMID[b * 128:(b + 1) * 128, :], xm[k][:], ["xm%d" % k], [], xms[k])
                self.norm_T(WN2, xm[k][:], ["xm%d" % k], 2048, P["gmodF"], P["modT"][:, 48:64], ["gmodF", "modT"],
                            lambda ci, k=k: h2[k][:, ci, :], ["h2st%d" % k])
                self.dma("sp", self.H2T[:, :, b * 128:(b + 1) * 128], h2[k][:], ["h2st%d" % k], [], h2s[k])
                pb, pk = self.pbank()
                for ci in range(16):
                    kb.op("pe", lambda e, ci=ci, pb=pb: e.matmul(pb[:, 0:NE], lhsT=h2[k][:, ci, :], rhs=wr[:, ci, :],
                                                                 start=(ci == 0), stop=(ci == 15)),
                          reads=["h2st%d" % k, "wr"], writes=[pk])
                D = lambda fn, r, w: kb.op("dve", fn, reads=r, writes=w)
                kb.op("act", lambda e, pb=pb: e.activation(out=sc[:], in_=pb[:, 0:NE], func=AF.Sigmoid), reads=[pk], writes=["r_sc"])
                D(lambda e: e.tensor_tensor(out=ch[:], in0=sc[:], in1=rb[:], op=ALU.add), ["r_sc", "rb"], ["r_ch"])
                for g in range(c.NG):
                    D(lambda e, g=g: e.max(out=m8[:, g, :], in_=ch[:, g * GS:(g + 1) * GS]), ["r_ch"], ["r_m8"])
                D(lambda e: e.tensor_tensor(out=grp[:].unsqueeze(2), in0=m8[:, :, 0:1], in1=m8[:, :, 1:2], op=ALU.add), ["r_m8"], ["r_grp"])
                D(lambda e: e.max(out=g8[:], in_=grp[:]), ["r_grp"], ["r_g8"])
                D(lambda e: e.tensor_scalar(out=gm[:], in0=grp[:], scalar1=g8[:, c.TOPG - 1:c.TOPG], scalar2=None, op0=ALU.is_ge),
                  ["r_grp", "r_g8"], ["r_gm"])
                D(lambda e: e.tensor_scalar(out=gm[:], in0=gm[:], scalar1=-1.0, scalar2=1e30, op0=ALU.add, op1=ALU.mult), [], ["r_gm"])
                D(lambda e: e.tensor_tensor(out=cm[:].rearrange("p (g s) -> p g s", s=GS), in0=ch[:].rearrange("p (g s) -> p g s", s=GS),
                                            in1=gm[:].unsqueeze(2).broadcast_to([128, c.NG, GS]), op=ALU.add), ["r_ch", "r_gm"], ["r_cm"])
                D(lambda e: e.max(out=e8[:], in_=cm[:]), ["r_cm"], ["r_e8"])
                D(lambda e: e.tensor_scalar(out=sel[:], in0=cm[:], scalar1=e8[:, c.TOPK - 1:c.TOPK], scalar2=None, op0=ALU.is_ge),
                  ["r_cm", "r_e8"], ["r_sel"])
                D(lambda e, b=b: e.tensor_copy(out=selb[:, b, :], in_=sel[:]), ["r_sel"], ["selb%d" % b])
                pp, ppk = self.pbank()
                for b2 in range(b):
                    kb.op("pe", lambda e, b2=b2, pp=pp: e.matmul(pp[:, 0:NE], lhsT=P["ones_b"][:], rhs=selb[:, b2, :],
                                                                 start=(b2 == 0), stop=False),
                          reads=["ones_b", "selb%d" % b2], writes=[ppk])
                kb.op("pe", lambda e, b=b, pp=pp: e.matmul(pp[:, 0:NE], lhsT=Ls[:], rhs=selb[:, b, :], start=(b == 0), stop=True),
                      reads=["Ls", "selb%d" % b], writes=[ppk])
                D(lambda e, pp=pp: e.tensor_scalar(out=d_selv[:], in0=pp[:, 0:NE], scalar1=float(CAP), scalar2=None, op0=ALU.is_lt),
                  [ppk], ["d_selv"])
                D(lambda e: e.tensor_tensor(out=d_selv[:], in0=d_selv[:], in1=sel[:], op=ALU.mult), ["r_sel"], ["d_selv"])
                D(lambda e, pp=pp: e.tensor_tensor(out=d_slot[:], in0=pp[:, 0:NE], in1=ebase[:], op=ALU.add), [ppk, "ebase"], ["d_slot"])
                D(lambda e: e.tensor_scalar(out=d_slot[:], in0=d_slot[:], scalar1=-1.0, scalar2=BIGK, op0=ALU.mult, op1=ALU.add),
                  [], ["d_slot"])
                D(lambda e: e.tensor_tensor(out=d_slot[:], in0=d_slot[:], in1=d_selv[:], op=ALU.mult), ["d_selv"], ["d_slot"])
                D(lambda e: e.max(out=d_k8[:], in_=d_slot[:]), ["d_slot"], ["d_k8"])
                D(lambda e: e.tensor_scalar(out=d_s8[:], in0=d_k8[:], scalar1=-1.0, scalar2=BIGK, op0=ALU.mult, op1=ALU.add),
                  ["d_k8"], ["d_s8"])
                D(lambda e, b=b: e.tensor_copy(out=P["slotI"][:, b, :], in_=d_s8[:]), ["d_s8"], ["slotI%d" % b])
                D(lambda e, b=b: e.tensor_scalar(out=d_e8i[:], in0=P["slotI"][:, b, :], scalar1=int(np.log2(CAP)), scalar2=None,
                                                 op0=ALU.arith_shift_right), ["slotI%d" % b], ["d_e8i"])
                D(lambda e: e.tensor_copy(out=d_e8f[:], in_=d_e8i[:]), ["d_e8i"], ["d_e8f"])
                xn_cur = WN2["xn"][(WN2["i"] - 1) % WN2["n"]]
                xn_key = "n4b_xn%d" % ((WN2["i"] - 1) % WN2["n"])
                for k8 in range(8):
                    sm = scs[sci % 16]
                    sci += 1
                    kb.op("pool", lambda e, b=b, k8=k8, xn_cur=xn_cur: e.indirect_dma_start(
                        out=self.XG[:, :], out_offset=bass.IndirectOffsetOnAxis(ap=P["slotI"][:, b, k8:k8 + 1], axis=0),
                        in_=xn_cur[:, :], in_offset=None, bounds_check=self.bnd_reg, oob_is_err=False),
                        reads=[xn_key, "slotI%d" % b], writes=[], dsem=sm)
                D(lambda e: e.tensor_tensor(out=sel[:], in0=sel[:], in1=sc[:], op=ALU.mult), ["r_sc"], ["r_sel"])
                D(lambda e: e.tensor_reduce(out=ws_[:, 0:1], in_=sel[:], axis=AX.X, op=ALU.add), ["r_sel"], ["r_ws"])
                D(lambda e: e.reciprocal(out=ws_[:, 1:2], in_=ws_[:, 0:1]), [], ["r_ws"])
                D(lambda e, b=b: e.tensor_scalar(out=P["Wall"][:, b, :], in0=sel[:], scalar1=ws_[:, 1:2], scalar2=float(c.ROUTED_SCALE),
                                                 op0=ALU.mult, op1=ALU.mult), ["r_sel", "r_ws"], ["Wall"])
                for k8 in range(8):
                    D(lambda e, b=b, k8=k8: e.scalar_tensor_tensor(
                        out=d_junk[:], in0=iot[:], scalar=d_e8f[:, k8:k8 + 1], in1=P["Wall"][:, b, :],
                        op0=ALU.is_equal, op1=ALU.mult, accum_out=P["w8"][:, b, k8:k8 + 1]),
                      ["iot", "d_e8f", "Wall"], ["d_junk", "w8_%d" % b])
            if c.debug:
                self.WALL = self.dscr("WALL", [128, NQ * NE], F32)
                self.dma("sp", self.WALL, P["Wall"][:].rearrange("p a b -> p (a b)"), ["Wall"], [], rbs)
            kb.barrier()
            kb.release_dsems(wos + [wrs, rbs, lss, ios] + obts + mas + xts + xms + h2s + scs)

    def phase6(self, shared_only=False):
        c, kb, P = self.cfg, self.kb, self.P
        NE = c.NE
        TH = c.CH // 2
        NTT = TH // 128
        with contextlib.ExitStack() as ph:
            self.alloc_psum(ph, 8, 0)
            gate_f = self.bcast_tile(ph, ph, "gate_f_bc", P["modT"][:, 80:96], "modT")
            fg = self.bcast_tile(ph, ph, "fg_bc", P["gvec"][:, 32:48], "gvec")
            h2T = self.sb(ph, "h2T_h", [128, 16, TH], BF16)
            h2s = kb.dsem("h2T_h")
            acc = self.sb(ph, "moe_acc", [128, NTT, 2048], F32)
            NW = 2
            wg = [self.sb(ph, "wg%d" % i, [128, 16, 128], BF16) for i in range(NW)]
            wu = [self.sb(ph, "wu%d" % i, [128, 16, 128], BF16) for i in range(NW)]
            wgs = [kb.dsem("wg%d" % i) for i in range(NW)]
            wus = [kb.dsem("wu%d" % i) for i in range(NW)]
            wd = [self.sb(ph, "wd%d" % i, [128, 4, 2048], BF16) for i in range(2)]
            wds = [kb.dsem("wd%d" % i) for i in range(2)]
            hT = [self.sb(ph, "ehT%d" % i, [128, 4, TH], BF16) for i in range(2)]
            sg = [self.sb(ph, "sg%d" % i, [128, 512], F32) for i in range(2)]
            xmt = self.sb(ph, "xmt", [128, 2048], F32)
            xmts = kb.dsem("xmt")
            st3 = self.sb(ph, "st3", [128, 4], F32)
            junk = self.sb(ph, "junk6", [128, 2048], BF16)
            wi = 0
            sgi = 0
            yshs = [kb.dsem("ysh%d" % i) for i in range(NTT)] if shared_only else []
            for half in range(2):
                self.dma("sp", h2T[:], self.H2T[:, :, half * TH:(half + 1) * TH], [], ["h2T_h"], h2s)
                for tt in range(NTT):
                    kb.op("pool", lambda e, tt=tt: e.memset(acc[:, tt, :], 0.0), writes=["acc%d" % tt])
                for ex in ([NE] if shared_only else range(NE + 1)):
                    eb = ex % 2
                    if ex < NE:
                        g_src, u_src, d_src = self.w_eg[ex], self.w_eu[ex], self.w_ed[ex]
                    else:
                        g_src, u_src, d_src = self.w_sg, self.w_su, self.w_sd
                    self.dma("pool", wd[eb][:], d_src.rearrange("(kc p) n -> p kc n", p=128), [], ["wd%d" % eb], wds[eb])
                    for hb in range(4):
                        w = wi % NW
                        wi += 1
                        self.dma("pool", wg[w][:], g_src[:, hb * 128:(hb + 1) * 128].rearrange("(kc p) n -> p kc n", p=128),
                                 [], ["wg%d" % w], wgs[w])
                        self.dma("pool", wu[w][:], u_src[:, hb * 128:(hb + 1) * 128].rearrange("(kc p) n -> p kc n", p=128),
                                 [], ["wu%d" % w], wus[w])
                        for tg in range(TH // 512):
                            pg, pgk = self.pbank()
                            pu, puk = self.pbank()
                            for (pp, ppk, wsrc, wk) in ((pg, pgk, wg[w], "wg%d" % w), (pu, puk, wu[w], "wu%d" % w)):
                                for kc in range(16):
                                    kb.op("pe", lambda e, pp=pp, wsrc=wsrc, kc=kc, tg=tg: e.matmul(
                                        pp[:], lhsT=wsrc[:, kc, :], rhs=h2T[:, kc, tg * 512:(tg + 1) * 512],
                                        start=(kc == 0), stop=(kc == 15)), reads=[wk, "h2T_h"], writes=[ppk])
                            si = sgi % 2
                            sgi += 1
                            kb.op("act", lambda e, pg=pg, si=si: e.activation(out=sg[si][:], in_=pg[:], func=AF.Silu),
                                  reads=[pgk], writes=["sg%d" % si])
                            kb.op("dve", lambda e, pu=pu, si=si, hb=hb, tg=tg: e.tensor_tensor(
                                out=hT[eb][:, hb, tg * 512:(tg + 1) * 512], in0=pu[:], in1=sg[si][:], op=ALU.mult),
                                reads=[puk, "sg%d" % si], writes=["ehT%d" % eb])
                    for tt in range(NTT):
                        tile = half * NTT + tt
                        for nb in range(4):
                            py, pyk = self.pbank()
                            for hb in range(4):
                                kb.op("pe", lambda e, py=py, hb=hb, tt=tt, nb=nb: e.matmul(
                                    py[:], lhsT=hT[eb][:, hb, tt * 128:(tt + 1) * 128], rhs=wd[eb][:, hb, nb * 512:(nb + 1) * 512],
                                    start=(hb == 0), stop=(hb == 3)), reads=["ehT%d" % eb, "wd%d" % eb], writes=[pyk])
                            scal = P["Wall"][:, tile, ex:ex + 1] if ex < NE else 1.0
                            kb.op("dve", lambda e, py=py, tt=tt, nb=nb, scal=scal: e.scalar_tensor_tensor(
                                out=acc[:, tt, nb * 512:(nb + 1) * 512], in0=py[:], scalar=scal,
                                in1=acc[:, tt, nb * 512:(nb + 1) * 512], op0=ALU.mult, op1=ALU.add),
                                reads=[pyk, "Wall"], writes=["acc%d" % tt])
                if shared_only:
                    for tt in range(NTT):
                        tile = half * NTT + tt
                        self.dma("sp", self.YSH[tile * 128:(tile + 1) * 128, :], acc[:, tt, :], ["acc%d" % tt], [], yshs[tt])
                    continue
                for tt in range(NTT):
                    tile = half * NTT + tt
                    self.dma("sp", xmt[:], self.XMID[tile * 128:(tile + 1) * 128, :], [], ["xmt"], xmts)
                    kb.op("dve", lambda e, tt=tt: e.tensor_tensor(out=acc[:, tt, :], in0=acc[:, tt, :], in1=gate_f[:], op=ALU.mult),
                          reads=["gate_f_bc"], writes=["acc%d" % tt])
                    kb.op("pool", lambda e, tt=tt: e.tensor_tensor(out=xmt[:], in0=xmt[:], in1=acc[:, tt, :], op=ALU.add),
                          reads=["acc%d" % tt], writes=["xmt"])
                    kb.op("act", lambda e: e.activation(out=junk[:], in_=xmt[:], func=AF.Square, accum_out=st3[:, 0:1]),
                          reads=["xmt"], writes=["junk6", "st3"])
                    kb.op("dve", lambda e: e.tensor_scalar(out=st3[:, 1:2], in0=st3[:, 0:1], scalar1=1.0 / c.D, scalar2=c.EPS,
                                                           op0=ALU.mult, op1=ALU.add), reads=[], writes=["st3"])
                    kb.op("act", lambda e: e.activation(out=st3[:, 2:3], in_=st3[:, 1:2], func=AF.Sqrt), reads=[], writes=["st3"])
                    kb.op("dve", lambda e: e.reciprocal(out=st3[:, 3:4], in_=st3[:, 2:3]), reads=[], writes=["st3"])
                    kb.op("dve", lambda e: e.scalar_tensor_tensor(out=xmt[:], in0=xmt[:], scalar=st3[:, 3:4], in1=fg[:],
                                                                  op0=ALU.mult, op1=ALU.mult), reads=["st3", "fg_bc"], writes=["xmt"])
                    self.dma("sp", self.out[tile * 128:(tile + 1) * 128, :], xmt[:], ["xmt"], [], xmts)
            kb.barrier()
            kb.release_dsems([h2s, xmts] + wgs + wus + wds + yshs)


class Phases8(Phases7):
    def phase6_routed(self):
        c, kb, P = self.cfg, self.kb, self.P
        NE, CAP = c.NE, c.CAP
        NB = CAP // 128
        with contextlib.ExitStack() as ph:
            self.alloc_psum(ph, 6, 2)
            xg = [self.sb(ph, "xg%d" % i, [128, 2048], BF16) for i in range(3)]
            xgs = [kb.dsem("xg%d" % i) for i in range(3)]
            xT = self.sb(ph, "xTe", [128, 16, CAP], BF16)
            NW = 2
            wg = [self.sb(ph, "rwg%d" % i, [128, 16, 128], BF16) for i in range(NW)]
            wu = [self.sb(ph, "rwu%d" % i, [128, 16, 128], BF16) for i in range(NW)]
            wgs = [kb.dsem("rwg%d" % i) for i in range(NW)]
            wus = [kb.dsem("rwu%d" % i) for i in range(NW)]
            wd = [self.sb(ph, "rwd%d" % i, [128, 4, 2048], BF16) for i in range(2)]
            wds = [kb.dsem("rwd%d" % i) for i in range(2)]
            hT = [self.sb(ph, "rhT%d" % i, [128, 4, CAP], BF16) for i in range(2)]
            sg = [self.sb(ph, "rsg%d" % i, [128, 512], F32) for i in range(2)]
            yst = [self.sb(ph, "yst%d" % i, [128, 2048], BF16) for i in range(2)]
            ysts = [kb.dsem("yst%d" % i) for i in range(2)]
            xTs = [xT, self.sb(ph, "xTe_b", [128, 16, CAP], BF16)]
            cnt = {"xgi": 0, "wi": 0, "sgi": 0, "yi": 0}

            def T_stage(ex):
                xTe = xTs[ex % 2]
                for blk in range(NB):
                    xi = cnt["xgi"] % 3
                    cnt["xgi"] += 1
                    r0 = ex * CAP + blk * 128
                    self.dma("sp", xg[xi][:], self.XG[r0:r0 + 128, :], [], ["xg%d" % xi], xgs[xi])
                    for c0 in range(0, 16, 8):
                        tb, tk = self.tbank()
                        for j in range(8):
                            ci = c0 + j
                            kb.op("pe", lambda e, j=j, ci=ci, tb=tb, xi=xi: e.transpose(
                                out=tb[:, j, :], in_=xg[xi][:, ci * 128:(ci + 1) * 128], identity=P["ident_b"][:]),
                                reads=["xg%d" % xi, "ident_b"], writes=[tk])
                        for j in range(8):
                            ci = c0 + j
                            kb.op("dve", lambda e, j=j, ci=ci, tb=tb, blk=blk: e.tensor_scalar(
                                out=xTe[:, ci, blk * 128:(blk + 1) * 128], in0=tb[:, j, :], scalar1=P["gmodF"][:, ci:ci + 1],
                                scalar2=P["modT"][:, 48 + ci:49 + ci], op0=ALU.mult, op1=ALU.add),
                                reads=[tk, "gmodF", "modT"], writes=["xTe%d_%d" % (ex % 2, blk // 4)])

            def G_stage(ex):
                eb = ex % 2
                xTe = xTs[ex % 2]
                self.dma("pool", wd[eb][:], self.w_ed[ex].rearrange("(kc p) n -> p kc n", p=128), [], ["rwd%d" % eb], wds[eb])
                for hb in range(4):
                    w = cnt["wi"] % NW
                    cnt["wi"] += 1
                    self.dma("pool", wg[w][:], self.w_eg[ex][:, hb * 128:(hb + 1) * 128].rearrange("(kc p) n -> p kc n", p=128),
                             [], ["rwg%d" % w], wgs[w])
                    self.dma("pool", wu[w][:], self.w_eu[ex][:, hb * 128:(hb + 1) * 128].rearrange("(kc p) n -> p kc n", p=128),
                             [], ["rwu%d" % w], wus[w])
                    for tg in range(CAP // 512):
                        pg, pgk = self.pbank()
                        pu, puk = self.pbank()
                        for (pp, ppk, wsrc, wk) in ((pg, pgk, wg[w], "rwg%d" % w), (pu, puk, wu[w], "rwu%d" % w)):
                            for kc in range(16):
                                kb.op("pe", lambda e, pp=pp, wsrc=wsrc, kc=kc, tg=tg: e.matmul(
                                    pp[:], lhsT=wsrc[:, kc, :], rhs=xTe[:, kc, tg * 512:(tg + 1) * 512],
                                    start=(kc == 0), stop=(kc == 15)), reads=[wk, "xTe%d_%d" % (ex % 2, tg)], writes=[ppk])
                        si = cnt["sgi"] % 2
                        cnt["sgi"] += 1
                        kb.op("act", lambda e, pg=pg, si=si: e.activation(out=sg[si][:], in_=pg[:], func=AF.Silu),
                              reads=[pgk], writes=["rsg%d" % si])
                        kb.op("dve", lambda e, pu=pu, si=si, hb=hb, tg=tg: e.tensor_tensor(
                            out=hT[eb][:, hb, tg * 512:(tg + 1) * 512], in0=pu[:], in1=sg[si][:], op=ALU.mult),
                            reads=[puk, "rsg%d" % si], writes=["rhT%d" % eb])
                for blk in range(NB):
                    y = cnt["yi"] % 2
                    cnt["yi"] += 1
                    for nb in range(4):
                        py, pyk = self.pbank()
                        for hb in range(4):
                            kb.op("pe", lambda e, py=py, hb=hb, blk=blk, nb=nb: e.matmul(
                                py[:], lhsT=hT[eb][:, hb, blk * 128:(blk + 1) * 128], rhs=wd[eb][:, hb, nb * 512:(nb + 1) * 512],
                                start=(hb == 0), stop=(hb == 3)), reads=["rhT%d" % eb, "rwd%d" % eb], writes=[pyk])
                        kb.op("act", lambda e, py=py, y=y, nb=nb: e.activation(out=yst[y][:, nb * 512:(nb + 1) * 512], in_=py[:], func=AF.Copy),
                              reads=[pyk], writes=["yst%d" % y])
                    r0 = ex * CAP + blk * 128
                    self.dma("sp", self.YS[r0:r0 + 128, :], yst[y][:], ["yst%d" % y], [], ysts[y])

            T_stage(0)
            for ex in range(NE):
                if ex + 1 < NE:
                    T_stage(ex + 1)
                G_stage(ex)
            kb.barrier()
            kb.release_dsems(xgs + wgs + wus + wds + ysts)

    def phase7(self):
        c, kb, P = self.cfg, self.kb, self.P
        NQ = c.CH // 128
        NE, CAP = c.NE, c.CAP
        with contextlib.ExitStack() as ph:
            self.alloc_psum(ph, 2, 0)
            gate_f = self.bcast_tile(ph, ph, "gate_f_bc2", P["modT"][:, 80:96], "modT")
            fg = self.bcast_tile(ph, ph, "fg_bc2", P["gvec"][:, 32:48], "gvec")
            acc = [self.sb(ph, "cacc%d" % i, [128, 2048], F32) for i in range(2)]
            accs = [kb.dsem("cacc%d" % i) for i in range(2)]
            xmt = [self.sb(ph, "cxm%d" % i, [128, 2048], F32) for i in range(2)]
            xmts = [kb.dsem("cxm%d" % i) for i in range(2)]
            NGB = 4
            gb = [self.sb(ph, "gb%d" % i, [128, 2048], BF16) for i in range(NGB)]
            gbs = [kb.dsem("gb%d" % i) for i in range(NGB)]
            st3 = self.sb(ph, "cst3", [128, 4], F32)
            junk = self.sb(ph, "cjunk", [128, 2048], BF16)
            for i in range(NGB):
                kb.op("pool", lambda e, i=i: e.memset(gb[i][:], 0.0), writes=["gb%d" % i])
            gi = 0
            for b in range(NQ):
                k = b % 2
                self.dma("sp", acc[k][:], self.YSH[b * 128:(b + 1) * 128, :], [], ["cacc%d" % k], accs[k])
                self.dma("sp", xmt[k][:], self.XMID[b * 128:(b + 1) * 128, :], [], ["cxm%d" % k], xmts[k])
                for k8 in range(8):
                    g = gi % NGB
                    gi += 1
                    kb.op("pool", lambda e, g=g, b=b, k8=k8: e.indirect_dma_start(
                        out=gb[g][:, :], out_offset=None, in_=self.YS[:, :],
                        in_offset=bass.IndirectOffsetOnAxis(ap=P["slotI"][:, b, k8:k8 + 1], axis=0),
                        bounds_check=self.bnd_reg, oob_is_err=False),
                        reads=["slotI%d" % b], writes=["gb%d" % g], dsem=gbs[g])
                    kb.op("dve", lambda e, g=g, b=b, k8=k8, k=k: e.scalar_tensor_tensor(
                        out=acc[k][:], in0=gb[g][:], scalar=P["w8"][:, b, k8:k8 + 1], in1=acc[k][:], op0=ALU.mult, op1=ALU.add),
                        reads=["gb%d" % g, "w8_%d" % b], writes=["cacc%d" % k])
                kb.op("dve", lambda e, k=k: e.tensor_tensor(out=acc[k][:], in0=acc[k][:], in1=gate_f[:], op=ALU.mult),
                      reads=["gate_f_bc2"], writes=["cacc%d" % k])
                kb.op("pool", lambda e, k=k: e.tensor_tensor(out=xmt[k][:], in0=xmt[k][:], in1=acc[k][:], op=ALU.add),
                      reads=["cacc%d" % k], writes=["cxm%d" % k])
                kb.op("act", lambda e, k=k: e.activation(out=junk[:], in_=xmt[k][:], func=AF.Square, accum_out=st3[:, 0:1]),
                      reads=["cxm%d" % k], writes=["cjunk", "cst3"])
                kb.op("dve", lambda e: e.tensor_scalar(out=st3[:, 1:2], in0=st3[:, 0:1], scalar1=1.0 / c.D, scalar2=c.EPS,
                                                       op0=ALU.mult, op1=ALU.add), reads=[], writes=["cst3"])
                kb.op("act", lambda e: e.activation(out=st3[:, 2:3], in_=st3[:, 1:2], func=AF.Sqrt), reads=[], writes=["cst3"])
                kb.op("dve", lambda e: e.reciprocal(out=st3[:, 3:4], in_=st3[:, 2:3]), reads=[], writes=["cst3"])
                kb.op("dve", lambda e, k=k: e.scalar_tensor_tensor(out=xmt[k][:], in0=xmt[k][:], scalar=st3[:, 3:4], in1=fg[:],
                                                                   op0=ALU.mult, op1=ALU.mult), reads=["cst3", "fg_bc2"], writes=["cxm%d" % k])
                self.dma("sp", self.out[b * 128:(b + 1) * 128, :], xmt[k][:], ["cxm%d" % k], [], xmts[k])
            kb.barrier()
            kb.release_dsems(accs + xmts + gbs)


def build(cfg):
    b = Phases8(cfg)
    b.declare_io()
    kb = b.kb
    with contextlib.ExitStack() as st:
        b.phase0(st)
        if cfg.stop_after >= 1:
            b.phase1a()
        if cfg.stop_after >= 2:
            b.phase1b()
            b.phase1c()
        if cfg.stop_after >= 3:
            b.phase2()
        if cfg.stop_after >= 4:
            b.phase3()
        if cfg.stop_after >= 5:
            b.phase4(st)
        if cfg.stop_after >= 6:
            if cfg.CAP:
                b.phase6(shared_only=True)
                b.phase6_routed()
                b.phase7()
            else:
                b.phase6()
        kb.barrier()
    return b


_BUILD_CACHE = {}


def kernel(**inputs):
    cfg = Cfg
    inp = {k: np.asarray(v) for k, v in inputs.items()}
    S = inp["x"].shape[1]
    nchunks = S // cfg.CH
    assert nchunks == cfg.NCORES and nchunks == cfg.NSLOT
    if "b" not in _BUILD_CACHE:
        _BUILD_CACHE["b"] = build(cfg)
    b = _BUILD_CACHE["b"]
    sh = prepare_shared(inp, cfg)
    maps = []
    for core in range(cfg.NCORES):
        m = dict(sh)
        m.update(prepare_core(inp, cfg, core, nchunks))
        maps.append(m)
    res = run_bass_kernel_spmd(b.nc, maps, core_ids=list(range(cfg.NCORES)))
    outs = [np.asarray(r["out"]) for r in res.results]
    return np.concatenate(outs, axis=0)[None].astype(np.float32)
```

```python
import contextlib
import numpy as np
import concourse.bass as bass
import concourse.mybir as mybir
from concourse.bass_utils import run_bass_kernel_spmd

F32 = mybir.dt.float32
BF16 = mybir.dt.bfloat16
I32 = mybir.dt.int32
AF = mybir.ActivationFunctionType
ALU = mybir.AluOpType
AX = mybir.AxisListType

SAME_ENGINE_SYNC = True


class Cfg:
    D = 2048
    CH = 2048
    NSLOT = 8
    NCORES = 8
    NE = 64
    NG = 8
    TOPG = 4
    TOPK = 8
    DE = 512
    NH = 8
    HD = 128
    QL = 512
    KVL = 256
    ROPE = 64
    NADA = 6
    EPS = 1e-6
    ROUTED_SCALE = 2.5
    WINDOWS = ((128, 1), (512, 4), (2048, 16))
    CAP = 1024
    debug = False
    stop_after = 99


class Sem:
    def __init__(self, h, name):
        self.h = h
        self.name = name
        self.count = 0


class KB:
    def __init__(self, nc):
        self.nc = nc
        self.E = {"pe": nc.tensor, "act": nc.scalar, "dve": nc.vector, "pool": nc.gpsimd, "sp": nc.sync}
        self.esem = {}
        self.allsems = []
        for e in ["pe", "act", "dve", "pool"]:
            self.esem[e] = self.newsem("es_" + e)
        self.waited = {e: {} for e in self.E}
        self.lastw = {}
        self.readers = {}
        self.free_dsems = []
        self.nwaits = 0
        self.nops = 0

    def newsem(self, name):
        s = Sem(self.nc.alloc_semaphore(name=name), name)
        self.allsems.append(s)
        return s

    def dsem(self, name="d"):
        if self.free_dsems:
            return self.free_dsems.pop()
        return self.newsem("ds%d_%s" % (len(self.allsems), name))

    def release_dsems(self, sems):
        self.free_dsems.extend(sems)

    def _wait(self, eng, sem, val):
        if self.waited[eng].get(sem, 0) >= val:
            return
        self.E[eng].wait_ge(sem.h, val)
        self.waited[eng][sem] = val
        self.nwaits += 1

    def op(self, eng, issue, reads=(), writes=(), dsem=None):
        need = {}
        def add(ev):
            sem, val, peng = ev
            if eng == "pe" and peng == "pe":
                return
            if (not SAME_ENGINE_SYNC) and peng == eng and peng != "dma":
                return
            if need.get(sem, 0) < val:
                need[sem] = val
        for r in reads:
            if r in self.lastw:
                add(self.lastw[r])
        for w in writes:
            if w in self.lastw:
                add(self.lastw[w])
            for ev in self.readers.get(w, ()):
                add(ev)
        for sem, val in need.items():
            self._wait(eng, sem, val)
        inst = issue(self.E[eng])
        if dsem is not None:
            if dsem.count > 0 and not any(w.get(dsem, 0) >= dsem.count for w in self.waited.values()):
                raise RuntimeError("DMA semaphore %s reused while previous DMA may be in flight" % dsem.name)
            dsem.count += 16
            inst.then_inc(dsem.h, 16)
            ev = (dsem, dsem.count, "dma")
        else:
            s = self.esem[eng]
            s.count += 1
            inst.then_inc(s.h, 1)
            ev = (s, s.count, eng)
        for w in writes:
            self.lastw[w] = ev
            self.readers[w] = []
        for r in reads:
            if r not in writes:
                self.readers.setdefault(r, []).append(ev)
        self.nops += 1
        return ev

    def barrier(self, engines=("pe", "act", "dve", "pool", "sp")):
        for e in engines:
            for s in self.allsems:
                if s.count > 0:
                    self._wait(e, s, s.count)
        self.lastw = {}
        self.readers = {}


def _slopes(nh):
    return [float(np.float32(2.0) ** np.float32(-8.0 * (h + 1) / nh)) for h in range(nh)]


class Builder:
    def __init__(self, cfg):
        self.cfg = cfg
        self.nc = bass.Bass("TRN2", target_bir_lowering=False)
        self.kb = KB(self.nc)
        self.dram_in = {}
        self.dram_out = {}
        self.scratch = {}

    def din(self, name, shape, dtype=F32):
        t = self.nc.dram_tensor(name, list(shape), dtype, kind="ExternalInput")
        self.dram_in[name] = (tuple(shape), dtype)
        return t.ap()

    def dscr(self, name, shape, dtype, internal=False):
        kind = "ExternalOutput" if (self.cfg.debug and not internal) else "Internal"
        t = self.nc.dram_tensor(name, list(shape), dtype, kind=kind)
        self.scratch[name] = (tuple(shape), dtype)
        return t.ap()

    def dout(self, name, shape, dtype=F32):
        t = self.nc.dram_tensor(name, list(shape), dtype, kind="ExternalOutput")
        self.dram_out[name] = (tuple(shape), dtype)
        return t.ap()


MAGIC = 12582912.0
TWO_PI = 2.0 * np.pi
CW1 = 6.28125
CW2 = float(np.float32(TWO_PI - CW1))
CW3 = float(TWO_PI - CW1 - CW2)


def _b(cls):
    return cls


class Phases(Builder):
    _uid = 0

    def _nm(self, name):
        Phases._uid += 1
        return "%s_u%d" % (name, Phases._uid)

    def sb(self, st, name, shape, dtype):
        return st.enter_context(self.nc.sbuf_tensor(self._nm(name), list(shape), dtype))

    def ps(self, st, name, shape, dtype=F32):
        return st.enter_context(self.nc.psum_tensor(self._nm(name), list(shape), dtype))

    def dma(self, eng, out, in_, reads, writes, sem, **kw):
        return self.kb.op(eng, lambda e: e.dma_start(out=out, in_=in_, **kw), reads=reads, writes=writes, dsem=sem)

    def declare_io(self):
        c = self.cfg
        NT = c.NSLOT * c.CH
        self.xk = self.din("xk", [NT, c.D])
        self.posk = self.din("posk", [1, NT], I32)
        self.flags = self.din("flags", [1, c.NSLOT * 8])
        self.cT = self.din("cT", [128, 16])
        self.w_ada = self.din("w_ada", [c.D, c.NADA * c.D])
        self.b_adaT = self.din("b_adaT", [128, 96])
        self.gvecT = self.din("gvecT", [128, 48])
        self.w_in = self.din("w_in", [c.D, 3968])
        self.gsmallT = self.din("gsmallT", [128, 22])
        self.w_uq = self.din("w_uq", [c.QL, 2048])
        self.w_ukv = self.din("w_ukv", [c.KVL, 2048])
        self.w_o = self.din("w_o", [c.D, c.D])
        self.w_router = self.din("w_router", [c.D, c.NE])
        self.rbias = self.din("rbias", [1, c.NE])
        nexp = c.NE if c.stop_after >= 6 else 1
        self.w_eg = self.din("w_eg", [nexp, c.D, c.DE])
        self.w_eu = self.din("w_eu", [nexp, c.D, c.DE])
        self.w_ed = self.din("w_ed", [nexp, c.DE, c.D])
        self.w_sg = self.din("w_sg", [c.D, c.DE])
        self.w_su = self.din("w_su", [c.D, c.DE])
        self.w_sd = self.din("w_sd", [c.DE, c.D])
        self.ident_d = self.din("ident", [128, 128])
        self.invfreq2 = self.din("invfreq2", [64, 1])
        self.lnmult = self.din("lnmult", [128, 17, 128])
        self.tri_d = self.din("tri", [128, 128])
        self.lstrict_d = self.din("lstrict", [128, 128])
        self.iota_d = self.din("iota64", [128, c.NE])
        self.out = self.dout("out", [c.CH, c.D])
        self.HT = self.dscr("HT", [2, 128, 16, c.CH], BF16)
        self.KT_A = self.dscr("KT_A", [c.NH, 128, NT], BF16)
        self.KT_B = self.dscr("KT_B", [65, NT], BF16)
        self.V_AUG = self.dscr("V_AUG", [NT, c.NH * 129], BF16)
        self.KAT = self.dscr("KAT", [c.NH, 128, 2 * c.CH], BF16)
        self.VA_AUG = self.dscr("VA_AUG", [2 * c.CH, c.NH * 129], BF16)
        self.QAT = self.dscr("QAT", [c.NH, 128, c.CH], BF16)
        self.NEGCA = self.dscr("NEGCA", [c.NH, c.CH], BF16)
        self.QT_A = self.dscr("QT_A", [c.NH, 128, c.CH], BF16)
        self.QT_B = self.dscr("QT_B", [c.NH, 65, c.CH], BF16)
        self.OB = self.dscr("OB", [c.CH, 1024], F32)
        self.MIXT = self.dscr("MIXT", [128, 16, c.CH], BF16)
        self.XMID = self.dscr("XMID", [c.CH, c.D], F32)
        self.H2T = self.dscr("H2T", [128, 16, c.CH], BF16)
        self.MODT = self.dscr("MODT", [128, 96], F32)
        self.XG = self.dscr("XG", [c.NE * max(c.CAP, 128), c.D], BF16, internal=True)
        self.YS = self.dscr("YS", [c.NE * max(c.CAP, 128), c.D], BF16, internal=True)
        self.YSH = self.dscr("YSH", [c.CH, c.D], F32)

    def phase0(self, st):
        c, kb, nc = self.cfg, self.kb, self.nc
        P = self.P = {}
        P["ident_f"] = self.sb(st, "ident_f", [128, 128], F32)
        P["ident_b"] = self.sb(st, "ident_b", [128, 128], BF16)
        P["ones_f"] = self.sb(st, "ones_f", [128, 128], F32)
        P["ones_b"] = self.sb(st, "ones_b", [128, 128], BF16)
        P["modT"] = self.sb(st, "modT", [128, 96], F32)
        P["gvec"] = self.sb(st, "gvec", [128, 48], F32)
        P["gsm"] = self.sb(st, "gsm", [128, 22], F32)
        P["gmodA"] = self.sb(st, "gmodA", [128, 16], F32)
        P["gmodF"] = self.sb(st, "gmodF", [128, 16], F32)
        P["flag8"] = self.sb(st, "flag8", [128, c.NSLOT * 8], F32)
        P["invf"] = self.sb(st, "invf", [64, 1], F32)
        P["sgn"] = self.sb(st, "sgn", [64, 1], F32)
        P["kmax2"] = self.sb(st, "kmax2", [1, 2], F32)
        cs = [kb.dsem("c%d" % i) for i in range(8)]
        s0 = cs[0]
        self.dma("sp", P["ident_f"][:], self.ident_d, [], ["ident_f"], cs[0])
        self.dma("pool", P["ident_b"][:], self.ident_d, [], ["ident_b"], cs[1])
        self.dma("sp", P["gvec"][:], self.gvecT, [], ["gvec"], cs[2])
        self.dma("sp", P["gsm"][:], self.gsmallT, [], ["gsm"], cs[3])
        self.dma("sp", P["flag8"][:], self.flags.partition_broadcast(128), [], ["flag8"], cs[4])
        self.dma("sp", P["invf"][:], self.invfreq2, [], ["invf"], cs[5])
        kb.op("dve", lambda e: e.memset(P["ones_f"][:], 1.0), writes=["ones_f"])
        kb.op("dve", lambda e: e.memset(P["ones_b"][:], 1.0), writes=["ones_b"])
        kb.op("dve", lambda e: e.memset(P["sgn"][0:32, :], -1.0), writes=["sgn0"])
        kb.op("dve", lambda e: e.memset(P["sgn"][32:64, :], 1.0), writes=["sgn1"])
        kb.op("dve", lambda e: e.memset(P["kmax2"][:], 0.0), writes=["kmax2"])

        with contextlib.ExitStack() as ph:
            cT = self.sb(ph, "cT_s", [128, 16], F32)
            sc = self.sb(ph, "sc_s", [128, 16], F32)
            bT = self.sb(ph, "bT_s", [128, 96], F32)
            wbuf = [self.sb(ph, "wada%d" % i, [128, 16, 512], F32) for i in range(2)]
            wsem = [kb.dsem("wada%d" % i) for i in range(2)]
            mps = self.ps(ph, "mod_ps", [128, 96], F32)
            self.dma("sp", cT[:], self.cT, [], ["cT"], cs[6])
            self.dma("sp", bT[:], self.b_adaT, [], ["bT"], cs[7])
            kb.op("act", lambda e: e.activation(out=sc[:], in_=cT[:], func=AF.Silu), reads=["cT"], writes=["sc"])
            wv = self.w_ada.rearrange("(kc p) n -> p kc n", p=128)
            npieces = (c.NADA * c.D) // 512
            for pi in range(npieces):
                b = pi % 2
                eng = "sp" if b == 0 else "pool"
                self.dma(eng, wbuf[b][:], wv[:, :, pi * 512:(pi + 1) * 512], [], ["wada%d" % b], wsem[b])
                for fb in range(4):
                    col = pi * 4 + fb
                    for kc in range(16):
                        kb.op("pe", lambda e, b=b, fb=fb, kc=kc, col=col: e.matmul(
                            mps[:, col:col + 1], lhsT=wbuf[b][:, kc, fb * 128:(fb + 1) * 128], rhs=sc[:, kc:kc + 1],
                            start=(kc == 0), stop=(kc == 15)),
                            reads=["wada%d" % b, "sc"], writes=["mod_ps"])
            kb.op("dve", lambda e: e.tensor_tensor(out=P["modT"][:], in0=mps[:], in1=bT[:], op=ALU.add),
                  reads=["mod_ps", "bT"], writes=["modT"])
            kb.op("dve", lambda e: e.scalar_tensor_tensor(out=P["gmodA"][:], in0=P["modT"][:, 16:32], scalar=1.0,
                                                          in1=P["gvec"][:, 0:16], op0=ALU.add, op1=ALU.mult),
                  reads=["modT", "gvec"], writes=["gmodA"])
            kb.op("dve", lambda e: e.scalar_tensor_tensor(out=P["gmodF"][:], in0=P["modT"][:, 64:80], scalar=1.0,
                                                          in1=P["gvec"][:, 16:32], op0=ALU.add, op1=ALU.mult),
                  reads=["modT", "gvec"], writes=["gmodF"])
            if c.debug:
                self.dma("sp", self.MODT, P["modT"][:], ["modT"], [], kb.dsem("dbg"))
            kb.barrier()
            kb.release_dsems(wsem + cs)

    def bcast_tile(self, st, ph, name, srcT, key):
        kb, P = self.kb, self.P
        dst = self.sb(st, name, [128, 2048], F32)
        dg = self.sb(ph, name + "_dg", [128, 128], F32)
        for ci in range(16):
            pst_full, pkk = self.pbank()
            pst = pst_full[:, 0:128]
            kb.op("dve", lambda e, ci=ci: e.tensor_scalar(out=dg[:], in0=P["ident_f"][:], scalar1=srcT[:, ci:ci + 1],
                                                         scalar2=None, op0=ALU.mult),
                  reads=["ident_f", key], writes=[name + "_dg"])
            kb.op("pe", lambda e, pst=pst: e.matmul(pst, lhsT=P["ones_f"][:], rhs=dg[:], start=True, stop=True),
                  reads=["ones_f", name + "_dg"], writes=[pkk])
            kb.op("act", lambda e, ci=ci, pst=pst: e.activation(out=dst[:, ci * 128:(ci + 1) * 128], in_=pst, func=AF.Copy),
                  reads=[pkk], writes=[name])
        return dst


def _T128(v):
    v = np.asarray(v).reshape(-1, 128)
    return np.ascontiguousarray(v.T)


def lnmult_table():
    tab = np.full((17, 128, 128), -1e30, np.float32)
    k = np.arange(128)[:, None]
    q = np.arange(128)[None, :]
    for dl in range(17):
        diff = 128 * dl + q - k
        cnt = np.zeros((128, 128), np.int32)
        for w, d in Cfg.WINDOWS:
            cnt += ((diff >= 0) & (diff <= w) & (diff % d == 0)).astype(np.int32)
        tab[dl] = np.where(cnt > 0, np.log(np.maximum(cnt, 1)).astype(np.float32), np.float32(-1e30))
    return np.ascontiguousarray(tab.transpose(1, 0, 2))


def prepare_shared(inp, cfg):
    c = cfg
    sh = {}
    sh["cT"] = _T128(inp["c"][0])
    sh["w_ada"] = np.ascontiguousarray(inp["w_ada"][0])
    sh["b_adaT"] = _T128(inp["b_ada"][0])
    sh["gvecT"] = np.concatenate([_T128(inp["norm_attn_g"][0]), _T128(inp["norm_ffn_g"][0]),
                                  _T128(inp["final_norm_g"])], axis=1)
    w_in = inp["w_in"][0]
    rope = w_in[:, 3840:3904]
    sh["w_in"] = np.concatenate([w_in, rope[:, 32:64], rope[:, 0:32]], axis=1)
    sh["gsmallT"] = np.concatenate([_T128(inp["g_q"][0]), _T128(inp["g_kv"][0]),
                                    _T128(inp["g_out_swa"][0]), _T128(inp["g_out_mla"][0])], axis=1)
    wq = inp["w_uq"][0].reshape(c.QL, c.NH, 192)
    sh["w_uq"] = np.concatenate([wq[:, :, 0:128].reshape(c.QL, -1), wq[:, :, 128:192].reshape(c.QL, -1),
                                 np.concatenate([wq[:, :, 160:192], wq[:, :, 128:160]], axis=2).reshape(c.QL, -1)],
                                axis=1)
    wkv = inp["w_ukv"][0].reshape(c.KVL, c.NH, 256)
    sh["w_ukv"] = np.concatenate([wkv[:, :, 0:128].reshape(c.KVL, -1), wkv[:, :, 128:256].reshape(c.KVL, -1)], axis=1)
    sh["w_o"] = np.ascontiguousarray(inp["w_o"][0])
    sh["w_router"] = np.ascontiguousarray(inp["w_router"][0])
    sh["rbias"] = np.ascontiguousarray(inp["router_bias"][0][None, :])
    sh["w_eg"] = inp["w_exp_gate"][0]
    sh["w_eu"] = inp["w_exp_up"][0]
    sh["w_ed"] = inp["w_exp_down"][0]
    sh["w_sg"] = inp["w_sh_gate"][0]
    sh["w_su"] = inp["w_sh_up"][0]
    sh["w_sd"] = inp["w_sh_down"][0]
    sh["ident"] = np.eye(128, dtype=np.float32)
    half = c.ROPE // 2
    invf = (np.float32(10000.0) ** (-np.arange(half, dtype=np.float32) / np.float32(half))).astype(np.float32)
    sh["invfreq2"] = np.concatenate([invf, invf])[:, None].astype(np.float32)
    sh["lnmult"] = lnmult_table()
    sh["tri"] = (np.arange(128)[:, None] <= np.arange(128)[None, :]).astype(np.float32)
    sh["lstrict"] = (np.arange(128)[:, None] < np.arange(128)[None, :]).astype(np.float32)
    sh["iota64"] = np.ascontiguousarray(np.broadcast_to(np.arange(c.NE, dtype=np.float32)[None, :], (128, c.NE)))
    return sh


def slot_order(core, nchunks, nslot):
    order = [core - s for s in range(core + 1)]
    rest = [j for j in range(nchunks) if j > core]
    order = order + rest
    order = order[:nslot] + [-1] * max(0, nslot - len(order))
    valid = [1.0 if s <= core and order[s] >= 0 else 0.0 for s in range(nslot)]
    return order, valid


def prepare_core(inp, cfg, core, nchunks):
    c = cfg
    x = inp["x"][0]
    pos = inp["positions"][0]
    order, valid = slot_order(core, nchunks, c.NSLOT)
    xs, ps = [], []
    for s, j in enumerate(order):
        if j >= 0:
            xs.append(x[j * c.CH:(j + 1) * c.CH])
            ps.append(pos[j * c.CH:(j + 1) * c.CH])
        else:
            xs.append(x[0:c.CH])
            ps.append(pos[0:c.CH])
    d = {}
    d["xk"] = np.concatenate(xs, axis=0)
    d["posk"] = np.concatenate(ps)[None, :].astype(np.int32)
    d["flags"] = np.repeat(np.asarray(valid, np.float32), 8)[None, :]
    return d


class Phases2(Phases):
    def alloc_psum(self, ph, nf32=6, nbf=2):
        self.pbanks = [self.ps(ph, "pb%d" % i, [128, 512], F32) for i in range(nf32)]
        self.pbi = 0
        self.tbanks = [self.ps(ph, "tb%d" % i, [128, 8, 128], BF16) for i in range(nbf)]
        self.tbi = 0

    def pbank(self):
        i = self.pbi % len(self.pbanks)
        self.pbi += 1
        return self.pbanks[i], "pb%d" % i

    def tbank(self):
        i = self.tbi % len(self.tbanks)
        self.tbi += 1
        return self.tbanks[i], "tb%d" % i

    def alloc_normT(self, ph, pref, Fmax, nslots=2):
        W = {"pref": pref, "n": nslots, "i": 0}
        W["junk"] = self.sb(ph, pref + "_junk", [128, Fmax], BF16)
        W["xn"] = [self.sb(ph, pref + "_xn%d" % i, [128, Fmax], BF16) for i in range(nslots)]
        W["st"] = [self.sb(ph, pref + "_st%d" % i, [128, 4], F32) for i in range(nslots)]
        return W

    def norm_T(self, W, src, src_keys, F, gT, shiftT, gkeys, dst_of, dst_keys, eps_scale=None):
        kb, P, c = self.kb, self.P, self.cfg
        i = W["i"] % W["n"]
        W["i"] += 1
        pref = W["pref"]
        junk, xn, stt = W["junk"], W["xn"][i], W["st"][i]
        kj, kx, ks = pref + "_junk", pref + "_xn%d" % i, pref + "_st%d" % i
        kb.op("act", lambda e: e.activation(out=junk[:, 0:F], in_=src, func=AF.Square, accum_out=stt[:, 0:1]),
              reads=src_keys, writes=[kj, ks])
        kb.op("dve", lambda e: e.tensor_scalar(out=stt[:, 1:2], in0=stt[:, 0:1], scalar1=1.0 / F, scalar2=c.EPS,
                                               op0=ALU.mult, op1=ALU.add), reads=[ks], writes=[ks])
        kb.op("act", lambda e: e.activation(out=stt[:, 2:3], in_=stt[:, 1:2], func=AF.Sqrt), reads=[ks], writes=[ks])
        kb.op("dve", lambda e: e.reciprocal(out=stt[:, 3:4], in_=stt[:, 2:3]), reads=[ks], writes=[ks])
        kb.op("act", lambda e: e.activation(out=xn[:, 0:F], in_=src, func=AF.Copy, scale=stt[:, 3:4]),
              reads=list(src_keys) + [ks], writes=[kx])
        nchunk = F // 128
        for c0 in range(0, nchunk, 8):
            tb, tk = self.tbank()
            n = min(8, nchunk - c0)
            for j in range(n):
                ci = c0 + j
                kb.op("pe", lambda e, j=j, ci=ci: e.transpose(out=tb[:, j, :], in_=xn[:, ci * 128:(ci + 1) * 128],
                                                              identity=P["ident_b"][:]),
                      reads=[kx, "ident_b"], writes=[tk])
            for j in range(n):
                ci = c0 + j
                if shiftT is not None:
                    kb.op("dve", lambda e, j=j, ci=ci: e.tensor_scalar(
                        out=dst_of(ci), in0=tb[:, j, :], scalar1=gT[:, ci:ci + 1], scalar2=shiftT[:, ci:ci + 1],
                        op0=ALU.mult, op1=ALU.add), reads=[tk] + gkeys, writes=dst_keys)
                else:
                    kb.op("dve", lambda e, j=j, ci=ci: e.tensor_scalar(
                        out=dst_of(ci), in0=tb[:, j, :], scalar1=gT[:, ci:ci + 1], scalar2=None,
                        op0=ALU.mult), reads=[tk] + gkeys, writes=dst_keys)

    def alloc_rope(self, ph, pref):
        R = {"pref": pref}
        for n in ["posi"]:
            R[n] = self.sb(ph, pref + n, [64, 512], I32)
        for n in ["ang", "t", "kk", "r", "r2", "sin", "cos"]:
            R[n] = self.sb(ph, pref + n, [64, 512], F32)
        R["sem"] = self.kb.dsem(pref)
        return R

    def rope_tables(self, R, pos_ap):
        kb, P = self.kb, self.P
        p = R["pref"]
        K = lambda n: p + n
        self.dma("sp", R["posi"][:], pos_ap.partition_broadcast(64), [], [K("posi")], R["sem"])
        kb.op("dve", lambda e: e.tensor_copy(out=R["ang"][:], in_=R["posi"][:]), reads=[K("posi")], writes=[K("ang")])
        kb.op("dve", lambda e: e.tensor_scalar(out=R["ang"][:], in0=R["ang"][:], scalar1=P["invf"][:, 0:1], scalar2=None,
                                               op0=ALU.mult), reads=["invf"], writes=[K("ang")])
        kb.op("dve", lambda e: e.tensor_scalar(out=R["t"][:], in0=R["ang"][:], scalar1=float(1.0 / TWO_PI), scalar2=MAGIC,
                                               op0=ALU.mult, op1=ALU.add), reads=[K("ang")], writes=[K("t")])
        kb.op("dve", lambda e: e.tensor_scalar(out=R["kk"][:], in0=R["t"][:], scalar1=-MAGIC, scalar2=None,
                                               op0=ALU.add), reads=[K("t")], writes=[K("kk")])
        kb.op("dve", lambda e: e.scalar_tensor_tensor(out=R["r"][:], in0=R["kk"][:], scalar=-CW1, in1=R["ang"][:],
                                                      op0=ALU.mult, op1=ALU.add), reads=[K("ang"), K("kk")], writes=[K("r")])
        for cw in (CW2, CW3):
            kb.op("dve", lambda e, cw=cw: e.scalar_tensor_tensor(out=R["r"][:], in0=R["kk"][:], scalar=-cw, in1=R["r"][:],
                                                                 op0=ALU.mult, op1=ALU.add), reads=[K("kk")], writes=[K("r")])
        kb.op("dve", lambda e: e.tensor_scalar(out=R["t"][:], in0=R["r"][:], scalar1=float(np.pi / 2), scalar2=None,
                                               op0=ALU.is_gt), reads=[K("r")], writes=[K("t")])
        kb.op("dve", lambda e: e.scalar_tensor_tensor(out=R["r2"][:], in0=R["t"][:], scalar=-float(TWO_PI), in1=R["r"][:],
                                                      op0=ALU.mult, op1=ALU.add), reads=[K("t"), K("r")], writes=[K("r2")])
        kb.op("dve", lambda e: e.tensor_scalar(out=R["r2"][:], in0=R["r2"][:], scalar1=float(np.pi / 2), scalar2=None,
                                               op0=ALU.add), reads=[], writes=[K("r2")])
        lim = 3.1415925
        for n in ["r", "r2"]:
            kb.op("dve", lambda e, n=n: e.tensor_scalar(out=R[n][:], in0=R[n][:], scalar1=lim, scalar2=-lim,
                                                        op0=ALU.min, op1=ALU.max), reads=[], writes=[K(n)])
        kb.op("act", lambda e: e.activation(out=R["sin"][:], in_=R["r"][:], func=AF.Sin), reads=[K("r")], writes=[K("sin")])
        kb.op("act", lambda e: e.activation(out=R["cos"][:], in_=R["r2"][:], func=AF.Sin), reads=[K("r2")], writes=[K("cos")])
        kb.op("dve", lambda e: e.tensor_scalar(out=R["sin"][:], in0=R["sin"][:], scalar1=P["sgn"][:, 0:1], scalar2=None,
                                               op0=ALU.mult), reads=["sgn0", "sgn1"], writes=[K("sin")])

    def apply_rope(self, R, a_ps, b_ps, ps_keys, out_ap, out_keys, tmp, tmpk):
        kb = self.kb
        p = R["pref"]
        kb.op("dve", lambda e: e.tensor_tensor(out=tmp[0][:], in0=a_ps, in1=R["cos"][:], op=ALU.mult),
              reads=ps_keys + [p + "cos"], writes=[tmpk + "0"])
        kb.op("dve", lambda e: e.tensor_tensor(out=tmp[1][:], in0=b_ps, in1=R["sin"][:], op=ALU.mult),
              reads=ps_keys + [p + "sin"], writes=[tmpk + "1"])
        kb.op("pool", lambda e: e.tensor_tensor(out=out_ap, in0=tmp[0][:], in1=tmp[1][:], op=ALU.add),
              reads=[tmpk + "0", tmpk + "1"], writes=out_keys)


class Phases3(Phases2):
    def phase1a(self):
        c, kb, P = self.cfg, self.kb, self.P
        NG = c.NSLOT * c.CH // 512
        gps = c.CH // 512
        with contextlib.ExitStack() as ph:
            self.alloc_psum(ph, 6, 2)
            wkv = self.sb(ph, "wkv", [128, 16, 384], BF16)
            wukv = self.sb(ph, "wukv", [128, 2, 2048], BF16)
            ws = kb.dsem("w1a")
            self.dma("pool", wkv[:], self.w_in[:, 3584:3968].rearrange("(kc p) n -> p kc n", p=128), [], ["wkv"], ws)
            ws2 = kb.dsem("w1a2")
            self.dma("pool", wukv[:], self.w_ukv.rearrange("(kc p) n -> p kc n", p=128), [], ["wukv"], ws2)
            NX = 3
            xt = [self.sb(ph, "xt%d" % i, [128, 2048], F32) for i in range(NX)]
            xs = [kb.dsem("xt%d" % i) for i in range(NX)]
            WN = self.alloc_normT(ph, "n1", 2048, 2)
            WNb = self.alloc_normT(ph, "n1b", 256, 2)
            hT = [self.sb(ph, "hT%d" % i, [128, 16, 512], BF16) for i in range(2)]
            hs = [kb.dsem("hT%d" % i) for i in range(2)]
            ckvnT = [self.sb(ph, "ckvnT%d" % i, [128, 2, 512], BF16) for i in range(2)]
            kT_st = [self.sb(ph, "kTst%d" % i, [128, 8, 512], BF16) for i in range(2)]
            kTs = [kb.dsem("kTst%d" % i) for i in range(2)]
            kb_st = [self.sb(ph, "kbst%d" % i, [65, 512], BF16) for i in range(2)]
            kbs = [kb.dsem("kbst%d" % i) for i in range(2)]
            va_st = [self.sb(ph, "vast%d" % i, [128, 4, 8 * 129], BF16) for i in range(2)]
            vas = [kb.dsem("vast%d" % i) for i in range(2)]
            sq = self.sb(ph, "sq1a", [128, 8, 512], BF16)
            sqr = self.sb(ph, "sqr1a", [64, 512], BF16)
            sqmax = self.sb(ph, "sqmax1a", [128, 512], BF16)
            kmrun = self.sb(ph, "kmrun", [1, 512], F32)
            tmp = [self.sb(ph, "rtmp%d" % i, [64, 512], F32) for i in range(2)]
            R = self.alloc_rope(ph, "r1a")
            kb.op("dve", lambda e: e.memset(kmrun[:], 0.0), writes=["kmrun"])
            for i in range(2):
                kb.op("dve", lambda e, i=i: e.memset(kb_st[i][64:65, :], 1.0), writes=["kbst%d_one" % i])
            xv = self.xk.rearrange("(n p) d -> n p d", p=128)
            cnt1a = [0]

            def stageA(g):
                slot = g // gps
                tok0 = g * 512
                b = g % 2
                for t in range(4):
                    xi = cnt1a[0] % NX
                    cnt1a[0] += 1
                    self.dma("sp", xt[xi][:], xv[g * 4 + t], [], ["xt%d" % xi], xs[xi])
                    self.norm_T(WN, xt[xi][:], ["xt%d" % xi], 2048, P["gmodA"], P["modT"], ["gmodA", "modT"],
                                lambda ci, t=t, b=b: hT[b][:, ci, t * 128:(t + 1) * 128], ["hT%d" % b])
                if slot < 2:
                    self.dma("sp", self.HT[slot, :, :, (g % gps) * 512:(g % gps + 1) * 512], hT[b][:], ["hT%d" % b], [], hs[b])

            def stageB(g):
                slot = g // gps
                tok0 = g * 512
                b = g % 2
                self.rope_tables(R, self.posk[:, tok0:tok0 + 512])
                for t in range(4):
                    pb, pk = self.pbank()
                    for ci in range(16):
                        kb.op("pe", lambda e, ci=ci, t=t, pb=pb: e.matmul(
                            pb[:, 0:256], lhsT=hT[b][:, ci, t * 128:(t + 1) * 128], rhs=wkv[:, ci, 0:256],
                            start=(ci == 0), stop=(ci == 15)), reads=["hT%d" % b, "wkv"], writes=[pk])
                    self.norm_T(WNb, pb[:, 0:256], [pk], 256, P["gsm"][:, 4:6], None, ["gsm"],
                                lambda ci, t=t, b=b: ckvnT[b][:, ci, t * 128:(t + 1) * 128], ["ckvnT%d" % b])
                pa, pak = self.pbank()
                pbb, pbk = self.pbank()
                for (pp, ppk, c0) in ((pa, pak, 256), (pbb, pbk, 320)):
                    for ci in range(16):
                        kb.op("pe", lambda e, ci=ci, pp=pp, c0=c0: e.matmul(
                            pp[0:64, :], lhsT=wkv[:, ci, c0:c0 + 64], rhs=hT[b][:, ci, :],
                            start=(ci == 0), stop=(ci == 15)), reads=["hT%d" % b, "wkv"], writes=[ppk])
                self.apply_rope(R, pa[0:64, :], pbb[0:64, :], [pak, pbk], kb_st[b][0:64, :], ["kbst%d" % b], tmp, "rtmp")
                kb.op("act", lambda e: e.activation(out=sqr[:], in_=kb_st[b][0:64, :], func=AF.Square),
                      reads=["kbst%d" % b], writes=["sqr1a"])
                for h in range(c.NH):
                    pb, pk = self.pbank()
                    for kc in range(2):
                        kb.op("pe", lambda e, kc=kc, h=h, pb=pb: e.matmul(
                            pb[:], lhsT=wukv[:, kc, h * 128:(h + 1) * 128], rhs=ckvnT[b][:, kc, :],
                            start=(kc == 0), stop=(kc == 1)), reads=["ckvnT%d" % b, "wukv"], writes=[pk])
                    kb.op("act", lambda e, h=h, pb=pb: e.activation(out=kT_st[b][:, h, :], in_=pb[:], func=AF.Copy),
                          reads=[pk], writes=["kTst%d" % b])
                    kb.op("act", lambda e, h=h, pb=pb: e.activation(out=sq[:, h, :], in_=pb[:], func=AF.Square),
                          reads=[pk], writes=["sq1a"])
                kb.op("dve", lambda e: e.tensor_reduce(out=sqmax[:], in_=sq[:].rearrange("p h t -> p t h"),
                                                       axis=AX.X, op=ALU.max), reads=["sq1a"], writes=["sqmax1a"])
                pu, puk = self.pbank()
                kb.op("pe", lambda e: e.matmul(pu[0:1, :], lhsT=P["ones_b"][:, 0:1], rhs=sqmax[:], start=True, stop=False),
                      reads=["ones_b", "sqmax1a"], writes=[puk])
                kb.op("pe", lambda e: e.matmul(pu[0:1, :], lhsT=P["ones_b"][0:64, 0:1], rhs=sqr[:], start=False, stop=True),
                      reads=["ones_b", "sqr1a"], writes=[puk])
                kb.op("dve", lambda e: e.tensor_tensor(out=kmrun[:], in0=pu[0:1, :], in1=kmrun[:], op=ALU.max),
                      reads=[puk], writes=["kmrun"])
                for t in range(4):
                    for half in range(2):
                        pb, pk = self.pbank()
                        for kc in range(2):
                            kb.op("pe", lambda e, kc=kc, t=t, half=half, pb=pb: e.matmul(
                                pb[:], lhsT=ckvnT[b][:, kc, t * 128:(t + 1) * 128],
                                rhs=wukv[:, kc, 1024 + half * 512:1024 + (half + 1) * 512],
                                start=(kc == 0), stop=(kc == 1)), reads=["ckvnT%d" % b, "wukv"], writes=[pk])
                        dstv = va_st[b][:, t, :].rearrange("p (h f) -> p h f", f=129)[:, half * 4:(half + 1) * 4, 0:128]
                        kb.op("dve", lambda e, pb=pb, dstv=dstv: e.tensor_scalar(
                            out=dstv, in0=pb[:].rearrange("p (h f) -> p h f", f=128),
                            scalar1=P["flag8"][:, slot * 8:slot * 8 + 1], scalar2=None, op0=ALU.mult),
                            reads=[pk, "flag8"], writes=["vast%d" % b])
                    onec = va_st[b][:, t, :].rearrange("p (h f) -> p h f", f=129)[:, :, 128:129]
                    kb.op("pool", lambda e, onec=onec: e.tensor_copy(
                        out=onec, in_=P["flag8"][:, slot * 8:(slot + 1) * 8].unsqueeze(2)),
                        reads=["flag8"], writes=["vast%d" % b])
                self.dma("sp", self.KT_A[:, :, tok0:tok0 + 512].rearrange("h p t -> p h t"), kT_st[b][:],
                         ["kTst%d" % b], [], kTs[b])
                self.dma("sp", self.KT_B[:, tok0:tok0 + 512], kb_st[b][:], ["kbst%d" % b, "kbst%d_one" % b], [], kbs[b])
                self.dma("sp", self.V_AUG[tok0:tok0 + 512, :].rearrange("(t p) f -> p t f", p=128), va_st[b][:],
                         ["vast%d" % b], [], vas[b])

            stageA(0)
            for g in range(NG):
                if g + 1 < NG:
                    stageA(g + 1)
                stageB(g)
            kb.op("dve", lambda e: e.tensor_reduce(out=P["kmax2"][0:1, 0:1], in_=kmrun[:], axis=AX.X, op=ALU.max),
                  reads=["kmrun"], writes=["kmax2"])
            kb.barrier()
            kb.release_dsems([ws, ws2] + xs + hs + kTs + kbs + vas + [R["sem"]])


class Phases4(Phases3):
    def phase1b(self):
        c, kb, P = self.cfg, self.kb, self.P
        gps = c.CH // 512
        with contextlib.ExitStack() as ph:
            self.alloc_psum(ph, 6, 2)
            wkva = self.sb(ph, "wkva", [128, 16, 2048], BF16)
            ws = [kb.dsem("w1b%d" % i) for i in range(2)]
            for i in range(2):
                self.dma("pool", wkva[:, :, i * 1024:(i + 1) * 1024],
                         self.w_in[:, 1024 + i * 1024:2048 + i * 1024].rearrange("(kc p) n -> p kc n", p=128),
                         [], ["wkva%d" % i], ws[i])
            hT = [self.sb(ph, "hTb%d" % i, [128, 16, 512], BF16) for i in range(2)]
            hs = [kb.dsem("hTb%d" % i) for i in range(2)]
            kT_st = [self.sb(ph, "kaTst%d" % i, [128, 8, 512], BF16) for i in range(2)]
            kTs = [kb.dsem("kaTst%d" % i) for i in range(2)]
            va_st = [self.sb(ph, "vastb%d" % i, [128, 4, 8 * 129], BF16) for i in range(2)]
            vas = [kb.dsem("vastb%d" % i) for i in range(2)]
            sq = self.sb(ph, "sq1b", [128, 8, 512], BF16)
            sqmax = self.sb(ph, "sqmax1b", [128, 512], BF16)
            kmrun = self.sb(ph, "kmrunb", [1, 512], F32)
            kb.op("dve", lambda e: e.memset(kmrun[:], 0.0), writes=["kmrunb"])
            for g in range(2 * gps):
                slot = g // gps
                b = g % 2
                tokd = (1 - slot) * c.CH + (g % gps) * 512
                self.dma("sp", hT[b][:], self.HT[slot, :, :, (g % gps) * 512:(g % gps + 1) * 512], [], ["hTb%d" % b], hs[b])
                for h in range(c.NH):
                    pb, pk = self.pbank()
                    for ci in range(16):
                        kb.op("pe", lambda e, ci=ci, h=h, pb=pb: e.matmul(
                            pb[:], lhsT=wkva[:, ci, h * 128:(h + 1) * 128], rhs=hT[b][:, ci, :],
                            start=(ci == 0), stop=(ci == 15)), reads=["hTb%d" % b, "wkva0"], writes=[pk])
                    kb.op("act", lambda e, h=h, pb=pb: e.activation(out=kT_st[b][:, h, :], in_=pb[:], func=AF.Copy),
                          reads=[pk], writes=["kaTst%d" % b])
                    kb.op("act", lambda e, h=h, pb=pb: e.activation(out=sq[:, h, :], in_=pb[:], func=AF.Square),
                          reads=[pk], writes=["sq1b"])
                kb.op("dve", lambda e: e.tensor_reduce(out=sqmax[:], in_=sq[:].rearrange("p h t -> p t h"),
                                                       axis=AX.X, op=ALU.max), reads=["sq1b"], writes=["sqmax1b"])
                pu, puk = self.pbank()
                kb.op("pe", lambda e: e.matmul(pu[0:1, :], lhsT=P["ones_b"][:, 0:1], rhs=sqmax[:], start=True, stop=True),
                      reads=["ones_b", "sqmax1b"], writes=[puk])
                kb.op("dve", lambda e: e.tensor_tensor(out=kmrun[:], in0=pu[0:1, :], in1=kmrun[:], op=ALU.max),
                      reads=[puk], writes=["kmrunb"])
                for t in range(4):
                    for half in range(2):
                        pb, pk = self.pbank()
                        for ci in range(16):
                            kb.op("pe", lambda e, ci=ci, t=t, half=half, pb=pb: e.matmul(
                                pb[:], lhsT=hT[b][:, ci, t * 128:(t + 1) * 128],
                                rhs=wkva[:, ci, 1024 + half * 512:1024 + (half + 1) * 512],
                                start=(ci == 0), stop=(ci == 15)), reads=["hTb%d" % b, "wkva1"], writes=[pk])
                        dstv = va_st[b][:, t, :].rearrange("p (h f) -> p h f", f=129)[:, half * 4:(half + 1) * 4, 0:128]
                        kb.op("dve", lambda e, pb=pb, dstv=dstv: e.tensor_scalar(
                            out=dstv, in0=pb[:].rearrange("p (h f) -> p h f", f=128),
                            scalar1=P["flag8"][:, slot * 8:slot * 8 + 1], scalar2=None, op0=ALU.mult),
                            reads=[pk, "flag8"], writes=["vastb%d" % b])
                    onec = va_st[b][:, t, :].rearrange("p (h f) -> p h f", f=129)[:, :, 128:129]
                    kb.op("pool", lambda e, onec=onec: e.tensor_copy(
                        out=onec, in_=P["flag8"][:, slot * 8:(slot + 1) * 8].unsqueeze(2)),
                        reads=["flag8"], writes=["vastb%d" % b])
                self.dma("sp", self.KAT[:, :, tokd:tokd + 512].rearrange("h p t -> p h t"), kT_st[b][:],
                         ["kaTst%d" % b], [], kTs[b])
                self.dma("sp", self.VA_AUG[tokd:tokd + 512, :].rearrange("(t p) f -> p t f", p=128), va_st[b][:],
                         ["vastb%d" % b], [], vas[b])
            kb.op("dve", lambda e: e.tensor_reduce(out=P["kmax2"][0:1, 1:2], in_=kmrun[:], axis=AX.X, op=ALU.max),
                  reads=["kmrunb"], writes=["kmax2"])
            kb.barrier()
            kb.release_dsems(ws + hs + kTs + vas)

    def negc_row(self, u_ps, upk, kcol, dst, dstk, tmpf):
        kb, P = self.kb, self.P
        kb.op("act", lambda e: e.activation(out=tmpf[:], in_=u_ps, func=AF.Sqrt, scale=P["kmax2"][0:1, kcol:kcol + 1]),
              reads=[upk, "kmax2"], writes=["negc_tmp"])
        kb.op("dve", lambda e: e.tensor_scalar(out=dst, in0=tmpf[:], scalar1=-1.0, scalar2=None, op0=ALU.mult),
              reads=["negc_tmp"], writes=[dstk])

    def phase1c(self):
        c, kb, P = self.cfg, self.kb, self.P
        gps = c.CH // 512
        with contextlib.ExitStack() as ph:
            self.alloc_psum(ph, 6, 2)
            wq = self.sb(ph, "wq", [128, 16, 1536], BF16)
            wuq = self.sb(ph, "wuq", [128, 4, 2048], BF16)
            ws = [kb.dsem("w1c%d" % i) for i in range(3)]
            self.dma("pool", wq[:, :, 0:1024], self.w_in[:, 0:1024].rearrange("(kc p) n -> p kc n", p=128), [], ["wq0"], ws[0])
            self.dma("pool", wq[:, :, 1024:1536], self.w_in[:, 3072:3584].rearrange("(kc p) n -> p kc n", p=128), [], ["wq1"], ws[1])
            self.dma("pool", wuq[:], self.w_uq.rearrange("(kc p) n -> p kc n", p=128), [], ["wuq"], ws[2])
            WN = self.alloc_normT(ph, "n1c", 512, 2)
            hT = [self.sb(ph, "hTc%d" % i, [128, 16, 512], BF16) for i in range(2)]
            hs = [kb.dsem("hTc%d" % i) for i in range(2)]
            cqnT = [self.sb(ph, "cqnT%d" % i, [128, 4, 512], BF16) for i in range(2)]
            qa_st = [self.sb(ph, "qast%d" % i, [128, 8, 512], BF16) for i in range(2)]
            qas = [kb.dsem("qast%d" % i) for i in range(2)]
            qn_st = [self.sb(ph, "qnst%d" % i, [128, 8, 512], BF16) for i in range(2)]
            qns = [kb.dsem("qnst%d" % i) for i in range(2)]
            qb_st = [self.sb(ph, "qbst%d" % i, [64, 8, 512], BF16) for i in range(2)]
            qbs = [kb.dsem("qbst%d" % i) for i in range(2)]
            nca_st = [self.sb(ph, "ncast0", [1, 8, 512], BF16)] * 2
            ncas = [kb.dsem("ncast0")] * 2
            ncb_st = [self.sb(ph, "ncbst0", [1, 8, 512], BF16)] * 2
            ncbs = [kb.dsem("ncbst0")] * 2
            sq = self.sb(ph, "sq1c", [128, 512], BF16)
            sqr = self.sb(ph, "sqr1c", [64, 512], BF16)
            tmpf = self.sb(ph, "negc_tmp", [1, 512], F32)
            tmp = [self.sb(ph, "rtmpc%d" % i, [64, 512], F32) for i in range(2)]
            R = self.alloc_rope(ph, "r1c")
            for g in range(gps):
                b = g % 2
                tok0 = g * 512
                self.rope_tables(R, self.posk[:, tok0:tok0 + 512])
                self.dma("sp", hT[b][:], self.HT[0, :, :, tok0:tok0 + 512], [], ["hTc%d" % b], hs[b])
                for h in range(c.NH):
                    pb, pk = self.pbank()
                    for ci in range(16):
                        kb.op("pe", lambda e, ci=ci, h=h, pb=pb: e.matmul(
                            pb[:], lhsT=wq[:, ci, h * 128:(h + 1) * 128], rhs=hT[b][:, ci, :],
                            start=(ci == 0), stop=(ci == 15)), reads=["hTc%d" % b, "wq0"], writes=[pk])
                    kb.op("act", lambda e, h=h, pb=pb: e.activation(out=qa_st[b][:, h, :], in_=pb[:], func=AF.Copy),
                          reads=[pk], writes=["qast%d" % b])
                    kb.op("act", lambda e, pb=pb: e.activation(out=sq[:], in_=pb[:], func=AF.Square),
                          reads=[pk], writes=["sq1c"])
                    pu, puk = self.pbank()
                    kb.op("pe", lambda e, pu=pu: e.matmul(pu[0:1, :], lhsT=P["ones_b"][:, 0:1], rhs=sq[:], start=True, stop=True),
                          reads=["ones_b", "sq1c"], writes=[puk])
                    self.negc_row(pu[0:1, :], puk, 1, nca_st[b][0:1, h, :], "ncast0", tmpf)
                for t in range(4):
                    pb, pk = self.pbank()
                    for ci in range(16):
                        kb.op("pe", lambda e, ci=ci, t=t, pb=pb: e.matmul(
                            pb[:], lhsT=hT[b][:, ci, t * 128:(t + 1) * 128], rhs=wq[:, ci, 1024:1536],
                            start=(ci == 0), stop=(ci == 15)), reads=["hTc%d" % b, "wq1"], writes=[pk])
                    self.norm_T(WN, pb[:], [pk], 512, P["gsm"][:, 0:4], None, ["gsm"],
                                lambda ci, t=t, b=b: cqnT[b][:, ci, t * 128:(t + 1) * 128], ["cqnT%d" % b])
                for h in range(c.NH):
                    pb, pk = self.pbank()
                    for kc in range(4):
                        kb.op("pe", lambda e, kc=kc, h=h, pb=pb: e.matmul(
                            pb[:], lhsT=wuq[:, kc, h * 128:(h + 1) * 128], rhs=cqnT[b][:, kc, :],
                            start=(kc == 0), stop=(kc == 3)), reads=["cqnT%d" % b, "wuq"], writes=[pk])
                    kb.op("act", lambda e, h=h, pb=pb: e.activation(out=qn_st[b][:, h, :], in_=pb[:], func=AF.Copy),
                          reads=[pk], writes=["qnst%d" % b])
                    kb.op("act", lambda e, pb=pb: e.activation(out=sq[:], in_=pb[:], func=AF.Square),
                          reads=[pk], writes=["sq1c"])
                    pa, pak = self.pbank()
                    pbb, pbk = self.pbank()
                    for (pp, ppk, c0) in ((pa, pak, 1024), (pbb, pbk, 1536)):
                        for kc in range(4):
                            kb.op("pe", lambda e, kc=kc, pp=pp, c0=c0, h=h: e.matmul(
                                pp[0:64, :], lhsT=wuq[:, kc, c0 + h * 64:c0 + (h + 1) * 64], rhs=cqnT[b][:, kc, :],
                                start=(kc == 0), stop=(kc == 3)), reads=["cqnT%d" % b, "wuq"], writes=[ppk])
                    self.apply_rope(R, pa[0:64, :], pbb[0:64, :], [pak, pbk], qb_st[b][:, h, :], ["qbst%d" % b], tmp, "rtmpc")
                    kb.op("act", lambda e, h=h: e.activation(out=sqr[:], in_=qb_st[b][:, h, :], func=AF.Square),
                          reads=["qbst%d" % b], writes=["sqr1c"])
                    pu, puk = self.pbank()
                    kb.op("pe", lambda e, pu=pu: e.matmul(pu[0:1, :], lhsT=P["ones_b"][:, 0:1], rhs=sq[:], start=True, stop=False),
                          reads=["ones_b", "sq1c"], writes=[puk])
                    kb.op("pe", lambda e, pu=pu: e.matmul(pu[0:1, :], lhsT=P["ones_b"][0:64, 0:1], rhs=sqr[:], start=False, stop=True),
                          reads=["ones_b", "sqr1c"], writes=[puk])
                    self.negc_row(pu[0:1, :], puk, 0, ncb_st[b][0:1, h, :], "ncbst0", tmpf)
                self.dma("sp", self.QAT[:, :, tok0:tok0 + 512].rearrange("h p t -> p h t"), qa_st[b][:], ["qast%d" % b], [], qas[b])
                self.dma("sp", self.NEGCA[:, tok0:tok0 + 512].rearrange("(o h) t -> o h t", o=1), nca_st[b][:], ["ncast0"], [], ncas[b])
                self.dma("sp", self.QT_A[:, :, tok0:tok0 + 512].rearrange("h p t -> p h t"), qn_st[b][:], ["qnst%d" % b], [], qns[b])
                self.dma("sp", self.QT_B[:, 0:64, tok0:tok0 + 512].rearrange("h p t -> p h t"), qb_st[b][:], ["qbst%d" % b], [], qbs[b])
                self.dma("sp", self.QT_B[:, 64:65, tok0:tok0 + 512].rearrange("h o t -> o h t"), ncb_st[b][:], ["ncbst0"], [], ncbs[b])
            kb.barrier()
            kb.release_dsems(ws + hs + qas + qns + qbs + [ncas[0], ncbs[0], R["sem"]])


class Phases5(Phases4):
    def phase2(self):
        c, kb, P = self.cfg, self.kb, self.P
        NQ = c.CH // 128
        slopes = _slopes(c.NH)
        scale = float(c.HD ** -0.5)
        with contextlib.ExitStack() as ph:
            stp = [self.ps(ph, "stp%d" % i, [128, 8, 128], F32) for i in range(2)]
            accp = [self.ps(ph, "accp%d" % i, [128, 3, 129], F32) for i in range(3)]
            self.tbanks = [self.ps(ph, "tbs", [128, 8, 128], BF16)]
            self.tbi = 0
            def acc_of(h):
                return accp[h // 3][:, h % 3, :], "accp%d" % (h // 3)
            kaT = self.sb(ph, "kaT", [128, 8, 2 * c.CH], BF16)
            VA = self.sb(ph, "VAr", [128, 2 * NQ, 8 * 129], BF16)
            sems = [kb.dsem("p2_%d" % i) for i in range(12)]
            self.dma("sp", kaT[:], self.KAT.rearrange("h p t -> p h t"), [], ["kaT"], sems[0])
            nva = 4
            per = 2 * NQ // nva
            for i in range(nva):
                self.dma("sp", VA[:, i * per:(i + 1) * per, :],
                         self.VA_AUG[i * per * 128:(i + 1) * per * 128, :].rearrange("(t p) f -> p t f", p=128),
                         [], ["VA%d" % i], sems[8 + i])
            posq_i = self.sb(ph, "posq_i", [128, c.CH], I32)
            posq = self.sb(ph, "posq", [128, c.CH], F32)
            posk_i = self.sb(ph, "posk_i", [128, 2 * NQ], I32)
            poskc = self.sb(ph, "poskc", [128, 2 * NQ], F32)
            lnm = self.sb(ph, "lnm", [128, 17, 128], F32)
            NS = self.sb(ph, "NS", [128, 8, 128], F32)
            self.dma("sp", posq_i[:], self.posk[:, 0:c.CH].partition_broadcast(128), [], ["posq_i"], sems[3])
            self.dma("sp", posk_i[:, 0:NQ], self.posk[:, c.CH:2 * c.CH].rearrange("o (t p) -> p (o t)", p=128),
                     [], ["posk_i0"], sems[4], allow_slow_non_contiguous=True)
            self.dma("sp", posk_i[:, NQ:2 * NQ], self.posk[:, 0:c.CH].rearrange("o (t p) -> p (o t)", p=128),
                     [], ["posk_i1"], sems[5], allow_slow_non_contiguous=True)
            self.dma("sp", lnm[:], self.lnmult, [], ["lnm"], sems[6])
            kb.op("dve", lambda e: e.tensor_copy(out=posq[:], in_=posq_i[:]), reads=["posq_i"], writes=["posq"])
            kb.op("dve", lambda e: e.tensor_copy(out=poskc[:], in_=posk_i[:]), reads=["posk_i0", "posk_i1"], writes=["poskc"])
            kb.op("dve", lambda e: e.tensor_scalar(out=poskc[:], in0=poskc[:], scalar1=-1.0, scalar2=None, op0=ALU.mult),
                  reads=[], writes=["poskc"])
            for h in range(c.NH):
                kb.op("dve", lambda e, h=h: e.memset(NS[:, h, :], -slopes[h]), writes=["NS"])
            qa = [self.sb(ph, "qa%d" % i, [128, 8, 128], BF16) for i in range(2)]
            qsem = [kb.dsem("qa%d" % i) for i in range(2)]
            ngc = [self.sb(ph, "ngc%d" % i, [1, 8, 128], BF16) for i in range(2)]
            nsem = [kb.dsem("ngc%d" % i) for i in range(2)]
            dist = [self.sb(ph, "dist%d" % i, [128, 128], F32) for i in range(2)]
            u = [self.sb(ph, "u%d" % i, [128, 8, 128], F32) for i in range(2)]
            sp_ = [self.sb(ph, "sp%d" % i, [128, 8, 128], F32) for i in range(2)]
            pT = [self.sb(ph, "pT%d" % i, [128, 8, 128], BF16) for i in range(2)]
            oa = self.sb(ph, "oa", [128, 1024], F32)
            rl = self.sb(ph, "rl", [128, 8], F32)
            WN = self.alloc_normT(ph, "n2", 1024, 1)
            mx = [self.sb(ph, "mxa%d" % i, [128, 8, 128], BF16) for i in range(2)]
            msem = [kb.dsem("mxa%d" % i) for i in range(2)]
            seq = [(b, dl) for b in range(NQ) for dl in range(16, -1, -1)]

            def front(n):
                b, dl = seq[n]
                qb = b % 2
                k = n % 2
                if dl == 16:
                    self.dma("sp", qa[qb][:], self.QAT[:, :, b * 128:(b + 1) * 128].rearrange("h p t -> p h t"), [], ["qa%d" % qb], qsem[qb])
                    self.dma("sp", ngc[qb][:], self.NEGCA[:, b * 128:(b + 1) * 128].rearrange("(o h) t -> o h t", o=1), [], ["ngc%d" % qb], nsem[qb])
                j = NQ + b - dl
                kb.op("act", lambda e: e.activation(
                    out=dist[k][:], in_=posq[:, b * 128:(b + 1) * 128], func=AF.Abs, bias=poskc[:, j:j + 1], scale=1.0),
                    reads=["posq", "poskc"], writes=["dist%d" % k])
                kb.op("pool", lambda e: e.tensor_tensor(
                    out=u[k][:], in0=dist[k][:].unsqueeze(1).broadcast_to([128, 8, 128]), in1=NS[:], op=ALU.mult),
                    reads=["dist%d" % k, "NS"], writes=["u%d" % k])
                kb.op("pool", lambda e: e.tensor_tensor(
                    out=u[k][:], in0=u[k][:], in1=lnm[:, dl, :].unsqueeze(1).broadcast_to([128, 8, 128]), op=ALU.add),
                    reads=["lnm"], writes=["u%d" % k])
                for h in range(c.NH):
                    kb.op("pe", lambda e, h=h: e.matmul(
                        stp[k][:, h, :], lhsT=kaT[:, h, j * 128:(j + 1) * 128], rhs=qa[qb][:, h, :], start=True, stop=False),
                        reads=["kaT", "qa%d" % qb], writes=["stp%d" % k])
                    kb.op("pe", lambda e, h=h: e.matmul(
                        stp[k][:, h, :], lhsT=P["ones_b"][0:1, :], rhs=ngc[qb][0:1, h, :], start=False, stop=True),
                        reads=["ones_b", "ngc%d" % qb], writes=["stp%d" % k])
                kb.op("dve", lambda e: e.scalar_tensor_tensor(
                    out=sp_[k][:], in0=stp[k][:], scalar=scale, in1=u[k][:], op0=ALU.mult, op1=ALU.add),
                    reads=["stp%d" % k, "u%d" % k], writes=["sp%d" % k])
                kb.op("act", lambda e: e.activation(out=pT[k][:], in_=sp_[k][:], func=AF.Exp),
                      reads=["sp%d" % k], writes=["pT%d" % k])

            def back(n):
                b, dl = seq[n]
                k = n % 2
                j = NQ + b - dl
                vak = "VA%d" % (j // per)
                if dl == 16:
                    for i3 in range(3):
                        kb.op("dve", lambda e, i3=i3: e.memset(accp[i3][:], 0.0), writes=["accp%d" % i3])
                for h in range(c.NH):
                    ah, ak = acc_of(h)
                    kb.op("pe", lambda e, h=h, ah=ah: e.matmul(
                        ah, lhsT=pT[k][:, h, :], rhs=VA[:, j, h * 129:(h + 1) * 129], start=False, stop=(dl == 0)),
                        reads=["pT%d" % k, vak], writes=[ak])
                if dl != 0:
                    return
                for i3 in range(3):
                    nh = 3 if i3 < 2 else 2
                    kb.op("dve", lambda e, i3=i3, nh=nh: e.reciprocal(
                        out=rl[:, i3 * 3:i3 * 3 + nh].unsqueeze(2), in_=accp[i3][:, 0:nh, 128:129]),
                        reads=["accp%d" % i3], writes=["rl"])
                for h in range(c.NH):
                    ah, ak = acc_of(h)
                    kb.op("dve", lambda e, h=h, ah=ah: e.tensor_scalar(
                        out=oa[:, h * 128:(h + 1) * 128], in0=ah[:, 0:128], scalar1=rl[:, h:h + 1], scalar2=None, op0=ALU.mult),
                        reads=[ak, "rl"], writes=["oa"])
                mb = b % 2
                self.norm_T(WN, oa[:], ["oa"], 1024, P["gsm"][:, 6:14], None, ["gsm"],
                            lambda ci, mb=mb: mx[mb][:, ci, :], ["mxa%d" % mb])
                self.dma("sp", self.MIXT[:, 0:8, b * 128:(b + 1) * 128], mx[mb][:], ["mxa%d" % mb], [], msem[mb])

            front(0)
            for n in range(len(seq)):
                if n + 1 < len(seq):
                    front(n + 1)
                back(n)
            kb.barrier()
            kb.release_dsems(sems + qsem + nsem + msem)


class Phases6(Phases5):
    def phase3(self):
        c, kb, P = self.cfg, self.kb, self.P
        NT = c.NSLOT * c.CH
        TPS = c.CH // 128
        NGQ = c.CH // 512
        scale = float((128 + c.ROPE) ** -0.5)
        with contextlib.ExitStack() as ph:
            self.alloc_psum(ph, 4, 0)
            NPT = 4
            accs = []
            for i in range(2):
                accs.append((self.ps(ph, "macc%da" % i, [128, 3, 129], F32), self.ps(ph, "macc%db" % i, [128, 1, 129], F32)))
            tri = self.sb(ph, "tri_b", [128, 128], BF16)
            ts = kb.dsem("tri")
            self.dma("pool", tri[:], self.tri_d, [], ["tri"], ts)
            ktB = self.sb(ph, "ktB", [65, NT], BF16)
            kbs = kb.dsem("ktB")
            self.dma("sp", ktB[:], self.KT_B, [], ["ktB"], kbs)
            ktA = [self.sb(ph, "ktA%d" % i, [128, NT], BF16) for i in range(2)]
            Vh = [self.sb(ph, "Vh%d" % i, [128, NT // 128, 129], BF16) for i in range(2)]
            kas = [[kb.dsem("ktA%d_%d" % (i, p)) for p in range(c.NSLOT)] for i in range(2)]
            vs = [[kb.dsem("Vh%d_%d" % (i, p)) for p in range(c.NSLOT)] for i in range(2)]
            qTa = [self.sb(ph, "qTa%d" % i, [128, c.CH], BF16) for i in range(2)]
            qTb = [self.sb(ph, "qTb%d" % i, [65, c.CH], BF16) for i in range(2)]
            qsa = [kb.dsem("qTa%d" % i) for i in range(2)]
            qsb = [kb.dsem("qTb%d" % i) for i in range(2)]
            pT = [self.sb(ph, "mpT%d" % i, [128, 512], BF16) for i in range(NPT)]
            obst = [self.sb(ph, "obst%d" % i, [128, 4, 128], F32) for i in range(2)]
            obs = [kb.dsem("obst%d" % i) for i in range(2)]
            rl = self.sb(ph, "mrl", [128, 4], F32)
            pti = 0
            it = 0
            for h in range(c.NH):
                hb = h % 2
                self.dma("sp", qTa[hb][:], self.QT_A[h], [], ["qTa%d" % hb], qsa[hb])
                self.dma("sp", qTb[hb][:], self.QT_B[h], [], ["qTb%d" % hb], qsb[hb])
                for p in range(c.NSLOT):
                    self.dma("sp", ktA[hb][:, p * c.CH:(p + 1) * c.CH], self.KT_A[h, :, p * c.CH:(p + 1) * c.CH],
                             [], ["ktA%d_%d" % (hb, p)], kas[hb][p])
                    self.dma("sp", Vh[hb][:, p * TPS:(p + 1) * TPS, :],
                             self.V_AUG[p * c.CH:(p + 1) * c.CH, h * 129:(h + 1) * 129].rearrange("(t p) f -> p t f", p=128),
                             [], ["Vh%d_%d" % (hb, p)], vs[hb][p])
                for g in range(NGQ):
                    ab = it % 2
                    it += 1
                    accA, accB = accs[ab]
                    def acc_of(i):
                        return (accA[:, i, :], "macc%da" % ab) if i < 3 else (accB[:, 0, :], "macc%db" % ab)
                    tiles = [(jt, max(0, jt - 4 * g), (jt - 4 * g) if jt >= 4 * g else None) for jt in range(4 * g + 4)]
                    tiles += [(kt, 0, None) for kt in range(TPS, c.NSLOT * TPS)]
                    kb.op("dve", lambda e: e.memset(accA[:], 0.0), writes=["macc%da" % ab])
                    kb.op("dve", lambda e: e.memset(accB[:], 0.0), writes=["macc%db" % ab])
                    first = {}
                    last = {}
                    for n, (kt, imin, idg) in enumerate(tiles):
                        for i in range(imin, 4):
                            first.setdefault(i, n)
                            last[i] = n
                    LAG = 2
                    pend = {}
                    for n2 in range(len(tiles) + LAG):
                        if n2 < len(tiles):
                            n = n2
                            kt, imin, idg = tiles[n]
                            p = kt // TPS
                            pb, pk = self.pbank()
                            q0 = g * 512 + imin * 128
                            q1 = (g + 1) * 512
                            kb.op("pe", lambda e, pb=pb, kt=kt, imin=imin, q0=q0, q1=q1: e.matmul(
                                pb[:, imin * 128:512], lhsT=ktA[hb][:, kt * 128:(kt + 1) * 128], rhs=qTa[hb][:, q0:q1],
                                start=True, stop=False), reads=["ktA%d_%d" % (hb, p), "qTa%d" % hb], writes=[pk])
                            kb.op("pe", lambda e, pb=pb, kt=kt, imin=imin, q0=q0, q1=q1: e.matmul(
                                pb[:, imin * 128:512], lhsT=ktB[:, kt * 128:(kt + 1) * 128], rhs=qTb[hb][:, q0:q1],
                                start=False, stop=True), reads=["ktB", "qTb%d" % hb], writes=[pk])
                            pi = pti % NPT
                            pti += 1
                            kb.op("act", lambda e, pb=pb, pi=pi, imin=imin: e.activation(
                                out=pT[pi][:, imin * 128:512], in_=pb[:, imin * 128:512], func=AF.Exp, scale=scale),
                                reads=[pk], writes=["mpT%d" % pi])
                            if idg is not None:
                                kb.op("pool", lambda e, pi=pi, idg=idg: e.tensor_tensor(
                                    out=pT[pi][:, idg * 128:(idg + 1) * 128], in0=pT[pi][:, idg * 128:(idg + 1) * 128],
                                    in1=tri[:], op=ALU.mult), reads=["tri"], writes=["mpT%d" % pi])
                            pend[n] = pi
                        if n2 >= LAG:
                            n = n2 - LAG
                            kt, imin, idg = tiles[n]
                            p = kt // TPS
                            pi = pend.pop(n)
                            for i in range(imin, 4):
                                ai, ak = acc_of(i)
                                kb.op("pe", lambda e, ai=ai, pi=pi, i=i, kt=kt, n=n: e.matmul(
                                    ai, lhsT=pT[pi][:, i * 128:(i + 1) * 128], rhs=Vh[hb][:, kt, :],
                                    start=False, stop=(last[i] == n)),
                                    reads=["mpT%d" % pi, "Vh%d_%d" % (hb, p)], writes=[ak])
                    ob = obst[ab]
                    kb.op("dve", lambda e: e.reciprocal(out=rl[:, 0:3].unsqueeze(2), in_=accA[:, :, 128:129]),
                          reads=["macc%da" % ab], writes=["mrl"])
                    kb.op("dve", lambda e: e.reciprocal(out=rl[:, 3:4].unsqueeze(2), in_=accB[:, :, 128:129]),
                          reads=["macc%db" % ab], writes=["mrl"])
                    for i in range(4):
                        ai, ak = acc_of(i)
                        kb.op("dve", lambda e, ai=ai, i=i, ob=ob: e.tensor_scalar(
                            out=ob[:, i, :], in0=ai[:, 0:128], scalar1=rl[:, i:i + 1], scalar2=None, op0=ALU.mult),
                            reads=[ak, "mrl"], writes=["obst%d" % ab])
                    self.dma("sp", self.OB[g * 512:(g + 1) * 512, h * 128:(h + 1) * 128].rearrange("(i p) f -> p i f", p=128),
                             ob[:], ["obst%d" % ab], [], obs[ab])
            kb.barrier()
            kb.release_dsems([ts, kbs] + kas[0] + kas[1] + vs[0] + vs[1] + qsa + qsb + obs)


class Phases7(Phases6):
    def phase4(self, st):
        c, kb, P = self.cfg, self.kb, self.P
        NQ = c.CH // 128
        NE = c.NE
        GS = NE // c.NG
        P["Wall"] = self.sb(st, "Wall", [128, NQ, NE], F32)
        P["slotI"] = self.sb(st, "slotI", [128, NQ, 8], I32)
        self.bnd_reg = self.nc.gpsimd.alloc_register("moe_bnd")
        self.nc.gpsimd.reg_mov(self.bnd_reg, NE * c.CAP - 1)
        P["w8"] = self.sb(st, "w8", [128, NQ, 8], F32)
        CAP = c.CAP
        BIGK = 131072.0
        with contextlib.ExitStack() as ph:
            self.alloc_psum(ph, 5, 2)
            Ls = self.sb(ph, "Ls", [128, 128], BF16)
            iot = self.sb(ph, "iot", [128, NE], F32)
            ebase = self.sb(ph, "ebase", [128, NE], F32)
            selb = self.sb(ph, "selb", [128, NQ, NE], BF16)
            lss = kb.dsem("Ls")
            ios = kb.dsem("iot")
            self.dma("pool", Ls[:], self.lstrict_d, [], ["Ls"], lss)
            self.dma("sp", iot[:], self.iota_d, [], ["iot"], ios)
            kb.op("dve", lambda e: e.tensor_scalar(out=ebase[:], in0=iot[:], scalar1=float(CAP), scalar2=None, op0=ALU.mult),
                  reads=["iot"], writes=["ebase"])
            d_selv = self.sb(ph, "d_selv", [128, NE], F32)
            d_slot = self.sb(ph, "d_slot", [128, NE], F32)
            d_k8 = self.sb(ph, "d_k8", [128, 8], F32)
            d_s8 = self.sb(ph, "d_s8", [128, 8], F32)
            d_e8i = self.sb(ph, "d_e8i", [128, 8], I32)
            d_e8f = self.sb(ph, "d_e8f", [128, 8], F32)
            d_junk = self.sb(ph, "d_junk", [128, NE], F32)
            scs = [kb.dsem("scat%d" % i) for i in range(16)]
            sci = 0
            gate_a = self.bcast_tile(ph, ph, "gate_a_bc", P["modT"][:, 32:48], "modT")
            wo = self.sb(ph, "wo", [128, 16, 2048], BF16)
            wos = [kb.dsem("wo%d" % i) for i in range(4)]
            for i in range(4):
                self.dma("pool", wo[:, :, i * 512:(i + 1) * 512],
                         self.w_o[:, i * 512:(i + 1) * 512].rearrange("(kc p) n -> p kc n", p=128), [], ["wo%d" % i], wos[i])
            wr = self.sb(ph, "wr", [128, 16, NE], BF16)
            wrs = kb.dsem("wr")
            self.dma("pool", wr[:], self.w_router.rearrange("(kc p) n -> p kc n", p=128), [], ["wr"], wrs)
            rb = self.sb(ph, "rb_bc", [128, NE], F32)
            rbs = kb.dsem("rb")
            self.dma("sp", rb[:], self.rbias.partition_broadcast(128), [], ["rb"], rbs)
            obt = [self.sb(ph, "obt%d" % i, [128, 1024], F32) for i in range(2)]
            obts = [kb.dsem("obt%d" % i) for i in range(2)]
            mixa = [self.sb(ph, "mixa%d" % i, [128, 8, 128], BF16) for i in range(2)]
            mas = [kb.dsem("mixa%d" % i) for i in range(2)]
            mixb = [self.sb(ph, "mixb%d" % i, [128, 8, 128], BF16) for i in range(2)]
            xt = [self.sb(ph, "xt4_%d" % i, [128, 2048], F32) for i in range(2)]
            xts = [kb.dsem("xt4_%d" % i) for i in range(2)]
            xm = [self.sb(ph, "xm%d" % i, [128, 2048], F32) for i in range(2)]
            xms = [kb.dsem("xm%d" % i) for i in range(2)]
            tmpm = [self.sb(ph, "tmpm%d" % i, [128, 512], F32) for i in range(2)]
            h2 = [self.sb(ph, "h2st%d" % i, [128, 16, 128], BF16) for i in range(2)]
            h2s = [kb.dsem("h2st%d" % i) for i in range(2)]
            WN1 = self.alloc_normT(ph, "n4a", 1024, 1)
            WN2 = self.alloc_normT(ph, "n4b", 2048, 2)
            sc = self.sb(ph, "r_sc", [128, NE], F32)
            ch = self.sb(ph, "r_ch", [128, NE], F32)
            cm = self.sb(ph, "r_cm", [128, NE], F32)
            m8 = self.sb(ph, "r_m8", [128, c.NG, 8], F32)
            grp = self.sb(ph, "r_grp", [128, 8], F32)
            g8 = self.sb(ph, "r_g8", [128, 8], F32)
            gm = self.sb(ph, "r_gm", [128, 8], F32)
            e8 = self.sb(ph, "r_e8", [128, 8], F32)
            sel = self.sb(ph, "r_sel", [128, NE], F32)
            ws_ = self.sb(ph, "r_ws", [128, 2], F32)
            xv = self.xk.rearrange("(n p) d -> n p d", p=128)
            for b in range(NQ):
                k = b % 2
                self.dma("sp", obt[k][:], self.OB[b * 128:(b + 1) * 128, :], [], ["obt%d" % k], obts[k])
                self.dma("sp", mixa[k][:], self.MIXT[:, 0:8, b * 128:(b + 1) * 128], [], ["mixa%d" % k], mas[k])
                self.dma("sp", xt[k][:], xv[b], [], ["xt4_%d" % k], xts[k])
                self.norm_T(WN1, obt[k][:], ["obt%d" % k], 1024, P["gsm"][:, 14:22], None, ["gsm"],
                            lambda ci, k=k: mixb[k][:, ci, :], ["mixb%d" % k])
                for nb in range(4):
                    pb, pk = self.pbank()
                    for ci in range(16):
                        src = mixa[k] if ci < 8 else mixb[k]
                        sk = ("mixa%d" % k) if ci < 8 else ("mixb%d" % k)
                        kb.op("pe", lambda e, ci=ci, nb=nb, pb=pb, src=src: e.matmul(
                            pb[:], lhsT=src[:, ci % 8, :], rhs=wo[:, ci, nb * 512:(nb + 1) * 512],
                            start=(ci == 0), stop=(ci == 15)), reads=[sk, "wo%d" % nb], writes=[pk])
                    tk = nb % 2
                    kb.op("dve", lambda e, nb=nb, pb=pb, tk=tk: e.tensor_tensor(
                        out=tmpm[tk][:], in0=pb[:], in1=gate_a[:, nb * 512:(nb + 1) * 512], op=ALU.mult),
                        reads=[pk, "gate_a_bc"], writes=["tmpm%d" % tk])
                    kb.op("pool", lambda e, nb=nb, tk=tk: e.tensor_tensor(
                        out=xm[k][:, nb * 512:(nb + 1) * 512], in0=tmpm[tk][:], in1=xt[k][:, nb * 512:(nb + 1) * 512], op=ALU.add),
                        reads=["tmpm%d" % tk, "xt4_%d" % k], writes=["xm%d" % k])
                self.dma("sp", self.XMID[b * 128:(b + 1) * 128, :], xm[k][:], ["xm%d" % k], [], xms[k])
                self.norm_T(WN2, xm[k][:], ["xm%d" % k], 2048, P["gmodF"], P["modT"][:, 48:64], ["gmodF", "modT"],
                            lambda ci, k=k: h2[k][:, ci, :], ["h2st%d" % k])
                self.dma("sp", self.H2T[:, :, b * 128:(b + 1) * 128], h2[k][:], ["h2st%d" % k], [], h2s[k])
                pb, pk = self.pbank()
                for ci in range(16):
                    kb.op("pe", lambda e, ci=ci, pb=pb: e.matmul(pb[:, 0:NE], lhsT=h2[k][:, ci, :], rhs=wr[:, ci, :],
                                                                 start=(ci == 0), stop=(ci == 15)),
                          reads=["h2st%d" % k, "wr"], writes=[pk])
                D = lambda fn, r, w: kb.op("dve", fn, reads=r, writes=w)
                kb.op("act", lambda e, pb=pb: e.activation(out=sc[:], in_=pb[:, 0:NE], func=AF.Sigmoid), reads=[pk], writes=["r_sc"])
                D(lambda e: e.tensor_tensor(out=ch[:], in0=sc[:], in1=rb[:], op=ALU.add), ["r_sc", "rb"], ["r_ch"])
                for g in range(c.NG):
                    D(lambda e, g=g: e.max(out=m8[:, g, :], in_=ch[:, g * GS:(g + 1) * GS]), ["r_ch"], ["r_m8"])
                D(lambda e: e.tensor_tensor(out=grp[:].unsqueeze(2), in0=m8[:, :, 0:1], in1=m8[:, :, 1:2], op=ALU.add), ["r_m8"], ["r_grp"])
                D(lambda e: e.max(out=g8[:], in_=grp[:]), ["r_grp"], ["r_g8"])
                D(lambda e: e.tensor_scalar(out=gm[:], in0=grp[:], scalar1=g8[:, c.TOPG - 1:c.TOPG], scalar2=None, op0=ALU.is_ge),
                  ["r_grp", "r_g8"], ["r_gm"])
                D(lambda e: e.tensor_scalar(out=gm[:], in0=gm[:], scalar1=-1.0, scalar2=1e30, op0=ALU.add, op1=ALU.mult), [], ["r_gm"])
                D(lambda e: e.tensor_tensor(out=cm[:].rearrange("p (g s) -> p g s", s=GS), in0=ch[:].rearrange("p (g s) -> p g s", s=GS),
                                            in1=gm[:].unsqueeze(2).broadcast_to([128, c.NG, GS]), op=ALU.add), ["r_ch", "r_gm"], ["r_cm"])
                D(lambda e: e.max(out=e8[:], in_=cm[:]), ["r_cm"], ["r_e8"])
                D(lambda e: e.tensor_scalar(out=sel[:], in0=cm[:], scalar1=e8[:, c.TOPK - 1:c.TOPK], scalar2=None, op0=ALU.is_ge),
                  ["r_cm", "r_e8"], ["r_sel"])
                D(lambda e, b=b: e.tensor_copy(out=selb[:, b, :], in_=sel[:]), ["r_sel"], ["selb%d" % b])
                pp, ppk = self.pbank()
                for b2 in range(b):
                    kb.op("pe", lambda e, b2=b2, pp=pp: e.matmul(pp[:, 0:NE], lhsT=P["ones_b"][:], rhs=selb[:, b2, :],
                                                                 start=(b2 == 0), stop=False),
                          reads=["ones_b", "selb%d" % b2], writes=[ppk])
                kb.op("pe", lambda e, b=b, pp=pp: e.matmul(pp[:, 0:NE], lhsT=Ls[:], rhs=selb[:, b, :], start=(b == 0), stop=True),
                      reads=["Ls", "selb%d" % b], writes=[ppk])
                D(lambda e, pp=pp: e.tensor_scalar(out=d_selv[:], in0=pp[:, 0:NE], scalar1=float(CAP), scalar2=None, op0=ALU.is_lt),
                  [ppk], ["d_selv"])
                D(lambda e: e.tensor_tensor(out=d_selv[:], in0=d_selv[:], in1=sel[:], op=ALU.mult), ["r_sel"], ["d_selv"])
                D(lambda e, pp=pp: e.tensor_tensor(out=d_slot[:], in0=pp[:, 0:NE], in1=ebase[:], op=ALU.add), [ppk, "ebase"], ["d_slot"])
                D(lambda e: e.tensor_scalar(out=d_slot[:], in0=d_slot[:], scalar1=-1.0, scalar2=BIGK, op0=ALU.mult, op1=ALU.add),
                  [], ["d_slot"])
                D(lambda e: e.tensor_tensor(out=d_slot[:], in0=d_slot[:], in1=d_selv[:], op=ALU.mult), ["d_selv"], ["d_slot"])
                D(lambda e: e.max(out=d_k8[:], in_=d_slot[:]), ["d_slot"], ["d_k8"])
                D(lambda e: e.tensor_scalar(out=d_s8[:], in0=d_k8[:], scalar1=-1.0, scalar2=BIGK, op0=ALU.mult, op1=ALU.add),
                  ["d_k8"], ["d_s8"])
                D(lambda e, b=b: e.tensor_copy(out=P["slotI"][:, b, :], in_=d_s8[:]), ["d_s8"], ["slotI%d" % b])
                D(lambda e, b=b: e.tensor_scalar(out=d_e8i[:], in0=P["slotI"][:, b, :], scalar1=int(np.log2(CAP)), scalar2=None,
                                                 op0=ALU.arith_shift_right), ["slotI%d" % b], ["d_e8i"])
                D(lambda e: e.tensor_copy(out=d_e8f[:], in_=d_e8i[:]), ["d_e8i"], ["d_e8f"])
                xn_cur = WN2["xn"][(WN2["i"] - 1) % WN2["n"]]
                xn_key = "n4b_xn%d" % ((WN2["i"] - 1) % WN2["n"])
                for k8 in range(8):
                    sm = scs[sci % 16]
                    sci += 1
                    kb.op("pool", lambda e, b=b, k8=k8, xn_cur=xn_cur: e.indirect_dma_start(
                        out=self.XG[:, :], out_offset=bass.IndirectOffsetOnAxis(ap=P["slotI"][:, b, k8:k8 + 1], axis=0),
                        in_=xn_cur[:, :], in_offset=None, bounds_check=self.bnd_reg, oob_is_err=False),
                        reads=[xn_key, "slotI%d" % b], writes=[], dsem=sm)
                D(lambda e: e.tensor_tensor(out=sel[:], in0=sel[:], in1=sc[:], op=ALU.mult), ["r_sc"], ["r_sel"])
                D(lambda e: e.tensor_reduce(out=ws_[:, 0:1], in_=sel[:], axis=AX.X, op=ALU.add), ["r_sel"], ["r_ws"])
                D(lambda e: e.reciprocal(out=ws_[:, 1:2], in_=ws_[:, 0:1]), [], ["r_ws"])
                D(lambda e, b=b: e.tensor_scalar(out=P["Wall"][:, b, :], in0=sel[:], scalar1=ws_[:, 1:2], scalar2=float(c.ROUTED_SCALE),
                                                 op0=ALU.mult, op1=ALU.mult), ["r_sel", "r_ws"], ["Wall"])
                for k8 in range(8):
                    D(lambda e, b=b, k8=k8: e.scalar_tensor_tensor(
                        out=d_junk[:], in0=iot[:], scalar=d_e8f[:, k8:k8 + 1], in1=P["Wall"][:, b, :],
                        op0=ALU.is_equal, op1=ALU.mult, accum_out=P["w8"][:, b, k8:k8 + 1]),
                      ["iot", "d_e8f", "Wall"], ["d_junk", "w8_%d" % b])
            if c.debug:
                self.WALL = self.dscr("WALL", [128, NQ * NE], F32)
                self.dma("sp", self.WALL, P["Wall"][:].rearrange("p a b -> p (a b)"), ["Wall"], [], rbs)
            kb.barrier()
            kb.release_dsems(wos + [wrs, rbs, lss, ios] + obts + mas + xts + xms + h2s + scs)

    def phase6(self, shared_only=False):
        c, kb, P = self.cfg, self.kb, self.P
        NE = c.NE
        TH = c.CH // 2
        NTT = TH // 128
        with contextlib.ExitStack() as ph:
            self.alloc_psum(ph, 8, 0)
            gate_f = self.bcast_tile(ph, ph, "gate_f_bc", P["modT"][:, 80:96], "modT")
            fg = self.bcast_tile(ph, ph, "fg_bc", P["gvec"][:, 32:48], "gvec")
            h2T = self.sb(ph, "h2T_h", [128, 16, TH], BF16)
            h2s = kb.dsem("h2T_h")
            acc = self.sb(ph, "moe_acc", [128, NTT, 2048], F32)
            NW = 2
            wg = [self.sb(ph, "wg%d" % i, [128, 16, 128], BF16) for i in range(NW)]
            wu = [self.sb(ph, "wu%d" % i, [128, 16, 128], BF16) for i in range(NW)]
            wgs = [kb.dsem("wg%d" % i) for i in range(NW)]
            wus = [kb.dsem("wu%d" % i) for i in range(NW)]
            wd = [self.sb(ph, "wd%d" % i, [128, 4, 2048], BF16) for i in range(2)]
            wds = [kb.dsem("wd%d" % i) for i in range(2)]
            hT = [self.sb(ph, "ehT%d" % i, [128, 4, TH], BF16) for i in range(2)]
            sg = [self.sb(ph, "sg%d" % i, [128, 512], F32) for i in range(2)]
            xmt = self.sb(ph, "xmt", [128, 2048], F32)
            xmts = kb.dsem("xmt")
            st3 = self.sb(ph, "st3", [128, 4], F32)
            junk = self.sb(ph, "junk6", [128, 2048], BF16)
            wi = 0
            sgi = 0
            yshs = [kb.dsem("ysh%d" % i) for i in range(NTT)] if shared_only else []
            for half in range(2):
                self.dma("sp", h2T[:], self.H2T[:, :, half * TH:(half + 1) * TH], [], ["h2T_h"], h2s)
                for tt in range(NTT):
                    kb.op("pool", lambda e, tt=tt: e.memset(acc[:, tt, :], 0.0), writes=["acc%d" % tt])
                for ex in ([NE] if shared_only else range(NE + 1)):
                    eb = ex % 2
                    if ex < NE:
                        g_src, u_src, d_src = self.w_eg[ex], self.w_eu[ex], self.w_ed[ex]
                    else:
                        g_src, u_src, d_src = self.w_sg, self.w_su, self.w_sd
                    self.dma("pool", wd[eb][:], d_src.rearrange("(kc p) n -> p kc n", p=128), [], ["wd%d" % eb], wds[eb])
                    for hb in range(4):
                        w = wi % NW
                        wi += 1
                        self.dma("pool", wg[w][:], g_src[:, hb * 128:(hb + 1) * 128].rearrange("(kc p) n -> p kc n", p=128),
                                 [], ["wg%d" % w], wgs[w])
                        self.dma("pool", wu[w][:], u_src[:, hb * 128:(hb + 1) * 128].rearrange("(kc p) n -> p kc n", p=128),
                                 [], ["wu%d" % w], wus[w])
                        for tg in range(TH // 512):
                            pg, pgk = self.pbank()
                            pu, puk = self.pbank()
                            for (pp, ppk, wsrc, wk) in ((pg, pgk, wg[w], "wg%d" % w), (pu, puk, wu[w], "wu%d" % w)):
                                for kc in range(16):
                                    kb.op("pe", lambda e, pp=pp, wsrc=wsrc, kc=kc, tg=tg: e.matmul(
                                        pp[:], lhsT=wsrc[:, kc, :], rhs=h2T[:, kc, tg * 512:(tg + 1) * 512],
                                        start=(kc == 0), stop=(kc == 15)), reads=[wk, "h2T_h"], writes=[ppk])
                            si = sgi % 2
                            sgi += 1
                            kb.op("act", lambda e, pg=pg, si=si: e.activation(out=sg[si][:], in_=pg[:], func=AF.Silu),
                                  reads=[pgk], writes=["sg%d" % si])
                            kb.op("dve", lambda e, pu=pu, si=si, hb=hb, tg=tg: e.tensor_tensor(
                                out=hT[eb][:, hb, tg * 512:(tg + 1) * 512], in0=pu[:], in1=sg[si][:], op=ALU.mult),
                                reads=[puk, "sg%d" % si], writes=["ehT%d" % eb])
                    for tt in range(NTT):
                        tile = half * NTT + tt
                        for nb in range(4):
                            py, pyk = self.pbank()
                            for hb in range(4):
                                kb.op("pe", lambda e, py=py, hb=hb, tt=tt, nb=nb: e.matmul(
                                    py[:], lhsT=hT[eb][:, hb, tt * 128:(tt + 1) * 128], rhs=wd[eb][:, hb, nb * 512:(nb + 1) * 512],
                                    start=(hb == 0), stop=(hb == 3)), reads=["ehT%d" % eb, "wd%d" % eb], writes=[pyk])
                            scal = P["Wall"][:, tile, ex:ex + 1] if ex < NE else 1.0
                            kb.op("dve", lambda e, py=py, tt=tt, nb=nb, scal=scal: e.scalar_tensor_tensor(
                                out=acc[:, tt, nb * 512:(nb + 1) * 512], in0=py[:], scalar=scal,
                                in1=acc[:, tt, nb * 512:(nb + 1) * 512], op0=ALU.mult, op1=ALU.add),
                                reads=[pyk, "Wall"], writes=["acc%d" % tt])
                if shared_only:
                    for tt in range(NTT):
                        tile = half * NTT + tt
                        self.dma("sp", self.YSH[tile * 128:(tile + 1) * 128, :], acc[:, tt, :], ["acc%d" % tt], [], yshs[tt])
                    continue
                for tt in range(NTT):
                    tile = half * NTT + tt
                    self.dma("sp", xmt[:], self.XMID[tile * 128:(tile + 1) * 128, :], [], ["xmt"], xmts)
                    kb.op("dve", lambda e, tt=tt: e.tensor_tensor(out=acc[:, tt, :], in0=acc[:, tt, :], in1=gate_f[:], op=ALU.mult),
                          reads=["gate_f_bc"], writes=["acc%d" % tt])
                    kb.op("pool", lambda e, tt=tt: e.tensor_tensor(out=xmt[:], in0=xmt[:], in1=acc[:, tt, :], op=ALU.add),
                          reads=["acc%d" % tt], writes=["xmt"])
                    kb.op("act", lambda e: e.activation(out=junk[:], in_=xmt[:], func=AF.Square, accum_out=st3[:, 0:1]),
                          reads=["xmt"], writes=["junk6", "st3"])
                    kb.op("dve", lambda e: e.tensor_scalar(out=st3[:, 1:2], in0=st3[:, 0:1], scalar1=1.0 / c.D, scalar2=c.EPS,
                                                           op0=ALU.mult, op1=ALU.add), reads=[], writes=["st3"])
                    kb.op("act", lambda e: e.activation(out=st3[:, 2:3], in_=st3[:, 1:2], func=AF.Sqrt), reads=[], writes=["st3"])
                    kb.op("dve", lambda e: e.reciprocal(out=st3[:, 3:4], in_=st3[:, 2:3]), reads=[], writes=["st3"])
                    kb.op("dve", lambda e: e.scalar_tensor_tensor(out=xmt[:], in0=xmt[:], scalar=st3[:, 3:4], in1=fg[:],
                                                                  op0=ALU.mult, op1=ALU.mult), reads=["st3", "fg_bc"], writes=["xmt"])
                    self.dma("sp", self.out[tile * 128:(tile + 1) * 128, :], xmt[:], ["xmt"], [], xmts)
            kb.barrier()
            kb.release_dsems([h2s, xmts] + wgs + wus + wds + yshs)


class Phases8(Phases7):
    def phase6_routed(self):
        c, kb, P = self.cfg, self.kb, self.P
        NE, CAP = c.NE, c.CAP
        NB = CAP // 128
        with contextlib.ExitStack() as ph:
            self.alloc_psum(ph, 6, 2)
            xg = [self.sb(ph, "xg%d" % i, [128, 2048], BF16) for i in range(3)]
            xgs = [kb.dsem("xg%d" % i) for i in range(3)]
            xT = self.sb(ph, "xTe", [128, 16, CAP], BF16)
            NW = 2
            wg = [self.sb(ph, "rwg%d" % i, [128, 16, 512], BF16) for i in range(NW)]
            wu = [self.sb(ph, "rwu%d" % i, [128, 16, 512], BF16) for i in range(NW)]
            wgs = [kb.dsem("rwg%d" % i) for i in range(NW)]
            wus = [kb.dsem("rwu%d" % i) for i in range(NW)]

            def W_load(ex):
                eb = ex % 2
                self.dma("pool", wg[eb][:], self.w_eg[ex].rearrange("(kc p) n -> p kc n", p=128), [], ["rwg%d" % eb], wgs[eb])
                self.dma("pool", wu[eb][:], self.w_eu[ex].rearrange("(kc p) n -> p kc n", p=128), [], ["rwu%d" % eb], wus[eb])
                self.dma("pool", wd[eb][:], self.w_ed[ex].rearrange("(kc p) n -> p kc n", p=128), [], ["rwd%d" % eb], wds[eb])
            wd = [self.sb(ph, "rwd%d" % i, [128, 4, 2048], BF16) for i in range(2)]
            wds = [kb.dsem("rwd%d" % i) for i in range(2)]
            hT = [self.sb(ph, "rhT%d" % i, [128, 4, CAP], BF16) for i in range(2)]
            sg = [self.sb(ph, "rsg%d" % i, [128, 512], F32) for i in range(2)]
            yst = [self.sb(ph, "yst%d" % i, [128, 2048], BF16) for i in range(2)]
            ysts = [kb.dsem("yst%d" % i) for i in range(2)]
            xTs = [xT, self.sb(ph, "xTe_b", [128, 16, CAP], BF16)]
            cnt = {"xgi": 0, "wi": 0, "sgi": 0, "yi": 0}

            def T_blk(ex, blk):
                xTe = xTs[ex % 2]
                xi = cnt["xgi"] % 3
                cnt["xgi"] += 1
                r0 = ex * CAP + blk * 128
                self.dma("sp", xg[xi][:], self.XG[r0:r0 + 128, :], [], ["xg%d" % xi], xgs[xi])
                for c0 in range(0, 16, 8):
                    tb, tk = self.tbank()
                    for j in range(8):
                        ci = c0 + j
                        kb.op("pe", lambda e, j=j, ci=ci: e.transpose(
                            out=tb[:, j, :], in_=xg[xi][:, ci * 128:(ci + 1) * 128], identity=P["ident_b"][:]),
                            reads=["xg%d" % xi, "ident_b"], writes=[tk])
                    for j in range(8):
                        ci = c0 + j
                        wkey = "xTe%d_%d" % (ex % 2, blk // 4)
                        if j % 2 == 0:
                            kb.op("dve", lambda e, j=j, ci=ci: e.tensor_scalar(
                                out=xTe[:, ci, blk * 128:(blk + 1) * 128], in0=tb[:, j, :], scalar1=P["gmodF"][:, ci:ci + 1],
                                scalar2=P["modT"][:, 48 + ci:49 + ci], op0=ALU.mult, op1=ALU.add),
                                reads=[tk, "gmodF", "modT"], writes=[wkey])
                        else:
                            kb.op("act", lambda e, j=j, ci=ci: e.activation(
                                out=xTe[:, ci, blk * 128:(blk + 1) * 128], in_=tb[:, j, :], func=AF.Identity,
                                scale=P["gmodF"][:, ci:ci + 1], bias=P["modT"][:, 48 + ci:49 + ci]),
                                reads=[tk, "gmodF", "modT"], writes=[wkey])

            def G_hb(ex, hb):
                eb = ex % 2
                xTe = xTs[ex % 2]
                w = eb
                for tg in range(CAP // 512):
                    pg, pgk = self.pbank()
                    pu, puk = self.pbank()
                    for (pp, ppk, wsrc, wk) in ((pg, pgk, wg[w], "rwg%d" % w), (pu, puk, wu[w], "rwu%d" % w)):
                        for kc in range(16):
                            kb.op("pe", lambda e, pp=pp, wsrc=wsrc, kc=kc, tg=tg: e.matmul(
                                pp[:], lhsT=wsrc[:, kc, hb * 128:(hb + 1) * 128], rhs=xTe[:, kc, tg * 512:(tg + 1) * 512],
                                start=(kc == 0), stop=(kc == 15)), reads=[wk, "xTe%d_%d" % (ex % 2, tg)], writes=[ppk])
                    si = cnt["sgi"] % 2
                    cnt["sgi"] += 1
                    kb.op("act", lambda e, pg=pg, si=si: e.activation(out=sg[si][:], in_=pg[:], func=AF.Silu),
                          reads=[pgk], writes=["rsg%d" % si])
                    kb.op("dve", lambda e, pu=pu, si=si, tg=tg: e.tensor_tensor(
                        out=hT[eb][:, hb, tg * 512:(tg + 1) * 512], in0=pu[:], in1=sg[si][:], op=ALU.mult),
                        reads=[puk, "rsg%d" % si], writes=["rhT%d" % eb])

            def G_down(ex, blk):
                eb = ex % 2
                y = cnt["yi"] % 2
                cnt["yi"] += 1
                for nb in range(4):
                    py, pyk = self.pbank()
                    for hb in range(4):
                        kb.op("pe", lambda e, py=py, hb=hb, nb=nb: e.matmul(
                            py[:], lhsT=hT[eb][:, hb, blk * 128:(blk + 1) * 128], rhs=wd[eb][:, hb, nb * 512:(nb + 1) * 512],
                            start=(hb == 0), stop=(hb == 3)), reads=["rhT%d" % eb, "rwd%d" % eb], writes=[pyk])
                    if nb % 2 == 0:
                        kb.op("act", lambda e, py=py, nb=nb: e.activation(out=yst[y][:, nb * 512:(nb + 1) * 512], in_=py[:], func=AF.Copy),
                              reads=[pyk], writes=["yst%d" % y])
                    else:
                        kb.op("dve", lambda e, py=py, nb=nb: e.tensor_copy(out=yst[y][:, nb * 512:(nb + 1) * 512], in_=py[:]),
                              reads=[pyk], writes=["yst%d" % y])
                r0 = ex * CAP + blk * 128
                self.dma("sp", self.YS[r0:r0 + 128, :], yst[y][:], ["yst%d" % y], [], ysts[y])

            W_load(0)
            for blk in range(NB):
                T_blk(0, blk)
            for ex in range(NE):
                if ex + 1 < NE:
                    W_load(ex + 1)
                nxt = [(ex + 1, blk) for blk in range(NB)] if ex + 1 < NE else []
                for hb in range(4):
                    G_hb(ex, hb)
                    if nxt:
                        T_blk(*nxt.pop(0))
                for blk in range(NB):
                    G_down(ex, blk)
                    if nxt and blk % 2 == 1:
                        T_blk(*nxt.pop(0))
                while nxt:
                    T_blk(*nxt.pop(0))
            kb.barrier()
            kb.release_dsems(xgs + wgs + wus + wds + ysts)

    def phase7(self):
        c, kb, P = self.cfg, self.kb, self.P
        NQ = c.CH // 128
        NE, CAP = c.NE, c.CAP
        with contextlib.ExitStack() as ph:
            self.alloc_psum(ph, 2, 0)
            gate_f = self.bcast_tile(ph, ph, "gate_f_bc2", P["modT"][:, 80:96], "modT")
            fg = self.bcast_tile(ph, ph, "fg_bc2", P["gvec"][:, 32:48], "gvec")
            acc = [self.sb(ph, "cacc%d" % i, [128, 2048], F32) for i in range(2)]
            accs = [kb.dsem("cacc%d" % i) for i in range(2)]
            xmt = [self.sb(ph, "cxm%d" % i, [128, 2048], F32) for i in range(2)]
            xmts = [kb.dsem("cxm%d" % i) for i in range(2)]
            NGB = 4
            gb = [self.sb(ph, "gb%d" % i, [128, 2048], BF16) for i in range(NGB)]
            gbs = [kb.dsem("gb%d" % i) for i in range(NGB)]
            st3 = self.sb(ph, "cst3", [128, 4], F32)
            junk = self.sb(ph, "cjunk", [128, 2048], BF16)
            for i in range(NGB):
                kb.op("pool", lambda e, i=i: e.memset(gb[i][:], 0.0), writes=["gb%d" % i])
            gi = 0
            for b in range(NQ):
                k = b % 2
                self.dma("sp", acc[k][:], self.YSH[b * 128:(b + 1) * 128, :], [], ["cacc%d" % k], accs[k])
                self.dma("sp", xmt[k][:], self.XMID[b * 128:(b + 1) * 128, :], [], ["cxm%d" % k], xmts[k])
                for k8 in range(8):
                    g = gi % NGB
                    gi += 1
                    kb.op("pool", lambda e, g=g, b=b, k8=k8: e.indirect_dma_start(
                        out=gb[g][:, :], out_offset=None, in_=self.YS[:, :],
                        in_offset=bass.IndirectOffsetOnAxis(ap=P["slotI"][:, b, k8:k8 + 1], axis=0),
                        bounds_check=self.bnd_reg, oob_is_err=False),
                        reads=["slotI%d" % b], writes=["gb%d" % g], dsem=gbs[g])
                    kb.op("dve", lambda e, g=g, b=b, k8=k8, k=k: e.scalar_tensor_tensor(
                        out=acc[k][:], in0=gb[g][:], scalar=P["w8"][:, b, k8:k8 + 1], in1=acc[k][:], op0=ALU.mult, op1=ALU.add),
                        reads=["gb%d" % g, "w8_%d" % b], writes=["cacc%d" % k])
                kb.op("dve", lambda e, k=k: e.tensor_tensor(out=acc[k][:], in0=acc[k][:], in1=gate_f[:], op=ALU.mult),
                      reads=["gate_f_bc2"], writes=["cacc%d" % k])
                kb.op("pool", lambda e, k=k: e.tensor_tensor(out=xmt[k][:], in0=xmt[k][:], in1=acc[k][:], op=ALU.add),
                      reads=["cacc%d" % k], writes=["cxm%d" % k])
                kb.op("act", lambda e, k=k: e.activation(out=junk[:], in_=xmt[k][:], func=AF.Square, accum_out=st3[:, 0:1]),
                      reads=["cxm%d" % k], writes=["cjunk", "cst3"])
                kb.op("dve", lambda e: e.tensor_scalar(out=st3[:, 1:2], in0=st3[:, 0:1], scalar1=1.0 / c.D, scalar2=c.EPS,
                                                       op0=ALU.mult, op1=ALU.add), reads=[], writes=["cst3"])
                kb.op("act", lambda e: e.activation(out=st3[:, 2:3], in_=st3[:, 1:2], func=AF.Sqrt), reads=[], writes=["cst3"])
                kb.op("dve", lambda e: e.reciprocal(out=st3[:, 3:4], in_=st3[:, 2:3]), reads=[], writes=["cst3"])
                kb.op("dve", lambda e, k=k: e.scalar_tensor_tensor(out=xmt[k][:], in0=xmt[k][:], scalar=st3[:, 3:4], in1=fg[:],
                                                                   op0=ALU.mult, op1=ALU.mult), reads=["cst3", "fg_bc2"], writes=["cxm%d" % k])
                self.dma("sp", self.out[b * 128:(b + 1) * 128, :], xmt[k][:], ["cxm%d" % k], [], xmts[k])
            kb.barrier()
            kb.release_dsems(accs + xmts + gbs)


def build(cfg):
    b = Phases8(cfg)
    b.declare_io()
    kb = b.kb
    with contextlib.ExitStack() as st:
        b.phase0(st)
        if cfg.stop_after >= 1:
            b.phase1a()
        if cfg.stop_after >= 2:
            b.phase1b()
            b.phase1c()
        if cfg.stop_after >= 3:
            b.phase2()
        if cfg.stop_after >= 4:
            b.phase3()
        if cfg.stop_after >= 5:
            b.phase4(st)
        if cfg.stop_after >= 6:
            if cfg.CAP:
                b.phase6(shared_only=True)
                b.phase6_routed()
                b.phase7()
            else:
                b.phase6()
        kb.barrier()
    return b


_BUILD_CACHE = {}


def kernel(**inputs):
    cfg = Cfg
    inp = {k: np.asarray(v) for k, v in inputs.items()}
    S = inp["x"].shape[1]
    nchunks = S // cfg.CH
    assert nchunks == cfg.NCORES and nchunks == cfg.NSLOT
    if "b" not in _BUILD_CACHE:
        _BUILD_CACHE["b"] = build(cfg)
    b = _BUILD_CACHE["b"]
    sh = prepare_shared(inp, cfg)
    maps = []
    for core in range(cfg.NCORES):
        m = dict(sh)
        m.update(prepare_core(inp, cfg, core, nchunks))
        maps.append(m)
    res = run_bass_kernel_spmd(b.nc, maps, core_ids=list(range(cfg.NCORES)))
    outs = [np.asarray(r["out"]) for r in res.results]
    return np.concatenate(outs, axis=0)[None].astype(np.float32)
```

```python
import contextlib
import numpy as np
import concourse.bass as bass
import concourse.mybir as mybir
from concourse.bass_utils import run_bass_kernel_spmd

F32 = mybir.dt.float32
BF16 = mybir.dt.bfloat16
I32 = mybir.dt.int32
AF = mybir.ActivationFunctionType
ALU = mybir.AluOpType
AX = mybir.AxisListType

SAME_ENGINE_SYNC = True


class Cfg:
    D = 2048
    CH = 2048
    NSLOT = 8
    NCORES = 8
    NE = 64
    NG = 8
    TOPG = 4
    TOPK = 8
    DE = 512
    NH = 8
    HD = 128
    QL = 512
    KVL = 256
    ROPE = 64
    NADA = 6
    EPS = 1e-6
    ROUTED_SCALE = 2.5
    WINDOWS = ((128, 1), (512, 4), (2048, 16))
    CAP = 1024
    debug = False
    stop_after = 99


class Sem:
    def __init__(self, h, name):
        self.h = h
        self.name = name
        self.count = 0


class KB:
    def __init__(self, nc):
        self.nc = nc
        self.E = {"pe": nc.tensor, "act": nc.scalar, "dve": nc.vector, "pool": nc.gpsimd, "sp": nc.sync}
        self.esem = {}
        self.allsems = []
        for e in ["pe", "act", "dve", "pool"]:
            self.esem[e] = self.newsem("es_" + e)
        self.waited = {e: {} for e in self.E}
        self.lastw = {}
        self.readers = {}
        self.free_dsems = []
        self.nwaits = 0
        self.nops = 0

    def newsem(self, name):
        s = Sem(self.nc.alloc_semaphore(name=name), name)
        self.allsems.append(s)
        return s

    def dsem(self, name="d"):
        if self.free_dsems:
            return self.free_dsems.pop()
        return self.newsem("ds%d_%s" % (len(self.allsems), name))

    def release_dsems(self, sems):
        self.free_dsems.extend(sems)

    def _wait(self, eng, sem, val):
        if self.waited[eng].get(sem, 0) >= val:
            return
        self.E[eng].wait_ge(sem.h, val)
        self.waited[eng][sem] = val
        self.nwaits += 1

    def op(self, eng, issue, reads=(), writes=(), dsem=None):
        need = {}
        def add(ev):
            sem, val, peng = ev
            if eng == "pe" and peng == "pe":
                return
            if (not SAME_ENGINE_SYNC) and peng == eng and peng != "dma":
                return
            if need.get(sem, 0) < val:
                need[sem] = val
        for r in reads:
            if r in self.lastw:
                add(self.lastw[r])
        for w in writes:
            if w in self.lastw:
                add(self.lastw[w])
            for ev in self.readers.get(w, ()):
                add(ev)
        for sem, val in need.items():
            self._wait(eng, sem, val)
        inst = issue(self.E[eng])
        if dsem is not None:
            if dsem.count > 0 and not any(w.get(dsem, 0) >= dsem.count for w in self.waited.values()):
                raise RuntimeError("DMA semaphore %s reused while previous DMA may be in flight" % dsem.name)
            dsem.count += 16
            inst.then_inc(dsem.h, 16)
            ev = (dsem, dsem.count, "dma")
        else:
            s = self.esem[eng]
            s.count += 1
            inst.then_inc(s.h, 1)
            ev = (s, s.count, eng)
        for w in writes:
            self.lastw[w] = ev
            self.readers[w] = []
        for r in reads:
            if r not in writes:
                self.readers.setdefault(r, []).append(ev)
        self.nops += 1
        return ev

    def barrier(self, engines=("pe", "act", "dve", "pool", "sp")):
        for e in engines:
            for s in self.allsems:
                if s.count > 0:
                    self._wait(e, s, s.count)
        self.lastw = {}
        self.readers = {}


def _slopes(nh):
    return [float(np.float32(2.0) ** np.float32(-8.0 * (h + 1) / nh)) for h in range(nh)]


class Builder:
    def __init__(self, cfg):
        self.cfg = cfg
        self.nc = bass.Bass("TRN2", target_bir_lowering=False)
        self.kb = KB(self.nc)
        self.dram_in = {}
        self.dram_out = {}
        self.scratch = {}

    def din(self, name, shape, dtype=F32):
        t = self.nc.dram_tensor(name, list(shape), dtype, kind="ExternalInput")
        self.dram_in[name] = (tuple(shape), dtype)
        return t.ap()

    def dscr(self, name, shape, dtype, internal=False):
        kind = "ExternalOutput" if (self.cfg.debug and not internal) else "Internal"
        t = self.nc.dram_tensor(name, list(shape), dtype, kind=kind)
        self.scratch[name] = (tuple(shape), dtype)
        return t.ap()

    def dout(self, name, shape, dtype=F32):
        t = self.nc.dram_tensor(name, list(shape), dtype, kind="ExternalOutput")
        self.dram_out[name] = (tuple(shape), dtype)
        return t.ap()


MAGIC = 12582912.0
TWO_PI = 2.0 * np.pi
CW1 = 6.28125
CW2 = float(np.float32(TWO_PI - CW1))
CW3 = float(TWO_PI - CW1 - CW2)


def _b(cls):
    return cls


class Phases(Builder):
    _uid = 0

    def _nm(self, name):
        Phases._uid += 1
        return "%s_u%d" % (name, Phases._uid)

    def sb(self, st, name, shape, dtype):
        return st.enter_context(self.nc.sbuf_tensor(self._nm(name), list(shape), dtype))

    def ps(self, st, name, shape, dtype=F32):
        return st.enter_context(self.nc.psum_tensor(self._nm(name), list(shape), dtype))

    def dma(self, eng, out, in_, reads, writes, sem, **kw):
        return self.kb.op(eng, lambda e: e.dma_start(out=out, in_=in_, **kw), reads=reads, writes=writes, dsem=sem)

    def declare_io(self):
        c = self.cfg
        NT = c.NSLOT * c.CH
        self.xk = self.din("xk", [NT, c.D])
        self.posk = self.din("posk", [1, NT], I32)
        self.flags = self.din("flags", [1, c.NSLOT * 8])
        self.cT = self.din("cT", [128, 16])
        self.w_ada = self.din("w_ada", [c.D, c.NADA * c.D])
        self.b_adaT = self.din("b_adaT", [128, 96])
        self.gvecT = self.din("gvecT", [128, 48])
        self.w_in = self.din("w_in", [c.D, 3968])
        self.gsmallT = self.din("gsmallT", [128, 22])
        self.w_uq = self.din("w_uq", [c.QL, 2048])
        self.w_ukv = self.din("w_ukv", [c.KVL, 2048])
        self.w_o = self.din("w_o", [c.D, c.D])
        self.w_router = self.din("w_router", [c.D, c.NE])
        self.rbias = self.din("rbias", [1, c.NE])
        nexp = c.NE if c.stop_after >= 6 else 1
        self.w_eg = self.din("w_eg", [nexp, c.D, c.DE])
        self.w_eu = self.din("w_eu", [nexp, c.D, c.DE])
        self.w_ed = self.din("w_ed", [nexp, c.DE, c.D])
        self.w_sg = self.din("w_sg", [c.D, c.DE])
        self.w_su = self.din("w_su", [c.D, c.DE])
        self.w_sd = self.din("w_sd", [c.DE, c.D])
        self.ident_d = self.din("ident", [128, 128])
        self.invfreq2 = self.din("invfreq2", [64, 1])
        self.lnmult = self.din("lnmult", [128, 17, 128])
        self.tri_d = self.din("tri", [128, 128])
        self.lstrict_d = self.din("lstrict", [128, 128])
        self.iota_d = self.din("iota64", [128, c.NE])
        self.out = self.dout("out", [c.CH, c.D])
        self.HT = self.dscr("HT", [2, 128, 16, c.CH], BF16)
        self.KT_A = self.dscr("KT_A", [c.NH, 128, NT], BF16)
        self.KT_B = self.dscr("KT_B", [65, NT], BF16)
        self.V_AUG = self.dscr("V_AUG", [NT, c.NH * 129], BF16)
        self.KAT = self.dscr("KAT", [c.NH, 128, 2 * c.CH], BF16)
        self.VA_AUG = self.dscr("VA_AUG", [2 * c.CH, c.NH * 129], BF16)
        self.QAT = self.dscr("QAT", [c.NH, 128, c.CH], BF16)
        self.NEGCA = self.dscr("NEGCA", [c.NH, c.CH], BF16)
        self.QT_A = self.dscr("QT_A", [c.NH, 128, c.CH], BF16)
        self.QT_B = self.dscr("QT_B", [c.NH, 65, c.CH], BF16)
        self.OB = self.dscr("OB", [c.CH, 1024], F32)
        self.MIXT = self.dscr("MIXT", [128, 16, c.CH], BF16)
        self.XMID = self.dscr("XMID", [c.CH, c.D], F32)
        self.H2T = self.dscr("H2T", [128, 16, c.CH], BF16)
        self.MODT = self.dscr("MODT", [128, 96], F32)
        self.XG = self.dscr("XG", [c.NE * max(c.CAP, 128), c.D], BF16, internal=True)
        self.YS = self.dscr("YS", [c.NE * max(c.CAP, 128), c.D], BF16, internal=True)
        self.YSH = self.dscr("YSH", [c.CH, c.D], F32)

    def phase0(self, st):
        c, kb, nc = self.cfg, self.kb, self.nc
        P = self.P = {}
        P["ident_f"] = self.sb(st, "ident_f", [128, 128], F32)
        P["ident_b"] = self.sb(st, "ident_b", [128, 128], BF16)
        P["ones_f"] = self.sb(st, "ones_f", [128, 128], F32)
        P["ones_b"] = self.sb(st, "ones_b", [128, 128], BF16)
        P["modT"] = self.sb(st, "modT", [128, 96], F32)
        P["gvec"] = self.sb(st, "gvec", [128, 48], F32)
        P["gsm"] = self.sb(st, "gsm", [128, 22], F32)
        P["gmodA"] = self.sb(st, "gmodA", [128, 16], F32)
        P["gmodF"] = self.sb(st, "gmodF", [128, 16], F32)
        P["flag8"] = self.sb(st, "flag8", [128, c.NSLOT * 8], F32)
        P["invf"] = self.sb(st, "invf", [64, 1], F32)
        P["sgn"] = self.sb(st, "sgn", [64, 1], F32)
        P["kmax2"] = self.sb(st, "kmax2", [1, 2], F32)
        cs = [kb.dsem("c%d" % i) for i in range(8)]
        s0 = cs[0]
        self.dma("sp", P["ident_f"][:], self.ident_d, [], ["ident_f"], cs[0])
        self.dma("pool", P["ident_b"][:], self.ident_d, [], ["ident_b"], cs[1])
        self.dma("sp", P["gvec"][:], self.gvecT, [], ["gvec"], cs[2])
        self.dma("sp", P["gsm"][:], self.gsmallT, [], ["gsm"], cs[3])
        self.dma("sp", P["flag8"][:], self.flags.partition_broadcast(128), [], ["flag8"], cs[4])
        self.dma("sp", P["invf"][:], self.invfreq2, [], ["invf"], cs[5])
        kb.op("dve", lambda e: e.memset(P["ones_f"][:], 1.0), writes=["ones_f"])
        kb.op("dve", lambda e: e.memset(P["ones_b"][:], 1.0), writes=["ones_b"])
        kb.op("dve", lambda e: e.memset(P["sgn"][0:32, :], -1.0), writes=["sgn0"])
        kb.op("dve", lambda e: e.memset(P["sgn"][32:64, :], 1.0), writes=["sgn1"])
        kb.op("dve", lambda e: e.memset(P["kmax2"][:], 0.0), writes=["kmax2"])

        with contextlib.ExitStack() as ph:
            cT = self.sb(ph, "cT_s", [128, 16], F32)
            sc = self.sb(ph, "sc_s", [128, 16], F32)
            bT = self.sb(ph, "bT_s", [128, 96], F32)
            wbuf = [self.sb(ph, "wada%d" % i, [128, 16, 512], F32) for i in range(2)]
            wsem = [kb.dsem("wada%d" % i) for i in range(2)]
            mps = self.ps(ph, "mod_ps", [128, 96], F32)
            self.dma("sp", cT[:], self.cT, [], ["cT"], cs[6])
            self.dma("sp", bT[:], self.b_adaT, [], ["bT"], cs[7])
            kb.op("act", lambda e: e.activation(out=sc[:], in_=cT[:], func=AF.Silu), reads=["cT"], writes=["sc"])
            wv = self.w_ada.rearrange("(kc p) n -> p kc n", p=128)
            npieces = (c.NADA * c.D) // 512
            for pi in range(npieces):
                b = pi % 2
                eng = "sp" if b == 0 else "pool"
                self.dma(eng, wbuf[b][:], wv[:, :, pi * 512:(pi + 1) * 512], [], ["wada%d" % b], wsem[b])
                for fb in range(4):
                    col = pi * 4 + fb
                    for kc in range(16):
                        kb.op("pe", lambda e, b=b, fb=fb, kc=kc, col=col: e.matmul(
                            mps[:, col:col + 1], lhsT=wbuf[b][:, kc, fb * 128:(fb + 1) * 128], rhs=sc[:, kc:kc + 1],
                            start=(kc == 0), stop=(kc == 15)),
                            reads=["wada%d" % b, "sc"], writes=["mod_ps"])
            kb.op("dve", lambda e: e.tensor_tensor(out=P["modT"][:], in0=mps[:], in1=bT[:], op=ALU.add),
                  reads=["mod_ps", "bT"], writes=["modT"])
            kb.op("dve", lambda e: e.scalar_tensor_tensor(out=P["gmodA"][:], in0=P["modT"][:, 16:32], scalar=1.0,
                                                          in1=P["gvec"][:, 0:16], op0=ALU.add, op1=ALU.mult),
                  reads=["modT", "gvec"], writes=["gmodA"])
            kb.op("dve", lambda e: e.scalar_tensor_tensor(out=P["gmodF"][:], in0=P["modT"][:, 64:80], scalar=1.0,
                                                          in1=P["gvec"][:, 16:32], op0=ALU.add, op1=ALU.mult),
                  reads=["modT", "gvec"], writes=["gmodF"])
            if c.debug:
                self.dma("sp", self.MODT, P["modT"][:], ["modT"], [], kb.dsem("dbg"))
            kb.barrier()
            kb.release_dsems(wsem + cs)

    def bcast_tile(self, st, ph, name, srcT, key):
        kb, P = self.kb, self.P
        dst = self.sb(st, name, [128, 2048], F32)
        dg = self.sb(ph, name + "_dg", [128, 128], F32)
        for ci in range(16):
            pst_full, pkk = self.pbank()
            pst = pst_full[:, 0:128]
            kb.op("dve", lambda e, ci=ci: e.tensor_scalar(out=dg[:], in0=P["ident_f"][:], scalar1=srcT[:, ci:ci + 1],
                                                         scalar2=None, op0=ALU.mult),
                  reads=["ident_f", key], writes=[name + "_dg"])
            kb.op("pe", lambda e, pst=pst: e.matmul(pst, lhsT=P["ones_f"][:], rhs=dg[:], start=True, stop=True),
                  reads=["ones_f", name + "_dg"], writes=[pkk])
            kb.op("act", lambda e, ci=ci, pst=pst: e.activation(out=dst[:, ci * 128:(ci + 1) * 128], in_=pst, func=AF.Copy),
                  reads=[pkk], writes=[name])
        return dst


def _T128(v):
    v = np.asarray(v).reshape(-1, 128)
    return np.ascontiguousarray(v.T)


def lnmult_table():
    tab = np.full((17, 128, 128), -1e30, np.float32)
    k = np.arange(128)[:, None]
    q = np.arange(128)[None, :]
    for dl in range(17):
        diff = 128 * dl + q - k
        cnt = np.zeros((128, 128), np.int32)
        for w, d in Cfg.WINDOWS:
            cnt += ((diff >= 0) & (diff <= w) & (diff % d == 0)).astype(np.int32)
        tab[dl] = np.where(cnt > 0, np.log(np.maximum(cnt, 1)).astype(np.float32), np.float32(-1e30))
    return np.ascontiguousarray(tab.transpose(1, 0, 2))


def prepare_shared(inp, cfg):
    c = cfg
    sh = {}
    sh["cT"] = _T128(inp["c"][0])
    sh["w_ada"] = np.ascontiguousarray(inp["w_ada"][0])
    sh["b_adaT"] = _T128(inp["b_ada"][0])
    sh["gvecT"] = np.concatenate([_T128(inp["norm_attn_g"][0]), _T128(inp["norm_ffn_g"][0]),
                                  _T128(inp["final_norm_g"])], axis=1)
    w_in = inp["w_in"][0]
    rope = w_in[:, 3840:3904]
    sh["w_in"] = np.concatenate([w_in, rope[:, 32:64], rope[:, 0:32]], axis=1)
    sh["gsmallT"] = np.concatenate([_T128(inp["g_q"][0]), _T128(inp["g_kv"][0]),
                                    _T128(inp["g_out_swa"][0]), _T128(inp["g_out_mla"][0])], axis=1)
    wq = inp["w_uq"][0].reshape(c.QL, c.NH, 192)
    sh["w_uq"] = np.concatenate([wq[:, :, 0:128].reshape(c.QL, -1), wq[:, :, 128:192].reshape(c.QL, -1),
                                 np.concatenate([wq[:, :, 160:192], wq[:, :, 128:160]], axis=2).reshape(c.QL, -1)],
                                axis=1)
    wkv = inp["w_ukv"][0].reshape(c.KVL, c.NH, 256)
    sh["w_ukv"] = np.concatenate([wkv[:, :, 0:128].reshape(c.KVL, -1), wkv[:, :, 128:256].reshape(c.KVL, -1)], axis=1)
    sh["w_o"] = np.ascontiguousarray(inp["w_o"][0])
    sh["w_router"] = np.ascontiguousarray(inp["w_router"][0])
    sh["rbias"] = np.ascontiguousarray(inp["router_bias"][0][None, :])
    sh["w_eg"] = inp["w_exp_gate"][0]
    sh["w_eu"] = inp["w_exp_up"][0]
    sh["w_ed"] = inp["w_exp_down"][0]
    sh["w_sg"] = inp["w_sh_gate"][0]
    sh["w_su"] = inp["w_sh_up"][0]
    sh["w_sd"] = inp["w_sh_down"][0]
    sh["ident"] = np.eye(128, dtype=np.float32)
    half = c.ROPE // 2
    invf = (np.float32(10000.0) ** (-np.arange(half, dtype=np.float32) / np.float32(half))).astype(np.float32)
    sh["invfreq2"] = np.concatenate([invf, invf])[:, None].astype(np.float32)
    sh["lnmult"] = lnmult_table()
    sh["tri"] = (np.arange(128)[:, None] <= np.arange(128)[None, :]).astype(np.float32)
    sh["lstrict"] = (np.arange(128)[:, None] < np.arange(128)[None, :]).astype(np.float32)
    sh["iota64"] = np.ascontiguousarray(np.broadcast_to(np.arange(c.NE, dtype=np.float32)[None, :], (128, c.NE)))
    return sh


def slot_order(core, nchunks, nslot):
    order = [core - s for s in range(core + 1)]
    rest = [j for j in range(nchunks) if j > core]
    order = order + rest
    order = order[:nslot] + [-1] * max(0, nslot - len(order))
    valid = [1.0 if s <= core and order[s] >= 0 else 0.0 for s in range(nslot)]
    return order, valid


def prepare_core(inp, cfg, core, nchunks):
    c = cfg
    x = inp["x"][0]
    pos = inp["positions"][0]
    order, valid = slot_order(core, nchunks, c.NSLOT)
    xs, ps = [], []
    for s, j in enumerate(order):
        if j >= 0:
            xs.append(x[j * c.CH:(j + 1) * c.CH])
            ps.append(pos[j * c.CH:(j + 1) * c.CH])
        else:
            xs.append(x[0:c.CH])
            ps.append(pos[0:c.CH])
    d = {}
    d["xk"] = np.concatenate(xs, axis=0)
    d["posk"] = np.concatenate(ps)[None, :].astype(np.int32)
    d["flags"] = np.repeat(np.asarray(valid, np.float32), 8)[None, :]
    return d


class Phases2(Phases):
    def alloc_psum(self, ph, nf32=6, nbf=2):
        self.pbanks = [self.ps(ph, "pb%d" % i, [128, 512], F32) for i in range(nf32)]
        self.pbi = 0
        self.tbanks = [self.ps(ph, "tb%d" % i, [128, 8, 128], BF16) for i in range(nbf)]
        self.tbi = 0

    def pbank(self):
        i = self.pbi % len(self.pbanks)
        self.pbi += 1
        return self.pbanks[i], "pb%d" % i

    def tbank(self):
        i = self.tbi % len(self.tbanks)
        self.tbi += 1
        return self.tbanks[i], "tb%d" % i

    def alloc_normT(self, ph, pref, Fmax, nslots=2):
        W = {"pref": pref, "n": nslots, "i": 0}
        W["junk"] = self.sb(ph, pref + "_junk", [128, Fmax], BF16)
        W["xn"] = [self.sb(ph, pref + "_xn%d" % i, [128, Fmax], BF16) for i in range(nslots)]
        W["st"] = [self.sb(ph, pref + "_st%d" % i, [128, 4], F32) for i in range(nslots)]
        return W

    def norm_T(self, W, src, src_keys, F, gT, shiftT, gkeys, dst_of, dst_keys, eps_scale=None):
        kb, P, c = self.kb, self.P, self.cfg
        i = W["i"] % W["n"]
        W["i"] += 1
        pref = W["pref"]
        junk, xn, stt = W["junk"], W["xn"][i], W["st"][i]
        kj, kx, ks = pref + "_junk", pref + "_xn%d" % i, pref + "_st%d" % i
        kb.op("act", lambda e: e.activation(out=junk[:, 0:F], in_=src, func=AF.Square, accum_out=stt[:, 0:1]),
              reads=src_keys, writes=[kj, ks])
        kb.op("dve", lambda e: e.tensor_scalar(out=stt[:, 1:2], in0=stt[:, 0:1], scalar1=1.0 / F, scalar2=c.EPS,
                                               op0=ALU.mult, op1=ALU.add), reads=[ks], writes=[ks])
        kb.op("act", lambda e: e.activation(out=stt[:, 2:3], in_=stt[:, 1:2], func=AF.Sqrt), reads=[ks], writes=[ks])
        kb.op("dve", lambda e: e.reciprocal(out=stt[:, 3:4], in_=stt[:, 2:3]), reads=[ks], writes=[ks])
        kb.op("act", lambda e: e.activation(out=xn[:, 0:F], in_=src, func=AF.Copy, scale=stt[:, 3:4]),
              reads=list(src_keys) + [ks], writes=[kx])
        nchunk = F // 128
        for c0 in range(0, nchunk, 8):
            tb, tk = self.tbank()
            n = min(8, nchunk - c0)
            for j in range(n):
                ci = c0 + j
                kb.op("pe", lambda e, j=j, ci=ci: e.transpose(out=tb[:, j, :], in_=xn[:, ci * 128:(ci + 1) * 128],
                                                              identity=P["ident_b"][:]),
                      reads=[kx, "ident_b"], writes=[tk])
            for j in range(n):
                ci = c0 + j
                if shiftT is not None:
                    kb.op("dve", lambda e, j=j, ci=ci: e.tensor_scalar(
                        out=dst_of(ci), in0=tb[:, j, :], scalar1=gT[:, ci:ci + 1], scalar2=shiftT[:, ci:ci + 1],
                        op0=ALU.mult, op1=ALU.add), reads=[tk] + gkeys, writes=dst_keys)
                else:
                    kb.op("dve", lambda e, j=j, ci=ci: e.tensor_scalar(
                        out=dst_of(ci), in0=tb[:, j, :], scalar1=gT[:, ci:ci + 1], scalar2=None,
                        op0=ALU.mult), reads=[tk] + gkeys, writes=dst_keys)

    def alloc_rope(self, ph, pref):
        R = {"pref": pref}
        for n in ["posi"]:
            R[n] = self.sb(ph, pref + n, [64, 512], I32)
        for n in ["ang", "t", "kk", "r", "r2", "sin", "cos"]:
            R[n] = self.sb(ph, pref + n, [64, 512], F32)
        R["sem"] = self.kb.dsem(pref)
        return R

    def rope_tables(self, R, pos_ap):
        kb, P = self.kb, self.P
        p = R["pref"]
        K = lambda n: p + n
        self.dma("sp", R["posi"][:], pos_ap.partition_broadcast(64), [], [K("posi")], R["sem"])
        kb.op("dve", lambda e: e.tensor_copy(out=R["ang"][:], in_=R["posi"][:]), reads=[K("posi")], writes=[K("ang")])
        kb.op("dve", lambda e: e.tensor_scalar(out=R["ang"][:], in0=R["ang"][:], scalar1=P["invf"][:, 0:1], scalar2=None,
                                               op0=ALU.mult), reads=["invf"], writes=[K("ang")])
        kb.op("dve", lambda e: e.tensor_scalar(out=R["t"][:], in0=R["ang"][:], scalar1=float(1.0 / TWO_PI), scalar2=MAGIC,
                                               op0=ALU.mult, op1=ALU.add), reads=[K("ang")], writes=[K("t")])
        kb.op("dve", lambda e: e.tensor_scalar(out=R["kk"][:], in0=R["t"][:], scalar1=-MAGIC, scalar2=None,
                                               op0=ALU.add), reads=[K("t")], writes=[K("kk")])
        kb.op("dve", lambda e: e.scalar_tensor_tensor(out=R["r"][:], in0=R["kk"][:], scalar=-CW1, in1=R["ang"][:],
                                                      op0=ALU.mult, op1=ALU.add), reads=[K("ang"), K("kk")], writes=[K("r")])
        for cw in (CW2, CW3):
            kb.op("dve", lambda e, cw=cw: e.scalar_tensor_tensor(out=R["r"][:], in0=R["kk"][:], scalar=-cw, in1=R["r"][:],
                                                                 op0=ALU.mult, op1=ALU.add), reads=[K("kk")], writes=[K("r")])
        kb.op("dve", lambda e: e.tensor_scalar(out=R["t"][:], in0=R["r"][:], scalar1=float(np.pi / 2), scalar2=None,
                                               op0=ALU.is_gt), reads=[K("r")], writes=[K("t")])
        kb.op("dve", lambda e: e.scalar_tensor_tensor(out=R["r2"][:], in0=R["t"][:], scalar=-float(TWO_PI), in1=R["r"][:],
                                                      op0=ALU.mult, op1=ALU.add), reads=[K("t"), K("r")], writes=[K("r2")])
        kb.op("dve", lambda e: e.tensor_scalar(out=R["r2"][:], in0=R["r2"][:], scalar1=float(np.pi / 2), scalar2=None,
                                               op0=ALU.add), reads=[], writes=[K("r2")])
        lim = 3.1415925
        for n in ["r", "r2"]:
            kb.op("dve", lambda e, n=n: e.tensor_scalar(out=R[n][:], in0=R[n][:], scalar1=lim, scalar2=-lim,
                                                        op0=ALU.min, op1=ALU.max), reads=[], writes=[K(n)])
        kb.op("act", lambda e: e.activation(out=R["sin"][:], in_=R["r"][:], func=AF.Sin), reads=[K("r")], writes=[K("sin")])
        kb.op("act", lambda e: e.activation(out=R["cos"][:], in_=R["r2"][:], func=AF.Sin), reads=[K("r2")], writes=[K("cos")])
        kb.op("dve", lambda e: e.tensor_scalar(out=R["sin"][:], in0=R["sin"][:], scalar1=P["sgn"][:, 0:1], scalar2=None,
                                               op0=ALU.mult), reads=["sgn0", "sgn1"], writes=[K("sin")])

    def apply_rope(self, R, a_ps, b_ps, ps_keys, out_ap, out_keys, tmp, tmpk):
        kb = self.kb
        p = R["pref"]
        kb.op("dve", lambda e: e.tensor_tensor(out=tmp[0][:], in0=a_ps, in1=R["cos"][:], op=ALU.mult),
              reads=ps_keys + [p + "cos"], writes=[tmpk + "0"])
        kb.op("dve", lambda e: e.tensor_tensor(out=tmp[1][:], in0=b_ps, in1=R["sin"][:], op=ALU.mult),
              reads=ps_keys + [p + "sin"], writes=[tmpk + "1"])
        kb.op("pool", lambda e: e.tensor_tensor(out=out_ap, in0=tmp[0][:], in1=tmp[1][:], op=ALU.add),
              reads=[tmpk + "0", tmpk + "1"], writes=out_keys)


class Phases3(Phases2):
    def phase1a(self):
        c, kb, P = self.cfg, self.kb, self.P
        NG = c.NSLOT * c.CH // 512
        gps = c.CH // 512
        with contextlib.ExitStack() as ph:
            self.alloc_psum(ph, 6, 2)
            wkv = self.sb(ph, "wkv", [128, 16, 384], BF16)
            wukv = self.sb(ph, "wukv", [128, 2, 2048], BF16)
            ws = kb.dsem("w1a")
            self.dma("pool", wkv[:], self.w_in[:, 3584:3968].rearrange("(kc p) n -> p kc n", p=128), [], ["wkv"], ws)
            ws2 = kb.dsem("w1a2")
            self.dma("pool", wukv[:], self.w_ukv.rearrange("(kc p) n -> p kc n", p=128), [], ["wukv"], ws2)
            NX = 3
            xt = [self.sb(ph, "xt%d" % i, [128, 2048], F32) for i in range(NX)]
            xs = [kb.dsem("xt%d" % i) for i in range(NX)]
            WN = self.alloc_normT(ph, "n1", 2048, 2)
            WNb = self.alloc_normT(ph, "n1b", 256, 2)
            hT = [self.sb(ph, "hT%d" % i, [128, 16, 512], BF16) for i in range(2)]
            hs = [kb.dsem("hT%d" % i) for i in range(2)]
            ckvnT = [self.sb(ph, "ckvnT%d" % i, [128, 2, 512], BF16) for i in range(2)]
            kT_st = [self.sb(ph, "kTst%d" % i, [128, 8, 512], BF16) for i in range(2)]
            kTs = [kb.dsem("kTst%d" % i) for i in range(2)]
            kb_st = [self.sb(ph, "kbst%d" % i, [65, 512], BF16) for i in range(2)]
            kbs = [kb.dsem("kbst%d" % i) for i in range(2)]
            va_st = [self.sb(ph, "vast%d" % i, [128, 4, 8 * 129], BF16) for i in range(2)]
            vas = [kb.dsem("vast%d" % i) for i in range(2)]
            sq = self.sb(ph, "sq1a", [128, 8, 512], BF16)
            sqr = self.sb(ph, "sqr1a", [64, 512], BF16)
            sqmax = self.sb(ph, "sqmax1a", [128, 512], BF16)
            kmrun = self.sb(ph, "kmrun", [1, 512], F32)
            tmp = [self.sb(ph, "rtmp%d" % i, [64, 512], F32) for i in range(2)]
            R = self.alloc_rope(ph, "r1a")
            kb.op("dve", lambda e: e.memset(kmrun[:], 0.0), writes=["kmrun"])
            for i in range(2):
                kb.op("dve", lambda e, i=i: e.memset(kb_st[i][64:65, :], 1.0), writes=["kbst%d_one" % i])
            xv = self.xk.rearrange("(n p) d -> n p d", p=128)
            cnt1a = [0]

            def stageA(g):
                slot = g // gps
                tok0 = g * 512
                b = g % 2
                for t in range(4):
                    xi = cnt1a[0] % NX
                    cnt1a[0] += 1
                    self.dma("sp", xt[xi][:], xv[g * 4 + t], [], ["xt%d" % xi], xs[xi])
                    self.norm_T(WN, xt[xi][:], ["xt%d" % xi], 2048, P["gmodA"], P["modT"], ["gmodA", "modT"],
                                lambda ci, t=t, b=b: hT[b][:, ci, t * 128:(t + 1) * 128], ["hT%d" % b])
                if slot < 2:
                    self.dma("pool", self.HT[slot, :, :, (g % gps) * 512:(g % gps + 1) * 512], hT[b][:], ["hT%d" % b], [], hs[b])

            def stageB(g):
                slot = g // gps
                tok0 = g * 512
                b = g % 2
                self.rope_tables(R, self.posk[:, tok0:tok0 + 512])
                for t in range(4):
                    pb, pk = self.pbank()
                    for ci in range(16):
                        kb.op("pe", lambda e, ci=ci, t=t, pb=pb: e.matmul(
                            pb[:, 0:256], lhsT=hT[b][:, ci, t * 128:(t + 1) * 128], rhs=wkv[:, ci, 0:256],
                            start=(ci == 0), stop=(ci == 15)), reads=["hT%d" % b, "wkv"], writes=[pk])
                    self.norm_T(WNb, pb[:, 0:256], [pk], 256, P["gsm"][:, 4:6], None, ["gsm"],
                                lambda ci, t=t, b=b: ckvnT[b][:, ci, t * 128:(t + 1) * 128], ["ckvnT%d" % b])
                pa, pak = self.pbank()
                pbb, pbk = self.pbank()
                for (pp, ppk, c0) in ((pa, pak, 256), (pbb, pbk, 320)):
                    for ci in range(16):
                        kb.op("pe", lambda e, ci=ci, pp=pp, c0=c0: e.matmul(
                            pp[0:64, :], lhsT=wkv[:, ci, c0:c0 + 64], rhs=hT[b][:, ci, :],
                            start=(ci == 0), stop=(ci == 15)), reads=["hT%d" % b, "wkv"], writes=[ppk])
                self.apply_rope(R, pa[0:64, :], pbb[0:64, :], [pak, pbk], kb_st[b][0:64, :], ["kbst%d" % b], tmp, "rtmp")
                kb.op("act", lambda e: e.activation(out=sqr[:], in_=kb_st[b][0:64, :], func=AF.Square),
                      reads=["kbst%d" % b], writes=["sqr1a"])
                for h in range(c.NH):
                    pb, pk = self.pbank()
                    for kc in range(2):
                        kb.op("pe", lambda e, kc=kc, h=h, pb=pb: e.matmul(
                            pb[:], lhsT=wukv[:, kc, h * 128:(h + 1) * 128], rhs=ckvnT[b][:, kc, :],
                            start=(kc == 0), stop=(kc == 1)), reads=["ckvnT%d" % b, "wukv"], writes=[pk])
                    kb.op("act", lambda e, h=h, pb=pb: e.activation(out=kT_st[b][:, h, :], in_=pb[:], func=AF.Copy),
                          reads=[pk], writes=["kTst%d" % b])
                    kb.op("act", lambda e, h=h, pb=pb: e.activation(out=sq[:, h, :], in_=pb[:], func=AF.Square),
                          reads=[pk], writes=["sq1a"])
                kb.op("dve", lambda e: e.tensor_reduce(out=sqmax[:], in_=sq[:].rearrange("p h t -> p t h"),
                                                       axis=AX.X, op=ALU.max), reads=["sq1a"], writes=["sqmax1a"])
                pu, puk = self.pbank()
                kb.op("pe", lambda e: e.matmul(pu[0:1, :], lhsT=P["ones_b"][:, 0:1], rhs=sqmax[:], start=True, stop=False),
                      reads=["ones_b", "sqmax1a"], writes=[puk])
                kb.op("pe", lambda e: e.matmul(pu[0:1, :], lhsT=P["ones_b"][0:64, 0:1], rhs=sqr[:], start=False, stop=True),
                      reads=["ones_b", "sqr1a"], writes=[puk])
                kb.op("dve", lambda e: e.tensor_tensor(out=kmrun[:], in0=pu[0:1, :], in1=kmrun[:], op=ALU.max),
                      reads=[puk], writes=["kmrun"])
                for t in range(4):
                    for half in range(2):
                        pb, pk = self.pbank()
                        for kc in range(2):
                            kb.op("pe", lambda e, kc=kc, t=t, half=half, pb=pb: e.matmul(
                                pb[:], lhsT=ckvnT[b][:, kc, t * 128:(t + 1) * 128],
                                rhs=wukv[:, kc, 1024 + half * 512:1024 + (half + 1) * 512],
                                start=(kc == 0), stop=(kc == 1)), reads=["ckvnT%d" % b, "wukv"], writes=[pk])
                        dstv = va_st[b][:, t, :].rearrange("p (h f) -> p h f", f=129)[:, half * 4:(half + 1) * 4, 0:128]
                        kb.op("dve", lambda e, pb=pb, dstv=dstv: e.tensor_scalar(
                            out=dstv, in0=pb[:].rearrange("p (h f) -> p h f", f=128),
                            scalar1=P["flag8"][:, slot * 8:slot * 8 + 1], scalar2=None, op0=ALU.mult),
                            reads=[pk, "flag8"], writes=["vast%d" % b])
                    onec = va_st[b][:, t, :].rearrange("p (h f) -> p h f", f=129)[:, :, 128:129]
                    kb.op("pool", lambda e, onec=onec: e.tensor_copy(
                        out=onec, in_=P["flag8"][:, slot * 8:(slot + 1) * 8].unsqueeze(2)),
                        reads=["flag8"], writes=["vast%d" % b])
                self.dma("pool", self.KT_A[:, :, tok0:tok0 + 512].rearrange("h p t -> p h t"), kT_st[b][:],
                         ["kTst%d" % b], [], kTs[b])
                self.dma("pool", self.KT_B[:, tok0:tok0 + 512], kb_st[b][:], ["kbst%d" % b, "kbst%d_one" % b], [], kbs[b])
                self.dma("pool", self.V_AUG[tok0:tok0 + 512, :].rearrange("(t p) f -> p t f", p=128), va_st[b][:],
                         ["vast%d" % b], [], vas[b])

            stageA(0)
            for g in range(NG):
                if g + 1 < NG:
                    stageA(g + 1)
                stageB(g)
            kb.op("dve", lambda e: e.tensor_reduce(out=P["kmax2"][0:1, 0:1], in_=kmrun[:], axis=AX.X, op=ALU.max),
                  reads=["kmrun"], writes=["kmax2"])
            kb.barrier()
            kb.release_dsems([ws, ws2] + xs + hs + kTs + kbs + vas + [R["sem"]])


class Phases4(Phases3):
    def phase1b(self):
        c, kb, P = self.cfg, self.kb, self.P
        gps = c.CH // 512
        with contextlib.ExitStack() as ph:
            self.alloc_psum(ph, 6, 2)
            wkva = self.sb(ph, "wkva", [128, 16, 2048], BF16)
            ws = [kb.dsem("w1b%d" % i) for i in range(2)]
            for i in range(2):
                self.dma("pool", wkva[:, :, i * 1024:(i + 1) * 1024],
                         self.w_in[:, 1024 + i * 1024:2048 + i * 1024].rearrange("(kc p) n -> p kc n", p=128),
                         [], ["wkva%d" % i], ws[i])
            hT = [self.sb(ph, "hTb%d" % i, [128, 16, 512], BF16) for i in range(2)]
            hs = [kb.dsem("hTb%d" % i) for i in range(2)]
            kT_st = [self.sb(ph, "kaTst%d" % i, [128, 8, 512], BF16) for i in range(2)]
            kTs = [kb.dsem("kaTst%d" % i) for i in range(2)]
            va_st = [self.sb(ph, "vastb%d" % i, [128, 4, 8 * 129], BF16) for i in range(2)]
            vas = [kb.dsem("vastb%d" % i) for i in range(2)]
            sq = self.sb(ph, "sq1b", [128, 8, 512], BF16)
            sqmax = self.sb(ph, "sqmax1b", [128, 512], BF16)
            kmrun = self.sb(ph, "kmrunb", [1, 512], F32)
            kb.op("dve", lambda e: e.memset(kmrun[:], 0.0), writes=["kmrunb"])
            for g in range(2 * gps):
                slot = g // gps
                b = g % 2
                tokd = (1 - slot) * c.CH + (g % gps) * 512
                self.dma("sp", hT[b][:], self.HT[slot, :, :, (g % gps) * 512:(g % gps + 1) * 512], [], ["hTb%d" % b], hs[b])
                for h in range(c.NH):
                    pb, pk = self.pbank()
                    for ci in range(16):
                        kb.op("pe", lambda e, ci=ci, h=h, pb=pb: e.matmul(
                            pb[:], lhsT=wkva[:, ci, h * 128:(h + 1) * 128], rhs=hT[b][:, ci, :],
                            start=(ci == 0), stop=(ci == 15)), reads=["hTb%d" % b, "wkva0"], writes=[pk])
                    kb.op("act", lambda e, h=h, pb=pb: e.activation(out=kT_st[b][:, h, :], in_=pb[:], func=AF.Copy),
                          reads=[pk], writes=["kaTst%d" % b])
                    kb.op("act", lambda e, h=h, pb=pb: e.activation(out=sq[:, h, :], in_=pb[:], func=AF.Square),
                          reads=[pk], writes=["sq1b"])
                kb.op("dve", lambda e: e.tensor_reduce(out=sqmax[:], in_=sq[:].rearrange("p h t -> p t h"),
                                                       axis=AX.X, op=ALU.max), reads=["sq1b"], writes=["sqmax1b"])
                pu, puk = self.pbank()
                kb.op("pe", lambda e: e.matmul(pu[0:1, :], lhsT=P["ones_b"][:, 0:1], rhs=sqmax[:], start=True, stop=True),
                      reads=["ones_b", "sqmax1b"], writes=[puk])
                kb.op("dve", lambda e: e.tensor_tensor(out=kmrun[:], in0=pu[0:1, :], in1=kmrun[:], op=ALU.max),
                      reads=[puk], writes=["kmrunb"])
                for t in range(4):
                    for half in range(2):
                        pb, pk = self.pbank()
                        for ci in range(16):
                            kb.op("pe", lambda e, ci=ci, t=t, half=half, pb=pb: e.matmul(
                                pb[:], lhsT=hT[b][:, ci, t * 128:(t + 1) * 128],
                                rhs=wkva[:, ci, 1024 + half * 512:1024 + (half + 1) * 512],
                                start=(ci == 0), stop=(ci == 15)), reads=["hTb%d" % b, "wkva1"], writes=[pk])
                        dstv = va_st[b][:, t, :].rearrange("p (h f) -> p h f", f=129)[:, half * 4:(half + 1) * 4, 0:128]
                        kb.op("dve", lambda e, pb=pb, dstv=dstv: e.tensor_scalar(
                            out=dstv, in0=pb[:].rearrange("p (h f) -> p h f", f=128),
                            scalar1=P["flag8"][:, slot * 8:slot * 8 + 1], scalar2=None, op0=ALU.mult),
                            reads=[pk, "flag8"], writes=["vastb%d" % b])
                    onec = va_st[b][:, t, :].rearrange("p (h f) -> p h f", f=129)[:, :, 128:129]
                    kb.op("pool", lambda e, onec=onec: e.tensor_copy(
                        out=onec, in_=P["flag8"][:, slot * 8:(slot + 1) * 8].unsqueeze(2)),
                        reads=["flag8"], writes=["vastb%d" % b])
                self.dma("pool", self.KAT[:, :, tokd:tokd + 512].rearrange("h p t -> p h t"), kT_st[b][:],
                         ["kaTst%d" % b], [], kTs[b])
                self.dma("pool", self.VA_AUG[tokd:tokd + 512, :].rearrange("(t p) f -> p t f", p=128), va_st[b][:],
                         ["vastb%d" % b], [], vas[b])
            kb.op("dve", lambda e: e.tensor_reduce(out=P["kmax2"][0:1, 1:2], in_=kmrun[:], axis=AX.X, op=ALU.max),
                  reads=["kmrunb"], writes=["kmax2"])
            kb.barrier()
            kb.release_dsems(ws + hs + kTs + vas)

    def negc_row(self, u_ps, upk, kcol, dst, dstk, tmpf):
        kb, P = self.kb, self.P
        kb.op("act", lambda e: e.activation(out=tmpf[:], in_=u_ps, func=AF.Sqrt, scale=P["kmax2"][0:1, kcol:kcol + 1]),
              reads=[upk, "kmax2"], writes=["negc_tmp"])
        kb.op("dve", lambda e: e.tensor_scalar(out=dst, in0=tmpf[:], scalar1=-1.0, scalar2=None, op0=ALU.mult),
              reads=["negc_tmp"], writes=[dstk])

    def phase1c(self):
        c, kb, P = self.cfg, self.kb, self.P
        gps = c.CH // 512
        with contextlib.ExitStack() as ph:
            self.alloc_psum(ph, 6, 2)
            wq = self.sb(ph, "wq", [128, 16, 1536], BF16)
            wuq = self.sb(ph, "wuq", [128, 4, 2048], BF16)
            ws = [kb.dsem("w1c%d" % i) for i in range(3)]
            self.dma("pool", wq[:, :, 0:1024], self.w_in[:, 0:1024].rearrange("(kc p) n -> p kc n", p=128), [], ["wq0"], ws[0])
            self.dma("pool", wq[:, :, 1024:1536], self.w_in[:, 3072:3584].rearrange("(kc p) n -> p kc n", p=128), [], ["wq1"], ws[1])
            self.dma("pool", wuq[:], self.w_uq.rearrange("(kc p) n -> p kc n", p=128), [], ["wuq"], ws[2])
            WN = self.alloc_normT(ph, "n1c", 512, 2)
            hT = [self.sb(ph, "hTc%d" % i, [128, 16, 512], BF16) for i in range(2)]
            hs = [kb.dsem("hTc%d" % i) for i in range(2)]
            cqnT = [self.sb(ph, "cqnT%d" % i, [128, 4, 512], BF16) for i in range(2)]
            qa_st = [self.sb(ph, "qast%d" % i, [128, 8, 512], BF16) for i in range(2)]
            qas = [kb.dsem("qast%d" % i) for i in range(2)]
            qn_st = [self.sb(ph, "qnst%d" % i, [128, 8, 512], BF16) for i in range(2)]
            qns = [kb.dsem("qnst%d" % i) for i in range(2)]
            qb_st = [self.sb(ph, "qbst%d" % i, [64, 8, 512], BF16) for i in range(2)]
            qbs = [kb.dsem("qbst%d" % i) for i in range(2)]
            nca_st = [self.sb(ph, "ncast0", [1, 8, 512], BF16)] * 2
            ncas = [kb.dsem("ncast0")] * 2
            ncb_st = [self.sb(ph, "ncbst0", [1, 8, 512], BF16)] * 2
            ncbs = [kb.dsem("ncbst0")] * 2
            sq = self.sb(ph, "sq1c", [128, 512], BF16)
            sqr = self.sb(ph, "sqr1c", [64, 512], BF16)
            tmpf = self.sb(ph, "negc_tmp", [1, 512], F32)
            tmp = [self.sb(ph, "rtmpc%d" % i, [64, 512], F32) for i in range(2)]
            R = self.alloc_rope(ph, "r1c")
            for g in range(gps):
                b = g % 2
                tok0 = g * 512
                self.rope_tables(R, self.posk[:, tok0:tok0 + 512])
                self.dma("sp", hT[b][:], self.HT[0, :, :, tok0:tok0 + 512], [], ["hTc%d" % b], hs[b])
                for h in range(c.NH):
                    pb, pk = self.pbank()
                    for ci in range(16):
                        kb.op("pe", lambda e, ci=ci, h=h, pb=pb: e.matmul(
                            pb[:], lhsT=wq[:, ci, h * 128:(h + 1) * 128], rhs=hT[b][:, ci, :],
                            start=(ci == 0), stop=(ci == 15)), reads=["hTc%d" % b, "wq0"], writes=[pk])
                    kb.op("act", lambda e, h=h, pb=pb: e.activation(out=qa_st[b][:, h, :], in_=pb[:], func=AF.Copy),
                          reads=[pk], writes=["qast%d" % b])
                    kb.op("act", lambda e, pb=pb: e.activation(out=sq[:], in_=pb[:], func=AF.Square),
                          reads=[pk], writes=["sq1c"])
                    pu, puk = self.pbank()
                    kb.op("pe", lambda e, pu=pu: e.matmul(pu[0:1, :], lhsT=P["ones_b"][:, 0:1], rhs=sq[:], start=True, stop=True),
                          reads=["ones_b", "sq1c"], writes=[puk])
                    self.negc_row(pu[0:1, :], puk, 1, nca_st[b][0:1, h, :], "ncast0", tmpf)
                for t in range(4):
                    pb, pk = self.pbank()
                    for ci in range(16):
                        kb.op("pe", lambda e, ci=ci, t=t, pb=pb: e.matmul(
                            pb[:], lhsT=hT[b][:, ci, t * 128:(t + 1) * 128], rhs=wq[:, ci, 1024:1536],
                            start=(ci == 0), stop=(ci == 15)), reads=["hTc%d" % b, "wq1"], writes=[pk])
                    self.norm_T(WN, pb[:], [pk], 512, P["gsm"][:, 0:4], None, ["gsm"],
                                lambda ci, t=t, b=b: cqnT[b][:, ci, t * 128:(t + 1) * 128], ["cqnT%d" % b])
                for h in range(c.NH):
                    pb, pk = self.pbank()
                    for kc in range(4):
                        kb.op("pe", lambda e, kc=kc, h=h, pb=pb: e.matmul(
                            pb[:], lhsT=wuq[:, kc, h * 128:(h + 1) * 128], rhs=cqnT[b][:, kc, :],
                            start=(kc == 0), stop=(kc == 3)), reads=["cqnT%d" % b, "wuq"], writes=[pk])
                    kb.op("act", lambda e, h=h, pb=pb: e.activation(out=qn_st[b][:, h, :], in_=pb[:], func=AF.Copy),
                          reads=[pk], writes=["qnst%d" % b])
                    kb.op("act", lambda e, pb=pb: e.activation(out=sq[:], in_=pb[:], func=AF.Square),
                          reads=[pk], writes=["sq1c"])
                    pa, pak = self.pbank()
                    pbb, pbk = self.pbank()
                    for (pp, ppk, c0) in ((pa, pak, 1024), (pbb, pbk, 1536)):
                        for kc in range(4):
                            kb.op("pe", lambda e, kc=kc, pp=pp, c0=c0, h=h: e.matmul(
                                pp[0:64, :], lhsT=wuq[:, kc, c0 + h * 64:c0 + (h + 1) * 64], rhs=cqnT[b][:, kc, :],
                                start=(kc == 0), stop=(kc == 3)), reads=["cqnT%d" % b, "wuq"], writes=[ppk])
                    self.apply_rope(R, pa[0:64, :], pbb[0:64, :], [pak, pbk], qb_st[b][:, h, :], ["qbst%d" % b], tmp, "rtmpc")
                    kb.op("act", lambda e, h=h: e.activation(out=sqr[:], in_=qb_st[b][:, h, :], func=AF.Square),
                          reads=["qbst%d" % b], writes=["sqr1c"])
                    pu, puk = self.pbank()
                    kb.op("pe", lambda e, pu=pu: e.matmul(pu[0:1, :], lhsT=P["ones_b"][:, 0:1], rhs=sq[:], start=True, stop=False),
                          reads=["ones_b", "sq1c"], writes=[puk])
                    kb.op("pe", lambda e, pu=pu: e.matmul(pu[0:1, :], lhsT=P["ones_b"][0:64, 0:1], rhs=sqr[:], start=False, stop=True),
                          reads=["ones_b", "sqr1c"], writes=[puk])
                    self.negc_row(pu[0:1, :], puk, 0, ncb_st[b][0:1, h, :], "ncbst0", tmpf)
                self.dma("pool", self.QAT[:, :, tok0:tok0 + 512].rearrange("h p t -> p h t"), qa_st[b][:], ["qast%d" % b], [], qas[b])
                self.dma("pool", self.NEGCA[:, tok0:tok0 + 512].rearrange("(o h) t -> o h t", o=1), nca_st[b][:], ["ncast0"], [], ncas[b])
                self.dma("pool", self.QT_A[:, :, tok0:tok0 + 512].rearrange("h p t -> p h t"), qn_st[b][:], ["qnst%d" % b], [], qns[b])
                self.dma("pool", self.QT_B[:, 0:64, tok0:tok0 + 512].rearrange("h p t -> p h t"), qb_st[b][:], ["qbst%d" % b], [], qbs[b])
                self.dma("pool", self.QT_B[:, 64:65, tok0:tok0 + 512].rearrange("h o t -> o h t"), ncb_st[b][:], ["ncbst0"], [], ncbs[b])
            kb.barrier()
            kb.release_dsems(ws + hs + qas + qns + qbs + [ncas[0], ncbs[0], R["sem"]])


class Phases5(Phases4):
    def phase2(self):
        c, kb, P = self.cfg, self.kb, self.P
        NQ = c.CH // 128
        slopes = _slopes(c.NH)
        scale = float(c.HD ** -0.5)
        with contextlib.ExitStack() as ph:
            stp = [self.ps(ph, "stp%d" % i, [128, 8, 128], F32) for i in range(2)]
            accp = [self.ps(ph, "accp%d" % i, [128, 3, 129], F32) for i in range(3)]
            self.tbanks = [self.ps(ph, "tbs", [128, 8, 128], BF16)]
            self.tbi = 0
            def acc_of(h):
                return accp[h // 3][:, h % 3, :], "accp%d" % (h // 3)
            kaT = self.sb(ph, "kaT", [128, 8, 2 * c.CH], BF16)
            VA = self.sb(ph, "VAr", [128, 2 * NQ, 8 * 129], BF16)
            sems = [kb.dsem("p2_%d" % i) for i in range(12)]
            self.dma("sp", kaT[:], self.KAT.rearrange("h p t -> p h t"), [], ["kaT"], sems[0])
            nva = 4
            per = 2 * NQ // nva
            for i in range(nva):
                self.dma("sp", VA[:, i * per:(i + 1) * per, :],
                         self.VA_AUG[i * per * 128:(i + 1) * per * 128, :].rearrange("(t p) f -> p t f", p=128),
                         [], ["VA%d" % i], sems[8 + i])
            posq_i = self.sb(ph, "posq_i", [128, c.CH], I32)
            posq = self.sb(ph, "posq", [128, c.CH], F32)
            posk_i = self.sb(ph, "posk_i", [128, 2 * NQ], I32)
            poskc = self.sb(ph, "poskc", [128, 2 * NQ], F32)
            lnm = self.sb(ph, "lnm", [128, 17, 128], F32)
            NS = self.sb(ph, "NS", [128, 8, 128], F32)
            self.dma("sp", posq_i[:], self.posk[:, 0:c.CH].partition_broadcast(128), [], ["posq_i"], sems[3])
            self.dma("sp", posk_i[:, 0:NQ], self.posk[:, c.CH:2 * c.CH].rearrange("o (t p) -> p (o t)", p=128),
                     [], ["posk_i0"], sems[4], allow_slow_non_contiguous=True)
            self.dma("sp", posk_i[:, NQ:2 * NQ], self.posk[:, 0:c.CH].rearrange("o (t p) -> p (o t)", p=128),
                     [], ["posk_i1"], sems[5], allow_slow_non_contiguous=True)
            self.dma("sp", lnm[:], self.lnmult, [], ["lnm"], sems[6])
            kb.op("dve", lambda e: e.tensor_copy(out=posq[:], in_=posq_i[:]), reads=["posq_i"], writes=["posq"])
            kb.op("dve", lambda e: e.tensor_copy(out=poskc[:], in_=posk_i[:]), reads=["posk_i0", "posk_i1"], writes=["poskc"])
            kb.op("dve", lambda e: e.tensor_scalar(out=poskc[:], in0=poskc[:], scalar1=-1.0, scalar2=None, op0=ALU.mult),
                  reads=[], writes=["poskc"])
            for h in range(c.NH):
                kb.op("dve", lambda e, h=h: e.memset(NS[:, h, :], -slopes[h]), writes=["NS"])
            qa = [self.sb(ph, "qa%d" % i, [128, 8, 128], BF16) for i in range(2)]
            qsem = [kb.dsem("qa%d" % i) for i in range(2)]
            ngc = [self.sb(ph, "ngc%d" % i, [1, 8, 128], BF16) for i in range(2)]
            nsem = [kb.dsem("ngc%d" % i) for i in range(2)]
            dist = [self.sb(ph, "dist%d" % i, [128, 128], F32) for i in range(2)]
            u = [self.sb(ph, "u%d" % i, [128, 8, 128], F32) for i in range(2)]
            sp_ = [self.sb(ph, "sp%d" % i, [128, 8, 128], F32) for i in range(2)]
            pT = [self.sb(ph, "pT%d" % i, [128, 8, 128], BF16) for i in range(2)]
            oa = self.sb(ph, "oa", [128, 1024], F32)
            rl = self.sb(ph, "rl", [128, 8], F32)
            WN = self.alloc_normT(ph, "n2", 1024, 1)
            mx = [self.sb(ph, "mxa%d" % i, [128, 8, 128], BF16) for i in range(2)]
            msem = [kb.dsem("mxa%d" % i) for i in range(2)]
            seq = [(b, dl) for b in range(NQ) for dl in range(16, -1, -1)]

            def front(n):
                b, dl = seq[n]
                qb = b % 2
                k = n % 2
                if dl == 16:
                    self.dma("sp", qa[qb][:], self.QAT[:, :, b * 128:(b + 1) * 128].rearrange("h p t -> p h t"), [], ["qa%d" % qb], qsem[qb])
                    self.dma("sp", ngc[qb][:], self.NEGCA[:, b * 128:(b + 1) * 128].rearrange("(o h) t -> o h t", o=1), [], ["ngc%d" % qb], nsem[qb])
                j = NQ + b - dl
                kb.op("act", lambda e: e.activation(
                    out=dist[k][:], in_=posq[:, b * 128:(b + 1) * 128], func=AF.Abs, bias=poskc[:, j:j + 1], scale=1.0),
                    reads=["posq", "poskc"], writes=["dist%d" % k])
                kb.op("pool", lambda e: e.tensor_tensor(
                    out=u[k][:], in0=dist[k][:].unsqueeze(1).broadcast_to([128, 8, 128]), in1=NS[:], op=ALU.mult),
                    reads=["dist%d" % k, "NS"], writes=["u%d" % k])
                kb.op("pool", lambda e: e.tensor_tensor(
                    out=u[k][:], in0=u[k][:], in1=lnm[:, dl, :].unsqueeze(1).broadcast_to([128, 8, 128]), op=ALU.add),
                    reads=["lnm"], writes=["u%d" % k])
                for h in range(c.NH):
                    kb.op("pe", lambda e, h=h: e.matmul(
                        stp[k][:, h, :], lhsT=kaT[:, h, j * 128:(j + 1) * 128], rhs=qa[qb][:, h, :], start=True, stop=False),
                        reads=["kaT", "qa%d" % qb], writes=["stp%d" % k])
                    kb.op("pe", lambda e, h=h: e.matmul(
                        stp[k][:, h, :], lhsT=P["ones_b"][0:1, :], rhs=ngc[qb][0:1, h, :], start=False, stop=True),
                        reads=["ones_b", "ngc%d" % qb], writes=["stp%d" % k])
                kb.op("dve", lambda e: e.scalar_tensor_tensor(
                    out=sp_[k][:], in0=stp[k][:], scalar=scale, in1=u[k][:], op0=ALU.mult, op1=ALU.add),
                    reads=["stp%d" % k, "u%d" % k], writes=["sp%d" % k])
                kb.op("act", lambda e: e.activation(out=pT[k][:], in_=sp_[k][:], func=AF.Exp),
                      reads=["sp%d" % k], writes=["pT%d" % k])

            def back(n):
                b, dl = seq[n]
                k = n % 2
                j = NQ + b - dl
                vak = "VA%d" % (j // per)
                if dl == 16:
                    for i3 in range(3):
                        kb.op("dve", lambda e, i3=i3: e.memset(accp[i3][:], 0.0), writes=["accp%d" % i3])
                for h in range(c.NH):
                    ah, ak = acc_of(h)
                    kb.op("pe", lambda e, h=h, ah=ah: e.matmul(
                        ah, lhsT=pT[k][:, h, :], rhs=VA[:, j, h * 129:(h + 1) * 129], start=False, stop=(dl == 0)),
                        reads=["pT%d" % k, vak], writes=[ak])
                if dl != 0:
                    return
                for i3 in range(3):
                    nh = 3 if i3 < 2 else 2
                    kb.op("dve", lambda e, i3=i3, nh=nh: e.reciprocal(
                        out=rl[:, i3 * 3:i3 * 3 + nh].unsqueeze(2), in_=accp[i3][:, 0:nh, 128:129]),
                        reads=["accp%d" % i3], writes=["rl"])
                for h in range(c.NH):
                    ah, ak = acc_of(h)
                    kb.op("dve", lambda e, h=h, ah=ah: e.tensor_scalar(
                        out=oa[:, h * 128:(h + 1) * 128], in0=ah[:, 0:128], scalar1=rl[:, h:h + 1], scalar2=None, op0=ALU.mult),
                        reads=[ak, "rl"], writes=["oa"])
                mb = b % 2
                self.norm_T(WN, oa[:], ["oa"], 1024, P["gsm"][:, 6:14], None, ["gsm"],
                            lambda ci, mb=mb: mx[mb][:, ci, :], ["mxa%d" % mb])
                self.dma("pool", self.MIXT[:, 0:8, b * 128:(b + 1) * 128], mx[mb][:], ["mxa%d" % mb], [], msem[mb])

            front(0)
            for n in range(len(seq)):
                if n + 1 < len(seq):
                    front(n + 1)
                back(n)
            kb.barrier()
            kb.release_dsems(sems + qsem + nsem + msem)


class Phases6(Phases5):
    def phase3(self):
        c, kb, P = self.cfg, self.kb, self.P
        NT = c.NSLOT * c.CH
        TPS = c.CH // 128
        NGQ = c.CH // 512
        scale = float((128 + c.ROPE) ** -0.5)
        with contextlib.ExitStack() as ph:
            self.alloc_psum(ph, 4, 0)
            NPT = 4
            accs = []
            for i in range(2):
                accs.append((self.ps(ph, "macc%da" % i, [128, 3, 129], F32), self.ps(ph, "macc%db" % i, [128, 1, 129], F32)))
            tri = self.sb(ph, "tri_b", [128, 128], BF16)
            ts = kb.dsem("tri")
            self.dma("pool", tri[:], self.tri_d, [], ["tri"], ts)
            ktB = self.sb(ph, "ktB", [65, NT], BF16)
            kbs = kb.dsem("ktB")
            self.dma("sp", ktB[:], self.KT_B, [], ["ktB"], kbs)
            ktA = [self.sb(ph, "ktA%d" % i, [128, NT], BF16) for i in range(2)]
            Vh = [self.sb(ph, "Vh%d" % i, [128, NT // 128, 129], BF16) for i in range(2)]
            kas = [[kb.dsem("ktA%d_%d" % (i, p)) for p in range(c.NSLOT)] for i in range(2)]
            vs = [[kb.dsem("Vh%d_%d" % (i, p)) for p in range(c.NSLOT)] for i in range(2)]
            qTa = [self.sb(ph, "qTa%d" % i, [128, c.CH], BF16) for i in range(2)]
            qTb = [self.sb(ph, "qTb%d" % i, [65, c.CH], BF16) for i in range(2)]
            qsa = [kb.dsem("qTa%d" % i) for i in range(2)]
            qsb = [kb.dsem("qTb%d" % i) for i in range(2)]
            pT = [self.sb(ph, "mpT%d" % i, [128, 512], BF16) for i in range(NPT)]
            obst = [self.sb(ph, "obst%d" % i, [128, 4, 128], F32) for i in range(2)]
            obs = [kb.dsem("obst%d" % i) for i in range(2)]
            rl = self.sb(ph, "mrl", [128, 4], F32)
            pti = 0
            it = 0
            for h in range(c.NH):
                hb = h % 2
                self.dma("sp", qTa[hb][:], self.QT_A[h], [], ["qTa%d" % hb], qsa[hb])
                self.dma("sp", qTb[hb][:], self.QT_B[h], [], ["qTb%d" % hb], qsb[hb])
                for p in range(c.NSLOT):
                    self.dma("sp", ktA[hb][:, p * c.CH:(p + 1) * c.CH], self.KT_A[h, :, p * c.CH:(p + 1) * c.CH],
                             [], ["ktA%d_%d" % (hb, p)], kas[hb][p])
                    self.dma("sp", Vh[hb][:, p * TPS:(p + 1) * TPS, :],
                             self.V_AUG[p * c.CH:(p + 1) * c.CH, h * 129:(h + 1) * 129].rearrange("(t p) f -> p t f", p=128),
                             [], ["Vh%d_%d" % (hb, p)], vs[hb][p])
                for g in range(NGQ):
                    ab = it % 2
                    it += 1
                    accA, accB = accs[ab]
                    def acc_of(i):
                        return (accA[:, i, :], "macc%da" % ab) if i < 3 else (accB[:, 0, :], "macc%db" % ab)
                    tiles = [(jt, max(0, jt - 4 * g), (jt - 4 * g) if jt >= 4 * g else None) for jt in range(4 * g + 4)]
                    tiles += [(kt, 0, None) for kt in range(TPS, c.NSLOT * TPS)]
                    kb.op("dve", lambda e: e.memset(accA[:], 0.0), writes=["macc%da" % ab])
                    kb.op("dve", lambda e: e.memset(accB[:], 0.0), writes=["macc%db" % ab])
                    first = {}
                    last = {}
                    for n, (kt, imin, idg) in enumerate(tiles):
                        for i in range(imin, 4):
                            first.setdefault(i, n)
                            last[i] = n
                    LAG = 2
                    pend = {}
                    for n2 in range(len(tiles) + LAG):
                        if n2 < len(tiles):
                            n = n2
                            kt, imin, idg = tiles[n]
                            p = kt // TPS
                            pb, pk = self.pbank()
                            q0 = g * 512 + imin * 128
                            q1 = (g + 1) * 512
                            kb.op("pe", lambda e, pb=pb, kt=kt, imin=imin, q0=q0, q1=q1: e.matmul(
                                pb[:, imin * 128:512], lhsT=ktA[hb][:, kt * 128:(kt + 1) * 128], rhs=qTa[hb][:, q0:q1],
                                start=True, stop=False), reads=["ktA%d_%d" % (hb, p), "qTa%d" % hb], writes=[pk])
                            kb.op("pe", lambda e, pb=pb, kt=kt, imin=imin, q0=q0, q1=q1: e.matmul(
                                pb[:, imin * 128:512], lhsT=ktB[:, kt * 128:(kt + 1) * 128], rhs=qTb[hb][:, q0:q1],
                                start=False, stop=True), reads=["ktB", "qTb%d" % hb], writes=[pk])
                            pi = pti % NPT
                            pti += 1
                            kb.op("act", lambda e, pb=pb, pi=pi, imin=imin: e.activation(
                                out=pT[pi][:, imin * 128:512], in_=pb[:, imin * 128:512], func=AF.Exp, scale=scale),
                                reads=[pk], writes=["mpT%d" % pi])
                            if idg is not None:
                                kb.op("pool", lambda e, pi=pi, idg=idg: e.tensor_tensor(
                                    out=pT[pi][:, idg * 128:(idg + 1) * 128], in0=pT[pi][:, idg * 128:(idg + 1) * 128],
                                    in1=tri[:], op=ALU.mult), reads=["tri"], writes=["mpT%d" % pi])
                            pend[n] = pi
                        if n2 >= LAG:
                            n = n2 - LAG
                            kt, imin, idg = tiles[n]
                            p = kt // TPS
                            pi = pend.pop(n)
                            for i in range(imin, 4):
                                ai, ak = acc_of(i)
                                kb.op("pe", lambda e, ai=ai, pi=pi, i=i, kt=kt, n=n: e.matmul(
                                    ai, lhsT=pT[pi][:, i * 128:(i + 1) * 128], rhs=Vh[hb][:, kt, :],
                                    start=False, stop=(last[i] == n)),
                                    reads=["mpT%d" % pi, "Vh%d_%d" % (hb, p)], writes=[ak])
                    ob = obst[ab]
                    kb.op("dve", lambda e: e.reciprocal(out=rl[:, 0:3].unsqueeze(2), in_=accA[:, :, 128:129]),
                          reads=["macc%da" % ab], writes=["mrl"])
                    kb.op("dve", lambda e: e.reciprocal(out=rl[:, 3:4].unsqueeze(2), in_=accB[:, :, 128:129]),
                          reads=["macc%db" % ab], writes=["mrl"])
                    for i in range(4):
                        ai, ak = acc_of(i)
                        kb.op("dve", lambda e, ai=ai, i=i, ob=ob: e.tensor_scalar(
                            out=ob[:, i, :], in0=ai[:, 0:128], scalar1=rl[:, i:i + 1], scalar2=None, op0=ALU.mult),
                            reads=[ak, "mrl"], writes=["obst%d" % ab])
                    self.dma("pool", self.OB[g * 512:(g + 1) * 512, h * 128:(h + 1) * 128].rearrange("(i p) f -> p i f", p=128),
                             ob[:], ["obst%d" % ab], [], obs[ab])
            kb.barrier()
            kb.release_dsems([ts, kbs] + kas[0] + kas[1] + vs[0] + vs[1] + qsa + qsb + obs)


class Phases7(Phases6):
    def phase4(self, st):
        c, kb, P = self.cfg, self.kb, self.P
        NQ = c.CH // 128
        NE = c.NE
        GS = NE // c.NG
        P["Wall"] = self.sb(st, "Wall", [128, NQ, NE], F32)
        P["slotI"] = self.sb(st, "slotI", [128, NQ, 8], I32)
        self.bnd_reg = self.nc.gpsimd.alloc_register("moe_bnd")
        self.nc.gpsimd.reg_mov(self.bnd_reg, NE * c.CAP - 1)
        P["w8"] = self.sb(st, "w8", [128, NQ, 8], F32)
        CAP = c.CAP
        BIGK = 131072.0
        with contextlib.ExitStack() as ph:
            self.alloc_psum(ph, 5, 2)
            Ls = self.sb(ph, "Ls", [128, 128], BF16)
            iot = self.sb(ph, "iot", [128, NE], F32)
            ebase = self.sb(ph, "ebase", [128, NE], F32)
            selb = self.sb(ph, "selb", [128, NQ, NE], BF16)
            lss = kb.dsem("Ls")
            ios = kb.dsem("iot")
            self.dma("pool", Ls[:], self.lstrict_d, [], ["Ls"], lss)
            self.dma("sp", iot[:], self.iota_d, [], ["iot"], ios)
            kb.op("dve", lambda e: e.tensor_scalar(out=ebase[:], in0=iot[:], scalar1=float(CAP), scalar2=None, op0=ALU.mult),
                  reads=["iot"], writes=["ebase"])
            d_selv = self.sb(ph, "d_selv", [128, NE], F32)
            d_slot = self.sb(ph, "d_slot", [128, NE], F32)
            d_k8 = self.sb(ph, "d_k8", [128, 8], F32)
            d_s8 = self.sb(ph, "d_s8", [128, 8], F32)
            d_e8i = self.sb(ph, "d_e8i", [128, 8], I32)
            d_e8f = self.sb(ph, "d_e8f", [128, 8], F32)
            d_junk = self.sb(ph, "d_junk", [128, NE], F32)
            scs = [kb.dsem("scat%d" % i) for i in range(16)]
            sci = 0
            gate_a = self.bcast_tile(ph, ph, "gate_a_bc", P["modT"][:, 32:48], "modT")
            wo = self.sb(ph, "wo", [128, 16, 2048], BF16)
            wos = [kb.dsem("wo%d" % i) for i in range(4)]
            for i in range(4):
                self.dma("pool", wo[:, :, i * 512:(i + 1) * 512],
                         self.w_o[:, i * 512:(i + 1) * 512].rearrange("(kc p) n -> p kc n", p=128), [], ["wo%d" % i], wos[i])
            wr = self.sb(ph, "wr", [128, 16, NE], BF16)
            wrs = kb.dsem("wr")
            self.dma("pool", wr[:], self.w_router.rearrange("(kc p) n -> p kc n", p=128), [], ["wr"], wrs)
            rb = self.sb(ph, "rb_bc", [128, NE], F32)
            rbs = kb.dsem("rb")
            self.dma("sp", rb[:], self.rbias.partition_broadcast(128), [], ["rb"], rbs)
            obt = [self.sb(ph, "obt%d" % i, [128, 1024], F32) for i in range(2)]
            obts = [kb.dsem("obt%d" % i) for i in range(2)]
            mixa = [self.sb(ph, "mixa%d" % i, [128, 8, 128], BF16) for i in range(2)]
            mas = [kb.dsem("mixa%d" % i) for i in range(2)]
            mixb = [self.sb(ph, "mixb%d" % i, [128, 8, 128], BF16) for i in range(2)]
            xt = [self.sb(ph, "xt4_%d" % i, [128, 2048], F32) for i in range(2)]
            xts = [kb.dsem("xt4_%d" % i) for i in range(2)]
            xm = [self.sb(ph, "xm%d" % i, [128, 2048], F32) for i in range(2)]
            xms = [kb.dsem("xm%d" % i) for i in range(2)]
            tmpm = [self.sb(ph, "tmpm%d" % i, [128, 512], F32) for i in range(2)]
            h2 = [self.sb(ph, "h2st%d" % i, [128, 16, 128], BF16) for i in range(2)]
            h2s = [kb.dsem("h2st%d" % i) for i in range(2)]
            WN1 = self.alloc_normT(ph, "n4a", 1024, 1)
            WN2 = self.alloc_normT(ph, "n4b", 2048, 2)
            sc = self.sb(ph, "r_sc", [128, NE], F32)
            ch = self.sb(ph, "r_ch", [128, NE], F32)
            cm = self.sb(ph, "r_cm", [128, NE], F32)
            m8 = self.sb(ph, "r_m8", [128, c.NG, 8], F32)
            grp = self.sb(ph, "r_grp", [128, 8], F32)
            g8 = self.sb(ph, "r_g8", [128, 8], F32)
            gm = self.sb(ph, "r_gm", [128, 8], F32)
            e8 = self.sb(ph, "r_e8", [128, 8], F32)
            sel = self.sb(ph, "r_sel", [128, NE], F32)
            ws_ = self.sb(ph, "r_ws", [128, 2], F32)
            xv = self.xk.rearrange("(n p) d -> n p d", p=128)
            for b in range(NQ):
                k = b % 2
                self.dma("sp", obt[k][:], self.OB[b * 128:(b + 1) * 128, :], [], ["obt%d" % k], obts[k])
                self.dma("sp", mixa[k][:], self.MIXT[:, 0:8, b * 128:(b + 1) * 128], [], ["mixa%d" % k], mas[k])
                self.dma("sp", xt[k][:], xv[b], [], ["xt4_%d" % k], xts[k])
                self.norm_T(WN1, obt[k][:], ["obt%d" % k], 1024, P["gsm"][:, 14:22], None, ["gsm"],
                            lambda ci, k=k: mixb[k][:, ci, :], ["mixb%d" % k])
                for nb in range(4):
                    pb, pk = self.pbank()
                    for ci in range(16):
                        src = mixa[k] if ci < 8 else mixb[k]
                        sk = ("mixa%d" % k) if ci < 8 else ("mixb%d" % k)
                        kb.op("pe", lambda e, ci=ci, nb=nb, pb=pb, src=src: e.matmul(
                            pb[:], lhsT=src[:, ci % 8, :], rhs=wo[:, ci, nb * 512:(nb + 1) * 512],
                            start=(ci == 0), stop=(ci == 15)), reads=[sk, "wo%d" % nb], writes=[pk])
                    tk = nb % 2
                    kb.op("dve", lambda e, nb=nb, pb=pb, tk=tk: e.tensor_tensor(
                        out=tmpm[tk][:], in0=pb[:], in1=gate_a[:, nb * 512:(nb + 1) * 512], op=ALU.mult),
                        reads=[pk, "gate_a_bc"], writes=["tmpm%d" % tk])
                    kb.op("pool", lambda e, nb=nb, tk=tk: e.tensor_tensor(
                        out=xm[k][:, nb * 512:(nb + 1) * 512], in0=tmpm[tk][:], in1=xt[k][:, nb * 512:(nb + 1) * 512], op=ALU.add),
                        reads=["tmpm%d" % tk, "xt4_%d" % k], writes=["xm%d" % k])
                self.dma("pool", self.XMID[b * 128:(b + 1) * 128, :], xm[k][:], ["xm%d" % k], [], xms[k])
                self.norm_T(WN2, xm[k][:], ["xm%d" % k], 2048, P["gmodF"], P["modT"][:, 48:64], ["gmodF", "modT"],
                            lambda ci, k=k: h2[k][:, ci, :], ["h2st%d" % k])
                self.dma("pool", self.H2T[:, :, b * 128:(b + 1) * 128], h2[k][:], ["h2st%d" % k], [], h2s[k])
                pb, pk = self.pbank()
                for ci in range(16):
                    kb.op("pe", lambda e, ci=ci, pb=pb: e.matmul(pb[:, 0:NE], lhsT=h2[k][:, ci, :], rhs=wr[:, ci, :],
                                                                 start=(ci == 0), stop=(ci == 15)),
                          reads=["h2st%d" % k, "wr"], writes=[pk])
                D = lambda fn, r, w: kb.op("dve", fn, reads=r, writes=w)
                kb.op("act", lambda e, pb=pb: e.activation(out=sc[:], in_=pb[:, 0:NE], func=AF.Sigmoid), reads=[pk], writes=["r_sc"])
                D(lambda e: e.tensor_tensor(out=ch[:], in0=sc[:], in1=rb[:], op=ALU.add), ["r_sc", "rb"], ["r_ch"])
                for g in range(c.NG):
                    D(lambda e, g=g: e.max(out=m8[:, g, :], in_=ch[:, g * GS:(g + 1) * GS]), ["r_ch"], ["r_m8"])
                D(lambda e: e.tensor_tensor(out=grp[:].unsqueeze(2), in0=m8[:, :, 0:1], in1=m8[:, :, 1:2], op=ALU.add), ["r_m8"], ["r_grp"])
                D(lambda e: e.max(out=g8[:], in_=grp[:]), ["r_grp"], ["r_g8"])
                D(lambda e: e.tensor_scalar(out=gm[:], in0=grp[:], scalar1=g8[:, c.TOPG - 1:c.TOPG], scalar2=None, op0=ALU.is_ge),
                  ["r_grp", "r_g8"], ["r_gm"])
                D(lambda e: e.tensor_scalar(out=gm[:], in0=gm[:], scalar1=-1.0, scalar2=1e30, op0=ALU.add, op1=ALU.mult), [], ["r_gm"])
                D(lambda e: e.tensor_tensor(out=cm[:].rearrange("p (g s) -> p g s", s=GS), in0=ch[:].rearrange("p (g s) -> p g s", s=GS),
                                            in1=gm[:].unsqueeze(2).broadcast_to([128, c.NG, GS]), op=ALU.add), ["r_ch", "r_gm"], ["r_cm"])
                D(lambda e: e.max(out=e8[:], in_=cm[:]), ["r_cm"], ["r_e8"])
                D(lambda e: e.tensor_scalar(out=sel[:], in0=cm[:], scalar1=e8[:, c.TOPK - 1:c.TOPK], scalar2=None, op0=ALU.is_ge),
                  ["r_cm", "r_e8"], ["r_sel"])
                D(lambda e, b=b: e.tensor_copy(out=selb[:, b, :], in_=sel[:]), ["r_sel"], ["selb%d" % b])
                pp, ppk = self.pbank()
                for b2 in range(b):
                    kb.op("pe", lambda e, b2=b2, pp=pp: e.matmul(pp[:, 0:NE], lhsT=P["ones_b"][:], rhs=selb[:, b2, :],
                                                                 start=(b2 == 0), stop=False),
                          reads=["ones_b", "selb%d" % b2], writes=[ppk])
                kb.op("pe", lambda e, b=b, pp=pp: e.matmul(pp[:, 0:NE], lhsT=Ls[:], rhs=selb[:, b, :], start=(b == 0), stop=True),
                      reads=["Ls", "selb%d" % b], writes=[ppk])
                D(lambda e, pp=pp: e.tensor_scalar(out=d_selv[:], in0=pp[:, 0:NE], scalar1=float(CAP), scalar2=None, op0=ALU.is_lt),
                  [ppk], ["d_selv"])
                D(lambda e: e.tensor_tensor(out=d_selv[:], in0=d_selv[:], in1=sel[:], op=ALU.mult), ["r_sel"], ["d_selv"])
                D(lambda e, pp=pp: e.tensor_tensor(out=d_slot[:], in0=pp[:, 0:NE], in1=ebase[:], op=ALU.add), [ppk, "ebase"], ["d_slot"])
                D(lambda e: e.tensor_scalar(out=d_slot[:], in0=d_slot[:], scalar1=-1.0, scalar2=BIGK, op0=ALU.mult, op1=ALU.add),
                  [], ["d_slot"])
                D(lambda e: e.tensor_tensor(out=d_slot[:], in0=d_slot[:], in1=d_selv[:], op=ALU.mult), ["d_selv"], ["d_slot"])
                D(lambda e: e.max(out=d_k8[:], in_=d_slot[:]), ["d_slot"], ["d_k8"])
                D(lambda e: e.tensor_scalar(out=d_s8[:], in0=d_k8[:], scalar1=-1.0, scalar2=BIGK, op0=ALU.mult, op1=ALU.add),
                  ["d_k8"], ["d_s8"])
                D(lambda e, b=b: e.tensor_copy(out=P["slotI"][:, b, :], in_=d_s8[:]), ["d_s8"], ["slotI%d" % b])
                D(lambda e, b=b: e.tensor_scalar(out=d_e8i[:], in0=P["slotI"][:, b, :], scalar1=int(np.log2(CAP)), scalar2=None,
                                                 op0=ALU.arith_shift_right), ["slotI%d" % b], ["d_e8i"])
                D(lambda e: e.tensor_copy(out=d_e8f[:], in_=d_e8i[:]), ["d_e8i"], ["d_e8f"])
                xn_cur = WN2["xn"][(WN2["i"] - 1) % WN2["n"]]
                xn_key = "n4b_xn%d" % ((WN2["i"] - 1) % WN2["n"])
                for k8 in range(8):
                    sm = scs[sci % 16]
                    sci += 1
                    kb.op("pool", lambda e, b=b, k8=k8, xn_cur=xn_cur: e.indirect_dma_start(
                        out=self.XG[:, :], out_offset=bass.IndirectOffsetOnAxis(ap=P["slotI"][:, b, k8:k8 + 1], axis=0),
                        in_=xn_cur[:, :], in_offset=None, bounds_check=self.bnd_reg, oob_is_err=False),
                        reads=[xn_key, "slotI%d" % b], writes=[], dsem=sm)
                D(lambda e: e.tensor_tensor(out=sel[:], in0=sel[:], in1=sc[:], op=ALU.mult), ["r_sc"], ["r_sel"])
                D(lambda e: e.tensor_reduce(out=ws_[:, 0:1], in_=sel[:], axis=AX.X, op=ALU.add), ["r_sel"], ["r_ws"])
                D(lambda e: e.reciprocal(out=ws_[:, 1:2], in_=ws_[:, 0:1]), [], ["r_ws"])
                D(lambda e, b=b: e.tensor_scalar(out=P["Wall"][:, b, :], in0=sel[:], scalar1=ws_[:, 1:2], scalar2=float(c.ROUTED_SCALE),
                                                 op0=ALU.mult, op1=ALU.mult), ["r_sel", "r_ws"], ["Wall"])
                for k8 in range(8):
                    D(lambda e, b=b, k8=k8: e.scalar_tensor_tensor(
                        out=d_junk[:], in0=iot[:], scalar=d_e8f[:, k8:k8 + 1], in1=P["Wall"][:, b, :],
                        op0=ALU.is_equal, op1=ALU.mult, accum_out=P["w8"][:, b, k8:k8 + 1]),
                      ["iot", "d_e8f", "Wall"], ["d_junk", "w8_%d" % b])
            if c.debug:
                self.WALL = self.dscr("WALL", [128, NQ * NE], F32)
                self.dma("sp", self.WALL, P["Wall"][:].rearrange("p a b -> p (a b)"), ["Wall"], [], rbs)
            kb.barrier()
            kb.release_dsems(wos + [wrs, rbs, lss, ios] + obts + mas + xts + xms + h2s + scs)

    def phase6(self, shared_only=False):
        c, kb, P = self.cfg, self.kb, self.P
        NE = c.NE
        TH = c.CH // 2
        NTT = TH // 128
        with contextlib.ExitStack() as ph:
            self.alloc_psum(ph, 8, 0)
            gate_f = self.bcast_tile(ph, ph, "gate_f_bc", P["modT"][:, 80:96], "modT")
            fg = self.bcast_tile(ph, ph, "fg_bc", P["gvec"][:, 32:48], "gvec")
            h2T = self.sb(ph, "h2T_h", [128, 16, TH], BF16)
            h2s = kb.dsem("h2T_h")
            acc = self.sb(ph, "moe_acc", [128, NTT, 2048], F32)
            NW = 2
            wg = [self.sb(ph, "wg%d" % i, [128, 16, 128], BF16) for i in range(NW)]
            wu = [self.sb(ph, "wu%d" % i, [128, 16, 128], BF16) for i in range(NW)]
            wgs = [kb.dsem("wg%d" % i) for i in range(NW)]
            wus = [kb.dsem("wu%d" % i) for i in range(NW)]
            wd = [self.sb(ph, "wd%d" % i, [128, 4, 2048], BF16) for i in range(2)]
            wds = [kb.dsem("wd%d" % i) for i in range(2)]
            hT = [self.sb(ph, "ehT%d" % i, [128, 4, TH], BF16) for i in range(2)]
            sg = [self.sb(ph, "sg%d" % i, [128, 512], F32) for i in range(2)]
            xmt = self.sb(ph, "xmt", [128, 2048], F32)
            xmts = kb.dsem("xmt")
            st3 = self.sb(ph, "st3", [128, 4], F32)
            junk = self.sb(ph, "junk6", [128, 2048], BF16)
            wi = 0
            sgi = 0
            yshs = [kb.dsem("ysh%d" % i) for i in range(NTT)] if shared_only else []
            for half in range(2):
                self.dma("sp", h2T[:], self.H2T[:, :, half * TH:(half + 1) * TH], [], ["h2T_h"], h2s)
                for tt in range(NTT):
                    kb.op("pool", lambda e, tt=tt: e.memset(acc[:, tt, :], 0.0), writes=["acc%d" % tt])
                for ex in ([NE] if shared_only else range(NE + 1)):
                    eb = ex % 2
                    if ex < NE:
                        g_src, u_src, d_src = self.w_eg[ex], self.w_eu[ex], self.w_ed[ex]
                    else:
                        g_src, u_src, d_src = self.w_sg, self.w_su, self.w_sd
                    self.dma("pool", wd[eb][:], d_src.rearrange("(kc p) n -> p kc n", p=128), [], ["wd%d" % eb], wds[eb])
                    for hb in range(4):
                        w = wi % NW
                        wi += 1
                        self.dma("pool", wg[w][:], g_src[:, hb * 128:(hb + 1) * 128].rearrange("(kc p) n -> p kc n", p=128),
                                 [], ["wg%d" % w], wgs[w])
                        self.dma("pool", wu[w][:], u_src[:, hb * 128:(hb + 1) * 128].rearrange("(kc p) n -> p kc n", p=128),
                                 [], ["wu%d" % w], wus[w])
                        for tg in range(TH // 512):
                            pg, pgk = self.pbank()
                            pu, puk = self.pbank()
                            for (pp, ppk, wsrc, wk) in ((pg, pgk, wg[w], "wg%d" % w), (pu, puk, wu[w], "wu%d" % w)):
                                for kc in range(16):
                                    kb.op("pe", lambda e, pp=pp, wsrc=wsrc, kc=kc, tg=tg: e.matmul(
                                        pp[:], lhsT=wsrc[:, kc, :], rhs=h2T[:, kc, tg * 512:(tg + 1) * 512],
                                        start=(kc == 0), stop=(kc == 15)), reads=[wk, "h2T_h"], writes=[ppk])
                            si = sgi % 2
                            sgi += 1
                            kb.op("act", lambda e, pg=pg, si=si: e.activation(out=sg[si][:], in_=pg[:], func=AF.Silu),
                                  reads=[pgk], writes=["sg%d" % si])
                            kb.op("dve", lambda e, pu=pu, si=si, hb=hb, tg=tg: e.tensor_tensor(
                                out=hT[eb][:, hb, tg * 512:(tg + 1) * 512], in0=pu[:], in1=sg[si][:], op=ALU.mult),
                                reads=[puk, "sg%d" % si], writes=["ehT%d" % eb])
                    for tt in range(NTT):
                        tile = half * NTT + tt
                        for nb in range(4):
                            py, pyk = self.pbank()
                            for hb in range(4):
                                kb.op("pe", lambda e, py=py, hb=hb, tt=tt, nb=nb: e.matmul(
                                    py[:], lhsT=hT[eb][:, hb, tt * 128:(tt + 1) * 128], rhs=wd[eb][:, hb, nb * 512:(nb + 1) * 512],
                                    start=(hb == 0), stop=(hb == 3)), reads=["ehT%d" % eb, "wd%d" % eb], writes=[pyk])
                            scal = P["Wall"][:, tile, ex:ex + 1] if ex < NE else 1.0
                            kb.op("dve", lambda e, py=py, tt=tt, nb=nb, scal=scal: e.scalar_tensor_tensor(
                                out=acc[:, tt, nb * 512:(nb + 1) * 512], in0=py[:], scalar=scal,
                                in1=acc[:, tt, nb * 512:(nb + 1) * 512], op0=ALU.mult, op1=ALU.add),
                                reads=[pyk, "Wall"], writes=["acc%d" % tt])
                if shared_only:
                    for tt in range(NTT):
                        tile = half * NTT + tt
                        self.dma("sp", self.YSH[tile * 128:(tile + 1) * 128, :], acc[:, tt, :], ["acc%d" % tt], [], yshs[tt])
                    continue
                for tt in range(NTT):
                    tile = half * NTT + tt
                    self.dma("sp", xmt[:], self.XMID[tile * 128:(tile + 1) * 128, :], [], ["xmt"], xmts)
                    kb.op("dve", lambda e, tt=tt: e.tensor_tensor(out=acc[:, tt, :], in0=acc[:, tt, :], in1=gate_f[:], op=ALU.mult),
                          reads=["gate_f_bc"], writes=["acc%d" % tt])
                    kb.op("pool", lambda e, tt=tt: e.tensor_tensor(out=xmt[:], in0=xmt[:], in1=acc[:, tt, :], op=ALU.add),
                          reads=["acc%d" % tt], writes=["xmt"])
                    kb.op("act", lambda e: e.activation(out=junk[:], in_=xmt[:], func=AF.Square, accum_out=st3[:, 0:1]),
                          reads=["xmt"], writes=["junk6", "st3"])
                    kb.op("dve", lambda e: e.tensor_scalar(out=st3[:, 1:2], in0=st3[:, 0:1], scalar1=1.0 / c.D, scalar2=c.EPS,
                                                           op0=ALU.mult, op1=ALU.add), reads=[], writes=["st3"])
                    kb.op("act", lambda e: e.activation(out=st3[:, 2:3], in_=st3[:, 1:2], func=AF.Sqrt), reads=[], writes=["st3"])
                    kb.op("dve", lambda e: e.reciprocal(out=st3[:, 3:4], in_=st3[:, 2:3]), reads=[], writes=["st3"])
                    kb.op("dve", lambda e: e.scalar_tensor_tensor(out=xmt[:], in0=xmt[:], scalar=st3[:, 3:4], in1=fg[:],
                                                                  op0=ALU.mult, op1=ALU.mult), reads=["st3", "fg_bc"], writes=["xmt"])
                    self.dma("sp", self.out[tile * 128:(tile + 1) * 128, :], xmt[:], ["xmt"], [], xmts)
            kb.barrier()
            kb.release_dsems([h2s, xmts] + wgs + wus + wds + yshs)


class Phases8(Phases7):
    def phase6_routed(self):
        c, kb, P = self.cfg, self.kb, self.P
        NE, CAP = c.NE, c.CAP
        NB = CAP // 128
        with contextlib.ExitStack() as ph:
            self.alloc_psum(ph, 6, 2)
            NXG = 8
            xg = [self.sb(ph, "xg%d" % i, [128, 2048], BF16) for i in range(NXG)]
            xgs = [kb.dsem("xg%d" % i) for i in range(NXG)]
            xT = self.sb(ph, "xTe", [128, 16, CAP], BF16)
            NW = 4
            wg = [self.sb(ph, "rwg%d" % i, [128, 16, 128], BF16) for i in range(NW)]
            wu = [self.sb(ph, "rwu%d" % i, [128, 16, 128], BF16) for i in range(NW)]
            wgs = [kb.dsem("rwg%d" % i) for i in range(NW)]
            wus = [kb.dsem("rwu%d" % i) for i in range(NW)]
            wd = [self.sb(ph, "rwd%d" % i, [128, 4, 2048], BF16) for i in range(2)]
            wds = [kb.dsem("rwd%d" % i) for i in range(2)]
            hT = [self.sb(ph, "rhT%d" % i, [128, 4, CAP], BF16) for i in range(2)]
            sg = [self.sb(ph, "rsg%d" % i, [128, 512], F32) for i in range(2)]
            yst = [self.sb(ph, "yst%d" % i, [128, 2048], BF16) for i in range(2)]
            ysts = [kb.dsem("yst%d" % i) for i in range(2)]
            xTs = [xT, self.sb(ph, "xTe_b", [128, 16, CAP], BF16)]
            cnt = {"xgi": 0, "wi": 0, "sgi": 0, "yi": 0}

            def T_blk(ex, blk):
                xTe = xTs[ex % 2]
                xi = blk % NXG
                for c0 in range(0, 16, 8):
                    tb, tk = self.tbank()
                    for j in range(8):
                        ci = c0 + j
                        kb.op("pe", lambda e, j=j, ci=ci: e.transpose(
                            out=tb[:, j, :], in_=xg[xi][:, ci * 128:(ci + 1) * 128], identity=P["ident_b"][:]),
                            reads=["xg%d" % xi, "ident_b"], writes=[tk])
                    for j in range(8):
                        ci = c0 + j
                        wkey = "xTe%d_%d" % (ex % 2, blk // 4)
                        if j % 2 == 0:
                            kb.op("dve", lambda e, j=j, ci=ci: e.tensor_scalar(
                                out=xTe[:, ci, blk * 128:(blk + 1) * 128], in0=tb[:, j, :], scalar1=P["gmodF"][:, ci:ci + 1],
                                scalar2=P["modT"][:, 48 + ci:49 + ci], op0=ALU.mult, op1=ALU.add),
                                reads=[tk, "gmodF", "modT"], writes=[wkey])
                        else:
                            kb.op("act", lambda e, j=j, ci=ci: e.activation(
                                out=xTe[:, ci, blk * 128:(blk + 1) * 128], in_=tb[:, j, :], func=AF.Identity,
                                scale=P["gmodF"][:, ci:ci + 1], bias=P["modT"][:, 48 + ci:49 + ci]),
                                reads=[tk, "gmodF", "modT"], writes=[wkey])

            def G_hb(ex, hb):
                eb = ex % 2
                xTe = xTs[ex % 2]
                if hb == 0:
                    self.dma("pool", wd[eb][:], self.w_ed[ex].rearrange("(kc p) n -> p kc n", p=128), [], ["rwd%d" % eb], wds[eb])
                w = cnt["wi"] % NW
                cnt["wi"] += 1
                self.dma("pool", wg[w][:], self.w_eg[ex][:, hb * 128:(hb + 1) * 128].rearrange("(kc p) n -> p kc n", p=128),
                         [], ["rwg%d" % w], wgs[w])
                self.dma("pool", wu[w][:], self.w_eu[ex][:, hb * 128:(hb + 1) * 128].rearrange("(kc p) n -> p kc n", p=128),
                         [], ["rwu%d" % w], wus[w])
                for tg in range(CAP // 512):
                    pg, pgk = self.pbank()
                    pu, puk = self.pbank()
                    for (pp, ppk, wsrc, wk) in ((pg, pgk, wg[w], "rwg%d" % w), (pu, puk, wu[w], "rwu%d" % w)):
                        for kc in range(16):
                            kb.op("pe", lambda e, pp=pp, wsrc=wsrc, kc=kc, tg=tg: e.matmul(
                                pp[:], lhsT=wsrc[:, kc, :], rhs=xTe[:, kc, tg * 512:(tg + 1) * 512],
                                start=(kc == 0), stop=(kc == 15)), reads=[wk, "xTe%d_%d" % (ex % 2, tg)], writes=[ppk])
                    si = cnt["sgi"] % 2
                    cnt["sgi"] += 1
                    kb.op("act", lambda e, pg=pg, si=si: e.activation(out=sg[si][:], in_=pg[:], func=AF.Silu),
                          reads=[pgk], writes=["rsg%d" % si])
                    kb.op("dve", lambda e, pu=pu, si=si, tg=tg: e.tensor_tensor(
                        out=hT[eb][:, hb, tg * 512:(tg + 1) * 512], in0=pu[:], in1=sg[si][:], op=ALU.mult),
                        reads=[puk, "rsg%d" % si], writes=["rhT%d" % eb])

            def G_down(ex, blk):
                eb = ex % 2
                y = cnt["yi"] % 2
                cnt["yi"] += 1
                for nb in range(4):
                    py, pyk = self.pbank()
                    for hb in range(4):
                        kb.op("pe", lambda e, py=py, hb=hb, nb=nb: e.matmul(
                            py[:], lhsT=hT[eb][:, hb, blk * 128:(blk + 1) * 128], rhs=wd[eb][:, hb, nb * 512:(nb + 1) * 512],
                            start=(hb == 0), stop=(hb == 3)), reads=["rhT%d" % eb, "rwd%d" % eb], writes=[pyk])
                    if nb % 2 == 0:
                        kb.op("act", lambda e, py=py, nb=nb: e.activation(out=yst[y][:, nb * 512:(nb + 1) * 512], in_=py[:], func=AF.Copy),
                              reads=[pyk], writes=["yst%d" % y])
                    else:
                        kb.op("dve", lambda e, py=py, nb=nb: e.tensor_copy(out=yst[y][:, nb * 512:(nb + 1) * 512], in_=py[:]),
                              reads=[pyk], writes=["yst%d" % y])
                r0 = ex * CAP + blk * 128
                self.dma("act", self.YS[r0:r0 + 128, :], yst[y][:], ["yst%d" % y], [], ysts[y])

            def X_load(ex):
                for blk in range(NB):
                    r0 = ex * CAP + blk * 128
                    self.dma("sp", xg[blk % NXG][:], self.XG[r0:r0 + 128, :], [], ["xg%d" % (blk % NXG)], xgs[blk % NXG])

            X_load(0)
            for blk in range(NB):
                T_blk(0, blk)
            X_load(1)
            for ex in range(NE):
                nxt = [(ex + 1, blk) for blk in range(NB)] if ex + 1 < NE else []
                for hb in range(4):
                    G_hb(ex, hb)
                    if nxt:
                        T_blk(*nxt.pop(0))
                for blk in range(NB):
                    G_down(ex, blk)
                    if nxt and blk % 2 == 1:
                        T_blk(*nxt.pop(0))
                while nxt:
                    T_blk(*nxt.pop(0))
                if ex + 2 < NE:
                    X_load(ex + 2)
            kb.barrier()
            kb.release_dsems(xgs + wgs + wus + wds + ysts)

    def phase7(self):
        c, kb, P = self.cfg, self.kb, self.P
        NQ = c.CH // 128
        NE, CAP = c.NE, c.CAP
        with contextlib.ExitStack() as ph:
            self.alloc_psum(ph, 2, 0)
            gate_f = self.bcast_tile(ph, ph, "gate_f_bc2", P["modT"][:, 80:96], "modT")
            fg = self.bcast_tile(ph, ph, "fg_bc2", P["gvec"][:, 32:48], "gvec")
            acc = [self.sb(ph, "cacc%d" % i, [128, 2048], F32) for i in range(2)]
            accs = [kb.dsem("cacc%d" % i) for i in range(2)]
            xmt = [self.sb(ph, "cxm%d" % i, [128, 2048], F32) for i in range(2)]
            xmts = [kb.dsem("cxm%d" % i) for i in range(2)]
            NGB = 4
            gb = [self.sb(ph, "gb%d" % i, [128, 2048], BF16) for i in range(NGB)]
            gbs = [kb.dsem("gb%d" % i) for i in range(NGB)]
            st3 = self.sb(ph, "cst3", [128, 4], F32)
            junk = self.sb(ph, "cjunk", [128, 2048], BF16)
            for i in range(NGB):
                kb.op("pool", lambda e, i=i: e.memset(gb[i][:], 0.0), writes=["gb%d" % i])
            gi = 0
            for b in range(NQ):
                k = b % 2
                self.dma("sp", acc[k][:], self.YSH[b * 128:(b + 1) * 128, :], [], ["cacc%d" % k], accs[k])
                self.dma("sp", xmt[k][:], self.XMID[b * 128:(b + 1) * 128, :], [], ["cxm%d" % k], xmts[k])
                for k8 in range(8):
                    g = gi % NGB
                    gi += 1
                    kb.op("pool", lambda e, g=g, b=b, k8=k8: e.indirect_dma_start(
                        out=gb[g][:, :], out_offset=None, in_=self.YS[:, :],
                        in_offset=bass.IndirectOffsetOnAxis(ap=P["slotI"][:, b, k8:k8 + 1], axis=0),
                        bounds_check=self.bnd_reg, oob_is_err=False),
                        reads=["slotI%d" % b], writes=["gb%d" % g], dsem=gbs[g])
                    kb.op("dve", lambda e, g=g, b=b, k8=k8, k=k: e.scalar_tensor_tensor(
                        out=acc[k][:], in0=gb[g][:], scalar=P["w8"][:, b, k8:k8 + 1], in1=acc[k][:], op0=ALU.mult, op1=ALU.add),
                        reads=["gb%d" % g, "w8_%d" % b], writes=["cacc%d" % k])
                kb.op("dve", lambda e, k=k: e.tensor_tensor(out=acc[k][:], in0=acc[k][:], in1=gate_f[:], op=ALU.mult),
                      reads=["gate_f_bc2"], writes=["cacc%d" % k])
                kb.op("pool", lambda e, k=k: e.tensor_tensor(out=xmt[k][:], in0=xmt[k][:], in1=acc[k][:], op=ALU.add),
                      reads=["cacc%d" % k], writes=["cxm%d" % k])
                kb.op("act", lambda e, k=k: e.activation(out=junk[:], in_=xmt[k][:], func=AF.Square, accum_out=st3[:, 0:1]),
                      reads=["cxm%d" % k], writes=["cjunk", "cst3"])
                kb.op("dve", lambda e: e.tensor_scalar(out=st3[:, 1:2], in0=st3[:, 0:1], scalar1=1.0 / c.D, scalar2=c.EPS,
                                                       op0=ALU.mult, op1=ALU.add), reads=[], writes=["cst3"])
                kb.op("act", lambda e: e.activation(out=st3[:, 2:3], in_=st3[:, 1:2], func=AF.Sqrt), reads=[], writes=["cst3"])
                kb.op("dve", lambda e: e.reciprocal(out=st3[:, 3:4], in_=st3[:, 2:3]), reads=[], writes=["cst3"])
                kb.op("dve", lambda e, k=k: e.scalar_tensor_tensor(out=xmt[k][:], in0=xmt[k][:], scalar=st3[:, 3:4], in1=fg[:],
                                                                   op0=ALU.mult, op1=ALU.mult), reads=["cst3", "fg_bc2"], writes=["cxm%d" % k])
                self.dma("act", self.out[b * 128:(b + 1) * 128, :], xmt[k][:], ["cxm%d" % k], [], xmts[k])
            kb.barrier()
            kb.release_dsems(accs + xmts + gbs)


def build(cfg):
    b = Phases8(cfg)
    b.declare_io()
    kb = b.kb
    with contextlib.ExitStack() as st:
        b.phase0(st)
        if cfg.stop_after >= 1:
            b.phase1a()
        if cfg.stop_after >= 2:
            b.phase1b()
            b.phase1c()
        if cfg.stop_after >= 3:
            b.phase2()
        if cfg.stop_after >= 4:
            b.phase3()
        if cfg.stop_after >= 5:
            b.phase4(st)
        if cfg.stop_after >= 6:
            if cfg.CAP:
                b.phase6(shared_only=True)
                b.phase6_routed()
                b.phase7()
            else:
                b.phase6()
        kb.barrier()
    return b


_BUILD_CACHE = {}


def kernel(**inputs):
    cfg = Cfg
    inp = {k: np.asarray(v) for k, v in inputs.items()}
    S = inp["x"].shape[1]
    nchunks = S // cfg.CH
    assert nchunks == cfg.NCORES and nchunks == cfg.NSLOT
    if "b" not in _BUILD_CACHE:
        _BUILD_CACHE["b"] = build(cfg)
    b = _BUILD_CACHE["b"]
    sh = prepare_shared(inp, cfg)
    maps = []
    for core in range(cfg.NCORES):
        m = dict(sh)
        m.update(prepare_core(inp, cfg, core, nchunks))
        maps.append(m)
    res = run_bass_kernel_spmd(b.nc, maps, core_ids=list(range(cfg.NCORES)))
    outs = [np.asarray(r["out"]) for r in res.results]
    return np.concatenate(outs, axis=0)[None].astype(np.float32)
```

```python
import contextlib
import numpy as np
import concourse.bass as bass
import concourse.mybir as mybir
from concourse.bass_utils import run_bass_kernel_spmd

F32 = mybir.dt.float32
BF16 = mybir.dt.bfloat16
I32 = mybir.dt.int32
AF = mybir.ActivationFunctionType
ALU = mybir.AluOpType
AX = mybir.AxisListType

SAME_ENGINE_SYNC = True


class Cfg:
    D = 2048
    CH = 2048
    NSLOT = 8
    NCORES = 8
    NE = 64
    NG = 8
    TOPG = 4
    TOPK = 8
    DE = 512
    NH = 8
    HD = 128
    QL = 512
    KVL = 256
    ROPE = 64
    NADA = 6
    EPS = 1e-6
    ROUTED_SCALE = 2.5
    WINDOWS = ((128, 1), (512, 4), (2048, 16))
    CAP = 1024
    debug = False
    stop_after = 99


class Sem:
    def __init__(self, h, name):
        self.h = h
        self.name = name
        self.count = 0


class KB:
    def __init__(self, nc):
        self.nc = nc
        self.E = {"pe": nc.tensor, "act": nc.scalar, "dve": nc.vector, "pool": nc.gpsimd, "sp": nc.sync}
        self.esem = {}
        self.allsems = []
        for e in ["pe", "act", "dve", "pool"]:
            self.esem[e] = self.newsem("es_" + e)
        self.waited = {e: {} for e in self.E}
        self.lastw = {}
        self.readers = {}
        self.free_dsems = []
        self.nwaits = 0
        self.nops = 0

    def newsem(self, name):
        s = Sem(self.nc.alloc_semaphore(name=name), name)
        self.allsems.append(s)
        return s

    def dsem(self, name="d"):
        if self.free_dsems:
            return self.free_dsems.pop()
        return self.newsem("ds%d_%s" % (len(self.allsems), name))

    def release_dsems(self, sems):
        self.free_dsems.extend(sems)

    def _wait(self, eng, sem, val):
        if self.waited[eng].get(sem, 0) >= val:
            return
        self.E[eng].wait_ge(sem.h, val)
        self.waited[eng][sem] = val
        self.nwaits += 1

    def op(self, eng, issue, reads=(), writes=(), dsem=None):
        need = {}
        def add(ev):
            sem, val, peng = ev
            if eng == "pe" and peng == "pe":
                return
            if (not SAME_ENGINE_SYNC) and peng == eng and peng != "dma":
                return
            if need.get(sem, 0) < val:
                need[sem] = val
        for r in reads:
            if r in self.lastw:
                add(self.lastw[r])
        for w in writes:
            if w in self.lastw:
                add(self.lastw[w])
            for ev in self.readers.get(w, ()):
                add(ev)
        for sem, val in need.items():
            self._wait(eng, sem, val)
        inst = issue(self.E[eng])
        if dsem is not None:
            if dsem.count > 0 and not any(w.get(dsem, 0) >= dsem.count for w in self.waited.values()):
                raise RuntimeError("DMA semaphore %s reused while previous DMA may be in flight" % dsem.name)
            dsem.count += 16
            inst.then_inc(dsem.h, 16)
            ev = (dsem, dsem.count, "dma")
        else:
            s = self.esem[eng]
            s.count += 1
            inst.then_inc(s.h, 1)
            ev = (s, s.count, eng)
        for w in writes:
            self.lastw[w] = ev
            self.readers[w] = []
        for r in reads:
            if r not in writes:
                self.readers.setdefault(r, []).append(ev)
        self.nops += 1
        return ev

    def barrier(self, engines=("pe", "act", "dve", "pool", "sp")):
        for e in engines:
            for s in self.allsems:
                if s.count > 0:
                    self._wait(e, s, s.count)
        self.lastw = {}
        self.readers = {}


def _slopes(nh):
    return [float(np.float32(2.0) ** np.float32(-8.0 * (h + 1) / nh)) for h in range(nh)]


class Builder:
    def __init__(self, cfg):
        self.cfg = cfg
        self.nc = bass.Bass("TRN2", target_bir_lowering=False)
        self.kb = KB(self.nc)
        self.dram_in = {}
        self.dram_out = {}
        self.scratch = {}

    def din(self, name, shape, dtype=F32):
        t = self.nc.dram_tensor(name, list(shape), dtype, kind="ExternalInput")
        self.dram_in[name] = (tuple(shape), dtype)
        return t.ap()

    def dscr(self, name, shape, dtype, internal=False):
        kind = "ExternalOutput" if (self.cfg.debug and not internal) else "Internal"
        t = self.nc.dram_tensor(name, list(shape), dtype, kind=kind)
        self.scratch[name] = (tuple(shape), dtype)
        return t.ap()

    def dout(self, name, shape, dtype=F32):
        t = self.nc.dram_tensor(name, list(shape), dtype, kind="ExternalOutput")
        self.dram_out[name] = (tuple(shape), dtype)
        return t.ap()


MAGIC = 12582912.0
TWO_PI = 2.0 * np.pi
CW1 = 6.28125
CW2 = float(np.float32(TWO_PI - CW1))
CW3 = float(TWO_PI - CW1 - CW2)


def _b(cls):
    return cls


class Phases(Builder):
    _uid = 0

    def _nm(self, name):
        Phases._uid += 1
        return "%s_u%d" % (name, Phases._uid)

    def sb(self, st, name, shape, dtype):
        return st.enter_context(self.nc.sbuf_tensor(self._nm(name), list(shape), dtype))

    def ps(self, st, name, shape, dtype=F32):
        return st.enter_context(self.nc.psum_tensor(self._nm(name), list(shape), dtype))

    def dma(self, eng, out, in_, reads, writes, sem, **kw):
        return self.kb.op(eng, lambda e: e.dma_start(out=out, in_=in_, **kw), reads=reads, writes=writes, dsem=sem)

    def declare_io(self):
        c = self.cfg
        NT = c.NSLOT * c.CH
        self.xk = self.din("xk", [NT, c.D])
        self.posk = self.din("posk", [1, NT], I32)
        self.flags = self.din("flags", [1, c.NSLOT * 8])
        self.cT = self.din("cT", [128, 16])
        self.w_ada = self.din("w_ada", [c.D, c.NADA * c.D])
        self.b_adaT = self.din("b_adaT", [128, 96])
        self.gvecT = self.din("gvecT", [128, 48])
        self.w_in = self.din("w_in", [c.D, 3968])
        self.gsmallT = self.din("gsmallT", [128, 22])
        self.w_uq = self.din("w_uq", [c.QL, 2048])
        self.w_ukv = self.din("w_ukv", [c.KVL, 2048])
        self.w_o = self.din("w_o", [c.D, c.D])
        self.w_router = self.din("w_router", [c.D, c.NE])
        self.rbias = self.din("rbias", [1, c.NE])
        nexp = c.NE if c.stop_after >= 6 else 1
        self.w_eg = self.din("w_eg", [nexp, c.D, c.DE])
        self.w_eu = self.din("w_eu", [nexp, c.D, c.DE])
        self.w_ed = self.din("w_ed", [nexp, c.DE, c.D])
        self.w_sg = self.din("w_sg", [c.D, c.DE])
        self.w_su = self.din("w_su", [c.D, c.DE])
        self.w_sd = self.din("w_sd", [c.DE, c.D])
        self.ident_d = self.din("ident", [128, 128])
        self.invfreq2 = self.din("invfreq2", [64, 1])
        self.lnmult = self.din("lnmult", [128, 17, 128])
        self.tri_d = self.din("tri", [128, 128])
        self.lstrict_d = self.din("lstrict", [128, 128])
        self.iota_d = self.din("iota64", [128, c.NE])
        self.out = self.dout("out", [c.CH, c.D])
        self.HT = self.dscr("HT", [2, 128, 16, c.CH], BF16)
        self.KT_A = self.dscr("KT_A", [c.NH, 128, NT], BF16)
        self.KT_B = self.dscr("KT_B", [65, NT], BF16)
        self.V_AUG = self.dscr("V_AUG", [NT, c.NH * 129], BF16)
        self.KAT = self.dscr("KAT", [c.NH, 128, 2 * c.CH], BF16)
        self.VA_AUG = self.dscr("VA_AUG", [2 * c.CH, c.NH * 129], BF16)
        self.QAT = self.dscr("QAT", [c.NH, 128, c.CH], BF16)
        self.NEGCA = self.dscr("NEGCA", [c.NH, c.CH], BF16)
        self.QT_A = self.dscr("QT_A", [c.NH, 128, c.CH], BF16)
        self.QT_B = self.dscr("QT_B", [c.NH, 65, c.CH], BF16)
        self.OB = self.dscr("OB", [c.CH, 1024], F32)
        self.MIXT = self.dscr("MIXT", [128, 16, c.CH], BF16)
        self.XMID = self.dscr("XMID", [c.CH, c.D], F32)
        self.H2T = self.dscr("H2T", [128, 16, c.CH], BF16)
        self.MODT = self.dscr("MODT", [128, 96], F32)
        self.XG = self.dscr("XG", [c.NE * max(c.CAP, 128), c.D], BF16, internal=True)
        self.YS = self.dscr("YS", [c.NE * max(c.CAP, 128), c.D], BF16, internal=True)
        self.YSH = self.dscr("YSH", [c.CH, c.D], F32)

    def phase0(self, st):
        c, kb, nc = self.cfg, self.kb, self.nc
        P = self.P = {}
        P["ident_f"] = self.sb(st, "ident_f", [128, 128], F32)
        P["ident_b"] = self.sb(st, "ident_b", [128, 128], BF16)
        P["ones_f"] = self.sb(st, "ones_f", [128, 128], F32)
        P["ones_b"] = self.sb(st, "ones_b", [128, 128], BF16)
        P["modT"] = self.sb(st, "modT", [128, 96], F32)
        P["gvec"] = self.sb(st, "gvec", [128, 48], F32)
        P["gsm"] = self.sb(st, "gsm", [128, 22], F32)
        P["gmodA"] = self.sb(st, "gmodA", [128, 16], F32)
        P["gmodF"] = self.sb(st, "gmodF", [128, 16], F32)
        P["flag8"] = self.sb(st, "flag8", [128, c.NSLOT * 8], F32)
        P["invf"] = self.sb(st, "invf", [64, 1], F32)
        P["sgn"] = self.sb(st, "sgn", [64, 1], F32)
        P["kmax2"] = self.sb(st, "kmax2", [1, 2], F32)
        cs = [kb.dsem("c%d" % i) for i in range(8)]
        s0 = cs[0]
        self.dma("sp", P["ident_f"][:], self.ident_d, [], ["ident_f"], cs[0])
        self.dma("pool", P["ident_b"][:], self.ident_d, [], ["ident_b"], cs[1])
        self.dma("sp", P["gvec"][:], self.gvecT, [], ["gvec"], cs[2])
        self.dma("sp", P["gsm"][:], self.gsmallT, [], ["gsm"], cs[3])
        self.dma("sp", P["flag8"][:], self.flags.partition_broadcast(128), [], ["flag8"], cs[4])
        self.dma("sp", P["invf"][:], self.invfreq2, [], ["invf"], cs[5])
        kb.op("dve", lambda e: e.memset(P["ones_f"][:], 1.0), writes=["ones_f"])
        kb.op("dve", lambda e: e.memset(P["ones_b"][:], 1.0), writes=["ones_b"])
        kb.op("dve", lambda e: e.memset(P["sgn"][0:32, :], -1.0), writes=["sgn0"])
        kb.op("dve", lambda e: e.memset(P["sgn"][32:64, :], 1.0), writes=["sgn1"])
        kb.op("dve", lambda e: e.memset(P["kmax2"][:], 0.0), writes=["kmax2"])

        with contextlib.ExitStack() as ph:
            cT = self.sb(ph, "cT_s", [128, 16], F32)
            sc = self.sb(ph, "sc_s", [128, 16], BF16)
            bT = self.sb(ph, "bT_s", [128, 96], F32)
            NWA = 3
            wbuf = [self.sb(ph, "wada%d" % i, [128, 16, 512], BF16) for i in range(NWA)]
            wsem = [kb.dsem("wada%d" % i) for i in range(NWA)]
            mps = self.ps(ph, "mod_ps", [128, 96], F32)
            self.dma("sp", cT[:], self.cT, [], ["cT"], cs[6])
            self.dma("sp", bT[:], self.b_adaT, [], ["bT"], cs[7])
            kb.op("act", lambda e: e.activation(out=sc[:], in_=cT[:], func=AF.Silu), reads=["cT"], writes=["sc"])
            wv = self.w_ada.rearrange("(kc p) n -> p kc n", p=128)
            npieces = (c.NADA * c.D) // 512
            for pi in range(npieces):
                b = pi % NWA
                eng = "pool"
                self.dma(eng, wbuf[b][:], wv[:, :, pi * 512:(pi + 1) * 512], [], ["wada%d" % b], wsem[b])
                for fb in range(4):
                    col = pi * 4 + fb
                    for kc in range(16):
                        kb.op("pe", lambda e, b=b, fb=fb, kc=kc, col=col: e.matmul(
                            mps[:, col:col + 1], lhsT=wbuf[b][:, kc, fb * 128:(fb + 1) * 128], rhs=sc[:, kc:kc + 1],
                            start=(kc == 0), stop=(kc == 15)),
                            reads=["wada%d" % b, "sc"], writes=["mod_ps"])
            kb.op("dve", lambda e: e.tensor_tensor(out=P["modT"][:], in0=mps[:], in1=bT[:], op=ALU.add),
                  reads=["mod_ps", "bT"], writes=["modT"])
            kb.op("dve", lambda e: e.scalar_tensor_tensor(out=P["gmodA"][:], in0=P["modT"][:, 16:32], scalar=1.0,
                                                          in1=P["gvec"][:, 0:16], op0=ALU.add, op1=ALU.mult),
                  reads=["modT", "gvec"], writes=["gmodA"])
            kb.op("dve", lambda e: e.scalar_tensor_tensor(out=P["gmodF"][:], in0=P["modT"][:, 64:80], scalar=1.0,
                                                          in1=P["gvec"][:, 16:32], op0=ALU.add, op1=ALU.mult),
                  reads=["modT", "gvec"], writes=["gmodF"])
            if c.debug:
                self.dma("sp", self.MODT, P["modT"][:], ["modT"], [], kb.dsem("dbg"))
            kb.barrier()
            kb.release_dsems(wsem + cs)

    def bcast_tile(self, st, ph, name, srcT, key):
        kb, P = self.kb, self.P
        dst = self.sb(st, name, [128, 2048], F32)
        dg = self.sb(ph, name + "_dg", [128, 128], F32)
        for ci in range(16):
            pst_full, pkk = self.pbank()
            pst = pst_full[:, 0:128]
            kb.op("dve", lambda e, ci=ci: e.tensor_scalar(out=dg[:], in0=P["ident_f"][:], scalar1=srcT[:, ci:ci + 1],
                                                         scalar2=None, op0=ALU.mult),
                  reads=["ident_f", key], writes=[name + "_dg"])
            kb.op("pe", lambda e, pst=pst: e.matmul(pst, lhsT=P["ones_f"][:], rhs=dg[:], start=True, stop=True),
                  reads=["ones_f", name + "_dg"], writes=[pkk])
            kb.op("act", lambda e, ci=ci, pst=pst: e.activation(out=dst[:, ci * 128:(ci + 1) * 128], in_=pst, func=AF.Copy),
                  reads=[pkk], writes=[name])
        return dst


def _T128(v):
    v = np.asarray(v).reshape(-1, 128)
    return np.ascontiguousarray(v.T)


def lnmult_table():
    tab = np.full((17, 128, 128), -1e30, np.float32)
    k = np.arange(128)[:, None]
    q = np.arange(128)[None, :]
    for dl in range(17):
        diff = 128 * dl + q - k
        cnt = np.zeros((128, 128), np.int32)
        for w, d in Cfg.WINDOWS:
            cnt += ((diff >= 0) & (diff <= w) & (diff % d == 0)).astype(np.int32)
        tab[dl] = np.where(cnt > 0, np.log(np.maximum(cnt, 1)).astype(np.float32), np.float32(-1e30))
    return np.ascontiguousarray(tab.transpose(1, 0, 2))


def prepare_shared(inp, cfg):
    c = cfg
    sh = {}
    sh["cT"] = _T128(inp["c"][0])
    sh["w_ada"] = np.ascontiguousarray(inp["w_ada"][0])
    sh["b_adaT"] = _T128(inp["b_ada"][0])
    sh["gvecT"] = np.concatenate([_T128(inp["norm_attn_g"][0]), _T128(inp["norm_ffn_g"][0]),
                                  _T128(inp["final_norm_g"])], axis=1)
    w_in = inp["w_in"][0]
    rope = w_in[:, 3840:3904]
    sh["w_in"] = np.concatenate([w_in, rope[:, 32:64], rope[:, 0:32]], axis=1)
    sh["gsmallT"] = np.concatenate([_T128(inp["g_q"][0]), _T128(inp["g_kv"][0]),
                                    _T128(inp["g_out_swa"][0]), _T128(inp["g_out_mla"][0])], axis=1)
    wq = inp["w_uq"][0].reshape(c.QL, c.NH, 192)
    sh["w_uq"] = np.concatenate([wq[:, :, 0:128].reshape(c.QL, -1), wq[:, :, 128:192].reshape(c.QL, -1),
                                 np.concatenate([wq[:, :, 160:192], wq[:, :, 128:160]], axis=2).reshape(c.QL, -1)],
                                axis=1)
    wkv = inp["w_ukv"][0].reshape(c.KVL, c.NH, 256)
    sh["w_ukv"] = np.concatenate([wkv[:, :, 0:128].reshape(c.KVL, -1), wkv[:, :, 128:256].reshape(c.KVL, -1)], axis=1)
    sh["w_o"] = np.ascontiguousarray(inp["w_o"][0])
    sh["w_router"] = np.ascontiguousarray(inp["w_router"][0])
    sh["rbias"] = np.ascontiguousarray(inp["router_bias"][0][None, :])
    sh["w_eg"] = inp["w_exp_gate"][0]
    sh["w_eu"] = inp["w_exp_up"][0]
    sh["w_ed"] = inp["w_exp_down"][0]
    sh["w_sg"] = inp["w_sh_gate"][0]
    sh["w_su"] = inp["w_sh_up"][0]
    sh["w_sd"] = inp["w_sh_down"][0]
    sh["ident"] = np.eye(128, dtype=np.float32)
    half = c.ROPE // 2
    invf = (np.float32(10000.0) ** (-np.arange(half, dtype=np.float32) / np.float32(half))).astype(np.float32)
    sh["invfreq2"] = np.concatenate([invf, invf])[:, None].astype(np.float32)
    sh["lnmult"] = lnmult_table()
    sh["tri"] = (np.arange(128)[:, None] <= np.arange(128)[None, :]).astype(np.float32)
    sh["lstrict"] = (np.arange(128)[:, None] < np.arange(128)[None, :]).astype(np.float32)
    sh["iota64"] = np.ascontiguousarray(np.broadcast_to(np.arange(c.NE, dtype=np.float32)[None, :], (128, c.NE)))
    return sh


def slot_order(core, nchunks, nslot):
    order = [core - s for s in range(core + 1)]
    rest = [j for j in range(nchunks) if j > core]
    order = order + rest
    order = order[:nslot] + [-1] * max(0, nslot - len(order))
    valid = [1.0 if s <= core and order[s] >= 0 else 0.0 for s in range(nslot)]
    return order, valid


def prepare_core(inp, cfg, core, nchunks):
    c = cfg
    x = inp["x"][0]
    pos = inp["positions"][0]
    order, valid = slot_order(core, nchunks, c.NSLOT)
    xs, ps = [], []
    for s, j in enumerate(order):
        if j >= 0:
            xs.append(x[j * c.CH:(j + 1) * c.CH])
            ps.append(pos[j * c.CH:(j + 1) * c.CH])
        else:
            xs.append(x[0:c.CH])
            ps.append(pos[0:c.CH])
    d = {}
    d["xk"] = np.concatenate(xs, axis=0)
    d["posk"] = np.concatenate(ps)[None, :].astype(np.int32)
    d["flags"] = np.repeat(np.asarray(valid, np.float32), 8)[None, :]
    return d


class Phases2(Phases):
    def alloc_psum(self, ph, nf32=6, nbf=2):
        self.pbanks = [self.ps(ph, "pb%d" % i, [128, 512], F32) for i in range(nf32)]
        self.pbi = 0
        self.tbanks = [self.ps(ph, "tb%d" % i, [128, 8, 128], BF16) for i in range(nbf)]
        self.tbi = 0

    def pbank(self):
        i = self.pbi % len(self.pbanks)
        self.pbi += 1
        return self.pbanks[i], "pb%d" % i

    def tbank(self):
        i = self.tbi % len(self.tbanks)
        self.tbi += 1
        return self.tbanks[i], "tb%d" % i

    def alloc_normT(self, ph, pref, Fmax, nslots=2):
        W = {"pref": pref, "n": nslots, "i": 0}
        W["junk"] = self.sb(ph, pref + "_junk", [128, Fmax], BF16)
        W["xn"] = [self.sb(ph, pref + "_xn%d" % i, [128, Fmax], BF16) for i in range(nslots)]
        W["st"] = [self.sb(ph, pref + "_st%d" % i, [128, 4], F32) for i in range(nslots)]
        return W

    def norm_T(self, W, src, src_keys, F, gT, shiftT, gkeys, dst_of, dst_keys, eps_scale=None):
        kb, P, c = self.kb, self.P, self.cfg
        i = W["i"] % W["n"]
        W["i"] += 1
        pref = W["pref"]
        junk, xn, stt = W["junk"], W["xn"][i], W["st"][i]
        kj, kx, ks = pref + "_junk", pref + "_xn%d" % i, pref + "_st%d" % i
        kb.op("act", lambda e: e.activation(out=junk[:, 0:F], in_=src, func=AF.Square, accum_out=stt[:, 0:1]),
              reads=src_keys, writes=[kj, ks])
        kb.op("dve", lambda e: e.tensor_scalar(out=stt[:, 1:2], in0=stt[:, 0:1], scalar1=1.0 / F, scalar2=c.EPS,
                                               op0=ALU.mult, op1=ALU.add), reads=[ks], writes=[ks])
        kb.op("act", lambda e: e.activation(out=stt[:, 2:3], in_=stt[:, 1:2], func=AF.Sqrt), reads=[ks], writes=[ks])
        kb.op("dve", lambda e: e.reciprocal(out=stt[:, 3:4], in_=stt[:, 2:3]), reads=[ks], writes=[ks])
        kb.op("act", lambda e: e.activation(out=xn[:, 0:F], in_=src, func=AF.Copy, scale=stt[:, 3:4]),
              reads=list(src_keys) + [ks], writes=[kx])
        nchunk = F // 128
        for c0 in range(0, nchunk, 8):
            tb, tk = self.tbank()
            n = min(8, nchunk - c0)
            for j in range(n):
                ci = c0 + j
                kb.op("pe", lambda e, j=j, ci=ci: e.transpose(out=tb[:, j, :], in_=xn[:, ci * 128:(ci + 1) * 128],
                                                              identity=P["ident_b"][:]),
                      reads=[kx, "ident_b"], writes=[tk])
            for j in range(n):
                ci = c0 + j
                if shiftT is not None:
                    kb.op("dve", lambda e, j=j, ci=ci: e.tensor_scalar(
                        out=dst_of(ci), in0=tb[:, j, :], scalar1=gT[:, ci:ci + 1], scalar2=shiftT[:, ci:ci + 1],
                        op0=ALU.mult, op1=ALU.add), reads=[tk] + gkeys, writes=dst_keys)
                else:
                    kb.op("dve", lambda e, j=j, ci=ci: e.tensor_scalar(
                        out=dst_of(ci), in0=tb[:, j, :], scalar1=gT[:, ci:ci + 1], scalar2=None,
                        op0=ALU.mult), reads=[tk] + gkeys, writes=dst_keys)

    def alloc_rope(self, ph, pref):
        R = {"pref": pref}
        for n in ["posi"]:
            R[n] = self.sb(ph, pref + n, [64, 512], I32)
        for n in ["ang", "t", "kk", "r", "r2", "sin", "cos"]:
            R[n] = self.sb(ph, pref + n, [64, 512], F32)
        R["sem"] = self.kb.dsem(pref)
        return R

    def rope_tables(self, R, pos_ap):
        kb, P = self.kb, self.P
        p = R["pref"]
        K = lambda n: p + n
        self.dma("sp", R["posi"][:], pos_ap.partition_broadcast(64), [], [K("posi")], R["sem"])
        kb.op("dve", lambda e: e.tensor_copy(out=R["ang"][:], in_=R["posi"][:]), reads=[K("posi")], writes=[K("ang")])
        kb.op("dve", lambda e: e.tensor_scalar(out=R["ang"][:], in0=R["ang"][:], scalar1=P["invf"][:, 0:1], scalar2=None,
                                               op0=ALU.mult), reads=["invf"], writes=[K("ang")])
        kb.op("dve", lambda e: e.tensor_scalar(out=R["t"][:], in0=R["ang"][:], scalar1=float(1.0 / TWO_PI), scalar2=MAGIC,
                                               op0=ALU.mult, op1=ALU.add), reads=[K("ang")], writes=[K("t")])
        kb.op("dve", lambda e: e.tensor_scalar(out=R["kk"][:], in0=R["t"][:], scalar1=-MAGIC, scalar2=None,
                                               op0=ALU.add), reads=[K("t")], writes=[K("kk")])
        kb.op("dve", lambda e: e.scalar_tensor_tensor(out=R["r"][:], in0=R["kk"][:], scalar=-CW1, in1=R["ang"][:],
                                                      op0=ALU.mult, op1=ALU.add), reads=[K("ang"), K("kk")], writes=[K("r")])
        for cw in (CW2, CW3):
            kb.op("dve", lambda e, cw=cw: e.scalar_tensor_tensor(out=R["r"][:], in0=R["kk"][:], scalar=-cw, in1=R["r"][:],
                                                                 op0=ALU.mult, op1=ALU.add), reads=[K("kk")], writes=[K("r")])
        kb.op("dve", lambda e: e.tensor_scalar(out=R["t"][:], in0=R["r"][:], scalar1=float(np.pi / 2), scalar2=None,
                                               op0=ALU.is_gt), reads=[K("r")], writes=[K("t")])
        kb.op("dve", lambda e: e.scalar_tensor_tensor(out=R["r2"][:], in0=R["t"][:], scalar=-float(TWO_PI), in1=R["r"][:],
                                                      op0=ALU.mult, op1=ALU.add), reads=[K("t"), K("r")], writes=[K("r2")])
        kb.op("dve", lambda e: e.tensor_scalar(out=R["r2"][:], in0=R["r2"][:], scalar1=float(np.pi / 2), scalar2=None,
                                               op0=ALU.add), reads=[], writes=[K("r2")])
        lim = 3.1415925
        for n in ["r", "r2"]:
            kb.op("dve", lambda e, n=n: e.tensor_scalar(out=R[n][:], in0=R[n][:], scalar1=lim, scalar2=-lim,
                                                        op0=ALU.min, op1=ALU.max), reads=[], writes=[K(n)])
        kb.op("act", lambda e: e.activation(out=R["sin"][:], in_=R["r"][:], func=AF.Sin), reads=[K("r")], writes=[K("sin")])
        kb.op("act", lambda e: e.activation(out=R["cos"][:], in_=R["r2"][:], func=AF.Sin), reads=[K("r2")], writes=[K("cos")])
        kb.op("dve", lambda e: e.tensor_scalar(out=R["sin"][:], in0=R["sin"][:], scalar1=P["sgn"][:, 0:1], scalar2=None,
                                               op0=ALU.mult), reads=["sgn0", "sgn1"], writes=[K("sin")])

    def apply_rope(self, R, a_ps, b_ps, ps_keys, out_ap, out_keys, tmp, tmpk):
        kb = self.kb
        p = R["pref"]
        kb.op("dve", lambda e: e.tensor_tensor(out=tmp[0][:], in0=a_ps, in1=R["cos"][:], op=ALU.mult),
              reads=ps_keys + [p + "cos"], writes=[tmpk + "0"])
        kb.op("dve", lambda e: e.tensor_tensor(out=tmp[1][:], in0=b_ps, in1=R["sin"][:], op=ALU.mult),
              reads=ps_keys + [p + "sin"], writes=[tmpk + "1"])
        kb.op("pool", lambda e: e.tensor_tensor(out=out_ap, in0=tmp[0][:], in1=tmp[1][:], op=ALU.add),
              reads=[tmpk + "0", tmpk + "1"], writes=out_keys)


class Phases3(Phases2):
    def phase1a(self):
        c, kb, P = self.cfg, self.kb, self.P
        NG = c.NSLOT * c.CH // 512
        gps = c.CH // 512
        with contextlib.ExitStack() as ph:
            self.alloc_psum(ph, 6, 2)
            wkv = self.sb(ph, "wkv", [128, 16, 384], BF16)
            wukv = self.sb(ph, "wukv", [128, 2, 2048], BF16)
            ws = kb.dsem("w1a")
            self.dma("pool", wkv[:], self.w_in[:, 3584:3968].rearrange("(kc p) n -> p kc n", p=128), [], ["wkv"], ws)
            ws2 = kb.dsem("w1a2")
            self.dma("pool", wukv[:], self.w_ukv.rearrange("(kc p) n -> p kc n", p=128), [], ["wukv"], ws2)
            NX = 3
            xt = [self.sb(ph, "xt%d" % i, [128, 2048], F32) for i in range(NX)]
            xs = [kb.dsem("xt%d" % i) for i in range(NX)]
            WN = self.alloc_normT(ph, "n1", 2048, 2)
            WNb = self.alloc_normT(ph, "n1b", 256, 2)
            hT = [self.sb(ph, "hT%d" % i, [128, 16, 512], BF16) for i in range(2)]
            hs = [kb.dsem("hT%d" % i) for i in range(2)]
            ckvnT = [self.sb(ph, "ckvnT%d" % i, [128, 2, 512], BF16) for i in range(2)]
            kT_st = [self.sb(ph, "kTst%d" % i, [128, 8, 512], BF16) for i in range(2)]
            kTs = [kb.dsem("kTst%d" % i) for i in range(2)]
            kb_st = [self.sb(ph, "kbst%d" % i, [65, 512], BF16) for i in range(2)]
            kbs = [kb.dsem("kbst%d" % i) for i in range(2)]
            va_st = [self.sb(ph, "vast%d" % i, [128, 4, 8 * 129], BF16) for i in range(2)]
            vas = [kb.dsem("vast%d" % i) for i in range(2)]
            sq = self.sb(ph, "sq1a", [128, 8, 512], BF16)
            sqr = self.sb(ph, "sqr1a", [64, 512], BF16)
            sqmax = self.sb(ph, "sqmax1a", [128, 512], BF16)
            kmrun = self.sb(ph, "kmrun", [1, 512], F32)
            tmp = [self.sb(ph, "rtmp%d" % i, [64, 512], F32) for i in range(2)]
            R = self.alloc_rope(ph, "r1a")
            kb.op("dve", lambda e: e.memset(kmrun[:], 0.0), writes=["kmrun"])
            for i in range(2):
                kb.op("dve", lambda e, i=i: e.memset(kb_st[i][64:65, :], 1.0), writes=["kbst%d_one" % i])
            xv = self.xk.rearrange("(n p) d -> n p d", p=128)
            cnt1a = [0]

            def stageA(g):
                slot = g // gps
                tok0 = g * 512
                b = g % 2
                for t in range(4):
                    xi = cnt1a[0] % NX
                    cnt1a[0] += 1
                    self.dma("sp", xt[xi][:], xv[g * 4 + t], [], ["xt%d" % xi], xs[xi])
                    self.norm_T(WN, xt[xi][:], ["xt%d" % xi], 2048, P["gmodA"], P["modT"], ["gmodA", "modT"],
                                lambda ci, t=t, b=b: hT[b][:, ci, t * 128:(t + 1) * 128], ["hT%d" % b])
                if slot < 2:
                    self.dma("pool", self.HT[slot, :, :, (g % gps) * 512:(g % gps + 1) * 512], hT[b][:], ["hT%d" % b], [], hs[b])

            def stageB(g):
                slot = g // gps
                tok0 = g * 512
                b = g % 2
                self.rope_tables(R, self.posk[:, tok0:tok0 + 512])
                for t in range(4):
                    pb, pk = self.pbank()
                    for ci in range(16):
                        kb.op("pe", lambda e, ci=ci, t=t, pb=pb: e.matmul(
                            pb[:, 0:256], lhsT=hT[b][:, ci, t * 128:(t + 1) * 128], rhs=wkv[:, ci, 0:256],
                            start=(ci == 0), stop=(ci == 15)), reads=["hT%d" % b, "wkv"], writes=[pk])
                    self.norm_T(WNb, pb[:, 0:256], [pk], 256, P["gsm"][:, 4:6], None, ["gsm"],
                                lambda ci, t=t, b=b: ckvnT[b][:, ci, t * 128:(t + 1) * 128], ["ckvnT%d" % b])
                pa, pak = self.pbank()
                pbb, pbk = self.pbank()
                for (pp, ppk, c0) in ((pa, pak, 256), (pbb, pbk, 320)):
                    for ci in range(16):
                        kb.op("pe", lambda e, ci=ci, pp=pp, c0=c0: e.matmul(
                            pp[0:64, :], lhsT=wkv[:, ci, c0:c0 + 64], rhs=hT[b][:, ci, :],
                            start=(ci == 0), stop=(ci == 15)), reads=["hT%d" % b, "wkv"], writes=[ppk])
                self.apply_rope(R, pa[0:64, :], pbb[0:64, :], [pak, pbk], kb_st[b][0:64, :], ["kbst%d" % b], tmp, "rtmp")
                kb.op("act", lambda e: e.activation(out=sqr[:], in_=kb_st[b][0:64, :], func=AF.Square),
                      reads=["kbst%d" % b], writes=["sqr1a"])
                for h in range(c.NH):
                    pb, pk = self.pbank()
                    for kc in range(2):
                        kb.op("pe", lambda e, kc=kc, h=h, pb=pb: e.matmul(
                            pb[:], lhsT=wukv[:, kc, h * 128:(h + 1) * 128], rhs=ckvnT[b][:, kc, :],
                            start=(kc == 0), stop=(kc == 1)), reads=["ckvnT%d" % b, "wukv"], writes=[pk])
                    kb.op("act", lambda e, h=h, pb=pb: e.activation(out=kT_st[b][:, h, :], in_=pb[:], func=AF.Copy),
                          reads=[pk], writes=["kTst%d" % b])
                    kb.op("act", lambda e, h=h, pb=pb: e.activation(out=sq[:, h, :], in_=pb[:], func=AF.Square),
                          reads=[pk], writes=["sq1a"])
                kb.op("dve", lambda e: e.tensor_reduce(out=sqmax[:], in_=sq[:].rearrange("p h t -> p t h"),
                                                       axis=AX.X, op=ALU.max), reads=["sq1a"], writes=["sqmax1a"])
                pu, puk = self.pbank()
                kb.op("pe", lambda e: e.matmul(pu[0:1, :], lhsT=P["ones_b"][:, 0:1], rhs=sqmax[:], start=True, stop=False),
                      reads=["ones_b", "sqmax1a"], writes=[puk])
                kb.op("pe", lambda e: e.matmul(pu[0:1, :], lhsT=P["ones_b"][0:64, 0:1], rhs=sqr[:], start=False, stop=True),
                      reads=["ones_b", "sqr1a"], writes=[puk])
                kb.op("dve", lambda e: e.tensor_tensor(out=kmrun[:], in0=pu[0:1, :], in1=kmrun[:], op=ALU.max),
                      reads=[puk], writes=["kmrun"])
                for t in range(4):
                    for half in range(2):
                        pb, pk = self.pbank()
                        for kc in range(2):
                            kb.op("pe", lambda e, kc=kc, t=t, half=half, pb=pb: e.matmul(
                                pb[:], lhsT=ckvnT[b][:, kc, t * 128:(t + 1) * 128],
                                rhs=wukv[:, kc, 1024 + half * 512:1024 + (half + 1) * 512],
                                start=(kc == 0), stop=(kc == 1)), reads=["ckvnT%d" % b, "wukv"], writes=[pk])
                        dstv = va_st[b][:, t, :].rearrange("p (h f) -> p h f", f=129)[:, half * 4:(half + 1) * 4, 0:128]
                        kb.op("dve", lambda e, pb=pb, dstv=dstv: e.tensor_scalar(
                            out=dstv, in0=pb[:].rearrange("p (h f) -> p h f", f=128),
                            scalar1=P["flag8"][:, slot * 8:slot * 8 + 1], scalar2=None, op0=ALU.mult),
                            reads=[pk, "flag8"], writes=["vast%d" % b])
                    onec = va_st[b][:, t, :].rearrange("p (h f) -> p h f", f=129)[:, :, 128:129]
                    kb.op("pool", lambda e, onec=onec: e.tensor_copy(
                        out=onec, in_=P["flag8"][:, slot * 8:(slot + 1) * 8].unsqueeze(2)),
                        reads=["flag8"], writes=["vast%d" % b])
                self.dma("pool", self.KT_A[:, :, tok0:tok0 + 512].rearrange("h p t -> p h t"), kT_st[b][:],
                         ["kTst%d" % b], [], kTs[b])
                self.dma("pool", self.KT_B[:, tok0:tok0 + 512], kb_st[b][:], ["kbst%d" % b, "kbst%d_one" % b], [], kbs[b])
                self.dma("pool", self.V_AUG[tok0:tok0 + 512, :].rearrange("(t p) f -> p t f", p=128), va_st[b][:],
                         ["vast%d" % b], [], vas[b])

            stageA(0)
            for g in range(NG):
                if g + 1 < NG:
                    stageA(g + 1)
                stageB(g)
            kb.op("dve", lambda e: e.tensor_reduce(out=P["kmax2"][0:1, 0:1], in_=kmrun[:], axis=AX.X, op=ALU.max),
                  reads=["kmrun"], writes=["kmax2"])
            kb.barrier()
            kb.release_dsems([ws, ws2] + xs + hs + kTs + kbs + vas + [R["sem"]])


class Phases4(Phases3):
    def phase1b(self):
        c, kb, P = self.cfg, self.kb, self.P
        gps = c.CH // 512
        with contextlib.ExitStack() as ph:
            self.alloc_psum(ph, 6, 2)
            wkva = self.sb(ph, "wkva", [128, 16, 2048], BF16)
            ws = [kb.dsem("w1b%d" % i) for i in range(2)]
            for i in range(2):
                self.dma("pool", wkva[:, :, i * 1024:(i + 1) * 1024],
                         self.w_in[:, 1024 + i * 1024:2048 + i * 1024].rearrange("(kc p) n -> p kc n", p=128),
                         [], ["wkva%d" % i], ws[i])
            hT = [self.sb(ph, "hTb%d" % i, [128, 16, 512], BF16) for i in range(2)]
            hs = [kb.dsem("hTb%d" % i) for i in range(2)]
            kT_st = [self.sb(ph, "kaTst%d" % i, [128, 8, 512], BF16) for i in range(2)]
            kTs = [kb.dsem("kaTst%d" % i) for i in range(2)]
            va_st = [self.sb(ph, "vastb%d" % i, [128, 4, 8 * 129], BF16) for i in range(2)]
            vas = [kb.dsem("vastb%d" % i) for i in range(2)]
            sq = self.sb(ph, "sq1b", [128, 8, 512], BF16)
            sqmax = self.sb(ph, "sqmax1b", [128, 512], BF16)
            kmrun = self.sb(ph, "kmrunb", [1, 512], F32)
            kb.op("dve", lambda e: e.memset(kmrun[:], 0.0), writes=["kmrunb"])
            for g in range(2 * gps):
                slot = g // gps
                b = g % 2
                tokd = (1 - slot) * c.CH + (g % gps) * 512
                self.dma("sp", hT[b][:], self.HT[slot, :, :, (g % gps) * 512:(g % gps + 1) * 512], [], ["hTb%d" % b], hs[b])
                for h in range(c.NH):
                    pb, pk = self.pbank()
                    for ci in range(16):
                        kb.op("pe", lambda e, ci=ci, h=h, pb=pb: e.matmul(
                            pb[:], lhsT=wkva[:, ci, h * 128:(h + 1) * 128], rhs=hT[b][:, ci, :],
                            start=(ci == 0), stop=(ci == 15)), reads=["hTb%d" % b, "wkva0"], writes=[pk])
                    kb.op("act", lambda e, h=h, pb=pb: e.activation(out=kT_st[b][:, h, :], in_=pb[:], func=AF.Copy),
                          reads=[pk], writes=["kaTst%d" % b])
                    kb.op("act", lambda e, h=h, pb=pb: e.activation(out=sq[:, h, :], in_=pb[:], func=AF.Square),
                          reads=[pk], writes=["sq1b"])
                kb.op("dve", lambda e: e.tensor_reduce(out=sqmax[:], in_=sq[:].rearrange("p h t -> p t h"),
                                                       axis=AX.X, op=ALU.max), reads=["sq1b"], writes=["sqmax1b"])
                pu, puk = self.pbank()
                kb.op("pe", lambda e: e.matmul(pu[0:1, :], lhsT=P["ones_b"][:, 0:1], rhs=sqmax[:], start=True, stop=True),
                      reads=["ones_b", "sqmax1b"], writes=[puk])
                kb.op("dve", lambda e: e.tensor_tensor(out=kmrun[:], in0=pu[0:1, :], in1=kmrun[:], op=ALU.max),
                      reads=[puk], writes=["kmrunb"])
                for t in range(4):
                    for half in range(2):
                        pb, pk = self.pbank()
                        for ci in range(16):
                            kb.op("pe", lambda e, ci=ci, t=t, half=half, pb=pb: e.matmul(
                                pb[:], lhsT=hT[b][:, ci, t * 128:(t + 1) * 128],
                                rhs=wkva[:, ci, 1024 + half * 512:1024 + (half + 1) * 512],
                                start=(ci == 0), stop=(ci == 15)), reads=["hTb%d" % b, "wkva1"], writes=[pk])
                        dstv = va_st[b][:, t, :].rearrange("p (h f) -> p h f", f=129)[:, half * 4:(half + 1) * 4, 0:128]
                        kb.op("dve", lambda e, pb=pb, dstv=dstv: e.tensor_scalar(
                            out=dstv, in0=pb[:].rearrange("p (h f) -> p h f", f=128),
                            scalar1=P["flag8"][:, slot * 8:slot * 8 + 1], scalar2=None, op0=ALU.mult),
                            reads=[pk, "flag8"], writes=["vastb%d" % b])
                    onec = va_st[b][:, t, :].rearrange("p (h f) -> p h f", f=129)[:, :, 128:129]
                    kb.op("pool", lambda e, onec=onec: e.tensor_copy(
                        out=onec, in_=P["flag8"][:, slot * 8:(slot + 1) * 8].unsqueeze(2)),
                        reads=["flag8"], writes=["vastb%d" % b])
                self.dma("pool", self.KAT[:, :, tokd:tokd + 512].rearrange("h p t -> p h t"), kT_st[b][:],
                         ["kaTst%d" % b], [], kTs[b])
                self.dma("pool", self.VA_AUG[tokd:tokd + 512, :].rearrange("(t p) f -> p t f", p=128), va_st[b][:],
                         ["vastb%d" % b], [], vas[b])
            kb.op("dve", lambda e: e.tensor_reduce(out=P["kmax2"][0:1, 1:2], in_=kmrun[:], axis=AX.X, op=ALU.max),
                  reads=["kmrunb"], writes=["kmax2"])
            kb.barrier()
            kb.release_dsems(ws + hs + kTs + vas)

    def negc_row(self, u_ps, upk, kcol, dst, dstk, tmpf):
        kb, P = self.kb, self.P
        kb.op("act", lambda e: e.activation(out=tmpf[:], in_=u_ps, func=AF.Sqrt, scale=P["kmax2"][0:1, kcol:kcol + 1]),
              reads=[upk, "kmax2"], writes=["negc_tmp"])
        kb.op("dve", lambda e: e.tensor_scalar(out=dst, in0=tmpf[:], scalar1=-1.0, scalar2=None, op0=ALU.mult),
              reads=["negc_tmp"], writes=[dstk])

    def phase1c(self):
        c, kb, P = self.cfg, self.kb, self.P
        gps = c.CH // 512
        with contextlib.ExitStack() as ph:
            self.alloc_psum(ph, 6, 2)
            wq = self.sb(ph, "wq", [128, 16, 1536], BF16)
            wuq = self.sb(ph, "wuq", [128, 4, 2048], BF16)
            ws = [kb.dsem("w1c%d" % i) for i in range(3)]
            self.dma("pool", wq[:, :, 0:1024], self.w_in[:, 0:1024].rearrange("(kc p) n -> p kc n", p=128), [], ["wq0"], ws[0])
            self.dma("pool", wq[:, :, 1024:1536], self.w_in[:, 3072:3584].rearrange("(kc p) n -> p kc n", p=128), [], ["wq1"], ws[1])
            self.dma("pool", wuq[:], self.w_uq.rearrange("(kc p) n -> p kc n", p=128), [], ["wuq"], ws[2])
            WN = self.alloc_normT(ph, "n1c", 512, 2)
            hT = [self.sb(ph, "hTc%d" % i, [128, 16, 512], BF16) for i in range(2)]
            hs = [kb.dsem("hTc%d" % i) for i in range(2)]
            cqnT = [self.sb(ph, "cqnT%d" % i, [128, 4, 512], BF16) for i in range(2)]
            qa_st = [self.sb(ph, "qast%d" % i, [128, 8, 512], BF16) for i in range(2)]
            qas = [kb.dsem("qast%d" % i) for i in range(2)]
            qn_st = [self.sb(ph, "qnst%d" % i, [128, 8, 512], BF16) for i in range(2)]
            qns = [kb.dsem("qnst%d" % i) for i in range(2)]
            qb_st = [self.sb(ph, "qbst%d" % i, [64, 8, 512], BF16) for i in range(2)]
            qbs = [kb.dsem("qbst%d" % i) for i in range(2)]
            nca_st = [self.sb(ph, "ncast0", [1, 8, 512], BF16)] * 2
            ncas = [kb.dsem("ncast0")] * 2
            ncb_st = [self.sb(ph, "ncbst0", [1, 8, 512], BF16)] * 2
            ncbs = [kb.dsem("ncbst0")] * 2
            sq = self.sb(ph, "sq1c", [128, 512], BF16)
            sqr = self.sb(ph, "sqr1c", [64, 512], BF16)
            tmpf = self.sb(ph, "negc_tmp", [1, 512], F32)
            tmp = [self.sb(ph, "rtmpc%d" % i, [64, 512], F32) for i in range(2)]
            R = self.alloc_rope(ph, "r1c")
            for g in range(gps):
                b = g % 2
                tok0 = g * 512
                self.rope_tables(R, self.posk[:, tok0:tok0 + 512])
                self.dma("sp", hT[b][:], self.HT[0, :, :, tok0:tok0 + 512], [], ["hTc%d" % b], hs[b])
                for h in range(c.NH):
                    pb, pk = self.pbank()
                    for ci in range(16):
                        kb.op("pe", lambda e, ci=ci, h=h, pb=pb: e.matmul(
                            pb[:], lhsT=wq[:, ci, h * 128:(h + 1) * 128], rhs=hT[b][:, ci, :],
                            start=(ci == 0), stop=(ci == 15)), reads=["hTc%d" % b, "wq0"], writes=[pk])
                    kb.op("act", lambda e, h=h, pb=pb: e.activation(out=qa_st[b][:, h, :], in_=pb[:], func=AF.Copy),
                          reads=[pk], writes=["qast%d" % b])
                    kb.op("act", lambda e, pb=pb: e.activation(out=sq[:], in_=pb[:], func=AF.Square),
                          reads=[pk], writes=["sq1c"])
                    pu, puk = self.pbank()
                    kb.op("pe", lambda e, pu=pu: e.matmul(pu[0:1, :], lhsT=P["ones_b"][:, 0:1], rhs=sq[:], start=True, stop=True),
                          reads=["ones_b", "sq1c"], writes=[puk])
                    self.negc_row(pu[0:1, :], puk, 1, nca_st[b][0:1, h, :], "ncast0", tmpf)
                for t in range(4):
                    pb, pk = self.pbank()
                    for ci in range(16):
                        kb.op("pe", lambda e, ci=ci, t=t, pb=pb: e.matmul(
                            pb[:], lhsT=hT[b][:, ci, t * 128:(t + 1) * 128], rhs=wq[:, ci, 1024:1536],
                            start=(ci == 0), stop=(ci == 15)), reads=["hTc%d" % b, "wq1"], writes=[pk])
                    self.norm_T(WN, pb[:], [pk], 512, P["gsm"][:, 0:4], None, ["gsm"],
                                lambda ci, t=t, b=b: cqnT[b][:, ci, t * 128:(t + 1) * 128], ["cqnT%d" % b])
                for h in range(c.NH):
                    pb, pk = self.pbank()
                    for kc in range(4):
                        kb.op("pe", lambda e, kc=kc, h=h, pb=pb: e.matmul(
                            pb[:], lhsT=wuq[:, kc, h * 128:(h + 1) * 128], rhs=cqnT[b][:, kc, :],
                            start=(kc == 0), stop=(kc == 3)), reads=["cqnT%d" % b, "wuq"], writes=[pk])
                    kb.op("act", lambda e, h=h, pb=pb: e.activation(out=qn_st[b][:, h, :], in_=pb[:], func=AF.Copy),
                          reads=[pk], writes=["qnst%d" % b])
                    kb.op("act", lambda e, pb=pb: e.activation(out=sq[:], in_=pb[:], func=AF.Square),
                          reads=[pk], writes=["sq1c"])
                    pa, pak = self.pbank()
                    pbb, pbk = self.pbank()
                    for (pp, ppk, c0) in ((pa, pak, 1024), (pbb, pbk, 1536)):
                        for kc in range(4):
                            kb.op("pe", lambda e, kc=kc, pp=pp, c0=c0, h=h: e.matmul(
                                pp[0:64, :], lhsT=wuq[:, kc, c0 + h * 64:c0 + (h + 1) * 64], rhs=cqnT[b][:, kc, :],
                                start=(kc == 0), stop=(kc == 3)), reads=["cqnT%d" % b, "wuq"], writes=[ppk])
                    self.apply_rope(R, pa[0:64, :], pbb[0:64, :], [pak, pbk], qb_st[b][:, h, :], ["qbst%d" % b], tmp, "rtmpc")
                    kb.op("act", lambda e, h=h: e.activation(out=sqr[:], in_=qb_st[b][:, h, :], func=AF.Square),
                          reads=["qbst%d" % b], writes=["sqr1c"])
                    pu, puk = self.pbank()
                    kb.op("pe", lambda e, pu=pu: e.matmul(pu[0:1, :], lhsT=P["ones_b"][:, 0:1], rhs=sq[:], start=True, stop=False),
                          reads=["ones_b", "sq1c"], writes=[puk])
                    kb.op("pe", lambda e, pu=pu: e.matmul(pu[0:1, :], lhsT=P["ones_b"][0:64, 0:1], rhs=sqr[:], start=False, stop=True),
                          reads=["ones_b", "sqr1c"], writes=[puk])
                    self.negc_row(pu[0:1, :], puk, 0, ncb_st[b][0:1, h, :], "ncbst0", tmpf)
                self.dma("pool", self.QAT[:, :, tok0:tok0 + 512].rearrange("h p t -> p h t"), qa_st[b][:], ["qast%d" % b], [], qas[b])
                self.dma("pool", self.NEGCA[:, tok0:tok0 + 512].rearrange("(o h) t -> o h t", o=1), nca_st[b][:], ["ncast0"], [], ncas[b])
                self.dma("pool", self.QT_A[:, :, tok0:tok0 + 512].rearrange("h p t -> p h t"), qn_st[b][:], ["qnst%d" % b], [], qns[b])
                self.dma("pool", self.QT_B[:, 0:64, tok0:tok0 + 512].rearrange("h p t -> p h t"), qb_st[b][:], ["qbst%d" % b], [], qbs[b])
                self.dma("pool", self.QT_B[:, 64:65, tok0:tok0 + 512].rearrange("h o t -> o h t"), ncb_st[b][:], ["ncbst0"], [], ncbs[b])
            kb.barrier()
            kb.release_dsems(ws + hs + qas + qns + qbs + [ncas[0], ncbs[0], R["sem"]])


class Phases5(Phases4):
    def phase2(self):
        c, kb, P = self.cfg, self.kb, self.P
        NQ = c.CH // 128
        slopes = _slopes(c.NH)
        scale = float(c.HD ** -0.5)
        with contextlib.ExitStack() as ph:
            stp = [self.ps(ph, "stp%d" % i, [128, 8, 128], F32) for i in range(2)]
            accp = [self.ps(ph, "accp%d" % i, [128, 3, 129], F32) for i in range(3)]
            self.tbanks = [self.ps(ph, "tbs", [128, 8, 128], BF16)]
            self.tbi = 0
            def acc_of(h):
                return accp[h // 3][:, h % 3, :], "accp%d" % (h // 3)
            kaT = self.sb(ph, "kaT", [128, 8, 2 * c.CH], BF16)
            VA = self.sb(ph, "VAr", [128, 2 * NQ, 8 * 129], BF16)
            sems = [kb.dsem("p2_%d" % i) for i in range(12)]
            self.dma("sp", kaT[:], self.KAT.rearrange("h p t -> p h t"), [], ["kaT"], sems[0])
            nva = 4
            per = 2 * NQ // nva
            for i in range(nva):
                self.dma("sp", VA[:, i * per:(i + 1) * per, :],
                         self.VA_AUG[i * per * 128:(i + 1) * per * 128, :].rearrange("(t p) f -> p t f", p=128),
                         [], ["VA%d" % i], sems[8 + i])
            posq_i = self.sb(ph, "posq_i", [128, c.CH], I32)
            posq = self.sb(ph, "posq", [128, c.CH], F32)
            posk_i = self.sb(ph, "posk_i", [128, 2 * NQ], I32)
            poskc = self.sb(ph, "poskc", [128, 2 * NQ], F32)
            lnm = self.sb(ph, "lnm", [128, 17, 128], F32)
            NS = self.sb(ph, "NS", [128, 8, 128], F32)
            self.dma("sp", posq_i[:], self.posk[:, 0:c.CH].partition_broadcast(128), [], ["posq_i"], sems[3])
            self.dma("sp", posk_i[:, 0:NQ], self.posk[:, c.CH:2 * c.CH].rearrange("o (t p) -> p (o t)", p=128),
                     [], ["posk_i0"], sems[4], allow_slow_non_contiguous=True)
            self.dma("sp", posk_i[:, NQ:2 * NQ], self.posk[:, 0:c.CH].rearrange("o (t p) -> p (o t)", p=128),
                     [], ["posk_i1"], sems[5], allow_slow_non_contiguous=True)
            self.dma("sp", lnm[:], self.lnmult, [], ["lnm"], sems[6])
            kb.op("dve", lambda e: e.tensor_copy(out=posq[:], in_=posq_i[:]), reads=["posq_i"], writes=["posq"])
            kb.op("dve", lambda e: e.tensor_copy(out=poskc[:], in_=posk_i[:]), reads=["posk_i0", "posk_i1"], writes=["poskc"])
            kb.op("dve", lambda e: e.tensor_scalar(out=poskc[:], in0=poskc[:], scalar1=-1.0, scalar2=None, op0=ALU.mult),
                  reads=[], writes=["poskc"])
            for h in range(c.NH):
                kb.op("dve", lambda e, h=h: e.memset(NS[:, h, :], -slopes[h]), writes=["NS"])
            qa = [self.sb(ph, "qa%d" % i, [128, 8, 128], BF16) for i in range(2)]
            qsem = [kb.dsem("qa%d" % i) for i in range(2)]
            ngc = [self.sb(ph, "ngc%d" % i, [1, 8, 128], BF16) for i in range(2)]
            nsem = [kb.dsem("ngc%d" % i) for i in range(2)]
            dist = [self.sb(ph, "dist%d" % i, [128, 128], F32) for i in range(2)]
            u = [self.sb(ph, "u%d" % i, [128, 8, 128], F32) for i in range(2)]
            sp_ = [self.sb(ph, "sp%d" % i, [128, 8, 128], F32) for i in range(2)]
            pT = [self.sb(ph, "pT%d" % i, [128, 8, 128], BF16) for i in range(2)]
            oa = self.sb(ph, "oa", [128, 1024], F32)
            rl = self.sb(ph, "rl", [128, 8], F32)
            WN = self.alloc_normT(ph, "n2", 1024, 1)
            mx = [self.sb(ph, "mxa%d" % i, [128, 8, 128], BF16) for i in range(2)]
            msem = [kb.dsem("mxa%d" % i) for i in range(2)]
            seq = [(b, dl) for b in range(NQ) for dl in range(16, -1, -1)]

            def front(n):
                b, dl = seq[n]
                qb = b % 2
                k = n % 2
                if dl == 16:
                    self.dma("sp", qa[qb][:], self.QAT[:, :, b * 128:(b + 1) * 128].rearrange("h p t -> p h t"), [], ["qa%d" % qb], qsem[qb])
                    self.dma("sp", ngc[qb][:], self.NEGCA[:, b * 128:(b + 1) * 128].rearrange("(o h) t -> o h t", o=1), [], ["ngc%d" % qb], nsem[qb])
                j = NQ + b - dl
                kb.op("act", lambda e: e.activation(
                    out=dist[k][:], in_=posq[:, b * 128:(b + 1) * 128], func=AF.Abs, bias=poskc[:, j:j + 1], scale=1.0),
                    reads=["posq", "poskc"], writes=["dist%d" % k])
                kb.op("pool", lambda e: e.tensor_tensor(
                    out=u[k][:], in0=dist[k][:].unsqueeze(1).broadcast_to([128, 8, 128]), in1=NS[:], op=ALU.mult),
                    reads=["dist%d" % k, "NS"], writes=["u%d" % k])
                kb.op("pool", lambda e: e.tensor_tensor(
                    out=u[k][:], in0=u[k][:], in1=lnm[:, dl, :].unsqueeze(1).broadcast_to([128, 8, 128]), op=ALU.add),
                    reads=["lnm"], writes=["u%d" % k])
                for h in range(c.NH):
                    kb.op("pe", lambda e, h=h: e.matmul(
                        stp[k][:, h, :], lhsT=kaT[:, h, j * 128:(j + 1) * 128], rhs=qa[qb][:, h, :], start=True, stop=False),
                        reads=["kaT", "qa%d" % qb], writes=["stp%d" % k])
                    kb.op("pe", lambda e, h=h: e.matmul(
                        stp[k][:, h, :], lhsT=P["ones_b"][0:1, :], rhs=ngc[qb][0:1, h, :], start=False, stop=True),
                        reads=["ones_b", "ngc%d" % qb], writes=["stp%d" % k])
                kb.op("dve", lambda e: e.scalar_tensor_tensor(
                    out=sp_[k][:], in0=stp[k][:], scalar=scale, in1=u[k][:], op0=ALU.mult, op1=ALU.add),
                    reads=["stp%d" % k, "u%d" % k], writes=["sp%d" % k])
                kb.op("act", lambda e: e.activation(out=pT[k][:], in_=sp_[k][:], func=AF.Exp),
                      reads=["sp%d" % k], writes=["pT%d" % k])

            def back(n):
                b, dl = seq[n]
                k = n % 2
                j = NQ + b - dl
                vak = "VA%d" % (j // per)
                if dl == 16:
                    for i3 in range(3):
                        kb.op("dve", lambda e, i3=i3: e.memset(accp[i3][:], 0.0), writes=["accp%d" % i3])
                for h in range(c.NH):
                    ah, ak = acc_of(h)
                    kb.op("pe", lambda e, h=h, ah=ah: e.matmul(
                        ah, lhsT=pT[k][:, h, :], rhs=VA[:, j, h * 129:(h + 1) * 129], start=False, stop=(dl == 0)),
                        reads=["pT%d" % k, vak], writes=[ak])
                if dl != 0:
                    return
                for i3 in range(3):
                    nh = 3 if i3 < 2 else 2
                    kb.op("dve", lambda e, i3=i3, nh=nh: e.reciprocal(
                        out=rl[:, i3 * 3:i3 * 3 + nh].unsqueeze(2), in_=accp[i3][:, 0:nh, 128:129]),
                        reads=["accp%d" % i3], writes=["rl"])
                for h in range(c.NH):
                    ah, ak = acc_of(h)
                    kb.op("dve", lambda e, h=h, ah=ah: e.tensor_scalar(
                        out=oa[:, h * 128:(h + 1) * 128], in0=ah[:, 0:128], scalar1=rl[:, h:h + 1], scalar2=None, op0=ALU.mult),
                        reads=[ak, "rl"], writes=["oa"])
                mb = b % 2
                self.norm_T(WN, oa[:], ["oa"], 1024, P["gsm"][:, 6:14], None, ["gsm"],
                            lambda ci, mb=mb: mx[mb][:, ci, :], ["mxa%d" % mb])
                self.dma("pool", self.MIXT[:, 0:8, b * 128:(b + 1) * 128], mx[mb][:], ["mxa%d" % mb], [], msem[mb])

            front(0)
            for n in range(len(seq)):
                if n + 1 < len(seq):
                    front(n + 1)
                back(n)
            kb.barrier()
            kb.release_dsems(sems + qsem + nsem + msem)


class Phases6(Phases5):
    def phase3(self):
        c, kb, P = self.cfg, self.kb, self.P
        NT = c.NSLOT * c.CH
        TPS = c.CH // 128
        NGQ = c.CH // 512
        scale = float((128 + c.ROPE) ** -0.5)
        with contextlib.ExitStack() as ph:
            self.alloc_psum(ph, 4, 0)
            NPT = 4
            accs = []
            for i in range(2):
                accs.append((self.ps(ph, "macc%da" % i, [128, 3, 129], F32), self.ps(ph, "macc%db" % i, [128, 1, 129], F32)))
            tri = self.sb(ph, "tri_b", [128, 128], BF16)
            ts = kb.dsem("tri")
            self.dma("pool", tri[:], self.tri_d, [], ["tri"], ts)
            ktB = self.sb(ph, "ktB", [65, NT], BF16)
            kbs = kb.dsem("ktB")
            self.dma("sp", ktB[:], self.KT_B, [], ["ktB"], kbs)
            ktA = [self.sb(ph, "ktA%d" % i, [128, NT], BF16) for i in range(2)]
            Vh = [self.sb(ph, "Vh%d" % i, [128, NT // 128, 129], BF16) for i in range(2)]
            kas = [[kb.dsem("ktA%d_%d" % (i, p)) for p in range(c.NSLOT)] for i in range(2)]
            vs = [[kb.dsem("Vh%d_%d" % (i, p)) for p in range(c.NSLOT)] for i in range(2)]
            qTa = [self.sb(ph, "qTa%d" % i, [128, c.CH], BF16) for i in range(2)]
            qTb = [self.sb(ph, "qTb%d" % i, [65, c.CH], BF16) for i in range(2)]
            qsa = [kb.dsem("qTa%d" % i) for i in range(2)]
            qsb = [kb.dsem("qTb%d" % i) for i in range(2)]
            pT = [self.sb(ph, "mpT%d" % i, [128, 512], BF16) for i in range(NPT)]
            obst = [self.sb(ph, "obst%d" % i, [128, 4, 128], F32) for i in range(2)]
            obs = [kb.dsem("obst%d" % i) for i in range(2)]
            rl = self.sb(ph, "mrl", [128, 4], F32)
            pti = 0
            it = 0
            for h in range(c.NH):
                hb = h % 2
                self.dma("sp", qTa[hb][:], self.QT_A[h], [], ["qTa%d" % hb], qsa[hb])
                self.dma("sp", qTb[hb][:], self.QT_B[h], [], ["qTb%d" % hb], qsb[hb])
                for p in range(c.NSLOT):
                    self.dma("sp", ktA[hb][:, p * c.CH:(p + 1) * c.CH], self.KT_A[h, :, p * c.CH:(p + 1) * c.CH],
                             [], ["ktA%d_%d" % (hb, p)], kas[hb][p])
                    self.dma("sp", Vh[hb][:, p * TPS:(p + 1) * TPS, :],
                             self.V_AUG[p * c.CH:(p + 1) * c.CH, h * 129:(h + 1) * 129].rearrange("(t p) f -> p t f", p=128),
                             [], ["Vh%d_%d" % (hb, p)], vs[hb][p])
                for g in range(NGQ):
                    ab = it % 2
                    it += 1
                    accA, accB = accs[ab]
                    def acc_of(i):
                        return (accA[:, i, :], "macc%da" % ab) if i < 3 else (accB[:, 0, :], "macc%db" % ab)
                    tiles = [(jt, max(0, jt - 4 * g), (jt - 4 * g) if jt >= 4 * g else None) for jt in range(4 * g + 4)]
                    tiles += [(kt, 0, None) for kt in range(TPS, c.NSLOT * TPS)]
                    kb.op("dve", lambda e: e.memset(accA[:], 0.0), writes=["macc%da" % ab])
                    kb.op("dve", lambda e: e.memset(accB[:], 0.0), writes=["macc%db" % ab])
                    first = {}
                    last = {}
                    for n, (kt, imin, idg) in enumerate(tiles):
                        for i in range(imin, 4):
                            first.setdefault(i, n)
                            last[i] = n
                    LAG = 2
                    pend = {}
                    for n2 in range(len(tiles) + LAG):
                        if n2 < len(tiles):
                            n = n2
                            kt, imin, idg = tiles[n]
                            p = kt // TPS
                            pb, pk = self.pbank()
                            q0 = g * 512 + imin * 128
                            q1 = (g + 1) * 512
                            kb.op("pe", lambda e, pb=pb, kt=kt, imin=imin, q0=q0, q1=q1: e.matmul(
                                pb[:, imin * 128:512], lhsT=ktA[hb][:, kt * 128:(kt + 1) * 128], rhs=qTa[hb][:, q0:q1],
                                start=True, stop=False), reads=["ktA%d_%d" % (hb, p), "qTa%d" % hb], writes=[pk])
                            kb.op("pe", lambda e, pb=pb, kt=kt, imin=imin, q0=q0, q1=q1: e.matmul(
                                pb[:, imin * 128:512], lhsT=ktB[:, kt * 128:(kt + 1) * 128], rhs=qTb[hb][:, q0:q1],
                                start=False, stop=True), reads=["ktB", "qTb%d" % hb], writes=[pk])
                            pi = pti % NPT
                            pti += 1
                            kb.op("act", lambda e, pb=pb, pi=pi, imin=imin: e.activation(
                                out=pT[pi][:, imin * 128:512], in_=pb[:, imin * 128:512], func=AF.Exp, scale=scale),
                                reads=[pk], writes=["mpT%d" % pi])
                            if idg is not None:
                                kb.op("pool", lambda e, pi=pi, idg=idg: e.tensor_tensor(
                                    out=pT[pi][:, idg * 128:(idg + 1) * 128], in0=pT[pi][:, idg * 128:(idg + 1) * 128],
                                    in1=tri[:], op=ALU.mult), reads=["tri"], writes=["mpT%d" % pi])
                            pend[n] = pi
                        if n2 >= LAG:
                            n = n2 - LAG
                            kt, imin, idg = tiles[n]
                            p = kt // TPS
                            pi = pend.pop(n)
                            for i in range(imin, 4):
                                ai, ak = acc_of(i)
                                kb.op("pe", lambda e, ai=ai, pi=pi, i=i, kt=kt, n=n: e.matmul(
                                    ai, lhsT=pT[pi][:, i * 128:(i + 1) * 128], rhs=Vh[hb][:, kt, :],
                                    start=False, stop=(last[i] == n)),
                                    reads=["mpT%d" % pi, "Vh%d_%d" % (hb, p)], writes=[ak])
                    ob = obst[ab]
                    kb.op("dve", lambda e: e.reciprocal(out=rl[:, 0:3].unsqueeze(2), in_=accA[:, :, 128:129]),
                          reads=["macc%da" % ab], writes=["mrl"])
                    kb.op("dve", lambda e: e.reciprocal(out=rl[:, 3:4].unsqueeze(2), in_=accB[:, :, 128:129]),
                          reads=["macc%db" % ab], writes=["mrl"])
                    for i in range(4):
                        ai, ak = acc_of(i)
                        kb.op("dve", lambda e, ai=ai, i=i, ob=ob: e.tensor_scalar(
                            out=ob[:, i, :], in0=ai[:, 0:128], scalar1=rl[:, i:i + 1], scalar2=None, op0=ALU.mult),
                            reads=[ak, "mrl"], writes=["obst%d" % ab])
                    self.dma("pool", self.OB[g * 512:(g + 1) * 512, h * 128:(h + 1) * 128].rearrange("(i p) f -> p i f", p=128),
                             ob[:], ["obst%d" % ab], [], obs[ab])
            kb.barrier()
            kb.release_dsems([ts, kbs] + kas[0] + kas[1] + vs[0] + vs[1] + qsa + qsb + obs)


class Phases7(Phases6):
    def phase4(self, st):
        c, kb, P = self.cfg, self.kb, self.P
        NQ = c.CH // 128
        NE = c.NE
        GS = NE // c.NG
        P["Wall"] = self.sb(st, "Wall", [128, NQ, NE], F32)
        P["slotI"] = self.sb(st, "slotI", [128, NQ, 8], I32)
        self.bnd_reg = self.nc.gpsimd.alloc_register("moe_bnd")
        self.nc.gpsimd.reg_mov(self.bnd_reg, NE * c.CAP - 1)
        P["w8"] = self.sb(st, "w8", [128, NQ, 8], F32)
        CAP = c.CAP
        BIGK = 131072.0
        with contextlib.ExitStack() as ph:
            self.alloc_psum(ph, 5, 2)
            Ls = self.sb(ph, "Ls", [128, 128], BF16)
            iot = self.sb(ph, "iot", [128, NE], F32)
            ebase = self.sb(ph, "ebase", [128, NE], F32)
            selb = self.sb(ph, "selb", [128, NQ, NE], BF16)
            lss = kb.dsem("Ls")
            ios = kb.dsem("iot")
            self.dma("pool", Ls[:], self.lstrict_d, [], ["Ls"], lss)
            self.dma("sp", iot[:], self.iota_d, [], ["iot"], ios)
            kb.op("dve", lambda e: e.tensor_scalar(out=ebase[:], in0=iot[:], scalar1=float(CAP), scalar2=None, op0=ALU.mult),
                  reads=["iot"], writes=["ebase"])
            d_selv = self.sb(ph, "d_selv", [128, NE], F32)
            d_slot = self.sb(ph, "d_slot", [128, NE], F32)
            d_k8 = self.sb(ph, "d_k8", [128, 8], F32)
            d_s8 = self.sb(ph, "d_s8", [128, 8], F32)
            d_e8i = self.sb(ph, "d_e8i", [128, 8], I32)
            d_e8f = self.sb(ph, "d_e8f", [128, 8], F32)
            d_junk = self.sb(ph, "d_junk", [128, NE], F32)
            scs = [kb.dsem("scat%d" % i) for i in range(16)]
            sci = 0
            gate_a = self.bcast_tile(ph, ph, "gate_a_bc", P["modT"][:, 32:48], "modT")
            wo = self.sb(ph, "wo", [128, 16, 2048], BF16)
            wos = [kb.dsem("wo%d" % i) for i in range(4)]
            for i in range(4):
                self.dma("pool", wo[:, :, i * 512:(i + 1) * 512],
                         self.w_o[:, i * 512:(i + 1) * 512].rearrange("(kc p) n -> p kc n", p=128), [], ["wo%d" % i], wos[i])
            wr = self.sb(ph, "wr", [128, 16, NE], BF16)
            wrs = kb.dsem("wr")
            self.dma("pool", wr[:], self.w_router.rearrange("(kc p) n -> p kc n", p=128), [], ["wr"], wrs)
            rb = self.sb(ph, "rb_bc", [128, NE], F32)
            rbs = kb.dsem("rb")
            self.dma("sp", rb[:], self.rbias.partition_broadcast(128), [], ["rb"], rbs)
            obt = [self.sb(ph, "obt%d" % i, [128, 1024], F32) for i in range(2)]
            obts = [kb.dsem("obt%d" % i) for i in range(2)]
            mixa = [self.sb(ph, "mixa%d" % i, [128, 8, 128], BF16) for i in range(2)]
            mas = [kb.dsem("mixa%d" % i) for i in range(2)]
            mixb = [self.sb(ph, "mixb%d" % i, [128, 8, 128], BF16) for i in range(2)]
            xt = [self.sb(ph, "xt4_%d" % i, [128, 2048], F32) for i in range(2)]
            xts = [kb.dsem("xt4_%d" % i) for i in range(2)]
            xm = [self.sb(ph, "xm%d" % i, [128, 2048], F32) for i in range(2)]
            xms = [kb.dsem("xm%d" % i) for i in range(2)]
            tmpm = [self.sb(ph, "tmpm%d" % i, [128, 512], F32) for i in range(2)]
            h2 = [self.sb(ph, "h2st%d" % i, [128, 16, 128], BF16) for i in range(2)]
            h2s = [kb.dsem("h2st%d" % i) for i in range(2)]
            WN1 = self.alloc_normT(ph, "n4a", 1024, 1)
            WN2 = self.alloc_normT(ph, "n4b", 2048, 2)
            sc = self.sb(ph, "r_sc", [128, NE], F32)
            ch = self.sb(ph, "r_ch", [128, NE], F32)
            cm = self.sb(ph, "r_cm", [128, NE], F32)
            m8 = self.sb(ph, "r_m8", [128, c.NG, 8], F32)
            grp = self.sb(ph, "r_grp", [128, 8], F32)
            g8 = self.sb(ph, "r_g8", [128, 8], F32)
            gm = self.sb(ph, "r_gm", [128, 8], F32)
            e8 = self.sb(ph, "r_e8", [128, 8], F32)
            sel = self.sb(ph, "r_sel", [128, NE], F32)
            ws_ = self.sb(ph, "r_ws", [128, 2], F32)
            xv = self.xk.rearrange("(n p) d -> n p d", p=128)
            for b in range(NQ):
                k = b % 2
                self.dma("sp", obt[k][:], self.OB[b * 128:(b + 1) * 128, :], [], ["obt%d" % k], obts[k])
                self.dma("sp", mixa[k][:], self.MIXT[:, 0:8, b * 128:(b + 1) * 128], [], ["mixa%d" % k], mas[k])
                self.dma("sp", xt[k][:], xv[b], [], ["xt4_%d" % k], xts[k])
                self.norm_T(WN1, obt[k][:], ["obt%d" % k], 1024, P["gsm"][:, 14:22], None, ["gsm"],
                            lambda ci, k=k: mixb[k][:, ci, :], ["mixb%d" % k])
                for nb in range(4):
                    pb, pk = self.pbank()
                    for ci in range(16):
                        src = mixa[k] if ci < 8 else mixb[k]
                        sk = ("mixa%d" % k) if ci < 8 else ("mixb%d" % k)
                        kb.op("pe", lambda e, ci=ci, nb=nb, pb=pb, src=src: e.matmul(
                            pb[:], lhsT=src[:, ci % 8, :], rhs=wo[:, ci, nb * 512:(nb + 1) * 512],
                            start=(ci == 0), stop=(ci == 15)), reads=[sk, "wo%d" % nb], writes=[pk])
                    tk = nb % 2
                    kb.op("dve", lambda e, nb=nb, pb=pb, tk=tk: e.tensor_tensor(
                        out=tmpm[tk][:], in0=pb[:], in1=gate_a[:, nb * 512:(nb + 1) * 512], op=ALU.mult),
                        reads=[pk, "gate_a_bc"], writes=["tmpm%d" % tk])
                    kb.op("pool", lambda e, nb=nb, tk=tk: e.tensor_tensor(
                        out=xm[k][:, nb * 512:(nb + 1) * 512], in0=tmpm[tk][:], in1=xt[k][:, nb * 512:(nb + 1) * 512], op=ALU.add),
                        reads=["tmpm%d" % tk, "xt4_%d" % k], writes=["xm%d" % k])
                self.dma("pool", self.XMID[b * 128:(b + 1) * 128, :], xm[k][:], ["xm%d" % k], [], xms[k])
                self.norm_T(WN2, xm[k][:], ["xm%d" % k], 2048, P["gmodF"], P["modT"][:, 48:64], ["gmodF", "modT"],
                            lambda ci, k=k: h2[k][:, ci, :], ["h2st%d" % k])
                self.dma("pool", self.H2T[:, :, b * 128:(b + 1) * 128], h2[k][:], ["h2st%d" % k], [], h2s[k])
                pb, pk = self.pbank()
                for ci in range(16):
                    kb.op("pe", lambda e, ci=ci, pb=pb: e.matmul(pb[:, 0:NE], lhsT=h2[k][:, ci, :], rhs=wr[:, ci, :],
                                                                 start=(ci == 0), stop=(ci == 15)),
                          reads=["h2st%d" % k, "wr"], writes=[pk])
                D = lambda fn, r, w: kb.op("dve", fn, reads=r, writes=w)
                kb.op("act", lambda e, pb=pb: e.activation(out=sc[:], in_=pb[:, 0:NE], func=AF.Sigmoid), reads=[pk], writes=["r_sc"])
                D(lambda e: e.tensor_tensor(out=ch[:], in0=sc[:], in1=rb[:], op=ALU.add), ["r_sc", "rb"], ["r_ch"])
                for g in range(c.NG):
                    D(lambda e, g=g: e.max(out=m8[:, g, :], in_=ch[:, g * GS:(g + 1) * GS]), ["r_ch"], ["r_m8"])
                D(lambda e: e.tensor_tensor(out=grp[:].unsqueeze(2), in0=m8[:, :, 0:1], in1=m8[:, :, 1:2], op=ALU.add), ["r_m8"], ["r_grp"])
                D(lambda e: e.max(out=g8[:], in_=grp[:]), ["r_grp"], ["r_g8"])
                D(lambda e: e.tensor_scalar(out=gm[:], in0=grp[:], scalar1=g8[:, c.TOPG - 1:c.TOPG], scalar2=None, op0=ALU.is_ge),
                  ["r_grp", "r_g8"], ["r_gm"])
                D(lambda e: e.tensor_scalar(out=gm[:], in0=gm[:], scalar1=-1.0, scalar2=1e30, op0=ALU.add, op1=ALU.mult), [], ["r_gm"])
                D(lambda e: e.tensor_tensor(out=cm[:].rearrange("p (g s) -> p g s", s=GS), in0=ch[:].rearrange("p (g s) -> p g s", s=GS),
                                            in1=gm[:].unsqueeze(2).broadcast_to([128, c.NG, GS]), op=ALU.add), ["r_ch", "r_gm"], ["r_cm"])
                D(lambda e: e.max(out=e8[:], in_=cm[:]), ["r_cm"], ["r_e8"])
                D(lambda e: e.tensor_scalar(out=sel[:], in0=cm[:], scalar1=e8[:, c.TOPK - 1:c.TOPK], scalar2=None, op0=ALU.is_ge),
                  ["r_cm", "r_e8"], ["r_sel"])
                D(lambda e, b=b: e.tensor_copy(out=selb[:, b, :], in_=sel[:]), ["r_sel"], ["selb%d" % b])
                pp, ppk = self.pbank()
                for b2 in range(b):
                    kb.op("pe", lambda e, b2=b2, pp=pp: e.matmul(pp[:, 0:NE], lhsT=P["ones_b"][:], rhs=selb[:, b2, :],
                                                                 start=(b2 == 0), stop=False),
                          reads=["ones_b", "selb%d" % b2], writes=[ppk])
                kb.op("pe", lambda e, b=b, pp=pp: e.matmul(pp[:, 0:NE], lhsT=Ls[:], rhs=selb[:, b, :], start=(b == 0), stop=True),
                      reads=["Ls", "selb%d" % b], writes=[ppk])
                D(lambda e, pp=pp: e.tensor_scalar(out=d_selv[:], in0=pp[:, 0:NE], scalar1=float(CAP), scalar2=None, op0=ALU.is_lt),
                  [ppk], ["d_selv"])
                D(lambda e: e.tensor_tensor(out=d_selv[:], in0=d_selv[:], in1=sel[:], op=ALU.mult), ["r_sel"], ["d_selv"])
                D(lambda e, pp=pp: e.tensor_tensor(out=d_slot[:], in0=pp[:, 0:NE], in1=ebase[:], op=ALU.add), [ppk, "ebase"], ["d_slot"])
                D(lambda e: e.tensor_scalar(out=d_slot[:], in0=d_slot[:], scalar1=-1.0, scalar2=BIGK, op0=ALU.mult, op1=ALU.add),
                  [], ["d_slot"])
                D(lambda e: e.tensor_tensor(out=d_slot[:], in0=d_slot[:], in1=d_selv[:], op=ALU.mult), ["d_selv"], ["d_slot"])
                D(lambda e: e.max(out=d_k8[:], in_=d_slot[:]), ["d_slot"], ["d_k8"])
                D(lambda e: e.tensor_scalar(out=d_s8[:], in0=d_k8[:], scalar1=-1.0, scalar2=BIGK, op0=ALU.mult, op1=ALU.add),
                  ["d_k8"], ["d_s8"])
                D(lambda e, b=b: e.tensor_copy(out=P["slotI"][:, b, :], in_=d_s8[:]), ["d_s8"], ["slotI%d" % b])
                D(lambda e, b=b: e.tensor_scalar(out=d_e8i[:], in0=P["slotI"][:, b, :], scalar1=int(np.log2(CAP)), scalar2=None,
                                                 op0=ALU.arith_shift_right), ["slotI%d" % b], ["d_e8i"])
                D(lambda e: e.tensor_copy(out=d_e8f[:], in_=d_e8i[:]), ["d_e8i"], ["d_e8f"])
                xn_cur = WN2["xn"][(WN2["i"] - 1) % WN2["n"]]
                xn_key = "n4b_xn%d" % ((WN2["i"] - 1) % WN2["n"])
                for k8 in range(8):
                    sm = scs[sci % 16]
                    sci += 1
                    kb.op("pool", lambda e, b=b, k8=k8, xn_cur=xn_cur: e.indirect_dma_start(
                        out=self.XG[:, :], out_offset=bass.IndirectOffsetOnAxis(ap=P["slotI"][:, b, k8:k8 + 1], axis=0),
                        in_=xn_cur[:, :], in_offset=None, bounds_check=self.bnd_reg, oob_is_err=False),
                        reads=[xn_key, "slotI%d" % b], writes=[], dsem=sm)
                D(lambda e: e.tensor_tensor(out=sel[:], in0=sel[:], in1=sc[:], op=ALU.mult), ["r_sc"], ["r_sel"])
                D(lambda e: e.tensor_reduce(out=ws_[:, 0:1], in_=sel[:], axis=AX.X, op=ALU.add), ["r_sel"], ["r_ws"])
                D(lambda e: e.reciprocal(out=ws_[:, 1:2], in_=ws_[:, 0:1]), [], ["r_ws"])
                D(lambda e, b=b: e.tensor_scalar(out=P["Wall"][:, b, :], in0=sel[:], scalar1=ws_[:, 1:2], scalar2=float(c.ROUTED_SCALE),
                                                 op0=ALU.mult, op1=ALU.mult), ["r_sel", "r_ws"], ["Wall"])
                for k8 in range(8):
                    D(lambda e, b=b, k8=k8: e.scalar_tensor_tensor(
                        out=d_junk[:], in0=iot[:], scalar=d_e8f[:, k8:k8 + 1], in1=P["Wall"][:, b, :],
                        op0=ALU.is_equal, op1=ALU.mult, accum_out=P["w8"][:, b, k8:k8 + 1]),
                      ["iot", "d_e8f", "Wall"], ["d_junk", "w8_%d" % b])
            if c.debug:
                self.WALL = self.dscr("WALL", [128, NQ * NE], F32)
                self.dma("sp", self.WALL, P["Wall"][:].rearrange("p a b -> p (a b)"), ["Wall"], [], rbs)
            kb.barrier()
            kb.release_dsems(wos + [wrs, rbs, lss, ios] + obts + mas + xts + xms + h2s + scs)

    def phase6(self, shared_only=False):
        c, kb, P = self.cfg, self.kb, self.P
        NE = c.NE
        TH = c.CH // 2
        NTT = TH // 128
        with contextlib.ExitStack() as ph:
            self.alloc_psum(ph, 8, 0)
            gate_f = self.bcast_tile(ph, ph, "gate_f_bc", P["modT"][:, 80:96], "modT")
            fg = self.bcast_tile(ph, ph, "fg_bc", P["gvec"][:, 32:48], "gvec")
            h2T = self.sb(ph, "h2T_h", [128, 16, TH], BF16)
            h2s = kb.dsem("h2T_h")
            acc = self.sb(ph, "moe_acc", [128, NTT, 2048], F32)
            NW = 2
            wg = [self.sb(ph, "wg%d" % i, [128, 16, 128], BF16) for i in range(NW)]
            wu = [self.sb(ph, "wu%d" % i, [128, 16, 128], BF16) for i in range(NW)]
            wgs = [kb.dsem("wg%d" % i) for i in range(NW)]
            wus = [kb.dsem("wu%d" % i) for i in range(NW)]
            wd = [self.sb(ph, "wd%d" % i, [128, 4, 2048], BF16) for i in range(2)]
            wds = [kb.dsem("wd%d" % i) for i in range(2)]
            hT = [self.sb(ph, "ehT%d" % i, [128, 4, TH], BF16) for i in range(2)]
            sg = [self.sb(ph, "sg%d" % i, [128, 512], F32) for i in range(2)]
            xmt = self.sb(ph, "xmt", [128, 2048], F32)
            xmts = kb.dsem("xmt")
            st3 = self.sb(ph, "st3", [128, 4], F32)
            junk = self.sb(ph, "junk6", [128, 2048], BF16)
            wi = 0
            sgi = 0
            yshs = [kb.dsem("ysh%d" % i) for i in range(NTT)] if shared_only else []
            for half in range(2):
                self.dma("sp", h2T[:], self.H2T[:, :, half * TH:(half + 1) * TH], [], ["h2T_h"], h2s)
                for tt in range(NTT):
                    kb.op("pool", lambda e, tt=tt: e.memset(acc[:, tt, :], 0.0), writes=["acc%d" % tt])
                for ex in ([NE] if shared_only else range(NE + 1)):
                    eb = ex % 2
                    if ex < NE:
                        g_src, u_src, d_src = self.w_eg[ex], self.w_eu[ex], self.w_ed[ex]
                    else:
                        g_src, u_src, d_src = self.w_sg, self.w_su, self.w_sd
                    self.dma("pool", wd[eb][:], d_src.rearrange("(kc p) n -> p kc n", p=128), [], ["wd%d" % eb], wds[eb])
                    for hb in range(4):
                        w = wi % NW
                        wi += 1
                        self.dma("pool", wg[w][:], g_src[:, hb * 128:(hb + 1) * 128].rearrange("(kc p) n -> p kc n", p=128),
                                 [], ["wg%d" % w], wgs[w])
                        self.dma("pool", wu[w][:], u_src[:, hb * 128:(hb + 1) * 128].rearrange("(kc p) n -> p kc n", p=128),
                                 [], ["wu%d" % w], wus[w])
                        for tg in range(TH // 512):
                            pg, pgk = self.pbank()
                            pu, puk = self.pbank()
                            for (pp, ppk, wsrc, wk) in ((pg, pgk, wg[w], "wg%d" % w), (pu, puk, wu[w], "wu%d" % w)):
                                for kc in range(16):
                                    kb.op("pe", lambda e, pp=pp, wsrc=wsrc, kc=kc, tg=tg: e.matmul(
                                        pp[:], lhsT=wsrc[:, kc, :], rhs=h2T[:, kc, tg * 512:(tg + 1) * 512],
                                        start=(kc == 0), stop=(kc == 15)), reads=[wk, "h2T_h"], writes=[ppk])
                            si = sgi % 2
                            sgi += 1
                            kb.op("act", lambda e, pg=pg, si=si: e.activation(out=sg[si][:], in_=pg[:], func=AF.Silu),
                                  reads=[pgk], writes=["sg%d" % si])
                            kb.op("dve", lambda e, pu=pu, si=si, hb=hb, tg=tg: e.tensor_tensor(
                                out=hT[eb][:, hb, tg * 512:(tg + 1) * 512], in0=pu[:], in1=sg[si][:], op=ALU.mult),
                                reads=[puk, "sg%d" % si], writes=["ehT%d" % eb])
                    for tt in range(NTT):
                        tile = half * NTT + tt
                        for nb in range(4):
                            py, pyk = self.pbank()
                            for hb in range(4):
                                kb.op("pe", lambda e, py=py, hb=hb, tt=tt, nb=nb: e.matmul(
                                    py[:], lhsT=hT[eb][:, hb, tt * 128:(tt + 1) * 128], rhs=wd[eb][:, hb, nb * 512:(nb + 1) * 512],
                                    start=(hb == 0), stop=(hb == 3)), reads=["ehT%d" % eb, "wd%d" % eb], writes=[pyk])
                            scal = P["Wall"][:, tile, ex:ex + 1] if ex < NE else 1.0
                            kb.op("dve", lambda e, py=py, tt=tt, nb=nb, scal=scal: e.scalar_tensor_tensor(
                                out=acc[:, tt, nb * 512:(nb + 1) * 512], in0=py[:], scalar=scal,
                                in1=acc[:, tt, nb * 512:(nb + 1) * 512], op0=ALU.mult, op1=ALU.add),
                                reads=[pyk, "Wall"], writes=["acc%d" % tt])
                if shared_only:
                    for tt in range(NTT):
                        tile = half * NTT + tt
                        self.dma("sp", self.YSH[tile * 128:(tile + 1) * 128, :], acc[:, tt, :], ["acc%d" % tt], [], yshs[tt])
                    continue
                for tt in range(NTT):
                    tile = half * NTT + tt
                    self.dma("sp", xmt[:], self.XMID[tile * 128:(tile + 1) * 128, :], [], ["xmt"], xmts)
                    kb.op("dve", lambda e, tt=tt: e.tensor_tensor(out=acc[:, tt, :], in0=acc[:, tt, :], in1=gate_f[:], op=ALU.mult),
                          reads=["gate_f_bc"], writes=["acc%d" % tt])
                    kb.op("pool", lambda e, tt=tt: e.tensor_tensor(out=xmt[:], in0=xmt[:], in1=acc[:, tt, :], op=ALU.add),
                          reads=["acc%d" % tt], writes=["xmt"])
                    kb.op("act", lambda e: e.activation(out=junk[:], in_=xmt[:], func=AF.Square, accum_out=st3[:, 0:1]),
                          reads=["xmt"], writes=["junk6", "st3"])
                    kb.op("dve", lambda e: e.tensor_scalar(out=st3[:, 1:2], in0=st3[:, 0:1], scalar1=1.0 / c.D, scalar2=c.EPS,
                                                           op0=ALU.mult, op1=ALU.add), reads=[], writes=["st3"])
                    kb.op("act", lambda e: e.activation(out=st3[:, 2:3], in_=st3[:, 1:2], func=AF.Sqrt), reads=[], writes=["st3"])
                    kb.op("dve", lambda e: e.reciprocal(out=st3[:, 3:4], in_=st3[:, 2:3]), reads=[], writes=["st3"])
                    kb.op("dve", lambda e: e.scalar_tensor_tensor(out=xmt[:], in0=xmt[:], scalar=st3[:, 3:4], in1=fg[:],
                                                                  op0=ALU.mult, op1=ALU.mult), reads=["st3", "fg_bc"], writes=["xmt"])
                    self.dma("sp", self.out[tile * 128:(tile + 1) * 128, :], xmt[:], ["xmt"], [], xmts)
            kb.barrier()
            kb.release_dsems([h2s, xmts] + wgs + wus + wds + yshs)


class Phases8(Phases7):
    def phase6_routed(self):
        c, kb, P = self.cfg, self.kb, self.P
        NE, CAP = c.NE, c.CAP
        NB = CAP // 128
        with contextlib.ExitStack() as ph:
            self.alloc_psum(ph, 6, 2)
            NXG = 8
            xg = [self.sb(ph, "xg%d" % i, [128, 2048], BF16) for i in range(NXG)]
            xgs = [kb.dsem("xg%d" % i) for i in range(NXG)]
            xT = self.sb(ph, "xTe", [128, 16, CAP], BF16)
            NW = 4
            wg = [self.sb(ph, "rwg%d" % i, [128, 16, 128], BF16) for i in range(NW)]
            wu = [self.sb(ph, "rwu%d" % i, [128, 16, 128], BF16) for i in range(NW)]
            wgs = [kb.dsem("rwg%d" % i) for i in range(NW)]
            wus = [kb.dsem("rwu%d" % i) for i in range(NW)]
            wd = [self.sb(ph, "rwd%d" % i, [128, 4, 2048], BF16) for i in range(2)]
            wds = [kb.dsem("rwd%d" % i) for i in range(2)]
            hT = [self.sb(ph, "rhT%d" % i, [128, 4, CAP], BF16) for i in range(2)]
            sg = [self.sb(ph, "rsg%d" % i, [128, 512], F32) for i in range(2)]
            yst = [self.sb(ph, "yst%d" % i, [128, 2048], BF16) for i in range(2)]
            ysts = [kb.dsem("yst%d" % i) for i in range(2)]
            xTs = [xT, self.sb(ph, "xTe_b", [128, 16, CAP], BF16)]
            cnt = {"xgi": 0, "wi": 0, "sgi": 0, "yi": 0}

            def T_blk(ex, blk):
                xTe = xTs[ex % 2]
                xi = blk % NXG
                for c0 in range(0, 16, 8):
                    tb, tk = self.tbank()
                    for j in range(8):
                        ci = c0 + j
                        kb.op("pe", lambda e, j=j, ci=ci: e.transpose(
                            out=tb[:, j, :], in_=xg[xi][:, ci * 128:(ci + 1) * 128], identity=P["ident_b"][:]),
                            reads=["xg%d" % xi, "ident_b"], writes=[tk])
                    for j in range(8):
                        ci = c0 + j
                        wkey = "xTe%d_%d" % (ex % 2, blk // 4)
                        if j % 2 == 0:
                            kb.op("dve", lambda e, j=j, ci=ci: e.tensor_scalar(
                                out=xTe[:, ci, blk * 128:(blk + 1) * 128], in0=tb[:, j, :], scalar1=P["gmodF"][:, ci:ci + 1],
                                scalar2=P["modT"][:, 48 + ci:49 + ci], op0=ALU.mult, op1=ALU.add),
                                reads=[tk, "gmodF", "modT"], writes=[wkey])
                        else:
                            kb.op("act", lambda e, j=j, ci=ci: e.activation(
                                out=xTe[:, ci, blk * 128:(blk + 1) * 128], in_=tb[:, j, :], func=AF.Identity,
                                scale=P["gmodF"][:, ci:ci + 1], bias=P["modT"][:, 48 + ci:49 + ci]),
                                reads=[tk, "gmodF", "modT"], writes=[wkey])

            def G_hb(ex, hb):
                eb = ex % 2
                xTe = xTs[ex % 2]
                if hb == 0:
                    self.dma("pool", wd[eb][:], self.w_ed[ex].rearrange("(kc p) n -> p kc n", p=128), [], ["rwd%d" % eb], wds[eb])
                w = cnt["wi"] % NW
                cnt["wi"] += 1
                self.dma("pool", wg[w][:], self.w_eg[ex][:, hb * 128:(hb + 1) * 128].rearrange("(kc p) n -> p kc n", p=128),
                         [], ["rwg%d" % w], wgs[w])
                self.dma("pool", wu[w][:], self.w_eu[ex][:, hb * 128:(hb + 1) * 128].rearrange("(kc p) n -> p kc n", p=128),
                         [], ["rwu%d" % w], wus[w])
                for tg in range(CAP // 512):
                    pg, pgk = self.pbank()
                    pu, puk = self.pbank()
                    for (pp, ppk, wsrc, wk) in ((pg, pgk, wg[w], "rwg%d" % w), (pu, puk, wu[w], "rwu%d" % w)):
                        for kc in range(16):
                            kb.op("pe", lambda e, pp=pp, wsrc=wsrc, kc=kc, tg=tg: e.matmul(
                                pp[:], lhsT=wsrc[:, kc, :], rhs=xTe[:, kc, tg * 512:(tg + 1) * 512],
                                start=(kc == 0), stop=(kc == 15)), reads=[wk, "xTe%d_%d" % (ex % 2, tg)], writes=[ppk])
                    si = cnt["sgi"] % 2
                    cnt["sgi"] += 1
                    kb.op("act", lambda e, pg=pg, si=si: e.activation(out=sg[si][:], in_=pg[:], func=AF.Silu),
                          reads=[pgk], writes=["rsg%d" % si])
                    kb.op("dve", lambda e, pu=pu, si=si, tg=tg: e.tensor_tensor(
                        out=hT[eb][:, hb, tg * 512:(tg + 1) * 512], in0=pu[:], in1=sg[si][:], op=ALU.mult),
                        reads=[puk, "rsg%d" % si], writes=["rhT%d" % eb])

            def G_down(ex, blk):
                eb = ex % 2
                y = cnt["yi"] % 2
                cnt["yi"] += 1
                for nb in range(4):
                    py, pyk = self.pbank()
                    for hb in range(4):
                        kb.op("pe", lambda e, py=py, hb=hb, nb=nb: e.matmul(
                            py[:], lhsT=hT[eb][:, hb, blk * 128:(blk + 1) * 128], rhs=wd[eb][:, hb, nb * 512:(nb + 1) * 512],
                            start=(hb == 0), stop=(hb == 3)), reads=["rhT%d" % eb, "rwd%d" % eb], writes=[pyk])
                    if nb % 2 == 0:
                        kb.op("act", lambda e, py=py, nb=nb: e.activation(out=yst[y][:, nb * 512:(nb + 1) * 512], in_=py[:], func=AF.Copy),
                              reads=[pyk], writes=["yst%d" % y])
                    else:
                        kb.op("dve", lambda e, py=py, nb=nb: e.tensor_copy(out=yst[y][:, nb * 512:(nb + 1) * 512], in_=py[:]),
                              reads=[pyk], writes=["yst%d" % y])
                r0 = ex * CAP + blk * 128
                self.dma("act", self.YS[r0:r0 + 128, :], yst[y][:], ["yst%d" % y], [], ysts[y])

            def X_load(ex):
                for blk in range(NB):
                    r0 = ex * CAP + blk * 128
                    self.dma("sp", xg[blk % NXG][:], self.XG[r0:r0 + 128, :], [], ["xg%d" % (blk % NXG)], xgs[blk % NXG])

            X_load(0)
            for blk in range(NB):
                T_blk(0, blk)
            X_load(1)
            for ex in range(NE):
                nxt = [(ex + 1, blk) for blk in range(NB)] if ex + 1 < NE else []
                for hb in range(4):
                    G_hb(ex, hb)
                    if nxt:
                        T_blk(*nxt.pop(0))
                for blk in range(NB):
                    G_down(ex, blk)
                    if nxt and blk % 2 == 1:
                        T_blk(*nxt.pop(0))
                while nxt:
                    T_blk(*nxt.pop(0))
                if ex + 2 < NE:
                    X_load(ex + 2)
            kb.barrier()
            kb.release_dsems(xgs + wgs + wus + wds + ysts)

    def phase7(self):
        c, kb, P = self.cfg, self.kb, self.P
        NQ = c.CH // 128
        NE, CAP = c.NE, c.CAP
        with contextlib.ExitStack() as ph:
            self.alloc_psum(ph, 2, 0)
            gate_f = self.bcast_tile(ph, ph, "gate_f_bc2", P["modT"][:, 80:96], "modT")
            fg = self.bcast_tile(ph, ph, "fg_bc2", P["gvec"][:, 32:48], "gvec")
            acc = [self.sb(ph, "cacc%d" % i, [128, 2048], F32) for i in range(2)]
            accs = [kb.dsem("cacc%d" % i) for i in range(2)]
            xmt = [self.sb(ph, "cxm%d" % i, [128, 2048], F32) for i in range(2)]
            xmts = [kb.dsem("cxm%d" % i) for i in range(2)]
            NGB = 4
            gb = [self.sb(ph, "gb%d" % i, [128, 2048], BF16) for i in range(NGB)]
            gbs = [kb.dsem("gb%d" % i) for i in range(NGB)]
            st3 = self.sb(ph, "cst3", [128, 4], F32)
            junk = self.sb(ph, "cjunk", [128, 2048], BF16)
            for i in range(NGB):
                kb.op("pool", lambda e, i=i: e.memset(gb[i][:], 0.0), writes=["gb%d" % i])
            gi = 0
            for b in range(NQ):
                k = b % 2
                self.dma("sp", acc[k][:], self.YSH[b * 128:(b + 1) * 128, :], [], ["cacc%d" % k], accs[k])
                self.dma("sp", xmt[k][:], self.XMID[b * 128:(b + 1) * 128, :], [], ["cxm%d" % k], xmts[k])
                for k8 in range(8):
                    g = gi % NGB
                    gi += 1
                    kb.op("pool", lambda e, g=g, b=b, k8=k8: e.indirect_dma_start(
                        out=gb[g][:, :], out_offset=None, in_=self.YS[:, :],
                        in_offset=bass.IndirectOffsetOnAxis(ap=P["slotI"][:, b, k8:k8 + 1], axis=0),
                        bounds_check=self.bnd_reg, oob_is_err=False),
                        reads=["slotI%d" % b], writes=["gb%d" % g], dsem=gbs[g])
                    kb.op("dve", lambda e, g=g, b=b, k8=k8, k=k: e.scalar_tensor_tensor(
                        out=acc[k][:], in0=gb[g][:], scalar=P["w8"][:, b, k8:k8 + 1], in1=acc[k][:], op0=ALU.mult, op1=ALU.add),
                        reads=["gb%d" % g, "w8_%d" % b], writes=["cacc%d" % k])
                kb.op("dve", lambda e, k=k: e.tensor_tensor(out=acc[k][:], in0=acc[k][:], in1=gate_f[:], op=ALU.mult),
                      reads=["gate_f_bc2"], writes=["cacc%d" % k])
                kb.op("pool", lambda e, k=k: e.tensor_tensor(out=xmt[k][:], in0=xmt[k][:], in1=acc[k][:], op=ALU.add),
                      reads=["cacc%d" % k], writes=["cxm%d" % k])
                kb.op("act", lambda e, k=k: e.activation(out=junk[:], in_=xmt[k][:], func=AF.Square, accum_out=st3[:, 0:1]),
                      reads=["cxm%d" % k], writes=["cjunk", "cst3"])
                kb.op("dve", lambda e: e.tensor_scalar(out=st3[:, 1:2], in0=st3[:, 0:1], scalar1=1.0 / c.D, scalar2=c.EPS,
                                                       op0=ALU.mult, op1=ALU.add), reads=[], writes=["cst3"])
                kb.op("act", lambda e: e.activation(out=st3[:, 2:3], in_=st3[:, 1:2], func=AF.Sqrt), reads=[], writes=["cst3"])
                kb.op("dve", lambda e: e.reciprocal(out=st3[:, 3:4], in_=st3[:, 2:3]), reads=[], writes=["cst3"])
                kb.op("dve", lambda e, k=k: e.scalar_tensor_tensor(out=xmt[k][:], in0=xmt[k][:], scalar=st3[:, 3:4], in1=fg[:],
                                                                   op0=ALU.mult, op1=ALU.mult), reads=["cst3", "fg_bc2"], writes=["cxm%d" % k])
                self.dma("act", self.out[b * 128:(b + 1) * 128, :], xmt[k][:], ["cxm%d" % k], [], xmts[k])
            kb.barrier()
            kb.release_dsems(accs + xmts + gbs)


def build(cfg):
    b = Phases8(cfg)
    b.declare_io()
    kb = b.kb
    with contextlib.ExitStack() as st:
        b.phase0(st)
        if cfg.stop_after >= 1:
            b.phase1a()
        if cfg.stop_after >= 2:
            b.phase1b()
            b.phase1c()
        if cfg.stop_after >= 3:
            b.phase2()
        if cfg.stop_after >= 4:
            b.phase3()
        if cfg.stop_after >= 5:
            b.phase4(st)
        if cfg.stop_after >= 6:
            if cfg.CAP:
                b.phase6(shared_only=True)
                b.phase6_routed()
                b.phase7()
            else:
                b.phase6()
        kb.barrier()
    return b


_BUILD_CACHE = {}


def kernel(**inputs):
    cfg = Cfg
    inp = {k: np.asarray(v) for k, v in inputs.items()}
    S = inp["x"].shape[1]
    nchunks = S // cfg.CH
    assert nchunks == cfg.NCORES and nchunks == cfg.NSLOT
    if "b" not in _BUILD_CACHE:
        _BUILD_CACHE["b"] = build(cfg)
    b = _BUILD_CACHE["b"]
    sh = prepare_shared(inp, cfg)
    maps = []
    for core in range(cfg.NCORES):
        m = dict(sh)
        m.update(prepare_core(inp, cfg, core, nchunks))
        maps.append(m)
    res = run_bass_kernel_spmd(b.nc, maps, core_ids=list(range(cfg.NCORES)))
    outs = [np.asarray(r["out"]) for r in res.results]
    return np.concatenate(outs, axis=0)[None].astype(np.float32)
```
